# Optimizing a Trainium2 kernel written in Bass

```python
import functools
import jax
import jax.numpy as jnp
from jax import lax
import numpy as np

D_MODEL = 1024
BATCH = 8
SEQ = 2048
DEPTH = 4

GRID_W = 64
CTX_LEN = 256
ROPE_THETA = 10000.0
NORM_EPS = 1e-6
NEG_BIG = -1e30
LB_FLOOR = 1e-30

D_MIX = D_MODEL
GROUP_W = D_MIX // 4

MLA_HEADS = 4
MLA_NOPE = 64
MLA_ROPE = 32
MLA_V = GROUP_W // MLA_HEADS
MLA_Q_RANK = 3 * D_MODEL // 16
MLA_KV_RANK = D_MODEL // 8
ATTN_QBLOCK = 128

HG_HEADS = 4
HG_DK = 64
HG_DV = GROUP_W // HG_HEADS
HG_CHUNK = 16

SWA_HEADS = 4
SWA_KV_HEADS = 2
SWA_HD = GROUP_W // SWA_HEADS
SWA_WINDOW = 128
SWA_BLOCK = 128

RET_HEADS = 4
RET_DK = 32
RET_DV = GROUP_W // RET_HEADS
RET_CHUNK = 64

D_FF = 2816
N_EXPERTS = 8
TOP_K = 2
D_FF_EXPERT = 3584

IN_SIZES = (
    MLA_Q_RANK, MLA_KV_RANK, MLA_ROPE,
    HG_HEADS * HG_DK, HG_HEADS * HG_DK, HG_HEADS * HG_DK, HG_HEADS * HG_DV, HG_HEADS * HG_DV,
    SWA_HEADS * SWA_HD, SWA_KV_HEADS * SWA_HD, SWA_KV_HEADS * SWA_HD,
    RET_HEADS * RET_DK, RET_HEADS * RET_DK, RET_HEADS * RET_DV, RET_HEADS * RET_DV,
)
D_IN = sum(IN_SIZES)

kernel_name = 'hybrid_mla_hgrn2_swa_retnet_moe_dit'


def rms_norm(x, g):
    xf = x.astype(jnp.float32)
    y = xf * lax.rsqrt(jnp.mean(xf * xf, axis=-1, keepdims=True) + NORM_EPS)
    return (y * g.astype(jnp.float32)).astype(x.dtype)


def modulate(h, shift, scale):
    return h * (1 + scale) + shift


def to_heads(t, n_heads):
    B, T, _ = t.shape
    return t.reshape(B, T, n_heads, -1).transpose(0, 2, 1, 3)


def apply_rope(x, ang):
    cos = jnp.cos(ang).astype(x.dtype)
    sin = jnp.sin(ang).astype(x.dtype)
    x1, x2 = jnp.split(x, 2, axis=-1)
    return jnp.concatenate([x1 * cos - x2 * sin, x1 * sin + x2 * cos], axis=-1)


def axial_angles(n_tok, rot_dim):
    rows = n_tok // GRID_W
    row = jnp.broadcast_to(jnp.arange(rows)[:, None], (rows, GRID_W)).reshape(-1)
    col = jnp.broadcast_to(jnp.arange(GRID_W)[None, :], (rows, GRID_W)).reshape(-1)
    n_freq = rot_dim // 4
    inv = ROPE_THETA ** (-jnp.arange(n_freq, dtype=jnp.float32) / n_freq)
    return jnp.concatenate([row.astype(jnp.float32)[:, None] * inv,
                            col.astype(jnp.float32)[:, None] * inv], axis=-1)


def linear_angles(pos, rot_dim):
    n_freq = rot_dim // 2
    inv = ROPE_THETA ** (-jnp.arange(n_freq, dtype=jnp.float32) / n_freq)
    return pos.astype(jnp.float32)[:, None] * inv


def dense_block_attention(q, k, v, scale):
    B, T, H, Dk = q.shape
    nb = T // ATTN_QBLOCK
    qb = jnp.moveaxis(q.reshape(B, nb, ATTN_QBLOCK, H, Dk), 1, 0)

    def one_block(qi):
        s = jnp.einsum('bqhd,bkhd->bhqk', qi, k).astype(jnp.float32) * scale
        p = jax.nn.softmax(s, axis=-1).astype(v.dtype)
        return jnp.einsum('bhqk,bkhd->bqhd', p, v)

    o = lax.map(one_block, qb)
    return jnp.moveaxis(o, 0, 1).reshape(B, T, H * v.shape[-1])


def mla_qkv(q_lat, kv_lat, k_pe, q_norm_g, w_uq, kv_norm_g, w_ukv, ang):
    B, T, _ = q_lat.shape
    q = (rms_norm(q_lat, q_norm_g) @ w_uq).reshape(B, T, MLA_HEADS, MLA_NOPE + MLA_ROPE)
    kv = (rms_norm(kv_lat, kv_norm_g) @ w_ukv).reshape(B, T, MLA_HEADS, MLA_NOPE + MLA_V)
    q_nope, q_pe = q[..., :MLA_NOPE], q[..., MLA_NOPE:]
    k_nope, v = kv[..., :MLA_NOPE], kv[..., MLA_NOPE:]
    if ang is not None:
        q_pe = apply_rope(q_pe, ang[:, None, :])
        k_pe = apply_rope(k_pe, ang)
    k_pe = jnp.broadcast_to(k_pe[:, :, None, :], (B, T, MLA_HEADS, MLA_ROPE))
    return (jnp.concatenate([q_nope, q_pe], axis=-1),
            jnp.concatenate([k_nope, k_pe], axis=-1), v)


def mla_mixer(lat_in, ctx_in, ang, q_norm_g, w_uq, kv_norm_g, w_ukv, with_ctx):
    ql, kl, vl = mla_qkv(*lat_in, q_norm_g, w_uq, kv_norm_g, w_ukv, ang)
    qc, kc, vc = mla_qkv(*ctx_in, q_norm_g, w_uq, kv_norm_g, w_ukv, None)
    scale = (MLA_NOPE + MLA_ROPE) ** -0.5
    o_lat = dense_block_attention(ql, jnp.concatenate([kc, kl], axis=1),
                                  jnp.concatenate([vc, vl], axis=1), scale)
    o_ctx = dense_block_attention(qc, kc, vc, scale) if with_ctx else None
    return o_lat, o_ctx


def hgrn_log_forget(z, lb):
    return jnp.logaddexp(jnp.log(jnp.maximum(lb, LB_FLOOR)), jnp.log1p(-lb) + jax.nn.log_sigmoid(z))


def hgrn_chunk_scan(q, k, v, log_f, s0):
    B, H, T, dk = q.shape
    dv = v.shape[-1]
    C = HG_CHUNK
    nc = T // C
    q, k, log_f = (t.reshape(B, H, nc, C, dk) for t in (q, k, log_f))
    v = v.reshape(B, H, nc, C, dv)
    b = jnp.cumsum(log_f, axis=3)
    incl = jnp.tril(jnp.ones((C, C), dtype=bool))[:, :, None]
    diff = b[:, :, :, :, None, :] - b[:, :, :, None, :, :]
    decay = jnp.where(incl, jnp.exp(jnp.where(incl, diff, 0.0)), 0.0)
    a = jnp.einsum('bhctk,bhctsk,bhcsk->bhcts', q, decay, k)
    o = jnp.einsum('bhcts,bhcsv->bhctv', a, v)
    b_last = b[:, :, :, -1:, :]
    u = jnp.einsum('bhcsk,bhcsv->bhckv', k * jnp.exp(b_last - b), v)
    d = jnp.exp(b_last[:, :, :, 0, :])

    def step(s, inp):
        d_c, u_c = inp
        return d_c[..., None] * s + u_c, s

    s_fin, s_start = lax.scan(step, s0, (jnp.moveaxis(d, 2, 0), jnp.moveaxis(u, 2, 0)))
    s_start = jnp.moveaxis(s_start, 0, 2)
    o = o + jnp.einsum('bhctk,bhckv->bhctv', q * jnp.exp(b), s_start)
    return o.reshape(B, H, T, dv), s_fin


def retention_chunk_scan(q, k, v, s0, log_g):
    B, H, T, dk = q.shape
    dv = v.shape[-1]
    C = RET_CHUNK
    nc = T // C
    q = q.reshape(B, H, nc, C, dk)
    k = k.reshape(B, H, nc, C, dk)
    v = v.reshape(B, H, nc, C, dv)
    idx = jnp.arange(C, dtype=jnp.float32)
    rel = idx[:, None] - idx[None, :]
    lg = log_g[:, None, None]
    d_intra = jnp.where(rel >= 0, jnp.exp(jnp.maximum(rel, 0.0) * lg), 0.0)
    a = jnp.einsum('bhctd,bhcsd->bhcts', q, k) * d_intra[None, :, None]
    o = jnp.einsum('bhcts,bhcsv->bhctv', a, v)
    xi = jnp.exp((idx + 1.0)[None, :] * log_g[:, None])
    zeta = jnp.exp((C - 1.0 - idx)[None, :] * log_g[:, None])
    u = jnp.einsum('bhcsd,hs,bhcsv->bhcdv', k, zeta, v)
    dc = jnp.exp(C * log_g)[None, :, None, None]

    def step(s, u_c):
        return dc * s + u_c, s

    s_fin, s_start = lax.scan(step, s0, jnp.moveaxis(u, 2, 0))
    s_start = jnp.moveaxis(s_start, 0, 2)
    o = o + jnp.einsum('bhctd,ht,bhcdv->bhctv', q, xi, s_start)
    return o.reshape(B, H, T, dv), s_fin


def bidirectional_scan(fns, ctx_seqs, lat_seqs, s0):
    o_lat, o_ctx = [], []
    for fn, cs, ls, rev in zip(fns, ctx_seqs, lat_seqs, (False, True)):
        if rev:
            cs = tuple(jnp.flip(t, axis=2) for t in cs)
            ls = tuple(jnp.flip(t, axis=2) for t in ls)
        oc, s_ctx = fn(*cs, s0)
        ol, _ = fn(*ls, s_ctx)
        if rev:
            oc, ol = jnp.flip(oc, axis=2), jnp.flip(ol, axis=2)
        o_lat.append(ol)
        o_ctx.append(oc)
    return o_lat[0] + o_lat[1], o_ctx[0] + o_ctx[1]


def hgrn2_mixer(lat_in, ctx_in, lb, norm_g, with_ctx):
    def prep(q, z_fwd, z_bwd, i):
        qh = to_heads(q, HG_HEADS).astype(jnp.float32) * HG_DK ** -0.5
        vh = to_heads(i, HG_HEADS).astype(jnp.float32)
        dirs = []
        for z, lb_d in ((z_fwd, lb[0]), (z_bwd, lb[1])):
            log_f = hgrn_log_forget(to_heads(z, HG_HEADS).astype(jnp.float32),
                                    lb_d.reshape(HG_HEADS, 1, HG_DK))
            dirs.append((qh, -jnp.expm1(log_f), vh, log_f))
        return dirs

    def finish(o, g):
        B, _, T, _ = o.shape
        y = rms_norm(jnp.swapaxes(o, 1, 2), norm_g.reshape(HG_HEADS, HG_DV)).reshape(B, T, GROUP_W)
        return (y * jax.nn.silu(g.astype(jnp.float32))).astype(g.dtype)

    B = lat_in[0].shape[0]
    s0 = jnp.zeros((B, HG_HEADS, HG_DK, HG_DV), jnp.float32)
    o_lat, o_ctx = bidirectional_scan((hgrn_chunk_scan, hgrn_chunk_scan),
                                      prep(*ctx_in[:4]), prep(*lat_in[:4]), s0)
    return finish(o_lat, lat_in[4]), (finish(o_ctx, ctx_in[4]) if with_ctx else None)


def swa_mixer(lat_in, ctx_in, ang, sink, with_ctx):
    ql, kl, vl = lat_in
    qc, kc, vc = ctx_in
    B, N, _ = ql.shape
    L = qc.shape[1]
    G = SWA_HEADS // SWA_KV_HEADS
    KVH, HD, BLK = SWA_KV_HEADS, SWA_HD, SWA_BLOCK
    ql = apply_rope(ql.reshape(B, N, KVH, G, HD), ang[:, None, None, :])
    kl = apply_rope(kl.reshape(B, N, KVH, HD), ang[:, None, :])
    vl = vl.reshape(B, N, KVH, HD)
    qc = qc.reshape(B, L, KVH, G, HD)
    kc = kc.reshape(B, L, KVH, HD)
    vc = vc.reshape(B, L, KVH, HD)
    scale = HD ** -0.5
    sink_hg = sink.astype(jnp.float32).reshape(KVH, G)

    nb = N // BLK
    qb = ql.reshape(B, nb, BLK, KVH, G, HD)
    pad = ((0, 0), (BLK, BLK), (0, 0), (0, 0))
    kp = jnp.pad(kl, pad).reshape(B, nb + 2, BLK, KVH, HD)
    vp = jnp.pad(vl, pad).reshape(B, nb + 2, BLK, KVH, HD)
    kw = jnp.concatenate([kp[:, :-2], kp[:, 1:-1], kp[:, 2:]], axis=2)
    vw = jnp.concatenate([vp[:, :-2], vp[:, 1:-1], vp[:, 2:]], axis=2)
    s_band = jnp.einsum('bnqhgd,bnkhd->bhgnqk', qb, kw).astype(jnp.float32) * scale
    qpos = jnp.arange(nb)[:, None] * BLK + jnp.arange(BLK)[None, :]
    kpos = (jnp.arange(nb)[:, None] - 1) * BLK + jnp.arange(3 * BLK)[None, :]
    valid = ((kpos >= 0) & (kpos < N))[:, None, :] & \
        (jnp.abs(qpos[:, :, None] - kpos[:, None, :]) <= SWA_WINDOW)
    s_band = jnp.where(valid, s_band, NEG_BIG)
    s_ctx = jnp.einsum('bnqhgd,bchd->bhgnqc', qb, kc).astype(jnp.float32) * scale
    s_sink = jnp.broadcast_to(sink_hg[None, :, :, None, None, None], s_ctx.shape[:-1] + (1,))
    p = jax.nn.softmax(jnp.concatenate([s_sink, s_ctx, s_band], axis=-1), axis=-1)
    p_ctx = p[..., 1:1 + L].astype(vl.dtype)
    p_band = p[..., 1 + L:].astype(vl.dtype)
    o = jnp.einsum('bhgnqc,bchd->bnqhgd', p_ctx, vc) + jnp.einsum('bhgnqk,bnkhd->bnqhgd', p_band, vw)
    o_lat = o.reshape(B, N, SWA_HEADS * HD)
    if not with_ctx:
        return o_lat, None
    s_cc = jnp.einsum('bqhgd,bkhd->bhgqk', qc, kc).astype(jnp.float32) * scale
    s_csink = jnp.broadcast_to(sink_hg[None, :, :, None, None], s_cc.shape[:-1] + (1,))
    pc = jax.nn.softmax(jnp.concatenate([s_csink, s_cc], axis=-1), axis=-1)[..., 1:].astype(vc.dtype)
    o_ctx = jnp.einsum('bhgqk,bkhd->bqhgd', pc, vc).reshape(B, L, SWA_HEADS * HD)
    return o_lat, o_ctx


def group_norm_heads(o, gain, bias):
    o = jnp.swapaxes(o, 1, 2)
    mu = jnp.mean(o, axis=-1, keepdims=True)
    var = jnp.mean(jnp.square(o - mu), axis=-1, keepdims=True)
    y = (o - mu) * lax.rsqrt(var + NORM_EPS)
    B, T, H, dv = y.shape
    return y.reshape(B, T, H * dv) * gain.astype(jnp.float32) + bias.astype(jnp.float32)


def retention_mixer(lat_in, ctx_in, log_gamma, gn_g, gn_b, with_ctx):
    L = ctx_in[0].shape[1]
    N = lat_in[0].shape[1]

    def prep(q, k, v, pos):
        ang = linear_angles(pos, RET_DK)
        qh = apply_rope(to_heads(q, RET_HEADS).astype(jnp.float32), ang) * RET_DK ** -0.5
        kh = apply_rope(to_heads(k, RET_HEADS).astype(jnp.float32), ang)
        vh = to_heads(v, RET_HEADS).astype(jnp.float32)
        return (qh, kh, vh)

    def finish(o, g):
        y = group_norm_heads(o, gn_g, gn_b)
        return (y * jax.nn.silu(g.astype(jnp.float32))).astype(g.dtype)

    cs = prep(*ctx_in[:3], jnp.arange(L))
    ls = prep(*lat_in[:3], L + jnp.arange(N))
    fns = tuple(functools.partial(retention_chunk_scan, log_g=log_gamma[d]) for d in range(2))
    B = lat_in[0].shape[0]
    s0 = jnp.zeros((B, RET_HEADS, RET_DK, RET_DV), jnp.float32)
    o_lat, o_ctx = bidirectional_scan(fns, (cs, cs), (ls, ls), s0)
    return finish(o_lat, lat_in[3]), (finish(o_ctx, ctx_in[3]) if with_ctx else None)


def token_mixers(p_lat, p_ctx, ang_mla, ang_swa, mla_q_norm_g, mla_w_uq, mla_kv_norm_g, mla_w_ukv,
                 hg_lb, hg_norm_g, swa_sink, ret_log_gamma, ret_gn_g, ret_gn_b, with_ctx):
    a_l, a_c = mla_mixer(p_lat[0:3], p_ctx[0:3], ang_mla, mla_q_norm_g, mla_w_uq,
                         mla_kv_norm_g, mla_w_ukv, with_ctx)
    b_l, b_c = hgrn2_mixer(p_lat[3:8], p_ctx[3:8], hg_lb, hg_norm_g, with_ctx)
    c_l, c_c = swa_mixer(p_lat[8:11], p_ctx[8:11], ang_swa, swa_sink, with_ctx)
    d_l, d_c = retention_mixer(p_lat[11:15], p_ctx[11:15], ret_log_gamma, ret_gn_g, ret_gn_b, with_ctx)
    o_lat = jnp.concatenate([a_l, b_l, c_l, d_l], axis=-1)
    o_ctx = jnp.concatenate([a_c, b_c, c_c, d_c], axis=-1) if with_ctx else None
    return o_lat, o_ctx


def swiglu(h, w_gate, w_up, w_down):
    return (jax.nn.silu(h @ w_gate) * (h @ w_up)) @ w_down


def moe_swiglu(h, w_router, w_gate, w_up, w_down):
    logits = (h @ w_router).astype(jnp.float32)
    top_val, top_idx = lax.top_k(logits, TOP_K)
    top_w = jax.nn.softmax(top_val, axis=-1)
    gates = jnp.sum(jax.nn.one_hot(top_idx, N_EXPERTS, dtype=jnp.float32) * top_w[..., None],
                    axis=-2).astype(h.dtype)
    out = jnp.zeros_like(h)
    for e in range(N_EXPERTS):
        out = out + gates[..., e:e + 1] * swiglu(h, w_gate[e], w_up[e], w_down[e])
    return out


def setup_inputs(seed: int = 0) -> dict:
    key = jax.random.key(seed)
    ks = iter(jax.random.split(key, 40))
    D = D_MODEL
    n_dense = (DEPTH + 1) // 2
    n_moe = DEPTH // 2

    def nrm(shape, scale):
        return jax.random.normal(next(ks), shape, jnp.float32) * scale

    ret_base = jnp.log(2.0 ** (5.0 + jnp.arange(RET_HEADS, dtype=jnp.float32)) - 1.0)
    return {
        'x': nrm((BATCH, SEQ, D), 1.0),
        'c': nrm((BATCH, D), 1.0),
        'ctx': nrm((BATCH, CTX_LEN, D), 1.0),
        'c_ctx': nrm((D,), 1.0),
        'w_ada': nrm((DEPTH, D, 6 * D), 0.5 * D ** -0.5),
        'b_ada': nrm((DEPTH, 6 * D), 0.02),
        'norm_mix_g': 1.0 + nrm((DEPTH, D), 0.02),
        'norm_ffn_g': 1.0 + nrm((DEPTH, D), 0.02),
        'w_in': nrm((DEPTH, D, D_IN), D ** -0.5),
        'w_out': nrm((DEPTH, D_MIX, D), D_MIX ** -0.5),
        'mla_q_norm_g': 1.0 + nrm((DEPTH, MLA_Q_RANK), 0.02),
        'mla_w_uq': nrm((DEPTH, MLA_Q_RANK, MLA_HEADS * (MLA_NOPE + MLA_ROPE)), MLA_Q_RANK ** -0.5),
        'mla_kv_norm_g': 1.0 + nrm((DEPTH, MLA_KV_RANK), 0.02),
        'mla_w_ukv': nrm((DEPTH, MLA_KV_RANK, MLA_HEADS * (MLA_NOPE + MLA_V)), MLA_KV_RANK ** -0.5),
        'hg_lb_logits': nrm((DEPTH, 2, HG_HEADS * HG_DK), 0.5),
        'hg_norm_g': 1.0 + nrm((DEPTH, HG_HEADS * HG_DV), 0.02),
        'swa_sink': nrm((DEPTH, SWA_HEADS), 0.5),
        'ret_decay_logit': ret_base + nrm((DEPTH, 2, RET_HEADS), 0.01),
        'ret_gn_g': 1.0 + nrm((DEPTH, RET_HEADS * RET_DV), 0.02),
        'ret_gn_b': nrm((DEPTH, RET_HEADS * RET_DV), 0.02),
        'ffn_w_gate': nrm((n_dense, D, D_FF), D ** -0.5),
        'ffn_w_up': nrm((n_dense, D, D_FF), D ** -0.5),
        'ffn_w_down': nrm((n_dense, D_FF, D), D_FF ** -0.5),
        'moe_w_router': nrm((n_moe, D, N_EXPERTS), D ** -0.5),
        'moe_w_gate': nrm((n_moe, N_EXPERTS, D, D_FF_EXPERT), D ** -0.5),
        'moe_w_up': nrm((n_moe, N_EXPERTS, D, D_FF_EXPERT), D ** -0.5),
        'moe_w_down': nrm((n_moe, N_EXPERTS, D_FF_EXPERT, D), D_FF_EXPERT ** -0.5),
        'final_norm_g': 1.0 + nrm((D,), 0.02),
    }


def reference(x, c, ctx, c_ctx, w_ada, b_ada, norm_mix_g, norm_ffn_g, w_in, w_out,
              mla_q_norm_g, mla_w_uq, mla_kv_norm_g, mla_w_ukv, hg_lb_logits, hg_norm_g,
              swa_sink, ret_decay_logit, ret_gn_g, ret_gn_b, ffn_w_gate, ffn_w_up, ffn_w_down,
              moe_w_router, moe_w_gate, moe_w_up, moe_w_down, final_norm_g):
    N = x.shape[1]
    ang_mla = axial_angles(N, MLA_ROPE)
    ang_swa = axial_angles(N, SWA_HD)
    lb_p = jax.nn.softmax(hg_lb_logits.astype(jnp.float32), axis=0)
    hg_lb = jnp.cumsum(lb_p, axis=0) - lb_p[0:1]
    ret_log_gamma = jax.nn.log_sigmoid(ret_decay_logit.astype(jnp.float32))
    split_points = [int(s) for s in np.cumsum(IN_SIZES)[:-1]]
    silu_c = jax.nn.silu(c)
    silu_cc = jax.nn.silu(c_ctx)

    h_lat, h_ctx = x, ctx
    for l in range(DEPTH):
        with_ctx = l < DEPTH - 1
        mod_l = silu_c @ w_ada[l] + b_ada[l]
        mod_c = silu_cc @ w_ada[l] + b_ada[l]
        sh_a, sc_a, g_a, sh_f, sc_f, g_f = jnp.split(mod_l[:, None, :], 6, axis=-1)
        csh_a, csc_a, cg_a, csh_f, csc_f, cg_f = jnp.split(mod_c, 6)

        a_lat = modulate(rms_norm(h_lat, norm_mix_g[l]), sh_a, sc_a)
        a_ctx = modulate(rms_norm(h_ctx, norm_mix_g[l]), csh_a, csc_a)
        p_lat = jnp.split(a_lat @ w_in[l], split_points, axis=-1)
        p_ctx = jnp.split(a_ctx @ w_in[l], split_points, axis=-1)
        o_lat, o_ctx = token_mixers(p_lat, p_ctx, ang_mla, ang_swa, mla_q_norm_g[l], mla_w_uq[l],
                                    mla_kv_norm_g[l], mla_w_ukv[l], hg_lb[l], hg_norm_g[l],
                                    swa_sink[l], ret_log_gamma[l], ret_gn_g[l], ret_gn_b[l], with_ctx)
        h_lat = h_lat + g_a * (o_lat @ w_out[l])
        if with_ctx:
            h_ctx = h_ctx + cg_a * (o_ctx @ w_out[l])

        if l % 2 == 0:
            ffn = functools.partial(swiglu, w_gate=ffn_w_gate[l // 2], w_up=ffn_w_up[l // 2],
                                    w_down=ffn_w_down[l // 2])
        else:
            ffn = functools.partial(moe_swiglu, w_router=moe_w_router[l // 2], w_gate=moe_w_gate[l // 2],
                                    w_up=moe_w_up[l // 2], w_down=moe_w_down[l // 2])
        h_lat = h_lat + g_f * ffn(modulate(rms_norm(h_lat, norm_ffn_g[l]), sh_f, sc_f))
        if with_ctx:
            h_ctx = h_ctx + cg_f * ffn(modulate(rms_norm(h_ctx, norm_ffn_g[l]), csh_f, csc_f))

    return rms_norm(h_lat, final_norm_g)
```

```python
import os
import numpy as np
import concourse.bass as bass
import concourse.mybir as mybir
from concourse.bass_utils import run_bass_kernel_spmd

AF = mybir.ActivationFunctionType
ALU = mybir.AluOpType
AX = mybir.AxisListType
F32 = mybir.dt.float32
BF16 = mybir.dt.bfloat16

ENGS = ['pe', 'act', 'dve', 'pool', 'sp']
NDMA = 56

D = 1024
T = 2304
NT = 18
LCTX = 256
NLAT = 2048
DEPTH = 4
D_FF = 2816
D_FFE = 3584
NEXP = 8
D_IN = 2912
EPS = 1e-6
BLKS = [(0, 256), (256, 512), (768, 512), (1280, 512), (1792, 512)]
C_MQ, C_MKV, C_MPE = 0, 192, 320
C_HQ, C_HZF, C_HZB, C_HI, C_HG = 352, 608, 864, 1120, 1376
C_SQ, C_SK, C_SV = 1632, 1888, 2016
C_RQ, C_RK, C_RV, C_RG = 2144, 2272, 2400, 2656


class Prog:
    def __init__(self, nc):
        self.nc = nc
        self.q = {e: [] for e in ENGS}
        self.sem = {e: nc.alloc_semaphore('cs_' + e) for e in ENGS}
        self.cnt = {e: 0 for e in ENGS}
        self.waited = {}
        self.lastw = {}
        self.readers = {}
        self.dma_sems = [nc.alloc_semaphore('ds%d' % i) for i in range(NDMA)]
        self.dma_cnt = [0] * NDMA
        self.dma_rr = {'sp': 0, 'pool': 0}
        self.out_tokens = []
        self.ninst = 0

    def _need(self, eng, tok):
        sem, val, prod = tok
        if eng == 'pe' and prod == 'pe':
            return
        k = (eng, sem.num)
        if self.waited.get(k, 0) >= val:
            return
        self.waited[k] = val
        self.q[eng].append(lambda e, sem=sem, val=val: e.wait_ge(sem, val))

    def _deps(self, eng, reads, writes):
        for r in reads:
            t = self.lastw.get(r)
            if t is not None:
                self._need(eng, t)
            if isinstance(r, tuple) and r[0] == 'ps':
                for t in self.readers.get(r, {}).values():
                    if t[2] != eng:
                        self._need(eng, t)
        for w in writes:
            t = self.lastw.get(w)
            if t is not None:
                self._need(eng, t)
            for t in self.readers.get(w, {}).values():
                self._need(eng, t)

    def _record(self, tok, reads, writes):
        for r in reads:
            d = self.readers.setdefault(r, {})
            k = tok[0].num
            if k not in d or d[k][1] < tok[1]:
                d[k] = tok
        for w in writes:
            self.lastw[w] = tok
            self.readers[w] = {}

    def op(self, eng, fn, reads=(), writes=(), inc=True):
        self._deps(eng, reads, writes)
        sem = self.sem[eng]
        self.ninst += 1
        if inc:
            self.cnt[eng] += 1
            tok = (sem, self.cnt[eng], eng)
            self.q[eng].append(lambda e, fn=fn, sem=sem: fn(e).then_inc(sem, 1))
        else:
            tok = (sem, self.cnt[eng] + 1, eng)
            self.q[eng].append(lambda e, fn=fn: fn(e))
        self._record(tok, reads, writes)
        return tok

    def dma(self, eng, out, in_, reads=(), writes=(), is_output=False):
        h = NDMA // 2
        j = self.dma_rr[eng]
        self.dma_rr[eng] = (j + 1) % h
        i = j if eng == 'sp' else h + j
        sem = self.dma_sems[i]
        if self.dma_cnt[i] > 0:
            self._need(eng, (sem, self.dma_cnt[i], 'dma'))
        self._deps(eng, reads, writes)
        self.dma_cnt[i] += 16
        tok = (sem, self.dma_cnt[i], 'dma')
        self.ninst += 1
        self.q[eng].append(
            lambda e, out=out, in_=in_, sem=sem: e.dma_start(out=out, in_=in_).then_inc(sem, 16))
        self._record(tok, reads, writes)
        if is_output:
            self.out_tokens.append(tok)
        return tok

    def barrier(self):
        toks = [(self.sem[e], self.cnt[e], e) for e in ENGS if self.cnt[e] > 0]
        toks += [(self.dma_sems[i], self.dma_cnt[i], 'dma') for i in range(NDMA) if self.dma_cnt[i] > 0]
        for f in ENGS:
            for t in toks:
                self._need(f, t)

    def emit(self):
        nc = self.nc
        for t in self.out_tokens:
            self._need('sp', t)
        for e in ['pe', 'act', 'dve', 'pool']:
            if self.cnt[e] > 0:
                self._need('sp', (self.sem[e], self.cnt[e], e))
        q = self.q
        with nc.Block() as block:
            @block.tensor
            def _(e):
                for f in q['pe']:
                    f(e)

            @block.scalar
            def _(e):
                for f in q['act']:
                    f(e)

            @block.vector
            def _(e):
                for f in q['dve']:
                    f(e)

            @block.gpsimd
            def _(e):
                for f in q['pool']:
                    f(e)

            @block.sync
            def _(e):
                for f in q['sp']:
                    f(e)


class SB:
    def __init__(self, nc, name, shape, dtype):
        self.t = nc.alloc_sbuf_tensor('sb_' + name, list(shape), dtype)
        self.k = name

    def __getitem__(self, idx):
        return self.t[idx]


class View:
    def __init__(self, ap, key):
        self.t = ap
        self.k = key

    def __getitem__(self, idx):
        return self.t[idx]


AR_WORDS = 19456


class Builder:
    def __init__(self, layers, mixers=(0, 1, 2, 3), ffn=True, final=True, h_in=False, h_out=False):
        self.layers = list(layers)
        self.mixers = tuple(mixers)
        self.ffn = ffn
        self.final = final
        self.h_in = h_in
        self.h_out = h_out
        self.nc = nc = bass.Bass("TRN2", target_bir_lowering=False)
        self.P = Prog(nc)
        self.din = {}
        self.ps = [nc.alloc_psum_tensor("psb%d" % i, [128, 512], F32) for i in range(8)]
        self.ps_rr = {}
        self.uid = 0
        self.arena = nc.alloc_sbuf_tensor('arena', [128, AR_WORDS], F32)
        self.ar_off = 0
        self.ar_phase = 0

    def inp(self, name, shape, dtype=F32):
        ap = self.nc.dram_tensor(name, list(shape), dtype, kind="ExternalInput").ap()
        self.din[name] = ap
        return ap

    def sb(self, name, shape, dtype=F32):
        return SB(self.nc, name, shape, dtype)

    def ar(self, name, shape, dtype=F32):
        nel = int(np.prod(shape[1:]))
        nb = nel * (2 if dtype == BF16 else 4)
        nw = ((nb + 31) // 32) * 8
        assert self.ar_off + nw <= AR_WORDS, (name, self.ar_off, nw)
        ap = self.arena[0:shape[0], self.ar_off:self.ar_off + nw]
        self.ar_off += nw
        if dtype == BF16:
            ap = ap.bitcast(BF16)
        ap = ap[:, 0:nel]
        if len(shape) == 3:
            ap = ap.rearrange("p (a b) -> p a b", a=shape[1])
        elif len(shape) == 4:
            ap = ap.rearrange("p (a b c) -> p a b c", a=shape[1], b=shape[2])
        elif len(shape) == 5:
            ap = ap.rearrange("p (a b c d) -> p a b c d", a=shape[1], b=shape[2], c=shape[3])
        self.ar_gen = getattr(self, 'ar_gen', 0)
        return View(ap, ('ar', self.ar_phase, name))

    def ar_release(self, mark):
        self.P.barrier()
        self.ar_off = mark
        self.ar_phase += 1

    def phase(self):
        self.P.barrier()
        self.ar_off = 0
        self.ar_phase += 1

    def psum(self, pool, banks):
        i = self.ps_rr.get(pool, 0)
        self.ps_rr[pool] = i + 1
        b = banks[i % len(banks)]
        return self.ps[b], ('ps', b)

    def mmg(self, out, pairs, reads, wkey):
        n = len(pairs)
        for i, (l, r) in enumerate(pairs):
            self.P.op('pe', lambda e, l=l, r=r, s=(i == 0), t=(i == n - 1): e.matmul(out, lhsT=l, rhs=r, start=s, stop=t),
                      reads=reads, writes=[wkey], inc=(i == n - 1))

    def tr(self, out, in_, ident, reads, wkey):
        self.P.op('pe', lambda e: e.transpose(out, in_, ident), reads=reads, writes=[wkey])

    def act(self, out, in_, func, reads, writes, bias=None, scale=None):
        kw = {}
        if bias is not None:
            kw['bias'] = bias
        if scale is not None:
            kw['scale'] = scale
        self.P.op('act', lambda e: e.activation(out=out, in_=in_, func=func, **kw), reads=reads, writes=writes)

    def tt(self, out, in0, in1, op, reads, writes, eng='dve'):
        self.P.op(eng, lambda e: e.tensor_tensor(out=out, in0=in0, in1=in1, op=op), reads=reads, writes=writes)

    def ts(self, out, in0, s1, s2, op0, op1, reads, writes, eng='dve'):
        if s2 is None:
            self.P.op(eng, lambda e: e.tensor_scalar(out=out, in0=in0, scalar1=s1, scalar2=None, op0=op0),
                      reads=reads, writes=writes)
        else:
            self.P.op(eng, lambda e: e.tensor_scalar(out=out, in0=in0, scalar1=s1, scalar2=s2, op0=op0, op1=op1),
                      reads=reads, writes=writes)

    def stt(self, out, in0, scalar, in1, op0, op1, reads, writes):
        self.P.op('dve', lambda e: e.scalar_tensor_tensor(out=out, in0=in0, scalar=scalar, in1=in1, op0=op0, op1=op1),
                  reads=reads, writes=writes)

    def cp(self, eng, out, in_, reads, writes):
        if eng == 'act':
            self.P.op('act', lambda e: e.copy(out=out, in_=in_), reads=reads, writes=writes)
        else:
            self.P.op(eng, lambda e: e.tensor_copy(out=out, in_=in_), reads=reads, writes=writes)

    def recip(self, out, in_, reads, writes):
        self.P.op('dve', lambda e: e.reciprocal(out=out, in_=in_), reads=reads, writes=writes)

    def memset(self, eng, ap, val, writes):
        self.P.op(eng, lambda e: e.memset(ap, val), writes=writes)

    def rows_to_cols(self, dram_rows, nrows, ncols, dst_ap, dst_key):
        self.uid += 1
        st = self.stage
        k = ('stage', self.uid % 2)
        sl = st[self.uid % 2]
        self.P.dma('sp', sl[0:nrows, 0:ncols], dram_rows, writes=[k])
        pt, pk = self.psum('tr', [0, 1])
        self.tr(pt[0:ncols, 0:nrows], sl[0:nrows, 0:ncols], self.ident[0:nrows, 0:nrows], [k, 'ident'], pk)
        self.cp('dve', dst_ap, pt[0:ncols, 0:nrows], [pk], [dst_key])

    def build(self):
        nc, P = self.nc, self.P
        L = DEPTH
        x = self.inp('x', [NLAT, D])
        ctx = self.inp('ctx', [LCTX, D])
        c2 = self.inp('c2', [2, D])
        identd = self.inp('ident', [128, 128])
        w_ada = self.inp('w_ada', [L, D, 6 * D])
        b_ada = self.inp('b_ada', [L, 6 * D])
        nmg = self.inp('norm_mix_g', [L, D])
        nfg = self.inp('norm_ffn_g', [L, D])
        fng = self.inp('final_norm_g', [D])
        self.w_in = self.inp('w_in', [L, D, D_IN])
        self.w_out = self.inp('w_out', [L, D, D])
        self.fwg = self.inp('ffn_w_gate', [2, D, D_FF])
        self.fwu = self.inp('ffn_w_up', [2, D, D_FF])
        self.fwd = self.inp('ffn_w_down', [2, D_FF, D])
        self.mwr = self.inp('moe_w_router', [2, D, NEXP])
        self.mwg = self.inp('moe_w_gate', [2, NEXP, D, D_FFE])
        self.mwu = self.inp('moe_w_up', [2, NEXP, D, D_FFE])
        self.mwd = self.inp('moe_w_down', [2, NEXP, D_FFE, D])
        seld = self.inp('sel', [NEXP, NEXP * 128])
        for nm, shp in [('mla_q_norm_g', [L, 192]), ('mla_w_uq', [L, 192, 384]), ('mla_kv_norm_g', [L, 128]),
                        ('mla_w_ukv', [L, 128, 512]), ('hg_lb_logits', [L, 2, 256]), ('hg_norm_g', [L, 256]),
                        ('swa_sink', [L, 4]), ('ret_decay_logit', [L, 2, 4]), ('ret_gn_g', [L, 256]),
                        ('ret_gn_b', [L, 256]), ('cos_mla', [32, T]), ('sin_mla', [32, T]), ('cos_swa', [64, T]),
                        ('sin_swa', [64, T]), ('cos_ret', [32, T]), ('sin_ret', [32, T]), ('rbig96', [96, 96]),
                        ('r128', [128, 128]), ('r64b', [64, 64]), ('swa_mask', [6, 128, 512]),
                        ('blk64', [128, 128]), ('ret_rel', [4, 128, 128]), ('ret_colc', [128, 16]),
                        ('ret_rowc', [64, 256]), ('ret_rowm', [64, 16]), ('hg_trie', [2, 128, 136]), ('hg_mt', [2, 128, 128]),
                        ('hg_e', [2, 128, 8])]:
            self.inp(nm, shp)
        if self.h_in:
            hin = self.inp('h_in', [128, 8 * T])
        out = nc.dram_tensor('out', [NLAT, D], F32, kind="ExternalOutput").ap()
        if self.h_out:
            hout = nc.dram_tensor('h_out', [128, 8 * T], F32, kind="ExternalOutput").ap()

        self.hT = hT = self.sb('hT', [128, 8, T], F32)
        self.aT = aT = self.sb('aT', [128, 8, T], BF16)
        self.ident = ident = self.sb('ident', [128, 128], F32)
        self.identb = identb = self.sb('identb', [128, 128], BF16)
        self.ones_bf = ones_bf = self.sb('ones_bf', [128, 128], BF16)
        self.stage = [self.ar('stage%d' % i, [128, 1024], F32).t for i in range(2)]
        self.modT = modT = self.sb('modT', [128, L, 48, 2], F32)
        self.gmix = gmix = self.sb('gmix', [128, L, 8], F32)
        self.gffn = gffn = self.sb('gffn', [128, L, 8], F32)
        self.gfin = gfin = self.sb('gfin', [128, 8], F32)
        self.A1 = A1 = self.sb('A1', [128, 8, 2], F32)
        self.rstd = rstd = self.sb('rstd', [128, 512], F32)
        self.sel = sel = self.sb('sel', [NEXP, NEXP * 128], F32)
        self.eps_col = self.sb('eps_col', [128, 1], F32)
        self.memset('dve', self.eps_col[:, :], EPS, ['eps_col'])
        self.gT = self.sb('gT', [NEXP, T], F32)

        P.dma('sp', ident[:, :], identd, writes=['ident'])
        P.dma('sp', sel[:, :], seld, writes=['sel'])
        self.cp('dve', identb[:, :], ident[:, :], ['ident'], ['identb'])
        self.memset('dve', ones_bf[:, :], 1.0, ['ones_bf'])

        if self.h_in:
            for c in range(8):
                P.dma('sp', hT[:, c, :], hin[:, c * T:(c + 1) * T], writes=[('hT', c, b) for b in range(5)])
        else:
            for t in range(NT):
                src = ctx[t * 128:(t + 1) * 128, :] if t < 2 else x[(t - 2) * 128:(t - 1) * 128, :]
                st = self.stage[t % 2]
                k = ('stage', t % 2)
                P.dma('sp', st[:, :], src, writes=[k])
                b = self.blk_of_tile(t)
                for half in range(2):
                    pt, pk = self.psum('tr', [0, 1])
                    for j in range(4):
                        c = half * 4 + j
                        self.tr(pt[:, j * 128:(j + 1) * 128], st[:, c * 128:(c + 1) * 128], ident[:, :], [k, 'ident'], pk)
                    self.cp('act' if half else 'dve', hT[:, half * 4:half * 4 + 4, t * 128:(t + 1) * 128],
                            pt[:, :].rearrange("p (j n) -> p j n", j=4), [pk],
                            [('hT', half * 4 + j, b) for j in range(4)])

        for l in range(L):
            self.rows_to_cols(nmg[l].rearrange("(c p) -> c p", p=128), 8, 128, gmix[:, l, :], 'gmix')
            self.rows_to_cols(nfg[l].rearrange("(c p) -> c p", p=128), 8, 128, gffn[:, l, :], 'gffn')
        self.rows_to_cols(fng.rearrange("(c p) -> c p", p=128), 8, 128, gfin[:, :], 'gfin')
        bada = self.sb('bada', [128, L, 48], F32)
        for l in range(L):
            self.rows_to_cols(b_ada[l].rearrange("(m p) -> m p", p=128), 48, 128, bada[:, l, :], 'bada')

        self.colv = colv = self.sb('colv', [128, L, 12], F32)
        for l in range(L):
            qg = self.din['mla_q_norm_g'][l]
            self.rows_to_cols(qg[0:128].rearrange("(o p) -> o p", o=1), 1, 128, colv[:, l, 0:1], 'colv')
            self.rows_to_cols(qg[128:192].rearrange("(o p) -> o p", o=1), 1, 64, colv[0:64, l, 1:2], 'colv')
            self.rows_to_cols(self.din['mla_kv_norm_g'][l].rearrange("(o p) -> o p", o=1), 1, 128, colv[:, l, 2:3], 'colv')
            self.rows_to_cols(self.din['hg_norm_g'][l].rearrange("(c p) -> c p", p=128), 2, 128, colv[:, l, 3:5], 'colv')
            self.rows_to_cols(self.din['ret_gn_g'][l].rearrange("(c p) -> c p", p=128), 2, 128, colv[:, l, 5:7], 'colv')
            self.rows_to_cols(self.din['ret_gn_b'][l].rearrange("(c p) -> c p", p=128), 2, 128, colv[:, l, 7:9], 'colv')

        c2s = self.ar('c2s', [2, D], F32)
        scT = self.sb('scT', [128, 8, 2], F32)
        P.dma('sp', c2s[:, :], c2, writes=['c2s'])
        self.act(c2s[:, :], c2s[:, :], AF.Silu, ['c2s'], ['c2s'])
        for c in range(8):
            pt, pk = self.psum('tr', [0, 1])
            self.tr(pt[:, 0:2], c2s[0:2, c * 128:(c + 1) * 128], ident[0:2, 0:2], ['c2s', 'ident'], pk)
            self.cp('dve', scT[:, c, :], pt[:, 0:2], [pk], ['scT'])
        NCB = 768
        wab = [self.ar('wab%d' % i, [128, 8, NCB], F32) for i in range(2)]
        it = 0
        for l in self.layers:
            for cb in range(6 * D // NCB):
                wt = wab[it % 2]
                it += 1
                P.dma('sp', wt[:, :, :], w_ada[l][:, cb * NCB:(cb + 1) * NCB].rearrange("(c p) n -> p c n", p=128),
                      writes=[wt.k])
                pt, pk = self.psum('tr', [0, 1])
                for m in range(NCB // 128):
                    self.mmg(pt[:, m * 2:(m + 1) * 2],
                             [(wt[:, kc, m * 128:(m + 1) * 128], scT[:, kc, :]) for kc in range(8)],
                             [wt.k, 'scT'], pk)
                nm = NCB // 128
                self.tt(modT[:, l, cb * nm:(cb + 1) * nm, :], pt[:, 0:2 * nm].rearrange("p (m r) -> p m r", r=2),
                        bada[:, l, cb * nm:(cb + 1) * nm].unsqueeze(2).broadcast_to([128, nm, 2]), ALU.add,
                        [pk, 'bada'], [('modT', l)])

        for l in self.layers:
            if len(self.mixers) > 0:
                self.phase()
                self.norm_mod(l, gmix, 0, 1, False)
                self.mixer_phase(l)
            if self.ffn:
                moe = (l % 2 == 1)
                self.phase()
                self.norm_mod(l, gffn, 3, 4, moe)
                if moe:
                    self.moe_route(l)
                self.phase()
                if moe:
                    self.moe_ffn(l)
                else:
                    self.dense_ffn(l)
        self.phase()

        if self.h_out:
            for c in range(8):
                P.dma('sp', hout[:, c * T:(c + 1) * T], hT[:, c, :], reads=[('hT', c, b) for b in range(5)],
                      is_output=True)
        self.final_out(out, gfin)
        P.emit()
        return nc

    def blk_of_tile(self, t):
        return 0 if t < 2 else 1 + (t - 2) // 4

    def norm_mod(self, l, gvec, i_shift, i_scale, want_f32):
        P = self.P
        hT, aT, modT, A1, rstd = self.hT, self.aT, self.modT, self.A1, self.rstd
        self.stt(A1[:, :, :], modT[:, l, i_scale * 8:(i_scale + 1) * 8, :], 1.0,
                 gvec[:, l, :].unsqueeze(2).broadcast_to([128, 8, 2]), ALU.add, ALU.mult,
                 [('modT', l), gvec.k], ['A1'])
        nsq = self.ar('nsq', [128, 8, 512], BF16)
        ntmp = self.ar('ntmp', [128, 8, 512], F32)
        if want_f32:
            self.a32 = self.ar('a32', [128, 8, 512], F32)
            self.wr = self.ar('wr', [128, 8, NEXP], F32)
            self.lgT = self.ar('lgT', [NEXP, T], F32)
            self.P.dma('sp', self.wr[:, :, :], self.mwr[l // 2].rearrange("(c p) e -> p c e", p=128), writes=[self.wr.k])
        for b, (t0, n) in enumerate(BLKS):
            r = 1 if b == 0 else 0
            hk = [('hT', c, b) for c in range(8)]
            self.act(nsq[:, :, 0:n], hT[:, :, t0:t0 + n], AF.Square, hk, [nsq.k])
            pt, pk = self.psum('nrm', [2, 3])
            self.mmg(pt[:, 0:n], [(self.ones_bf[:, :], nsq[:, c, 0:n]) for c in range(8)], [nsq.k, 'ones_bf'], pk)
            self.act(rstd[:, 0:n], pt[:, 0:n], AF.Sqrt, [pk], ['rstd'], bias=self.eps_col[:, 0:1], scale=1.0 / D)
            self.recip(rstd[:, 0:n], rstd[:, 0:n], ['rstd'], ['rstd'])
            self.tt(ntmp[:, :, 0:n], hT[:, :, t0:t0 + n], rstd[:, 0:n].unsqueeze(1).broadcast_to([128, 8, n]),
                    ALU.mult, hk + ['rstd'], [ntmp.k])
            for c in range(8):
                if want_f32:
                    self.ts(self.a32[:, c, 0:n], ntmp[:, c, 0:n], A1[:, c, r:r + 1],
                            modT[:, l, i_shift * 8 + c, r:r + 1], ALU.mult, ALU.add,
                            [ntmp.k, 'A1', ('modT', l)], [('a32', c)])
                    self.cp('act', aT[:, c, t0:t0 + n], self.a32[:, c, 0:n], [('a32', c)], [('aT', b)])
                else:
                    self.ts(aT[:, c, t0:t0 + n], ntmp[:, c, 0:n], A1[:, c, r:r + 1],
                            modT[:, l, i_shift * 8 + c, r:r + 1], ALU.mult, ALU.add,
                            [ntmp.k, 'A1', ('modT', l)], [('aT', b)])
            if want_f32:
                self.route_block(l, b, t0, n)

    def load_w(self, dst, dram, kchunks, ncols, key):
        step = max(1, 4096 // ncols)
        for k0 in range(0, kchunks, step):
            k1 = min(kchunks, k0 + step)
            self.P.dma('pool', dst[:, k0:k1, :], dram[k0 * 128:k1 * 128, :].rearrange("(c p) n -> p c n", p=128),
                       writes=[key])

    def ffn_slices(self, dff):
        s = []
        f = 0
        while f < dff:
            n = min(512, dff - f)
            s.append((f, n))
            f += n
        return s

    def ffn_core(self, l, wg_d, wu_d, wd_d, dff, gate_tile=None, tagbase=''):
        P = self.P
        hT, aT, modT = self.hT, self.aT, self.modT
        for (f0, fn) in self.ffn_slices(dff):
            wg, wu, wd = self.fw[self.fit % 2]
            self.fit += 1
            nj = fn // 128
            self.load_w(wg[:, :, 0:fn], wg_d[:, f0:f0 + fn], 8, fn, wg.k)
            self.load_w(wu[:, :, 0:fn], wu_d[:, f0:f0 + fn], 8, fn, wu.k)
            self.load_w(wd[:, 0:nj, :], wd_d[f0:f0 + fn, :], nj, 1024, wd.k)
            for b, (t0, n) in enumerate(BLKS):
                r = 1 if b == 0 else 0
                hid = self.fhid[self.fh % 2]
                self.fh += 1
                for j in range(nj):
                    pg, pgk = self.psum('ffg', [0, 1])
                    pu, puk = self.psum('ffu', [2, 3])
                    self.mmg(pg[:, 0:n], [(wg[:, kc, j * 128:(j + 1) * 128], aT[:, kc, t0:t0 + n]) for kc in range(8)],
                             [wg.k, ('aT', b)], pgk)
                    self.mmg(pu[:, 0:n], [(wu[:, kc, j * 128:(j + 1) * 128], aT[:, kc, t0:t0 + n]) for kc in range(8)],
                             [wu.k, ('aT', b)], puk)
                    sg = self.fsg[self.fj % 2]
                    self.fj += 1
                    self.act(sg[:, 0:n], pg[:, 0:n], AF.Silu, [pgk], [sg.k])
                    if gate_tile is not None:
                        self.tt(sg[:, 0:n], sg[:, 0:n], gate_tile[:, t0:t0 + n], ALU.mult, [sg.k, gate_tile.k], [sg.k])
                    self.tt(hid[:, j, 0:n], pu[:, 0:n], sg[:, 0:n], ALU.mult, [puk, sg.k], [(hid.k, j)])
                for oc in range(8):
                    py, pyk = self.psum('ffy', [4, 5, 6, 7])
                    self.mmg(py[:, 0:n], [(wd[:, j, oc * 128:(oc + 1) * 128], hid[:, j, 0:n]) for j in range(nj)],
                             [wd.k] + [(hid.k, j) for j in range(nj)], pyk)
                    self.stt(hT[:, oc, t0:t0 + n], py[:, 0:n], modT[:, l, 5 * 8 + oc, r:r + 1], hT[:, oc, t0:t0 + n],
                             ALU.mult, ALU.add, [pyk, ('modT', l), ('hT', oc, b)], [('hT', oc, b)])

    def ffn_alloc(self):
        self.fw = [(self.ar('fwg%d' % i, [128, 8, 512], BF16), self.ar('fwu%d' % i, [128, 8, 512], BF16),
                    self.ar('fwd%d' % i, [128, 4, 1024], BF16)) for i in range(2)]
        self.fsg = [self.ar('fsg%d' % i, [128, 512], BF16) for i in range(2)]
        self.fhid = [self.ar('fhid%d' % i, [128, 4, 512], BF16) for i in range(2)]
        self.fit = 0
        self.fj = 0
        self.fh = 0

    def dense_ffn(self, l):
        i = l // 2
        self.ffn_alloc()
        self.ffn_core(l, self.fwg[i], self.fwu[i], self.fwd[i], D_FF)

    def route_block(self, l, b, t0, n):
        pt, pk = self.psum('nrm', [2, 3])
        self.mmg(pt[0:NEXP, 0:n], [(self.wr[:, c, :], self.a32[:, c, 0:n]) for c in range(8)],
                 [self.wr.k] + [('a32', c) for c in range(8)], pk)
        self.cp('dve', self.lgT[:, t0:t0 + n], pt[0:NEXP, 0:n], [pk], ['lgT'])

    def moe_route(self, l):
        ident = self.ident
        lg = self.ar('lg', [128, NT, NEXP], F32)
        mk1 = self.ar('mk1', [128, NT, NEXP], F32)
        mk2 = self.ar('mk2', [128, NT, NEXP], F32)
        m1 = self.ar('m1', [128, NT], F32)
        m2 = self.ar('m2', [128, NT], F32)
        w1 = self.ar('w1', [128, NT], F32)
        gT = self.gT
        lg.k, mk1.k, mk2.k, m1.k, m2.k, w1.k = 'lg', 'mk1', 'mk2', 'm1', 'm2', 'w1'
        for t in range(NT):
            pt, pk = self.psum('tr', [0, 1])
            self.tr(pt[:, 0:NEXP], self.lgT[0:NEXP, t * 128:(t + 1) * 128], ident[0:NEXP, 0:NEXP], ['lgT', 'ident'], pk)
            self.cp('dve', lg[:, t, :], pt[:, 0:NEXP], [pk], ['lg'])
        P = self.P
        P.op('dve', lambda e: e.tensor_reduce(out=m1[:, :], in_=lg[:, :, :], axis=AX.X, op=ALU.max), reads=['lg'], writes=['m1'])
        self.tt(mk1[:, :, :], lg[:, :, :], m1[:, :].unsqueeze(2).broadcast_to([128, NT, NEXP]), ALU.is_equal,
                ['lg', 'm1'], ['mk1'])
        self.stt(mk2[:, :, :], mk1[:, :, :], -1e30, lg[:, :, :], ALU.mult, ALU.add, ['mk1', 'lg'], ['mk2'])
        P.op('dve', lambda e: e.tensor_reduce(out=m2[:, :], in_=mk2[:, :, :], axis=AX.X, op=ALU.max), reads=['mk2'], writes=['m2'])
        self.tt(mk2[:, :, :], mk2[:, :, :], m2[:, :].unsqueeze(2).broadcast_to([128, NT, NEXP]), ALU.is_equal,
                ['mk2', 'm2'], ['mk2'])
        self.tt(w1[:, :], m1[:, :], m2[:, :], ALU.subtract, ['m1', 'm2'], ['w1'])
        self.act(w1[:, :], w1[:, :], AF.Sigmoid, ['w1'], ['w1'])
        self.tt(mk1[:, :, :], mk1[:, :, :], w1[:, :].unsqueeze(2).broadcast_to([128, NT, NEXP]), ALU.mult,
                ['mk1', 'w1'], ['mk1'])
        self.ts(w1[:, :], w1[:, :], -1.0, 1.0, ALU.mult, ALU.add, ['w1'], ['w1'])
        self.tt(mk2[:, :, :], mk2[:, :, :], w1[:, :].unsqueeze(2).broadcast_to([128, NT, NEXP]), ALU.mult,
                ['mk2', 'w1'], ['mk2'])
        self.tt(mk1[:, :, :], mk1[:, :, :], mk2[:, :, :], ALU.add, ['mk1', 'mk2'], ['mk1'])
        for t in range(NT):
            pt, pk = self.psum('tr', [0, 1])
            self.tr(pt[0:NEXP, 0:128], mk1[:, t, :], ident[:, :], ['mk1', 'ident'], pk)
            self.cp('dve', gT[:, t * 128:(t + 1) * 128], pt[0:NEXP, 0:128], [pk], ['gT'])

    def moe_ffn(self, l):
        i = l // 2
        gT = self.gT
        gbc = [self.ar('gbc%d' % j, [128, T], BF16) for j in range(2)]
        self.ffn_alloc()
        for e_ in range(NEXP):
            gb = gbc[e_ % 2]
            for b, (t0, n) in enumerate(BLKS):
                pt, pk = self.psum('nrm', [2, 3])
                self.mmg(pt[:, 0:n], [(self.sel[:, e_ * 128:(e_ + 1) * 128], gT[:, t0:t0 + n])], ['sel', 'gT'], pk)
                self.cp('act', gb[:, t0:t0 + n], pt[:, 0:n], [pk], [gb.k])
            self.ffn_core(l, self.mwg[i][e_], self.mwu[i][e_], self.mwd[i][e_], D_FFE, gate_tile=gb)


    def mixer_phase(self, l):
        if 0 in self.mixers:
            self.phase()
            self.mla(l)
        if 2 in self.mixers:
            self.phase()
            self.swa(l)
        if 3 in self.mixers:
            self.phase()
            self.ret(l)
        if 1 in self.mixers:
            self.phase()
            self.hgrn(l)

    def colvec(self, dst_ap, dram_vec, n, key):
        self.P.dma('sp', dst_ap, dram_vec.rearrange("(p o) -> p o", o=1), writes=[key])

    def proj(self, out, w, wcols, rhs_fn, reads, pk, kchunks=8):
        c0, c1 = wcols
        self.mmg(out, [(w[:, kc, c0:c1], rhs_fn(kc)) for kc in range(kchunks)], reads, pk)

    def rstd_from(self, dst, src_ps, n_feat, reads, writes):
        p = dst.shape[0] if hasattr(dst, 'shape') else 128
        self.act(dst, src_ps, AF.Sqrt, reads, writes, bias=self.eps_col[0:p, 0:1], scale=1.0 / n_feat)
        self.recip(dst, dst, writes, writes)

    def outproj(self, l, wo, o_blk, okeys, b, t0, n):
        r = 1 if b == 0 else 0
        hT, modT = self.hT, self.modT
        for oc in range(8):
            py, pyk = self.psum('ffy', [4, 5, 6, 7])
            self.mmg(py[:, 0:n], [(wo[:, p, oc * 128:(oc + 1) * 128], o_blk[:, p, 0:n]) for p in range(2)],
                     [wo.k] + okeys, pyk)
            self.stt(hT[:, oc, t0:t0 + n], py[:, 0:n], modT[:, l, 2 * 8 + oc, r:r + 1], hT[:, oc, t0:t0 + n],
                     ALU.mult, ALU.add, [pyk, ('modT', l), ('hT', oc, b)], [('hT', oc, b)])

    def load_wo(self, l, grp):
        wo = self.ar('wo', [128, 2, 1024], BF16)
        self.load_w(wo[:, :, :], self.w_out[l][grp * 256:(grp + 1) * 256, :], 2, 1024, wo.k)
        return wo

    def attn(self, spairs, vfn, tiles, scale, n, half, out_ap, out_key, reads, sink_col=None):
        P = self.P
        pO, pOk = self.psum('aO', [4, 6])
        pD, pDk = self.psum('aD', [5, 7])
        nt = len(tiles)
        for i, (tile, mask) in enumerate(tiles):
            pS, pSk = self.psum('aS', [0, 1, 2])
            self.mmg(pS[:, 0:n], spairs(tile), reads, pSk)
            pt = self.PT[self.pti % 3]
            self.pti += 1
            self.act(pt[:, 0:n], pS[:, 0:n], AF.Exp, [pSk], [pt.k], scale=scale)
            if mask is not None:
                self.tt(pt[:, 0:n], pt[:, 0:n], mask[:, 0:n], ALU.mult, [pt.k, 'swamask'], [pt.k])
            v = vfn(tile)
            P.op('pe', lambda e, v=v, pt=pt, s=(i == 0), t=(i == nt - 1): e.matmul(pO[:, 0:n], lhsT=v, rhs=pt[:, 0:n], start=s, stop=t),
                 reads=reads + [pt.k], writes=[pOk], inc=False)
            P.op('pe', lambda e, pt=pt, s=(i == 0), t=(i == nt - 1): e.matmul(pD[:, 0:n], lhsT=self.ones_bf[:, :], rhs=pt[:, 0:n], start=s, stop=t),
                 reads=[pt.k, 'ones_bf'], writes=[pOk, pDk], inc=(i == nt - 1))
        r0 = half * 64
        rd = self.rd
        if sink_col is not None:
            self.ts(rd[r0:r0 + 64, 0:n], pD[r0:r0 + 64, 0:n], sink_col[r0:r0 + 64, :], None, ALU.add, None, [pDk, 'esink'], ['rd'])
            self.recip(rd[r0:r0 + 64, 0:n], rd[r0:r0 + 64, 0:n], ['rd'], ['rd'])
        else:
            self.recip(rd[r0:r0 + 64, 0:n], pD[r0:r0 + 64, 0:n], [pDk], ['rd'])
        self.tt(out_ap, pO[r0:r0 + 64, 0:n], rd[r0:r0 + 64, 0:n], ALU.mult, [pOk, 'rd'], [out_key])

    def mla(self, l):
        P = self.P
        aT = self.aT
        wm = self.ar('wm', [128, 8, 352], BF16)
        self.load_w(wm[:, :, :], self.w_in[l][:, 0:352], 8, 352, wm.k)
        wuq = self.ar('wuq', [128, 2, 384], BF16)
        P.dma('pool', wuq[:, 0, :], self.din['mla_w_uq'][l][0:128, :], writes=[wuq.k])
        P.dma('pool', wuq[0:64, 1, :], self.din['mla_w_uq'][l][128:192, :], writes=[wuq.k])
        wukv = self.ar('wukv', [128, 512], BF16)
        P.dma('pool', wukv[:, :], self.din['mla_w_ukv'][l], writes=[wukv.k])
        cv = self.colv
        self.ts(wuq[:, 0, :], wuq[:, 0, :], cv[:, l, 0:1], None, ALU.mult, None, [wuq.k, 'colv'], [wuq.k])
        self.ts(wuq[0:64, 1, :], wuq[0:64, 1, :], cv[0:64, l, 1:2], None, ALU.mult, None, [wuq.k, 'colv'], [wuq.k])
        self.ts(wukv[:, :], wukv[:, :], cv[:, l, 2:3], None, ALU.mult, None, [wukv.k, 'colv'], [wukv.k])
        r32 = self.ar('r32', [32, 32], BF16)
        P.dma('pool', r32[:, :], self.din['r64b'][0:32, 0:32], writes=[r32.k])
        wo = self.load_wo(l, 0)
        kN = self.ar('kN', [64, 4, T], BF16)
        kP = self.ar('kP', [32, T], BF16)
        vt = self.ar('vt', [128, NT, 256], BF16)
        cs = self.ar('cs', [32, 2, 512], F32)
        kvl = self.ar('kvl', [128, 512], BF16)
        sq = self.ar('sq', [128, 2, 512], BF16)
        rs = self.ar('rs', [128, 512], F32)
        rsc = self.ar('rsc', [128, NT], F32)
        xb = self.ar('xb', [32, 512], BF16)
        t1 = self.ar('t1', [32, 512], F32)
        t2 = self.ar('t2', [32, 512], F32)
        qlb = self.ar('qlb', [128, 2, 512], BF16)
        qp32 = self.ar('qp32', [32, 512], F32)
        qn = [self.ar('qn%d' % i, [64, 512], BF16) for i in range(2)]
        qp = [self.ar('qp%d' % i, [32, 512], BF16) for i in range(2)]
        oblk = [self.ar('oblk%d' % i, [128, 2, 512], BF16) for i in range(2)]
        self.PT = [self.ar('PT%d' % i, [128, 512], BF16) for i in range(3)]
        self.pti = 0
        self.rd = self.ar('rd', [128, 512], F32)
        self.rd.k = 'rd'
        cosd, sind = self.din['cos_mla'], self.din['sin_mla']

        def rope32(dst, src, skey, n, wkey):
            self.cp('act', xb[:, 0:n], src, [skey], [xb.k])
            pr, prk = self.psum('mB', [2, 3])
            self.mmg(pr[0:32, 0:n], [(r32[:, :], xb[:, 0:n])], [r32.k, xb.k], prk)
            RV = int(os.environ.get('ROPEV', '9'))
            if RV == 1:
                self.tt(t1[:, 0:n], src, src, ALU.mult, [skey], [t1.k])
                return
            if RV == 5:
                return
            if RV == 3:
                self.tt(t1[:, 0:n], rs[0:32, 0:n], rs[0:32, 0:n], ALU.mult, [rs.k], [t1.k])
                return
            if RV == 4:
                self.tt(rs[0:32, 0:n], cs[:, 0, 0:n], cs[:, 0, 0:n], ALU.mult, [cs.k, rs.k], [rs.k])
                return
            if RV == 2:
                self.tt(t1[:, 0:n], cs[:, 0, 0:n], cs[:, 0, 0:n], ALU.mult, [cs.k], [t1.k])
                return
            self.tt(t1[:, 0:n], src, cs[:, 0, 0:n], ALU.mult, [skey, cs.k, xb.k], [t1.k])
            self.tt(t2[:, 0:n], pr[0:32, 0:n], cs[:, 1, 0:n], ALU.mult, [prk, cs.k], [t2.k])
            self.tt(dst, t1[:, 0:n], t2[:, 0:n], ALU.add, [t1.k, t2.k], [wkey])

        for b, (t0, n) in enumerate(BLKS):
            ak = ('aT', b)
            P.dma('sp', cs[:, 0, 0:n], cosd[:, t0:t0 + n], writes=[cs.k])
            P.dma('sp', cs[:, 1, 0:n], sind[:, t0:t0 + n], writes=[cs.k])
            pk_, pkk = self.psum('mA', [0, 1])
            self.proj(pk_[:, 0:n], wm, (192, 320), lambda kc: aT[:, kc, t0:t0 + n], [wm.k, ak], pkk)
            self.cp('act', kvl[:, 0:n], pk_[:, 0:n], [pkk], [kvl.k])
            self.act(sq[:, 0, 0:n], pk_[:, 0:n], AF.Square, [pkk], [sq.k])
            ps_, psk = self.psum('mB', [2, 3])
            self.mmg(ps_[:, 0:n], [(self.ones_bf[:, :], sq[:, 0, 0:n])], [sq.k, 'ones_bf'], psk)
            self.rstd_from(rs[:, 0:n], ps_[:, 0:n], 128.0, [psk], [rs.k])
            for tt_ in range(n // 128):
                tile = t0 // 128 + tt_
                pc, pck = self.psum('mB', [2, 3])
                self.mmg(pc[:, 0:1], [(sq[:, 0, tt_ * 128:(tt_ + 1) * 128], self.ones_bf[:, 0:1])], [sq.k, 'ones_bf'], pck)
                self.rstd_from(rsc[:, tile:tile + 1], pc[:, 0:1], 128.0, [pck], [rsc.k])
                pv, pvk = self.psum('mA', [0, 1])
                for h in range(4):
                    self.mmg(pv[:, h * 64:(h + 1) * 64], [(kvl[:, tt_ * 128:(tt_ + 1) * 128], wukv[:, h * 128 + 64:h * 128 + 128])],
                             [kvl.k, wukv.k], pvk)
                self.ts(vt[:, tile, :], pv[:, 0:256], rsc[:, tile:tile + 1], None, ALU.mult, None, [pvk, rsc.k], [vt.k])
            for h in range(4):
                pk2, pk2k = self.psum('mA', [0, 1])
                self.mmg(pk2[0:64, 0:n], [(wukv[:, h * 128:h * 128 + 64], kvl[:, 0:n])], [kvl.k, wukv.k], pk2k)
                self.tt(kN[:, h, t0:t0 + n], pk2[0:64, 0:n], rs[0:64, 0:n], ALU.mult, [pk2k, rs.k], [kN.k])
            pp, ppk = self.psum('mA', [0, 1])
            self.proj(pp[0:32, 0:n], wm, (320, 352), lambda kc: aT[:, kc, t0:t0 + n], [wm.k, ak], ppk)
            rope32(kP[:, t0:t0 + n], pp[0:32, 0:n], ppk, n, kP.k)
        STOP = int(os.environ.get('MLA_STOP', '9'))
        if STOP <= 2:
            return
        scale = 96.0 ** -0.5
        qi = 0
        for b, (t0, n) in enumerate(BLKS):
            ak = ('aT', b)
            P.dma('sp', cs[:, 0, 0:n], cosd[:, t0:t0 + n], writes=[cs.k])
            P.dma('sp', cs[:, 1, 0:n], sind[:, t0:t0 + n], writes=[cs.k])
            p0, p0k = self.psum('mA', [0, 1])
            self.proj(p0[:, 0:n], wm, (0, 128), lambda kc: aT[:, kc, t0:t0 + n], [wm.k, ak], p0k)
            p1, p1k = self.psum('mA', [0, 1])
            self.proj(p1[0:64, 0:n], wm, (128, 192), lambda kc: aT[:, kc, t0:t0 + n], [wm.k, ak], p1k)
            self.cp('act', qlb[:, 0, 0:n], p0[:, 0:n], [p0k], [qlb.k])
            self.cp('act', qlb[0:64, 1, 0:n], p1[0:64, 0:n], [p1k], [qlb.k])
            self.act(sq[:, 0, 0:n], p0[:, 0:n], AF.Square, [p0k], [sq.k])
            self.act(sq[0:64, 1, 0:n], p1[0:64, 0:n], AF.Square, [p1k], [sq.k])
            ps_, psk = self.psum('mB', [2, 3])
            self.mmg(ps_[:, 0:n], [(self.ones_bf[:, :], sq[:, 0, 0:n]), (self.ones_bf[0:64, :], sq[0:64, 1, 0:n])],
                     [sq.k, 'ones_bf'], psk)
            self.rstd_from(rs[:, 0:n], ps_[:, 0:n], 192.0, [psk], [rs.k])
            ob = oblk[b % 2]
            tiles = [(0, None), (1, None)] if b == 0 else [(t, None) for t in range(NT)]
            if STOP <= 3:
                continue
            for h in range(4):
                qn_, qp_ = qn[qi % 2], qp[qi % 2]
                qi += 1
                pq, pqk = self.psum('mB', [2, 3])
                self.mmg(pq[0:64, 0:n], [(wuq[:, 0, h * 96:h * 96 + 64], qlb[:, 0, 0:n]),
                                         (wuq[0:64, 1, h * 96:h * 96 + 64], qlb[0:64, 1, 0:n])], [wuq.k, qlb.k], pqk)
                self.tt(qn_[:, 0:n], pq[0:64, 0:n], rs[0:64, 0:n], ALU.mult, [pqk, rs.k], [qn_.k])
                pq2, pq2k = self.psum('mB', [2, 3])
                self.mmg(pq2[0:32, 0:n], [(wuq[:, 0, h * 96 + 64:h * 96 + 96], qlb[:, 0, 0:n]),
                                          (wuq[0:64, 1, h * 96 + 64:h * 96 + 96], qlb[0:64, 1, 0:n])], [wuq.k, qlb.k], pq2k)
                self.tt(qp32[:, 0:n], pq2[0:32, 0:n], rs[0:32, 0:n], ALU.mult, [pq2k, rs.k], [qp32.k])
                rope32(qp_[:, 0:n], qp32[:, 0:n], qp32.k, n, qp_.k)
                p_, hh = h // 2, h % 2
                if STOP <= 4:
                    continue
                self.attn(lambda t, h=h, qn_=qn_, qp_=qp_, n=n: [(kN[:, h, t * 128:(t + 1) * 128], qn_[:, 0:n]),
                                                                 (kP[:, t * 128:(t + 1) * 128], qp_[:, 0:n])],
                          lambda t, p_=p_: vt[:, t, p_ * 128:(p_ + 1) * 128], tiles, scale, n, hh,
                          ob[hh * 64:hh * 64 + 64, p_, 0:n], (ob.k, h), [kN.k, kP.k, vt.k, qn_.k, qp_.k])
            if STOP <= 5:
                continue
            self.outproj(l, wo, ob, [(ob.k, h) for h in range(4)], b, t0, n)

    def swa(self, l):
        P = self.P
        aT = self.aT
        ws = self.ar('ws', [128, 8, 512], BF16)
        self.load_w(ws[:, :, :], self.w_in[l][:, C_SQ:C_SQ + 512], 8, 512, ws.k)
        r128 = self.ar('r128', [128, 128], BF16)
        P.dma('pool', r128[:, :], self.din['r128'], writes=[r128.k])
        wo = self.load_wo(l, 2)
        msk = self.ar('swamask', [128, 6, 512], BF16)
        msk.k = 'swamask'
        for r in range(6):
            P.dma('pool', msk[:, r, :], self.din['swa_mask'][r], writes=['swamask'])
        esink = self.ar('esink', [128, 4], F32)
        esink.k = 'esink'
        P.dma('sp', esink[:, :], self.din['swa_sink'][l:l + 1, :].broadcast_to([128, 4]), writes=['esink'])
        self.act(esink[:, :], esink[:, :], AF.Exp, ['esink'], ['esink'])
        kx = self.ar('kx', [128, T], BF16)
        kxs = self.ar('kxs', [128, T], BF16)
        vn = self.ar('vn', [128, NT, 128], BF16)
        vs = self.ar('vs', [128, NT, 128], BF16)
        cs = self.ar('cs', [128, 2, 512], F32)
        xb = self.ar('xb', [128, 512], BF16)
        t1 = self.ar('t1', [128, 512], F32)
        t2 = self.ar('t2', [128, 512], F32)
        qx = [self.ar('qx%d' % i, [128, 2, 512], BF16) for i in range(2)]
        oblk = [self.ar('oblk%d' % i, [128, 2, 512], BF16) for i in range(2)]
        self.PT = [self.ar('PT%d' % i, [128, 512], BF16) for i in range(3)]
        self.pti = 0
        self.rd = self.ar('rd', [128, 512], F32)
        self.rd.k = 'rd'
        cosd, sind = self.din['cos_swa'], self.din['sin_swa']

        def rope(dst, ps_x, psk, n, wkey):
            self.cp('act', xb[:, 0:n], ps_x[:, 0:n], [psk], [xb.k])
            pr, prk = self.psum('mB', [2, 3])
            self.mmg(pr[:, 0:n], [(r128[:, :], xb[:, 0:n])], [r128.k, xb.k], prk)
            self.tt(t1[:, 0:n], ps_x[:, 0:n], cs[:, 0, 0:n], ALU.mult, [psk, cs.k], [t1.k])
            self.tt(t2[:, 0:n], pr[:, 0:n], cs[:, 1, 0:n], ALU.mult, [prk, cs.k], [t2.k])
            self.tt(dst, t1[:, 0:n], t2[:, 0:n], ALU.add, [t1.k, t2.k], [wkey])

        for b, (t0, n) in enumerate(BLKS):
            ak = ('aT', b)
            for hf in range(2):
                P.dma('sp', cs[hf * 64:(hf + 1) * 64, 0, 0:n], cosd[:, t0:t0 + n], writes=[cs.k])
                P.dma('sp', cs[hf * 64:(hf + 1) * 64, 1, 0:n], sind[:, t0:t0 + n], writes=[cs.k])
            pk_, pkk = self.psum('mA', [0, 1])
            self.proj(pk_[:, 0:n], ws, (256, 384), lambda kc: aT[:, kc, t0:t0 + n], [ws.k, ak], pkk)
            rope(kx[:, t0:t0 + n], pk_, pkk, n, kx.k)
            for tt_ in range(n // 128):
                tile = t0 // 128 + tt_
                pv, pvk = self.psum('mA', [0, 1])
                self.mmg(pv[:, 0:128], [(aT[:, kc, tile * 128:(tile + 1) * 128], ws[:, kc, 384:512]) for kc in range(8)],
                         [ws.k, ak], pvk)
                self.cp('act', vn[:, tile, :], pv[:, 0:128], [pvk], [vn.k])
                self.cp('dve', vs[:, tile, 0:64], pv[:, 64:128], [pvk], [vs.k])
                self.cp('dve', vs[:, tile, 64:128], pv[:, 0:64], [pvk], [vs.k])
        P.dma('sp', kxs[0:64, :], kx[64:128, :], reads=[kx.k], writes=[kxs.k])
        P.dma('sp', kxs[64:128, :], kx[0:64, :], reads=[kx.k], writes=[kxs.k])
        scale = 64.0 ** -0.5
        for b, (t0, n) in enumerate(BLKS):
            ak = ('aT', b)
            for hf in range(2):
                P.dma('sp', cs[hf * 64:(hf + 1) * 64, 0, 0:n], cosd[:, t0:t0 + n], writes=[cs.k])
                P.dma('sp', cs[hf * 64:(hf + 1) * 64, 1, 0:n], sind[:, t0:t0 + n], writes=[cs.k])
            q_ = qx[b % 2]
            for p_ in range(2):
                pq, pqk = self.psum('mA', [0, 1])
                self.proj(pq[:, 0:n], ws, (p_ * 128, (p_ + 1) * 128), lambda kc: aT[:, kc, t0:t0 + n], [ws.k, ak], pqk)
                rope(q_[:, p_, 0:n], pq, pqk, n, q_.k)
            ob = oblk[b % 2]
            if b == 0:
                tiles = [(0, None), (1, None)]
            else:
                g0 = t0 // 128
                tiles = [(0, None), (1, None)]
                for r in range(-1, 5):
                    j = g0 + r
                    if 2 <= j <= 17:
                        tiles.append((j, msk[:, r + 1, :]))
            for h in range(4):
                g, hh, p_ = h // 2, h % 2, h // 2
                ksrc = kx if g == hh else kxs
                vsrc = vn if g == hh else vs
                self.attn(lambda t, ksrc=ksrc, hh=hh, q_=q_, p_=p_, n=n: [(ksrc[hh * 64:hh * 64 + 64, t * 128:(t + 1) * 128],
                                                                               q_[hh * 64:hh * 64 + 64, p_, 0:n])],
                          lambda t, vsrc=vsrc: vsrc[:, t, :], tiles, scale, n, hh,
                          ob[hh * 64:hh * 64 + 64, p_, 0:n], (ob.k, h), [kx.k, kxs.k, vn.k, vs.k, q_.k],
                          sink_col=esink[:, h:h + 1])
            self.outproj(l, wo, ob, [(ob.k, h) for h in range(4)], b, t0, n)

    def headnorm_gate(self, l, o32, okey, n, t0, b, w, gcols, center, gcol, bcol, oblk, ak):
        aT = self.aT
        ob16 = self.hn_b16
        c32 = self.hn_c32
        blk64 = self.blk64
        for p in range(2):
            src = o32[:, p, 0:n]
            if center:
                self.cp('act', ob16[:, 0:n], src, [okey], [ob16.k])
                pm, pmk = self.psum('mB', [2, 3])
                self.mmg(pm[:, 0:n], [(blk64[:, :], ob16[:, 0:n])], [ob16.k, blk64.k], pmk)
                self.stt(c32[:, 0:n], pm[:, 0:n], -1.0, src, ALU.mult, ALU.add, [okey, pmk], [c32.k])
                cs_ = c32[:, 0:n]
                ck = c32.k
            else:
                cs_ = src
                ck = okey
            self.act(ob16[:, 0:n], cs_, AF.Square, [ck], [ob16.k])
            pv_, pvk = self.psum('mB', [2, 3])
            self.mmg(pv_[:, 0:n], [(blk64[:, :], ob16[:, 0:n])], [ob16.k, blk64.k], pvk)
            rs = self.hn_rs
            self.rstd_from(rs[:, 0:n], pv_[:, 0:n], 1.0, [pvk], [rs.k])
            self.tt(c32[:, 0:n], cs_, rs[:, 0:n], ALU.mult, [ck, rs.k], [c32.k])
            if bcol is not None:
                self.ts(c32[:, 0:n], c32[:, 0:n], self.colv[:, l, gcol + p:gcol + p + 1],
                        self.colv[:, l, bcol + p:bcol + p + 1], ALU.mult, ALU.add, [c32.k, 'colv'], [c32.k])
            else:
                self.ts(c32[:, 0:n], c32[:, 0:n], self.colv[:, l, gcol + p:gcol + p + 1], None, ALU.mult, None,
                        [c32.k, 'colv'], [c32.k])
            pg, pgk = self.psum('mA', [0, 1])
            self.proj(pg[:, 0:n], w, (gcols + p * 128, gcols + (p + 1) * 128), lambda kc: aT[:, kc, t0:t0 + n], [w.k, ak], pgk)
            sg = self.hn_sg
            self.act(sg[:, 0:n], pg[:, 0:n], AF.Silu, [pgk], [sg.k])
            self.tt(oblk[:, p, 0:n], c32[:, 0:n], sg[:, 0:n], ALU.mult, [c32.k, sg.k], [(oblk.k, p)])

    def hn_alloc(self):
        self.hn_b16 = self.ar('hn_b16', [128, 512], BF16)
        self.hn_c32 = self.ar('hn_c32', [128, 512], F32)
        self.hn_rs = self.ar('hn_rs', [128, 512], F32)
        self.hn_sg = self.ar('hn_sg', [128, 512], BF16)
        self.blk64 = self.ar('blk64', [128, 128], BF16)
        self.P.dma('pool', self.blk64[:, :], self.din['blk64'], writes=[self.blk64.k])

    def ret(self, l):
        P = self.P
        aT = self.aT
        ident = self.ident
        r64b = self.ar('r64b', [64, 64], BF16)
        P.dma('pool', r64b[:, :], self.din['r64b'], writes=[r64b.k])
        wo = self.load_wo(l, 3)
        qx = self.ar('qx', [64, 2, T], BF16)
        kx = self.ar('kx', [64, 2, T], BF16)
        kxt = self.ar('kxt', [128, NT, 128], BF16)
        vt = self.ar('vt', [128, NT, 256], BF16)
        Sf = self.ar('Sf', [64, NT, 2, 128], BF16)
        Dsum = self.ar('Dsum', [128, 4, 128], BF16)
        zeta = self.ar('zeta', [128, 2, 4], F32)
        lgcol = self.ar('lgcol', [64, 2, 2], F32)
        xi = self.ar('xi', [64, 2, 2, 128], F32)
        dc = self.ar('dc', [64, 2, 2], F32)
        mark0 = self.ar_off
        w = self.ar('wr_', [128, 8, 512], BF16)
        self.load_w(w[:, :, :], self.w_in[l][:, C_RQ:C_RQ + 512], 8, 512, w.k)
        mark1 = self.ar_off
        lgb = self.ar('lgb', [128, 8], F32)
        rel = self.ar('rel', [128, 4, 128], F32)
        colc = self.ar('colc', [128, 16], F32)
        rowc = self.ar('rowc', [64, 2, 128], F32)
        dtmp = self.ar('dtmp', [128, 2, 128], F32)
        cosd, sind = self.din['cos_ret'], self.din['sin_ret']
        P.dma('sp', lgb[:, :], self.din['ret_decay_logit'][l:l + 1].rearrange("o d h -> o (d h)").broadcast_to([128, 8]),
              writes=[lgb.k])
        P.dma('sp', rel[:, :, :], self.din['ret_rel'].rearrange("r s t -> s r t"), writes=[rel.k])
        P.dma('sp', colc[:, :], self.din['ret_colc'], writes=[colc.k])
        P.dma('sp', rowc[:, :, :], self.din['ret_rowc'].rearrange("p (d t) -> p d t", d=2), writes=[rowc.k])
        self.act(lgb[:, :], lgb[:, :], AF.Exp, [lgb.k], [lgb.k], scale=-1.0)
        self.ts(lgb[:, :], lgb[:, :], 1.0, None, ALU.add, None, [lgb.k], [lgb.k])
        self.act(lgb[:, :], lgb[:, :], AF.Ln, [lgb.k], [lgb.k])
        self.ts(lgb[:, :], lgb[:, :], -1.0, None, ALU.mult, None, [lgb.k], [lgb.k])
        for h in range(4):
            self.act(dtmp[:, 0, :], rel[:, 0, :], AF.Exp, [rel.k, lgb.k], [dtmp.k], scale=lgb[:, h:h + 1])
            self.tt(dtmp[:, 0, :], dtmp[:, 0, :], rel[:, 1, :], ALU.mult, [dtmp.k, rel.k], [dtmp.k])
            self.act(dtmp[:, 1, :], rel[:, 2, :], AF.Exp, [rel.k, lgb.k], [dtmp.k], scale=lgb[:, 4 + h:5 + h])
            self.tt(dtmp[:, 1, :], dtmp[:, 1, :], rel[:, 3, :], ALU.mult, [dtmp.k, rel.k], [dtmp.k])
            self.tt(Dsum[:, h, :], dtmp[:, 0, :], dtmp[:, 1, :], ALU.add, [dtmp.k], [Dsum.k])
        for d in range(2):
            self.act(zeta[:, d, :], lgb[:, d * 4:d * 4 + 4], AF.Exp, [lgb.k, colc.k], [zeta.k], scale=colc[:, d:d + 1])
            for p in range(2):
                self.cp('dve', lgcol[0:32, d, p:p + 1], lgb[0:32, d * 4 + 2 * p:d * 4 + 2 * p + 1], [lgb.k], [lgcol.k])
                self.cp('dve', lgcol[32:64, d, p:p + 1], lgb[32:64, d * 4 + 2 * p + 1:d * 4 + 2 * p + 2], [lgb.k], [lgcol.k])
        for d in range(2):
            for p in range(2):
                self.act(xi[:, d, p, :], rowc[:, d, :], AF.Exp, [rowc.k, lgcol.k], [xi.k], scale=lgcol[:, d, p:p + 1])
        self.act(dc[:, :, :], lgcol[:, :, :], AF.Exp, [lgcol.k], [dc.k], scale=128.0)
        RSTOP = int(os.environ.get('RET_STOP', '9'))
        if RSTOP <= 1:
            return
        self.ar_release(mark1)
        cs = self.ar('cs', [64, 2, 512], F32)
        xb = self.ar('xb', [64, 512], BF16)
        t1 = self.ar('t1', [64, 512], F32)
        t2 = self.ar('t2', [64, 512], F32)

        def rope64(dst, ps_x, psk, n, wkey):
            self.cp('act', xb[:, 0:n], ps_x, [psk], [xb.k])
            pr, prk = self.psum('mB', [2, 3])
            self.mmg(pr[0:64, 0:n], [(r64b[:, :], xb[:, 0:n])], [r64b.k, xb.k], prk)
            self.tt(t1[:, 0:n], ps_x, cs[:, 0, 0:n], ALU.mult, [psk, cs.k], [t1.k])
            self.tt(t2[:, 0:n], pr[0:64, 0:n], cs[:, 1, 0:n], ALU.mult, [prk, cs.k], [t2.k])
            self.tt(t1[:, 0:n], t1[:, 0:n], t2[:, 0:n], ALU.add, [t1.k, t2.k], [t1.k])
            self.cp('act', dst, t1[:, 0:n], [t1.k], [wkey])

        for b, (t0, n) in enumerate(BLKS):
            ak = ('aT', b)
            for hf in range(2):
                P.dma('sp', cs[hf * 32:(hf + 1) * 32, 0, 0:n], cosd[:, t0:t0 + n], writes=[cs.k])
                P.dma('sp', cs[hf * 32:(hf + 1) * 32, 1, 0:n], sind[:, t0:t0 + n], writes=[cs.k])
            for p in range(2):
                pq, pqk = self.psum('mA', [0, 1])
                self.proj(pq[0:64, 0:n], w, (p * 64, (p + 1) * 64), lambda kc: aT[:, kc, t0:t0 + n], [w.k, ak], pqk)
                rope64(qx[:, p, t0:t0 + n], pq[0:64, 0:n], pqk, n, qx.k)
                pk_, pkk = self.psum('mA', [0, 1])
                self.proj(pk_[0:64, 0:n], w, (128 + p * 64, 128 + (p + 1) * 64), lambda kc: aT[:, kc, t0:t0 + n], [w.k, ak], pkk)
                rope64(kx[:, p, t0:t0 + n], pk_[0:64, 0:n], pkk, n, kx.k)
                for tt_ in range(n // 128):
                    tile = t0 // 128 + tt_
                    ptr, ptk = self.psum('mB', [2, 3])
                    self.tr(ptr[:, 0:64], t1[:, tt_ * 128:(tt_ + 1) * 128], ident[0:64, 0:64], [t1.k, 'ident'], ptk)
                    self.cp('act', kxt[:, tile, p * 64:(p + 1) * 64], ptr[:, 0:64], [ptk], [kxt.k])
            for tt_ in range(n // 128):
                tile = t0 // 128 + tt_
                pv, pvk = self.psum('mA', [0, 1])
                self.mmg(pv[:, 0:256], [(aT[:, kc, tile * 128:(tile + 1) * 128], w[:, kc, 256:512]) for kc in range(8)],
                         [w.k, ak], pvk)
                self.cp('act', vt[:, tile, :], pv[:, 0:256], [pvk], [vt.k])

        if RSTOP <= 2:
            return
        self.ar_release(mark0)
        w = self.ar('wg_', [128, 8, 256], BF16)
        self.load_w(w[:, :, :], self.w_in[l][:, C_RG:C_RG + 256], 8, 256, w.k)
        self.hn_alloc()
        S = self.ar('S', [64, 2, 128], F32)
        Sbb = self.ar('Sbb', [64, 2, 128], BF16)
        vz = self.ar('vz', [128, 256], BF16)
        qxm = self.ar('qxm', [64, 2, 2, 128], BF16)
        qxd = self.ar('qxd', [64, 2, 2, 2, 128], BF16)
        rowm = self.ar('rowm', [64, 16], F32)
        P.dma('sp', rowm[:, :], self.din['ret_rowm'], writes=[rowm.k])
        AT = self.ar('AT', [128, 4, 128], BF16)
        o32 = self.ar('o32', [128, 2, 512], F32)
        oblk = self.ar('oblk', [128, 2, 512], BF16)

        def state_step(d, tile):
            self.tt(vz[:, :].rearrange("s (h v) -> s h v", h=4), vt[:, tile, :].rearrange("s (h v) -> s h v", h=4),
                    zeta[:, d, :].unsqueeze(2).broadcast_to([128, 4, 64]), ALU.mult, [vt.k, zeta.k], [vz.k])
            pu, puk = self.psum('mA', [0, 1])
            for p in range(2):
                self.mmg(pu[0:64, p * 128:(p + 1) * 128], [(kxt[:, tile, p * 64:(p + 1) * 64], vz[:, p * 128:(p + 1) * 128])],
                         [kxt.k, vz.k], puk)
            for p in range(2):
                self.stt(S[:, p, :], S[:, p, :], dc[:, d, p:p + 1], pu[0:64, p * 128:(p + 1) * 128], ALU.mult, ALU.add,
                         [S.k, dc.k, puk], [S.k])

        self.memset('dve', S[:, :, :], 0.0, [S.k])
        for tile in range(NT):
            self.cp('act', Sf[:, tile, :, :], S[:, :, :], [S.k], [(Sf.k, tile)])
            if tile < NT - 1:
                state_step(0, tile)
        if RSTOP <= 3:
            return
        self.memset('dve', S[:, :, :], 0.0, [S.k])
        order = [1, 0] + list(range(NT - 1, 1, -1))
        for idx, tile in enumerate(order):
            self.cp('act', Sbb[:, :, :], S[:, :, :], [S.k], [Sbb.k])
            for hh in range(2):
                self.ts(qxm[:, hh, :, :], qx[:, :, tile * 128:(tile + 1) * 128], rowm[:, hh:hh + 1], None, ALU.mult, None,
                        [qx.k, rowm.k], [qxm.k])
            for d in range(2):
                for hh in range(2):
                    self.tt(qxd[:, d, hh, :, :], qxm[:, hh, :, :], xi[:, d, :, :], ALU.mult, [qxm.k, xi.k], [qxd.k])
            pA, pAk = self.psum('rA', [4, 5])
            for h in range(4):
                hh, p = h % 2, h // 2
                self.mmg(pA[:, h * 128:(h + 1) * 128], [(kx[:, p, tile * 128:(tile + 1) * 128], qxm[:, hh, p, :])],
                         [kx.k, qxm.k], pAk)
            self.tt(AT[:, :, :], pA[:, :].rearrange("s (h t) -> s h t", h=4), Dsum[:, :, :], ALU.mult, [pAk, Dsum.k], [AT.k])
            if RSTOP <= 4:
                continue
            pO, pOk = self.psum('rO', [6, 7])
            for h in range(4):
                hh, p = h % 2, h // 2
                self.mmg(pO[:, h * 128:(h + 1) * 128],
                         [(vt[:, tile, p * 128:(p + 1) * 128], AT[:, h, :]),
                          (Sf[:, tile, p, :], qxd[:, 0, hh, p, :]),
                          (Sbb[:, p, :], qxd[:, 1, hh, p, :])],
                         [vt.k, AT.k, (Sf.k, tile), Sbb.k, qxd.k], pOk)
            b = self.blk_of_tile(tile)
            t0, n = BLKS[b]
            off = tile * 128 - t0
            pv4 = pO[:, :].rearrange("q (p j t) -> q p j t", p=2, j=2)
            for hh in range(2):
                self.ts(o32[hh * 64:hh * 64 + 64, :, off:off + 128], pv4[hh * 64:hh * 64 + 64, :, hh, :], 32.0 ** -0.5, None,
                        ALU.mult, None, [pOk], [(o32.k, tile % 4, hh)])
            if RSTOP <= 5:
                continue
            if idx < NT - 1:
                state_step(1, tile)
            if RSTOP <= 6:
                continue
            if tile * 128 == t0:
                ntile = n // 128
                okeys = [(o32.k, (t0 // 128 + j) % 4, hh) for j in range(ntile) for hh in range(2)]
                self.P.op('dve', lambda e: e.tensor_copy(out=o32[:, :, 0:1], in_=o32[:, :, 0:1]), reads=okeys, writes=[o32.k] + okeys)
                self.headnorm_gate(l, o32, o32.k, n, t0, b, w, 0, True, 5, 7, oblk, ('aT', b))
                self.outproj(l, wo, oblk, [(oblk.k, 0), (oblk.k, 1)], b, t0, n)


    def hgrn(self, l):
        P = self.P
        aT = self.aT
        ident = self.ident
        wh = self.ar('wh', [128, 8, 1280], BF16)
        self.load_w(wh[:, :, :], self.w_in[l][:, C_HQ:C_HQ + 1280], 8, 1280, wh.k)
        itm = self.ar('itm', [128, NT, 256], BF16)
        ohg = self.ar('ohg', [128, 2, T], BF16)
        trie = self.ar('trie', [128, 2, 136], F32)
        P.dma('sp', trie[:, :, :], self.din['hg_trie'].rearrange("d s c -> s d c"), writes=[trie.k])
        mt = self.ar('mt', [128, 2, 128], BF16)
        P.dma('pool', mt[:, :, :], self.din['hg_mt'].rearrange("d s c -> s d c"), writes=[mt.k])
        ee = self.ar('ee', [128, 2, 8], BF16)
        P.dma('pool', ee[:, :, :], self.din['hg_e'].rearrange("d s c -> s d c"), writes=[ee.k])
        omlF = self.ar('omlF', [64, 8], F32)
        nomlF = self.ar('nomlF', [64, 8], F32)
        lbB = self.ar('lbB', [128, 512], F32)
        omlB = self.ar('omlB', [128, 512], F32)
        mark = self.ar_off
        st = self.ar('st', [32, 64], F32)
        lbl = self.ar('lbl', [64, 4, 8], F32)
        sF = self.ar('sF', [64, 8], F32)
        cF = self.ar('cF', [64, 8], F32)
        P.dma('sp', st[:, :], self.din['hg_lb_logits'].rearrange("l d (h k) -> (l d h) k", k=64), writes=[st.k])
        pt, pk = self.psum('mA', [0, 1])
        self.tr(pt[0:64, 0:32], st[:, :], ident[0:32, 0:32], [st.k, 'ident'], pk)
        self.act(lbl[:, :, :], pt[0:64, 0:32].rearrange("k (l x) -> k l x", l=4), AF.Exp, [pk], [lbl.k])
        self.tt(sF[:, :], lbl[:, 0, :], lbl[:, 1, :], ALU.add, [lbl.k], [sF.k])
        self.tt(sF[:, :], sF[:, :], lbl[:, 2, :], ALU.add, [lbl.k, sF.k], [sF.k])
        self.tt(sF[:, :], sF[:, :], lbl[:, 3, :], ALU.add, [lbl.k, sF.k], [sF.k])
        self.recip(sF[:, :], sF[:, :], [sF.k], [sF.k])
        self.memset('dve', cF[:, :], 0.0, [cF.k])
        for j in range(1, l + 1):
            self.tt(cF[:, :], cF[:, :], lbl[:, j, :], ALU.add, [lbl.k, cF.k], [cF.k])
        self.tt(cF[:, :], cF[:, :], sF[:, :], ALU.mult, [cF.k, sF.k], [cF.k])
        self.ts(omlF[:, :], cF[:, :], -1.0, 1.0, ALU.mult, ALU.add, [cF.k], [omlF.k])
        self.ts(nomlF[:, :], cF[:, :], -1.0, None, ALU.add, None, [cF.k], [nomlF.k])
        eb_ = self.ar('ebig', [128, 4, 512], F32)
        sB = self.ar('sB', [128, 512], F32)
        P.dma('sp', eb_[:, :, :], self.din['hg_lb_logits'].rearrange("(o l) d c -> o l (d c)", o=1).broadcast_to([128, 4, 512]),
              writes=[eb_.k])
        self.act(eb_[:, :, :], eb_[:, :, :], AF.Exp, [eb_.k], [eb_.k])
        self.tt(sB[:, :], eb_[:, 0, :], eb_[:, 1, :], ALU.add, [eb_.k], [sB.k])
        self.tt(sB[:, :], sB[:, :], eb_[:, 2, :], ALU.add, [eb_.k, sB.k], [sB.k])
        self.tt(sB[:, :], sB[:, :], eb_[:, 3, :], ALU.add, [eb_.k, sB.k], [sB.k])
        self.recip(sB[:, :], sB[:, :], [sB.k], [sB.k])
        self.memset('dve', lbB[:, :], 0.0, [lbB.k])
        for j in range(1, l + 1):
            self.tt(lbB[:, :], lbB[:, :], eb_[:, j, :], ALU.add, [eb_.k, lbB.k], [lbB.k])
        self.tt(lbB[:, :], lbB[:, :], sB[:, :], ALU.mult, [lbB.k, sB.k], [lbB.k])
        self.ts(omlB[:, :], lbB[:, :], -1.0, 1.0, ALU.mult, ALU.add, [lbB.k], [omlB.k])
        self.ar_release(mark)
        for tile in range(NT):
            ak = ('aT', self.blk_of_tile(tile))
            pv, pvk = self.psum('mA', [0, 1])
            self.mmg(pv[:, 0:256], [(aT[:, kc, tile * 128:(tile + 1) * 128], wh[:, kc, 768:1024]) for kc in range(8)],
                     [wh.k, ak], pvk)
            self.cp('act', itm[:, tile, :], pv[:, 0:256], [pvk], [itm.k])
        ftm = self.ar('ftm', [128, 256], F32)
        lftm = self.ar('lftm', [128, 256], F32)
        ktm = self.ar('ktm', [128, 256], F32)
        kbt = self.ar('kbt', [128, 256], BF16)
        kTt = self.ar('kTt', [64, 4, 128], F32)
        ebt = self.ar('ebt', [64, 4, 128], F32)
        enb = self.ar('enb', [64, 4, 128], F32)
        dd = self.ar('dd', [64, 4, 8], F32)
        qb = self.ar('qb', [64, 4, 128], BF16)
        kb = self.ar('kb', [64, 4, 128], BF16)
        vexp = [self.ar('vexp%d' % i, [128, 8, 64], BF16) for i in range(2)]
        u = self.ar('u', [64, 8, 4, 64], F32)
        S = self.ar('S', [64, 4, 64], F32)
        tmpS = self.ar('tmpS', [64, 4, 64], F32)
        Sbf = self.ar('Sbf', [64, 8, 4, 64], BF16)
        AT = self.ar('AT', [128, 4, 128], BF16)
        vi = 0
        for d in range(2):
            order = list(range(NT)) if d == 0 else [1, 0] + list(range(NT - 1, 1, -1))
            self.memset('dve', S[:, :, :], 0.0, [S.k])
            zc = 256 + d * 256
            for tile in order:
                ak = ('aT', self.blk_of_tile(tile))
                tcs = slice(tile * 128, (tile + 1) * 128)
                pz, pzk = self.psum('h0', [0, 1])
                self.mmg(pz[:, 0:256], [(aT[:, kc, tcs], wh[:, kc, zc:zc + 256]) for kc in range(8)], [wh.k, ak], pzk)
                self.act(ftm[:, :], pz[:, 0:256], AF.Sigmoid, [pzk], [ftm.k])
                self.tt(ftm[:, :], ftm[:, :], omlB[:, d * 256:(d + 1) * 256], ALU.mult, [ftm.k, omlB.k], [ftm.k])
                self.tt(ftm[:, :], ftm[:, :], lbB[:, d * 256:(d + 1) * 256], ALU.add, [ftm.k, lbB.k], [ftm.k])
                self.act(lftm[:, :], ftm[:, :], AF.Ln, [ftm.k], [lftm.k])
                self.ts(ktm[:, :], ftm[:, :], -1.0, 1.0, ALU.mult, ALU.add, [ftm.k], [ktm.k])
                pb, pbk = self.psum('h0', [0, 1])
                self.mmg(pb[:, 0:256], [(trie[:, d, 0:128], lftm[:, :])], [trie.k, lftm.k], pbk)
                self.act(ftm[:, :], pb[:, 0:256], AF.Exp, [pbk], [ftm.k], scale=-1.0)
                self.tt(kbt[:, :], ktm[:, :], ftm[:, :], ALU.mult, [ktm.k, ftm.k], [kbt.k])
                pzT, pzTk = self.psum('h0', [0, 1])
                for h in range(4):
                    self.mmg(pzT[0:64, h * 128:(h + 1) * 128],
                             [(wh[:, kc, zc + h * 64:zc + (h + 1) * 64], aT[:, kc, tcs]) for kc in range(8)], [wh.k, ak], pzTk)
                self.act(kTt[:, :, :], pzT[0:64, :].rearrange("k (h t) -> k h t", h=4), AF.Sigmoid, [pzTk], [kTt.k])
                self.tt(kTt[:, :, :], kTt[:, :, :], nomlF[:, d * 4:(d + 1) * 4].unsqueeze(2).broadcast_to([64, 4, 128]),
                        ALU.mult, [kTt.k, nomlF.k], [kTt.k])
                self.tt(kTt[:, :, :], kTt[:, :, :], omlF[:, d * 4:(d + 1) * 4].unsqueeze(2).broadcast_to([64, 4, 128]),
                        ALU.add, [kTt.k, omlF.k], [kTt.k])
                pbT, pbTk = self.psum('h1', [2, 3])
                for h in range(4):
                    self.mmg(pbT[0:64, h * 128:(h + 1) * 128], [(lftm[:, h * 64:(h + 1) * 64], trie[:, d, 0:128])],
                             [lftm.k, trie.k], pbTk)
                ptot, ptotk = self.psum('h1', [2, 3])
                for h in range(4):
                    self.mmg(ptot[0:64, h * 8:(h + 1) * 8], [(lftm[:, h * 64:(h + 1) * 64], trie[:, d, 128:136])],
                             [lftm.k, trie.k], ptotk)
                self.act(ebt[:, :, :], pbT[0:64, :].rearrange("k (h t) -> k h t", h=4), AF.Exp, [pbTk], [ebt.k])
                self.act(enb[:, :, :], pbT[0:64, :].rearrange("k (h t) -> k h t", h=4), AF.Exp, [pbTk], [enb.k], scale=-1.0)
                self.act(dd[:, :, :], ptot[0:64, 0:32].rearrange("k (h j) -> k h j", h=4), AF.Exp, [ptotk], [dd.k])
                pq, pqk = self.psum('h0', [0, 1])
                for h in range(4):
                    self.mmg(pq[0:64, h * 128:(h + 1) * 128],
                             [(wh[:, kc, h * 64:(h + 1) * 64], aT[:, kc, tcs]) for kc in range(8)], [wh.k, ak], pqk)
                self.stt(qb[:, :, :], pq[0:64, :].rearrange("k (h t) -> k h t", h=4), 0.125, ebt[:, :, :], ALU.mult, ALU.mult,
                         [pqk, ebt.k], [qb.k])
                self.tt(kb[:, :, :], kTt[:, :, :], enb[:, :, :], ALU.mult, [kTt.k, enb.k], [kb.k])
                for h in range(4):
                    ve = vexp[vi % 2]
                    vi += 1
                    self.tt(ve[:, :, :], itm[:, tile, h * 64:(h + 1) * 64].unsqueeze(1).broadcast_to([128, 8, 64]),
                            ee[:, d, :].unsqueeze(2).broadcast_to([128, 8, 64]), ALU.mult, [itm.k, ee.k], [ve.k])
                    pu, puk = self.psum('h2', [4, 5])
                    self.mmg(pu[0:64, :], [(kbt[:, h * 64:(h + 1) * 64], ve[:, :, :].rearrange("s j v -> s (j v)"))],
                             [kbt.k, ve.k], puk)
                    self.tt(u[:, :, h, :], pu[0:64, :].rearrange("k (j v) -> k j v", j=8),
                            dd[:, h, :].unsqueeze(2).broadcast_to([64, 8, 64]), ALU.mult, [puk, dd.k], [u.k])
                for j in range(8):
                    self.cp('act', Sbf[:, j, :, :], S[:, :, :], [S.k], [(Sbf.k, j)])
                    self.tt(tmpS[:, :, :], S[:, :, :], dd[:, :, j:j + 1].broadcast_to([64, 4, 64]), ALU.mult,
                            [S.k, dd.k], [tmpS.k])
                    self.tt(S[:, :, :], tmpS[:, :, :], u[:, j, :, :], ALU.add, [tmpS.k, u.k], [S.k])
                pA, pAk = self.psum('h3', [6])
                for h in range(4):
                    self.mmg(pA[:, h * 128:(h + 1) * 128], [(kb[:, h, :], qb[:, h, :])], [kb.k, qb.k], pAk)
                self.tt(AT[:, :, :], pA[:, :].rearrange("s (h t) -> s h t", h=4),
                        mt[:, d, :].unsqueeze(1).broadcast_to([128, 4, 128]), ALU.mult, [pAk, mt.k], [AT.k])
                pO, pOk = self.psum('h4', [7])
                rk = [itm.k, AT.k, qb.k] + [(Sbf.k, j) for j in range(8)]
                for h in range(4):
                    p = h // 2
                    P.op('pe', lambda e, h=h, p=p, tile=tile: e.matmul(pO[:, h * 128:(h + 1) * 128], lhsT=itm[:, tile, p * 128:(p + 1) * 128],
                                                           rhs=AT[:, h, :], start=True, stop=False),
                         reads=rk, writes=[pOk], inc=False)
                    for j in range(8):
                        cj = j if d == 0 else 7 - j
                        P.op('pe', lambda e, h=h, p=p, j=j, cj=cj: e.matmul(
                            pO[:, h * 128 + cj * 16:h * 128 + (cj + 1) * 16],
                            lhsT=Sbf[:, j, 2 * p:2 * p + 2, :].rearrange("k h v -> k (h v)"),
                            rhs=qb[:, h, cj * 16:(cj + 1) * 16], start=False, stop=(j == 7)),
                             reads=rk, writes=[pOk], inc=(h == 3 and j == 7))
                pv4 = pO[:, :].rearrange("q (p j t) -> q p j t", p=2, j=2)
                for hh in range(2):
                    if d == 0:
                        self.cp('act' if hh else 'dve', ohg[hh * 64:hh * 64 + 64, :, tcs], pv4[hh * 64:hh * 64 + 64, :, hh, :],
                                [pOk], [(ohg.k, tile, hh)])
                    else:
                        self.tt(ohg[hh * 64:hh * 64 + 64, :, tcs], pv4[hh * 64:hh * 64 + 64, :, hh, :],
                                ohg[hh * 64:hh * 64 + 64, :, tcs], ALU.add, [pOk, (ohg.k, tile, hh)], [(ohg.k, tile, hh)])
        if os.environ.get('HG_DBG'):
            dbg = self.nc.dram_tensor('dbg', [128, 2 * T], F32, kind="ExternalOutput").ap()
            P.dma('pool', dbg.rearrange("q (p t) -> q p t", p=2), ohg[:, :, :],
                  reads=[(ohg.k, t_, hh) for t_ in range(NT) for hh in range(2)], is_output=True)
        self.ar_release(mark)
        wo = self.load_wo(l, 1)
        self.hn_alloc()
        oblk = self.ar('oblk', [128, 2, 512], BF16)
        o32 = self.ar('o32h', [128, 2, 512], F32)
        for b, (t0, n) in enumerate(BLKS):
            okeys = [(ohg.k, t0 // 128 + j, hh) for j in range(n // 128) for hh in range(2)]
            self.cp('dve', o32[:, :, 0:n], ohg[:, :, t0:t0 + n], okeys, [o32.k])
            self.headnorm_gate(l, o32, o32.k, n, t0, b, wh, 1024, False, 3, None, oblk, ('aT', b))
            self.outproj(l, wo, oblk, [(oblk.k, 0), (oblk.k, 1)], b, t0, n)


    def final_out(self, out, gfin):
        P = self.P
        hT, rstd, ident = self.hT, self.rstd, self.ident
        nsq = self.ar('nsq', [128, 8, 512], BF16)
        ntmp = self.ar('ntmp', [128, 8, 512], F32)
        nsq.k, ntmp.k = 'nsq', 'ntmp'
        ost = [self.ar('ost%d' % i, [128, 1024], F32) for i in range(2)]
        oi = 0
        for b, (t0, n) in enumerate(BLKS):
            if b == 0:
                continue
            hk = [('hT', c, b) for c in range(8)]
            if self.final:
                self.act(nsq[:, :, 0:n], hT[:, :, t0:t0 + n], AF.Square, hk, ['nsq'])
                pt, pk = self.psum('nrm', [2, 3])
                self.mmg(pt[:, 0:n], [(self.ones_bf[:, :], nsq[:, c, 0:n]) for c in range(8)], ['nsq', 'ones_bf'], pk)
                self.act(rstd[:, 0:n], pt[:, 0:n], AF.Sqrt, [pk], ['rstd'], bias=self.eps_col[:, 0:1], scale=1.0 / D)
                self.recip(rstd[:, 0:n], rstd[:, 0:n], ['rstd'], ['rstd'])
                self.tt(ntmp[:, :, 0:n], hT[:, :, t0:t0 + n], rstd[:, 0:n].unsqueeze(1).broadcast_to([128, 8, n]),
                        ALU.mult, hk + ['rstd'], ['ntmp'])
                self.tt(ntmp[:, :, 0:n], ntmp[:, :, 0:n], gfin[:, :].unsqueeze(2).broadcast_to([128, 8, n]),
                        ALU.mult, ['ntmp', 'gfin'], ['ntmp'])
            else:
                self.cp('dve', ntmp[:, :, 0:n], hT[:, :, t0:t0 + n], hk, ['ntmp'])
            for tt_ in range(n // 128):
                os_ = ost[oi % 2]
                oi += 1
                for half in range(2):
                    pt, pk = self.psum('tr', [0, 1])
                    for j in range(4):
                        c = half * 4 + j
                        self.tr(pt[:, j * 128:(j + 1) * 128], ntmp[:, c, tt_ * 128:(tt_ + 1) * 128], ident[:, :],
                                ['ntmp', 'ident'], pk)
                    self.cp('act' if half else 'dve', os_[:, half * 512:(half + 1) * 512], pt[:, :], [pk], [(os_.k, half)])
                row0 = t0 - LCTX + tt_ * 128
                P.dma('sp', out[row0:row0 + 128, :], os_[:, :], reads=[(os_.k, 0), (os_.k, 1)], is_output=True)


def _rot_T(half):
    n = 2 * half
    R = np.zeros((n, n), np.float32)
    for m in range(half):
        R[m, m + half] = -1.0
        R[m + half, m] = 1.0
    return R.T.copy()


def _consts():
    sel = np.zeros((NEXP, NEXP * 128), np.float32)
    for e in range(NEXP):
        sel[e, e * 128:(e + 1) * 128] = 1.0
    c = {'ident': np.eye(128, dtype=np.float32), 'sel': sel}
    theta = 10000.0
    tok = np.arange(NLAT)
    row, col = (tok // 64).astype(np.float64), (tok % 64).astype(np.float64)

    def axial(rot_dim):
        nf = rot_dim // 4
        inv = theta ** (-np.arange(nf, dtype=np.float64) / nf)
        inv = inv.astype(np.float32).astype(np.float64)
        ang = np.concatenate([row[:, None] * inv, col[:, None] * inv], axis=1)
        ang = ang.astype(np.float32).astype(np.float64)
        full = np.zeros((T, rot_dim // 2))
        full[LCTX:] = ang
        a2 = np.concatenate([full, full], axis=1)
        return np.cos(a2).T.astype(np.float32).copy(), np.sin(a2).T.astype(np.float32).copy()

    c['cos_mla'], c['sin_mla'] = axial(32)
    c['cos_swa'], c['sin_swa'] = axial(64)
    inv = (theta ** (-np.arange(16, dtype=np.float64) / 16)).astype(np.float32).astype(np.float64)
    ang = (np.arange(T, dtype=np.float64)[:, None] * inv).astype(np.float32).astype(np.float64)
    a2 = np.concatenate([ang, ang], axis=1)
    c['cos_ret'], c['sin_ret'] = np.cos(a2).T.astype(np.float32).copy(), np.sin(a2).T.astype(np.float32).copy()
    rb = np.zeros((96, 96), np.float32)
    rb[64:96, 64:96] = _rot_T(16)
    c['rbig96'] = rb
    r128 = np.zeros((128, 128), np.float32)
    r128[0:64, 0:64] = _rot_T(32)
    r128[64:128, 64:128] = _rot_T(32)
    c['r128'] = r128
    r64b = np.zeros((64, 64), np.float32)
    r64b[0:32, 0:32] = _rot_T(16)
    r64b[32:64, 32:64] = _rot_T(16)
    c['r64b'] = r64b
    m = np.zeros((6, 128, 512), np.float32)
    s_ = np.arange(128)[:, None]
    t_ = np.arange(512)[None, :]
    for r in range(-1, 5):
        m[r + 1] = (np.abs(t_ - s_ - 128 * r) <= 128).astype(np.float32)
    c['swa_mask'] = m
    b64 = np.zeros((128, 128), np.float32)
    b64[0:64, 0:64] = 1.0 / 64
    b64[64:128, 64:128] = 1.0 / 64
    c['blk64'] = b64
    s_ = np.arange(128, dtype=np.float32)[:, None]
    t_ = np.arange(128, dtype=np.float32)[None, :]
    c['ret_rel'] = np.stack([np.maximum(t_ - s_, 0), (t_ >= s_).astype(np.float32),
                             np.maximum(s_ - t_, 0), (s_ >= t_).astype(np.float32)]).astype(np.float32)
    cc = np.zeros((128, 16), np.float32)
    cc[:, 0] = 127.0 - np.arange(128)
    cc[:, 1] = np.arange(128)
    c['ret_colc'] = cc
    rc = np.concatenate([np.arange(128) + 1.0, 128.0 - np.arange(128)])[None, :].repeat(64, axis=0)
    c['ret_rowc'] = rc.astype(np.float32)
    rm = np.zeros((64, 16), np.float32)
    rm[0:32, 0] = 1.0
    rm[32:64, 1] = 1.0
    c['ret_rowm'] = rm
    si = np.arange(128)[:, None]
    ti = np.arange(128)[None, :]
    same = (si // 16) == (ti // 16)
    trif = (same & (si <= ti)).astype(np.float32)
    trib = (same & (si >= ti)).astype(np.float32)
    ef = ((si // 16) == np.arange(8)[None, :]).astype(np.float32)
    eb = ((si // 16) == (7 - np.arange(8))[None, :]).astype(np.float32)
    c['hg_trie'] = np.stack([np.concatenate([trif, ef], axis=1), np.concatenate([trib, eb], axis=1)]).astype(np.float32)
    c['hg_mt'] = np.stack([trif, trib]).astype(np.float32)
    c['hg_e'] = np.stack([ef, eb]).astype(np.float32)
    return c


_CACHE = {}


def _get_prog(key, **kw):
    if key not in _CACHE:
        b = Builder(**kw)
        nc = b.build()
        _CACHE[key] = (nc, b)
    return _CACHE[key]


def run_partial(inputs, layers=(0, 1, 2, 3), mixers=(0, 1, 2, 3), ffn=True, final=True, cores=8):
    nc, b = _get_prog(('p', tuple(layers), tuple(mixers), ffn, final), layers=layers, mixers=mixers, ffn=ffn, final=final)
    consts = _consts()
    f = lambda a: np.ascontiguousarray(np.asarray(a, dtype=np.float32))
    shared = {k: f(inputs[k]) for k in b.din if k in inputs and k not in ('x', 'ctx')}
    for k in consts:
        if k in b.din:
            shared[k] = consts[k]
    in_maps = []
    for i in range(cores):
        m = dict(shared)
        m['x'] = f(inputs['x'][i])
        m['ctx'] = f(inputs['ctx'][i])
        m['c2'] = np.ascontiguousarray(np.stack([np.asarray(inputs['c'][i], np.float32),
                                                 np.asarray(inputs['c_ctx'], np.float32)]))
        in_maps.append(m)
    res = run_bass_kernel_spmd(nc, in_maps, core_ids=list(range(cores)))
    return np.stack([r['out'] for r in res.results], axis=0)


def kernel(**inputs):
    return run_partial(inputs)
```

```python
import os
import numpy as np
import concourse.bass as bass
import concourse.mybir as mybir
from concourse.bass_utils import run_bass_kernel_spmd

AF = mybir.ActivationFunctionType
ALU = mybir.AluOpType
AX = mybir.AxisListType
F32 = mybir.dt.float32
BF16 = mybir.dt.bfloat16

ENGS = ['pe', 'act', 'dve', 'pool', 'sp']
NDMA = 56

D = 1024
T = 2304
NT = 18
LCTX = 256
NLAT = 2048
DEPTH = 4
D_FF = 2816
D_FFE = 3584
NEXP = 8
D_IN = 2912
EPS = 1e-6
BLKS = [(0, 256), (256, 512), (768, 512), (1280, 512), (1792, 512)]
C_MQ, C_MKV, C_MPE = 0, 192, 320
C_HQ, C_HZF, C_HZB, C_HI, C_HG = 352, 608, 864, 1120, 1376
C_SQ, C_SK, C_SV = 1632, 1888, 2016
C_RQ, C_RK, C_RV, C_RG = 2144, 2272, 2400, 2656


class Prog:
    def __init__(self, nc):
        self.nc = nc
        self.q = {e: [] for e in ENGS}
        self.sem = {e: nc.alloc_semaphore('cs_' + e) for e in ENGS}
        self.cnt = {e: 0 for e in ENGS}
        self.waited = {}
        self.lastw = {}
        self.readers = {}
        self.dma_sems = [nc.alloc_semaphore('ds%d' % i) for i in range(NDMA)]
        self.dma_cnt = [0] * NDMA
        self.dma_rr = {'sp': 0, 'pool': 0}
        self.out_tokens = []
        self.ninst = 0

    def _need(self, eng, tok):
        sem, val, prod = tok
        if eng == 'pe' and prod == 'pe':
            return
        k = (eng, sem.num)
        if self.waited.get(k, 0) >= val:
            return
        self.waited[k] = val
        self.q[eng].append(lambda e, sem=sem, val=val: e.wait_ge(sem, val))

    def _deps(self, eng, reads, writes):
        for r in reads:
            t = self.lastw.get(r)
            if t is not None:
                self._need(eng, t)
            if isinstance(r, tuple) and r[0] == 'ps':
                for t in self.readers.get(r, {}).values():
                    if t[2] != eng:
                        self._need(eng, t)
        for w in writes:
            t = self.lastw.get(w)
            if t is not None:
                self._need(eng, t)
            for t in self.readers.get(w, {}).values():
                self._need(eng, t)

    def _record(self, tok, reads, writes):
        for r in reads:
            d = self.readers.setdefault(r, {})
            k = tok[0].num
            if k not in d or d[k][1] < tok[1]:
                d[k] = tok
        for w in writes:
            self.lastw[w] = tok
            self.readers[w] = {}

    def op(self, eng, fn, reads=(), writes=(), inc=True):
        self._deps(eng, reads, writes)
        sem = self.sem[eng]
        self.ninst += 1
        if inc:
            self.cnt[eng] += 1
            tok = (sem, self.cnt[eng], eng)
            self.q[eng].append(lambda e, fn=fn, sem=sem: fn(e).then_inc(sem, 1))
        else:
            tok = (sem, self.cnt[eng] + 1, eng)
            self.q[eng].append(lambda e, fn=fn: fn(e))
        self._record(tok, reads, writes)
        return tok

    def dma(self, eng, out, in_, reads=(), writes=(), is_output=False):
        h = NDMA // 2
        j = self.dma_rr[eng]
        self.dma_rr[eng] = (j + 1) % h
        i = j if eng == 'sp' else h + j
        sem = self.dma_sems[i]
        if self.dma_cnt[i] > 0:
            self._need(eng, (sem, self.dma_cnt[i], 'dma'))
        self._deps(eng, reads, writes)
        self.dma_cnt[i] += 16
        tok = (sem, self.dma_cnt[i], 'dma')
        self.ninst += 1
        self.q[eng].append(
            lambda e, out=out, in_=in_, sem=sem: e.dma_start(out=out, in_=in_).then_inc(sem, 16))
        self._record(tok, reads, writes)
        if is_output:
            self.out_tokens.append(tok)
        return tok

    def barrier(self):
        toks = [(self.sem[e], self.cnt[e], e) for e in ENGS if self.cnt[e] > 0]
        toks += [(self.dma_sems[i], self.dma_cnt[i], 'dma') for i in range(NDMA) if self.dma_cnt[i] > 0]
        for f in ENGS:
            for t in toks:
                self._need(f, t)

    def emit(self):
        nc = self.nc
        for t in self.out_tokens:
            self._need('sp', t)
        for e in ['pe', 'act', 'dve', 'pool']:
            if self.cnt[e] > 0:
                self._need('sp', (self.sem[e], self.cnt[e], e))
        q = self.q
        with nc.Block() as block:
            @block.tensor
            def _(e):
                for f in q['pe']:
                    f(e)

            @block.scalar
            def _(e):
                for f in q['act']:
                    f(e)

            @block.vector
            def _(e):
                for f in q['dve']:
                    f(e)

            @block.gpsimd
            def _(e):
                for f in q['pool']:
                    f(e)

            @block.sync
            def _(e):
                for f in q['sp']:
                    f(e)


class SB:
    def __init__(self, nc, name, shape, dtype):
        self.t = nc.alloc_sbuf_tensor('sb_' + name, list(shape), dtype)
        self.k = name

    def __getitem__(self, idx):
        return self.t[idx]


class View:
    def __init__(self, ap, key):
        self.t = ap
        self.k = key

    def __getitem__(self, idx):
        return self.t[idx]


AR_WORDS = 19456


class Builder:
    def __init__(self, layers, mixers=(0, 1, 2, 3), ffn=True, final=True, h_in=False, h_out=False):
        self.layers = list(layers)
        self.mixers = tuple(mixers)
        self.ffn = ffn
        self.final = final
        self.h_in = h_in
        self.h_out = h_out
        self.nc = nc = bass.Bass("TRN2", target_bir_lowering=False)
        self.P = Prog(nc)
        self.din = {}
        self.ps = [nc.alloc_psum_tensor("psb%d" % i, [128, 512], F32) for i in range(8)]
        self.ps_rr = {}
        self.uid = 0
        self.arena = nc.alloc_sbuf_tensor('arena', [128, AR_WORDS], F32)
        self.ar_off = 0
        self.ar_phase = 0

    def inp(self, name, shape, dtype=F32):
        ap = self.nc.dram_tensor(name, list(shape), dtype, kind="ExternalInput").ap()
        self.din[name] = ap
        return ap

    def sb(self, name, shape, dtype=F32):
        return SB(self.nc, name, shape, dtype)

    def ar(self, name, shape, dtype=F32):
        nel = int(np.prod(shape[1:]))
        nb = nel * (2 if dtype == BF16 else 4)
        nw = ((nb + 31) // 32) * 8
        assert self.ar_off + nw <= AR_WORDS, (name, self.ar_off, nw)
        ap = self.arena[0:shape[0], self.ar_off:self.ar_off + nw]
        self.ar_off += nw
        if dtype == BF16:
            ap = ap.bitcast(BF16)
        ap = ap[:, 0:nel]
        if len(shape) == 3:
            ap = ap.rearrange("p (a b) -> p a b", a=shape[1])
        elif len(shape) == 4:
            ap = ap.rearrange("p (a b c) -> p a b c", a=shape[1], b=shape[2])
        elif len(shape) == 5:
            ap = ap.rearrange("p (a b c d) -> p a b c d", a=shape[1], b=shape[2], c=shape[3])
        self.ar_gen = getattr(self, 'ar_gen', 0)
        return View(ap, ('ar', self.ar_phase, name))

    def ar_release(self, mark):
        self.P.barrier()
        self.ar_off = mark
        self.ar_phase += 1

    def phase(self):
        self.P.barrier()
        self.ar_off = 0
        self.ar_phase += 1

    def psum(self, pool, banks):
        i = self.ps_rr.get(pool, 0)
        self.ps_rr[pool] = i + 1
        b = banks[i % len(banks)]
        return self.ps[b], ('ps', b)

    def mmg(self, out, pairs, reads, wkey):
        n = len(pairs)
        for i, (l, r) in enumerate(pairs):
            self.P.op('pe', lambda e, l=l, r=r, s=(i == 0), t=(i == n - 1): e.matmul(out, lhsT=l, rhs=r, start=s, stop=t),
                      reads=reads, writes=[wkey], inc=(i == n - 1))

    def tr(self, out, in_, ident, reads, wkey):
        self.P.op('pe', lambda e: e.transpose(out, in_, ident), reads=reads, writes=[wkey])

    def act(self, out, in_, func, reads, writes, bias=None, scale=None):
        kw = {}
        if bias is not None:
            kw['bias'] = bias
        if scale is not None:
            kw['scale'] = scale
        self.P.op('act', lambda e: e.activation(out=out, in_=in_, func=func, **kw), reads=reads, writes=writes)

    def tt(self, out, in0, in1, op, reads, writes, eng='dve'):
        self.P.op(eng, lambda e: e.tensor_tensor(out=out, in0=in0, in1=in1, op=op), reads=reads, writes=writes)

    def ts(self, out, in0, s1, s2, op0, op1, reads, writes, eng='dve'):
        if s2 is None:
            self.P.op(eng, lambda e: e.tensor_scalar(out=out, in0=in0, scalar1=s1, scalar2=None, op0=op0),
                      reads=reads, writes=writes)
        else:
            self.P.op(eng, lambda e: e.tensor_scalar(out=out, in0=in0, scalar1=s1, scalar2=s2, op0=op0, op1=op1),
                      reads=reads, writes=writes)

    def stt(self, out, in0, scalar, in1, op0, op1, reads, writes):
        self.P.op('dve', lambda e: e.scalar_tensor_tensor(out=out, in0=in0, scalar=scalar, in1=in1, op0=op0, op1=op1),
                  reads=reads, writes=writes)

    def cp(self, eng, out, in_, reads, writes):
        if eng == 'act':
            self.P.op('act', lambda e: e.copy(out=out, in_=in_), reads=reads, writes=writes)
        else:
            self.P.op(eng, lambda e: e.tensor_copy(out=out, in_=in_), reads=reads, writes=writes)

    def recip(self, out, in_, reads, writes):
        self.P.op('dve', lambda e: e.reciprocal(out=out, in_=in_), reads=reads, writes=writes)

    def memset(self, eng, ap, val, writes):
        self.P.op(eng, lambda e: e.memset(ap, val), writes=writes)

    def rows_to_cols(self, dram_rows, nrows, ncols, dst_ap, dst_key):
        self.uid += 1
        st = self.stage
        k = ('stage', self.uid % 2)
        sl = st[self.uid % 2]
        self.P.dma('sp', sl[0:nrows, 0:ncols], dram_rows, writes=[k])
        pt, pk = self.psum('tr', [0, 1])
        self.tr(pt[0:ncols, 0:nrows], sl[0:nrows, 0:ncols], self.ident[0:nrows, 0:nrows], [k, 'ident'], pk)
        self.cp('dve', dst_ap, pt[0:ncols, 0:nrows], [pk], [dst_key])

    def build(self):
        nc, P = self.nc, self.P
        L = DEPTH
        x = self.inp('x', [NLAT, D])
        ctx = self.inp('ctx', [LCTX, D])
        c2 = self.inp('c2', [2, D])
        identd = self.inp('ident', [128, 128])
        w_ada = self.inp('w_ada', [L, D, 6 * D])
        b_ada = self.inp('b_ada', [L, 6 * D])
        nmg = self.inp('norm_mix_g', [L, D])
        nfg = self.inp('norm_ffn_g', [L, D])
        fng = self.inp('final_norm_g', [D])
        self.w_in = self.inp('w_in', [L, D, D_IN])
        self.w_out = self.inp('w_out', [L, D, D])
        self.fwg = self.inp('ffn_w_gate', [2, D, D_FF])
        self.fwu = self.inp('ffn_w_up', [2, D, D_FF])
        self.fwd = self.inp('ffn_w_down', [2, D_FF, D])
        self.mwr = self.inp('moe_w_router', [2, D, NEXP])
        self.mwg = self.inp('moe_w_gate', [2, NEXP, D, D_FFE])
        self.mwu = self.inp('moe_w_up', [2, NEXP, D, D_FFE])
        self.mwd = self.inp('moe_w_down', [2, NEXP, D_FFE, D])
        seld = self.inp('sel', [NEXP, NEXP * 128])
        for nm, shp in [('mla_q_norm_g', [L, 192]), ('mla_w_uq', [L, 192, 384]), ('mla_kv_norm_g', [L, 128]),
                        ('mla_w_ukv', [L, 128, 512]), ('hg_lb_logits', [L, 2, 256]), ('hg_norm_g', [L, 256]),
                        ('swa_sink', [L, 4]), ('ret_decay_logit', [L, 2, 4]), ('ret_gn_g', [L, 256]),
                        ('ret_gn_b', [L, 256]), ('cos_mla', [32, T]), ('sin_mla', [32, T]), ('cos_swa', [64, T]),
                        ('sin_swa', [64, T]), ('cos_ret', [32, T]), ('sin_ret', [32, T]), ('rbig96', [96, 96]),
                        ('r128', [128, 128]), ('r64b', [64, 64]), ('swa_mask', [6, 128, 512]),
                        ('blk64', [128, 128]), ('ret_rel', [4, 128, 128]), ('ret_colc', [128, 16]),
                        ('ret_rowc', [64, 256]), ('ret_rowm', [64, 16]), ('hg_trie', [2, 128, 136]), ('hg_mt', [2, 128, 128]),
                        ('hg_e', [2, 128, 8])]:
            self.inp(nm, shp)
        if self.h_in:
            hin = self.inp('h_in', [128, 8 * T])
        out = nc.dram_tensor('out', [NLAT, D], F32, kind="ExternalOutput").ap()
        if self.h_out:
            hout = nc.dram_tensor('h_out', [128, 8 * T], F32, kind="ExternalOutput").ap()

        self.hT = hT = self.sb('hT', [128, 8, T], F32)
        self.aT = aT = self.sb('aT', [128, 8, T], BF16)
        self.ident = ident = self.sb('ident', [128, 128], F32)
        self.identb = identb = self.sb('identb', [128, 128], BF16)
        self.ones_bf = ones_bf = self.sb('ones_bf', [128, 128], BF16)
        self.stage = [self.ar('stage%d' % i, [128, 1024], F32).t for i in range(2)]
        self.modT = modT = self.sb('modT', [128, L, 48, 2], F32)
        self.gmix = gmix = self.sb('gmix', [128, L, 8], F32)
        self.gffn = gffn = self.sb('gffn', [128, L, 8], F32)
        self.gfin = gfin = self.sb('gfin', [128, 8], F32)
        self.A1 = A1 = self.sb('A1', [128, 8, 2], F32)
        self.rstd = rstd = self.sb('rstd', [128, 512], F32)
        self.sel = sel = self.sb('sel', [NEXP, NEXP * 128], F32)
        self.eps_col = self.sb('eps_col', [128, 1], F32)
        self.memset('dve', self.eps_col[:, :], EPS, ['eps_col'])
        self.gT = self.sb('gT', [NEXP, T], F32)

        P.dma('sp', ident[:, :], identd, writes=['ident'])
        P.dma('sp', sel[:, :], seld, writes=['sel'])
        self.cp('dve', identb[:, :], ident[:, :], ['ident'], ['identb'])
        self.memset('dve', ones_bf[:, :], 1.0, ['ones_bf'])

        if self.h_in:
            for c in range(8):
                P.dma('sp', hT[:, c, :], hin[:, c * T:(c + 1) * T], writes=[('hT', c, b) for b in range(5)])
        else:
            for t in range(NT):
                src = ctx[t * 128:(t + 1) * 128, :] if t < 2 else x[(t - 2) * 128:(t - 1) * 128, :]
                st = self.stage[t % 2]
                k = ('stage', t % 2)
                P.dma('sp', st[:, :], src, writes=[k])
                b = self.blk_of_tile(t)
                for half in range(2):
                    pt, pk = self.psum('tr', [0, 1])
                    for j in range(4):
                        c = half * 4 + j
                        self.tr(pt[:, j * 128:(j + 1) * 128], st[:, c * 128:(c + 1) * 128], ident[:, :], [k, 'ident'], pk)
                    self.cp('act' if half else 'dve', hT[:, half * 4:half * 4 + 4, t * 128:(t + 1) * 128],
                            pt[:, :].rearrange("p (j n) -> p j n", j=4), [pk],
                            [('hT', half * 4 + j, b) for j in range(4)])

        for l in range(L):
            self.rows_to_cols(nmg[l].rearrange("(c p) -> c p", p=128), 8, 128, gmix[:, l, :], 'gmix')
            self.rows_to_cols(nfg[l].rearrange("(c p) -> c p", p=128), 8, 128, gffn[:, l, :], 'gffn')
        self.rows_to_cols(fng.rearrange("(c p) -> c p", p=128), 8, 128, gfin[:, :], 'gfin')
        bada = self.sb('bada', [128, L, 48], F32)
        for l in range(L):
            self.rows_to_cols(b_ada[l].rearrange("(m p) -> m p", p=128), 48, 128, bada[:, l, :], 'bada')

        self.colv = colv = self.sb('colv', [128, L, 12], F32)
        for l in range(L):
            qg = self.din['mla_q_norm_g'][l]
            self.rows_to_cols(qg[0:128].rearrange("(o p) -> o p", o=1), 1, 128, colv[:, l, 0:1], 'colv')
            self.rows_to_cols(qg[128:192].rearrange("(o p) -> o p", o=1), 1, 64, colv[0:64, l, 1:2], 'colv')
            self.rows_to_cols(self.din['mla_kv_norm_g'][l].rearrange("(o p) -> o p", o=1), 1, 128, colv[:, l, 2:3], 'colv')
            self.rows_to_cols(self.din['hg_norm_g'][l].rearrange("(c p) -> c p", p=128), 2, 128, colv[:, l, 3:5], 'colv')
            self.rows_to_cols(self.din['ret_gn_g'][l].rearrange("(c p) -> c p", p=128), 2, 128, colv[:, l, 5:7], 'colv')
            self.rows_to_cols(self.din['ret_gn_b'][l].rearrange("(c p) -> c p", p=128), 2, 128, colv[:, l, 7:9], 'colv')

        c2s = self.ar('c2s', [2, D], F32)
        scT = self.sb('scT', [128, 8, 2], F32)
        P.dma('sp', c2s[:, :], c2, writes=['c2s'])
        self.act(c2s[:, :], c2s[:, :], AF.Silu, ['c2s'], ['c2s'])
        for c in range(8):
            pt, pk = self.psum('tr', [0, 1])
            self.tr(pt[:, 0:2], c2s[0:2, c * 128:(c + 1) * 128], ident[0:2, 0:2], ['c2s', 'ident'], pk)
            self.cp('dve', scT[:, c, :], pt[:, 0:2], [pk], ['scT'])
        NCB = 768
        wab = [self.ar('wab%d' % i, [128, 8, NCB], F32) for i in range(2)]
        it = 0
        for l in self.layers:
            for cb in range(6 * D // NCB):
                wt = wab[it % 2]
                it += 1
                P.dma('sp', wt[:, :, :], w_ada[l][:, cb * NCB:(cb + 1) * NCB].rearrange("(c p) n -> p c n", p=128),
                      writes=[wt.k])
                pt, pk = self.psum('tr', [0, 1])
                for m in range(NCB // 128):
                    self.mmg(pt[:, m * 2:(m + 1) * 2],
                             [(wt[:, kc, m * 128:(m + 1) * 128], scT[:, kc, :]) for kc in range(8)],
                             [wt.k, 'scT'], pk)
                nm = NCB // 128
                self.tt(modT[:, l, cb * nm:(cb + 1) * nm, :], pt[:, 0:2 * nm].rearrange("p (m r) -> p m r", r=2),
                        bada[:, l, cb * nm:(cb + 1) * nm].unsqueeze(2).broadcast_to([128, nm, 2]), ALU.add,
                        [pk, 'bada'], [('modT', l)])

        for l in self.layers:
            if len(self.mixers) > 0:
                self.phase()
                self.norm_mod(l, gmix, 0, 1, False)
                self.mixer_phase(l)
            if self.ffn:
                moe = (l % 2 == 1)
                self.phase()
                self.norm_mod(l, gffn, 3, 4, moe)
                if moe:
                    self.moe_route(l)
                self.phase()
                if moe:
                    self.moe_ffn(l)
                else:
                    self.dense_ffn(l)
        self.phase()

        if self.h_out:
            for c in range(8):
                P.dma('sp', hout[:, c * T:(c + 1) * T], hT[:, c, :], reads=[('hT', c, b) for b in range(5)],
                      is_output=True)
        self.final_out(out, gfin)
        P.emit()
        return nc

    def blk_of_tile(self, t):
        return 0 if t < 2 else 1 + (t - 2) // 4

    def norm_mod(self, l, gvec, i_shift, i_scale, want_f32):
        P = self.P
        hT, aT, modT, A1, rstd = self.hT, self.aT, self.modT, self.A1, self.rstd
        self.stt(A1[:, :, :], modT[:, l, i_scale * 8:(i_scale + 1) * 8, :], 1.0,
                 gvec[:, l, :].unsqueeze(2).broadcast_to([128, 8, 2]), ALU.add, ALU.mult,
                 [('modT', l), gvec.k], ['A1'])
        nsq = self.ar('nsq', [128, 8, 512], BF16)
        ntmp = self.ar('ntmp', [128, 8, 512], F32)
        if want_f32:
            self.a32 = self.ar('a32', [128, 8, 512], F32)
            self.wr = self.ar('wr', [128, 8, NEXP], F32)
            self.lgT = self.ar('lgT', [NEXP, T], F32)
            self.P.dma('sp', self.wr[:, :, :], self.mwr[l // 2].rearrange("(c p) e -> p c e", p=128), writes=[self.wr.k])
        for b, (t0, n) in enumerate(BLKS):
            r = 1 if b == 0 else 0
            hk = [('hT', c, b) for c in range(8)]
            self.act(nsq[:, :, 0:n], hT[:, :, t0:t0 + n], AF.Square, hk, [nsq.k])
            pt, pk = self.psum('nrm', [2, 3])
            self.mmg(pt[:, 0:n], [(self.ones_bf[:, :], nsq[:, c, 0:n]) for c in range(8)], [nsq.k, 'ones_bf'], pk)
            self.act(rstd[:, 0:n], pt[:, 0:n], AF.Sqrt, [pk], ['rstd'], bias=self.eps_col[:, 0:1], scale=1.0 / D)
            self.recip(rstd[:, 0:n], rstd[:, 0:n], ['rstd'], ['rstd'])
            self.tt(ntmp[:, :, 0:n], hT[:, :, t0:t0 + n], rstd[:, 0:n].unsqueeze(1).broadcast_to([128, 8, n]),
                    ALU.mult, hk + ['rstd'], [ntmp.k])
            for c in range(8):
                if want_f32:
                    self.ts(self.a32[:, c, 0:n], ntmp[:, c, 0:n], A1[:, c, r:r + 1],
                            modT[:, l, i_shift * 8 + c, r:r + 1], ALU.mult, ALU.add,
                            [ntmp.k, 'A1', ('modT', l)], [('a32', c)])
                    self.cp('act', aT[:, c, t0:t0 + n], self.a32[:, c, 0:n], [('a32', c)], [('aT', b)])
                else:
                    self.ts(aT[:, c, t0:t0 + n], ntmp[:, c, 0:n], A1[:, c, r:r + 1],
                            modT[:, l, i_shift * 8 + c, r:r + 1], ALU.mult, ALU.add,
                            [ntmp.k, 'A1', ('modT', l)], [('aT', b)])
            if want_f32:
                self.route_block(l, b, t0, n)

    def load_w(self, dst, dram, kchunks, ncols, key):
        step = max(1, 4096 // ncols)
        for k0 in range(0, kchunks, step):
            k1 = min(kchunks, k0 + step)
            self.P.dma('pool', dst[:, k0:k1, :], dram[k0 * 128:k1 * 128, :].rearrange("(c p) n -> p c n", p=128),
                       writes=[key])

    def ffn_slices(self, dff):
        s = []
        f = 0
        while f < dff:
            n = min(512, dff - f)
            s.append((f, n))
            f += n
        return s

    def ffn_core(self, l, wg_d, wu_d, wd_d, dff, gate_tile=None, tagbase=''):
        P = self.P
        hT, aT, modT = self.hT, self.aT, self.modT
        for (f0, fn) in self.ffn_slices(dff):
            wg, wu, wd = self.fw[self.fit % 2]
            self.fit += 1
            nj = fn // 128
            self.load_w(wg[:, :, 0:fn], wg_d[:, f0:f0 + fn], 8, fn, wg.k)
            self.load_w(wu[:, :, 0:fn], wu_d[:, f0:f0 + fn], 8, fn, wu.k)
            self.load_w(wd[:, 0:nj, :], wd_d[f0:f0 + fn, :], nj, 1024, wd.k)
            def gu(b, t0, n):
                hid = self.fhid[self.fh % 2]
                self.fh += 1
                for j in range(nj):
                    pg, pgk = self.psum('ffg', [0, 1])
                    pu, puk = self.psum('ffu', [2, 3])
                    self.mmg(pg[:, 0:n], [(wg[:, kc, j * 128:(j + 1) * 128], aT[:, kc, t0:t0 + n]) for kc in range(8)],
                             [wg.k, ('aT', b)], pgk)
                    self.mmg(pu[:, 0:n], [(wu[:, kc, j * 128:(j + 1) * 128], aT[:, kc, t0:t0 + n]) for kc in range(8)],
                             [wu.k, ('aT', b)], puk)
                    sg = self.fsg[self.fj % 2]
                    self.fj += 1
                    self.act(sg[:, 0:n], pg[:, 0:n], AF.Silu, [pgk], [sg.k])
                    if gate_tile is not None:
                        self.tt(sg[:, 0:n], sg[:, 0:n], gate_tile[:, t0:t0 + n], ALU.mult, [sg.k, gate_tile.k], [sg.k])
                    self.tt(hid[:, j, 0:n], pu[:, 0:n], sg[:, 0:n], ALU.mult, [puk, sg.k], [(hid.k, j)])
                return hid

            def down(b, t0, n, hid):
                r = 1 if b == 0 else 0
                for oc in range(8):
                    py, pyk = self.psum('ffy', [4, 5, 6, 7])
                    self.mmg(py[:, 0:n], [(wd[:, j, oc * 128:(oc + 1) * 128], hid[:, j, 0:n]) for j in range(nj)],
                             [wd.k] + [(hid.k, j) for j in range(nj)], pyk)
                    self.stt(hT[:, oc, t0:t0 + n], py[:, 0:n], modT[:, l, 5 * 8 + oc, r:r + 1], hT[:, oc, t0:t0 + n],
                             ALU.mult, ALU.add, [pyk, ('modT', l), ('hT', oc, b)], [('hT', oc, b)])

            prev = None
            for b, (t0, n) in enumerate(BLKS):
                hid = gu(b, t0, n)
                if prev is not None:
                    down(*prev)
                prev = (b, t0, n, hid)
            down(*prev)

    def ffn_alloc(self):
        self.fw = [(self.ar('fwg%d' % i, [128, 8, 512], BF16), self.ar('fwu%d' % i, [128, 8, 512], BF16),
                    self.ar('fwd%d' % i, [128, 4, 1024], BF16)) for i in range(2)]
        self.fsg = [self.ar('fsg%d' % i, [128, 512], BF16) for i in range(2)]
        self.fhid = [self.ar('fhid%d' % i, [128, 4, 512], BF16) for i in range(2)]
        self.fit = 0
        self.fj = 0
        self.fh = 0

    def dense_ffn(self, l):
        i = l // 2
        self.ffn_alloc()
        self.ffn_core(l, self.fwg[i], self.fwu[i], self.fwd[i], D_FF)

    def route_block(self, l, b, t0, n):
        pt, pk = self.psum('nrm', [2, 3])
        self.mmg(pt[0:NEXP, 0:n], [(self.wr[:, c, :], self.a32[:, c, 0:n]) for c in range(8)],
                 [self.wr.k] + [('a32', c) for c in range(8)], pk)
        self.cp('dve', self.lgT[:, t0:t0 + n], pt[0:NEXP, 0:n], [pk], ['lgT'])

    def moe_route(self, l):
        ident = self.ident
        lg = self.ar('lg', [128, NT, NEXP], F32)
        mk1 = self.ar('mk1', [128, NT, NEXP], F32)
        mk2 = self.ar('mk2', [128, NT, NEXP], F32)
        m1 = self.ar('m1', [128, NT], F32)
        m2 = self.ar('m2', [128, NT], F32)
        w1 = self.ar('w1', [128, NT], F32)
        gT = self.gT
        lg.k, mk1.k, mk2.k, m1.k, m2.k, w1.k = 'lg', 'mk1', 'mk2', 'm1', 'm2', 'w1'
        for t in range(NT):
            pt, pk = self.psum('tr', [0, 1])
            self.tr(pt[:, 0:NEXP], self.lgT[0:NEXP, t * 128:(t + 1) * 128], ident[0:NEXP, 0:NEXP], ['lgT', 'ident'], pk)
            self.cp('dve', lg[:, t, :], pt[:, 0:NEXP], [pk], ['lg'])
        P = self.P
        P.op('dve', lambda e: e.tensor_reduce(out=m1[:, :], in_=lg[:, :, :], axis=AX.X, op=ALU.max), reads=['lg'], writes=['m1'])
        self.tt(mk1[:, :, :], lg[:, :, :], m1[:, :].unsqueeze(2).broadcast_to([128, NT, NEXP]), ALU.is_equal,
                ['lg', 'm1'], ['mk1'])
        self.stt(mk2[:, :, :], mk1[:, :, :], -1e30, lg[:, :, :], ALU.mult, ALU.add, ['mk1', 'lg'], ['mk2'])
        P.op('dve', lambda e: e.tensor_reduce(out=m2[:, :], in_=mk2[:, :, :], axis=AX.X, op=ALU.max), reads=['mk2'], writes=['m2'])
        self.tt(mk2[:, :, :], mk2[:, :, :], m2[:, :].unsqueeze(2).broadcast_to([128, NT, NEXP]), ALU.is_equal,
                ['mk2', 'm2'], ['mk2'])
        self.tt(w1[:, :], m1[:, :], m2[:, :], ALU.subtract, ['m1', 'm2'], ['w1'])
        self.act(w1[:, :], w1[:, :], AF.Sigmoid, ['w1'], ['w1'])
        self.tt(mk1[:, :, :], mk1[:, :, :], w1[:, :].unsqueeze(2).broadcast_to([128, NT, NEXP]), ALU.mult,
                ['mk1', 'w1'], ['mk1'])
        self.ts(w1[:, :], w1[:, :], -1.0, 1.0, ALU.mult, ALU.add, ['w1'], ['w1'])
        self.tt(mk2[:, :, :], mk2[:, :, :], w1[:, :].unsqueeze(2).broadcast_to([128, NT, NEXP]), ALU.mult,
                ['mk2', 'w1'], ['mk2'])
        self.tt(mk1[:, :, :], mk1[:, :, :], mk2[:, :, :], ALU.add, ['mk1', 'mk2'], ['mk1'])
        for t in range(NT):
            pt, pk = self.psum('tr', [0, 1])
            self.tr(pt[0:NEXP, 0:128], mk1[:, t, :], ident[:, :], ['mk1', 'ident'], pk)
            self.cp('dve', gT[:, t * 128:(t + 1) * 128], pt[0:NEXP, 0:128], [pk], ['gT'])

    def moe_ffn(self, l):
        i = l // 2
        gT = self.gT
        gbc = [self.ar('gbc%d' % j, [128, T], BF16) for j in range(2)]
        self.ffn_alloc()
        for e_ in range(NEXP):
            gb = gbc[e_ % 2]
            for b, (t0, n) in enumerate(BLKS):
                pt, pk = self.psum('nrm', [2, 3])
                self.mmg(pt[:, 0:n], [(self.sel[:, e_ * 128:(e_ + 1) * 128], gT[:, t0:t0 + n])], ['sel', 'gT'], pk)
                self.cp('act', gb[:, t0:t0 + n], pt[:, 0:n], [pk], [gb.k])
            self.ffn_core(l, self.mwg[i][e_], self.mwu[i][e_], self.mwd[i][e_], D_FFE, gate_tile=gb)


    def mixer_phase(self, l):
        if 0 in self.mixers:
            self.phase()
            self.mla(l)
        if 2 in self.mixers:
            self.phase()
            self.swa(l)
        if 3 in self.mixers:
            self.phase()
            self.ret(l)
        if 1 in self.mixers:
            self.phase()
            self.hgrn(l)

    def colvec(self, dst_ap, dram_vec, n, key):
        self.P.dma('sp', dst_ap, dram_vec.rearrange("(p o) -> p o", o=1), writes=[key])

    def proj(self, out, w, wcols, rhs_fn, reads, pk, kchunks=8):
        c0, c1 = wcols
        self.mmg(out, [(w[:, kc, c0:c1], rhs_fn(kc)) for kc in range(kchunks)], reads, pk)

    def rstd_from(self, dst, src_ps, n_feat, reads, writes):
        p = dst.shape[0] if hasattr(dst, 'shape') else 128
        self.act(dst, src_ps, AF.Sqrt, reads, writes, bias=self.eps_col[0:p, 0:1], scale=1.0 / n_feat)
        self.recip(dst, dst, writes, writes)

    def outproj(self, l, wo, o_blk, okeys, b, t0, n):
        r = 1 if b == 0 else 0
        hT, modT = self.hT, self.modT
        for oc in range(8):
            py, pyk = self.psum('ffy', [4, 5, 6, 7])
            self.mmg(py[:, 0:n], [(wo[:, p, oc * 128:(oc + 1) * 128], o_blk[:, p, 0:n]) for p in range(2)],
                     [wo.k] + okeys, pyk)
            self.stt(hT[:, oc, t0:t0 + n], py[:, 0:n], modT[:, l, 2 * 8 + oc, r:r + 1], hT[:, oc, t0:t0 + n],
                     ALU.mult, ALU.add, [pyk, ('modT', l), ('hT', oc, b)], [('hT', oc, b)])

    def load_wo(self, l, grp):
        wo = self.ar('wo', [128, 2, 1024], BF16)
        self.load_w(wo[:, :, :], self.w_out[l][grp * 256:(grp + 1) * 256, :], 2, 1024, wo.k)
        return wo

    def attn(self, spairs, vfn, tiles, scale, n, half, out_ap, out_key, reads, sink_col=None):
        P = self.P
        pO, pOk = self.psum('aO', [4, 6])
        pD, pDk = self.psum('aD', [5, 7])
        nt = len(tiles)

        def smm(i):
            pS, pSk = self.psum('aS', [0, 1, 2])
            self.mmg(pS[:, 0:n], spairs(tiles[i][0]), reads, pSk)
            return pS, pSk

        nxt = smm(0)
        for i, (tile, mask) in enumerate(tiles):
            pS, pSk = nxt
            if i + 1 < nt:
                nxt = smm(i + 1)
            pt = self.PT[self.pti % 3]
            self.pti += 1
            self.act(pt[:, 0:n], pS[:, 0:n], AF.Exp, [pSk], [pt.k], scale=scale)
            if mask is not None:
                self.tt(pt[:, 0:n], pt[:, 0:n], mask[:, 0:n], ALU.mult, [pt.k, 'swamask'], [pt.k])
            v = vfn(tile)
            P.op('pe', lambda e, v=v, pt=pt, s=(i == 0), t=(i == nt - 1): e.matmul(pO[:, 0:n], lhsT=v, rhs=pt[:, 0:n], start=s, stop=t),
                 reads=reads + [pt.k], writes=[pOk], inc=False)
            P.op('pe', lambda e, pt=pt, s=(i == 0), t=(i == nt - 1): e.matmul(pD[:, 0:n], lhsT=self.ones_bf[:, :], rhs=pt[:, 0:n], start=s, stop=t),
                 reads=[pt.k, 'ones_bf'], writes=[pOk, pDk], inc=True)
        r0 = half * 64
        rd = self.rd
        if sink_col is not None:
            self.ts(rd[r0:r0 + 64, 0:n], pD[r0:r0 + 64, 0:n], sink_col[r0:r0 + 64, :], None, ALU.add, None, [pDk, 'esink'], ['rd'])
            self.recip(rd[r0:r0 + 64, 0:n], rd[r0:r0 + 64, 0:n], ['rd'], ['rd'])
        else:
            self.recip(rd[r0:r0 + 64, 0:n], pD[r0:r0 + 64, 0:n], [pDk], ['rd'])
        self.tt(out_ap, pO[r0:r0 + 64, 0:n], rd[r0:r0 + 64, 0:n], ALU.mult, [pOk, 'rd'], [out_key])

    def mla(self, l):
        P = self.P
        aT = self.aT
        wm = self.ar('wm', [128, 8, 352], BF16)
        self.load_w(wm[:, :, :], self.w_in[l][:, 0:352], 8, 352, wm.k)
        wuq = self.ar('wuq', [128, 2, 384], BF16)
        P.dma('pool', wuq[:, 0, :], self.din['mla_w_uq'][l][0:128, :], writes=[wuq.k])
        P.dma('pool', wuq[0:64, 1, :], self.din['mla_w_uq'][l][128:192, :], writes=[wuq.k])
        wukv = self.ar('wukv', [128, 512], BF16)
        P.dma('pool', wukv[:, :], self.din['mla_w_ukv'][l], writes=[wukv.k])
        cv = self.colv
        self.ts(wuq[:, 0, :], wuq[:, 0, :], cv[:, l, 0:1], None, ALU.mult, None, [wuq.k, 'colv'], [wuq.k])
        self.ts(wuq[0:64, 1, :], wuq[0:64, 1, :], cv[0:64, l, 1:2], None, ALU.mult, None, [wuq.k, 'colv'], [wuq.k])
        self.ts(wukv[:, :], wukv[:, :], cv[:, l, 2:3], None, ALU.mult, None, [wukv.k, 'colv'], [wukv.k])
        r32 = self.ar('r32', [32, 32], BF16)
        P.dma('pool', r32[:, :], self.din['r64b'][0:32, 0:32], writes=[r32.k])
        wo = self.load_wo(l, 0)
        kN = self.ar('kN', [64, 4, T], BF16)
        kP = self.ar('kP', [32, T], BF16)
        vt = self.ar('vt', [128, NT, 256], BF16)
        cs = self.ar('cs', [32, 2, 512], F32)
        kvl = self.ar('kvl', [128, 512], BF16)
        sq = self.ar('sq', [128, 2, 512], BF16)
        rs = self.ar('rs', [128, 512], F32)
        rsc = self.ar('rsc', [128, NT], F32)
        xb = self.ar('xb', [32, 512], BF16)
        t1 = self.ar('t1', [32, 512], F32)
        t2 = self.ar('t2', [32, 512], F32)
        qlb = self.ar('qlb', [128, 2, 512], BF16)
        qp32 = self.ar('qp32', [32, 512], F32)
        qn = [self.ar('qn%d' % i, [64, 512], BF16) for i in range(2)]
        qp = [self.ar('qp%d' % i, [32, 512], BF16) for i in range(2)]
        oblk = [self.ar('oblk%d' % i, [128, 2, 512], BF16) for i in range(2)]
        self.PT = [self.ar('PT%d' % i, [128, 512], BF16) for i in range(3)]
        self.pti = 0
        self.rd = self.ar('rd', [128, 512], F32)
        self.rd.k = 'rd'
        cosd, sind = self.din['cos_mla'], self.din['sin_mla']

        def rope32(dst, src, skey, n, wkey):
            self.cp('act', xb[:, 0:n], src, [skey], [xb.k])
            pr, prk = self.psum('mB', [2, 3])
            self.mmg(pr[0:32, 0:n], [(r32[:, :], xb[:, 0:n])], [r32.k, xb.k], prk)
            RV = int(os.environ.get('ROPEV', '9'))
            if RV == 1:
                self.tt(t1[:, 0:n], src, src, ALU.mult, [skey], [t1.k])
                return
            if RV == 5:
                return
            if RV == 3:
                self.tt(t1[:, 0:n], rs[0:32, 0:n], rs[0:32, 0:n], ALU.mult, [rs.k], [t1.k])
                return
            if RV == 4:
                self.tt(rs[0:32, 0:n], cs[:, 0, 0:n], cs[:, 0, 0:n], ALU.mult, [cs.k, rs.k], [rs.k])
                return
            if RV == 2:
                self.tt(t1[:, 0:n], cs[:, 0, 0:n], cs[:, 0, 0:n], ALU.mult, [cs.k], [t1.k])
                return
            self.tt(t1[:, 0:n], src, cs[:, 0, 0:n], ALU.mult, [skey, cs.k, xb.k], [t1.k])
            self.tt(t2[:, 0:n], pr[0:32, 0:n], cs[:, 1, 0:n], ALU.mult, [prk, cs.k], [t2.k])
            self.tt(dst, t1[:, 0:n], t2[:, 0:n], ALU.add, [t1.k, t2.k], [wkey])

        for b, (t0, n) in enumerate(BLKS):
            ak = ('aT', b)
            P.dma('sp', cs[:, 0, 0:n], cosd[:, t0:t0 + n], writes=[cs.k])
            P.dma('sp', cs[:, 1, 0:n], sind[:, t0:t0 + n], writes=[cs.k])
            pk_, pkk = self.psum('mA', [0, 1])
            self.proj(pk_[:, 0:n], wm, (192, 320), lambda kc: aT[:, kc, t0:t0 + n], [wm.k, ak], pkk)
            self.cp('act', kvl[:, 0:n], pk_[:, 0:n], [pkk], [kvl.k])
            self.act(sq[:, 0, 0:n], pk_[:, 0:n], AF.Square, [pkk], [sq.k])
            ps_, psk = self.psum('mB', [2, 3])
            self.mmg(ps_[:, 0:n], [(self.ones_bf[:, :], sq[:, 0, 0:n])], [sq.k, 'ones_bf'], psk)
            self.rstd_from(rs[:, 0:n], ps_[:, 0:n], 128.0, [psk], [rs.k])
            for tt_ in range(n // 128):
                tile = t0 // 128 + tt_
                pc, pck = self.psum('mB', [2, 3])
                self.mmg(pc[:, 0:1], [(sq[:, 0, tt_ * 128:(tt_ + 1) * 128], self.ones_bf[:, 0:1])], [sq.k, 'ones_bf'], pck)
                self.rstd_from(rsc[:, tile:tile + 1], pc[:, 0:1], 128.0, [pck], [rsc.k])
                pv, pvk = self.psum('mA', [0, 1])
                for h in range(4):
                    self.mmg(pv[:, h * 64:(h + 1) * 64], [(kvl[:, tt_ * 128:(tt_ + 1) * 128], wukv[:, h * 128 + 64:h * 128 + 128])],
                             [kvl.k, wukv.k], pvk)
                self.ts(vt[:, tile, :], pv[:, 0:256], rsc[:, tile:tile + 1], None, ALU.mult, None, [pvk, rsc.k], [vt.k])
            for h in range(4):
                pk2, pk2k = self.psum('mA', [0, 1])
                self.mmg(pk2[0:64, 0:n], [(wukv[:, h * 128:h * 128 + 64], kvl[:, 0:n])], [kvl.k, wukv.k], pk2k)
                self.tt(kN[:, h, t0:t0 + n], pk2[0:64, 0:n], rs[0:64, 0:n], ALU.mult, [pk2k, rs.k], [kN.k])
            pp, ppk = self.psum('mA', [0, 1])
            self.proj(pp[0:32, 0:n], wm, (320, 352), lambda kc: aT[:, kc, t0:t0 + n], [wm.k, ak], ppk)
            rope32(kP[:, t0:t0 + n], pp[0:32, 0:n], ppk, n, kP.k)
        STOP = int(os.environ.get('MLA_STOP', '9'))
        if STOP <= 2:
            return
        scale = 96.0 ** -0.5
        qi = 0
        for b, (t0, n) in enumerate(BLKS):
            ak = ('aT', b)
            P.dma('sp', cs[:, 0, 0:n], cosd[:, t0:t0 + n], writes=[cs.k])
            P.dma('sp', cs[:, 1, 0:n], sind[:, t0:t0 + n], writes=[cs.k])
            p0, p0k = self.psum('mA', [0, 1])
            self.proj(p0[:, 0:n], wm, (0, 128), lambda kc: aT[:, kc, t0:t0 + n], [wm.k, ak], p0k)
            p1, p1k = self.psum('mA', [0, 1])
            self.proj(p1[0:64, 0:n], wm, (128, 192), lambda kc: aT[:, kc, t0:t0 + n], [wm.k, ak], p1k)
            self.cp('act', qlb[:, 0, 0:n], p0[:, 0:n], [p0k], [qlb.k])
            self.cp('act', qlb[0:64, 1, 0:n], p1[0:64, 0:n], [p1k], [qlb.k])
            self.act(sq[:, 0, 0:n], p0[:, 0:n], AF.Square, [p0k], [sq.k])
            self.act(sq[0:64, 1, 0:n], p1[0:64, 0:n], AF.Square, [p1k], [sq.k])
            ps_, psk = self.psum('mB', [2, 3])
            self.mmg(ps_[:, 0:n], [(self.ones_bf[:, :], sq[:, 0, 0:n]), (self.ones_bf[0:64, :], sq[0:64, 1, 0:n])],
                     [sq.k, 'ones_bf'], psk)
            self.rstd_from(rs[:, 0:n], ps_[:, 0:n], 192.0, [psk], [rs.k])
            ob = oblk[b % 2]
            tiles = [(0, None), (1, None)] if b == 0 else [(t, None) for t in range(NT)]
            if STOP <= 3:
                continue
            for h in range(4):
                qn_, qp_ = qn[qi % 2], qp[qi % 2]
                qi += 1
                pq, pqk = self.psum('mB', [2, 3])
                self.mmg(pq[0:64, 0:n], [(wuq[:, 0, h * 96:h * 96 + 64], qlb[:, 0, 0:n]),
                                         (wuq[0:64, 1, h * 96:h * 96 + 64], qlb[0:64, 1, 0:n])], [wuq.k, qlb.k], pqk)
                self.tt(qn_[:, 0:n], pq[0:64, 0:n], rs[0:64, 0:n], ALU.mult, [pqk, rs.k], [qn_.k])
                pq2, pq2k = self.psum('mB', [2, 3])
                self.mmg(pq2[0:32, 0:n], [(wuq[:, 0, h * 96 + 64:h * 96 + 96], qlb[:, 0, 0:n]),
                                          (wuq[0:64, 1, h * 96 + 64:h * 96 + 96], qlb[0:64, 1, 0:n])], [wuq.k, qlb.k], pq2k)
                self.tt(qp32[:, 0:n], pq2[0:32, 0:n], rs[0:32, 0:n], ALU.mult, [pq2k, rs.k], [qp32.k])
                rope32(qp_[:, 0:n], qp32[:, 0:n], qp32.k, n, qp_.k)
                p_, hh = h // 2, h % 2
                if STOP <= 4:
                    continue
                self.attn(lambda t, h=h, qn_=qn_, qp_=qp_, n=n: [(kN[:, h, t * 128:(t + 1) * 128], qn_[:, 0:n]),
                                                                 (kP[:, t * 128:(t + 1) * 128], qp_[:, 0:n])],
                          lambda t, p_=p_: vt[:, t, p_ * 128:(p_ + 1) * 128], tiles, scale, n, hh,
                          ob[hh * 64:hh * 64 + 64, p_, 0:n], (ob.k, h), [kN.k, kP.k, vt.k, qn_.k, qp_.k])
            if STOP <= 5:
                continue
            self.outproj(l, wo, ob, [(ob.k, h) for h in range(4)], b, t0, n)

    def swa(self, l):
        P = self.P
        aT = self.aT
        ws = self.ar('ws', [128, 8, 512], BF16)
        self.load_w(ws[:, :, :], self.w_in[l][:, C_SQ:C_SQ + 512], 8, 512, ws.k)
        r128 = self.ar('r128', [128, 128], BF16)
        P.dma('pool', r128[:, :], self.din['r128'], writes=[r128.k])
        wo = self.load_wo(l, 2)
        msk = self.ar('swamask', [128, 6, 512], BF16)
        msk.k = 'swamask'
        for r in range(6):
            P.dma('pool', msk[:, r, :], self.din['swa_mask'][r], writes=['swamask'])
        esink = self.ar('esink', [128, 4], F32)
        esink.k = 'esink'
        P.dma('sp', esink[:, :], self.din['swa_sink'][l:l + 1, :].broadcast_to([128, 4]), writes=['esink'])
        self.act(esink[:, :], esink[:, :], AF.Exp, ['esink'], ['esink'])
        kx = self.ar('kx', [128, T], BF16)
        kxs = self.ar('kxs', [128, T], BF16)
        vn = self.ar('vn', [128, NT, 128], BF16)
        vs = self.ar('vs', [128, NT, 128], BF16)
        cs = self.ar('cs', [128, 2, 512], F32)
        xb = self.ar('xb', [128, 512], BF16)
        t1 = self.ar('t1', [128, 512], F32)
        t2 = self.ar('t2', [128, 512], F32)
        qx = [self.ar('qx%d' % i, [128, 2, 512], BF16) for i in range(2)]
        oblk = [self.ar('oblk%d' % i, [128, 2, 512], BF16) for i in range(2)]
        self.PT = [self.ar('PT%d' % i, [128, 512], BF16) for i in range(3)]
        self.pti = 0
        self.rd = self.ar('rd', [128, 512], F32)
        self.rd.k = 'rd'
        cosd, sind = self.din['cos_swa'], self.din['sin_swa']

        def rope(dst, ps_x, psk, n, wkey):
            self.cp('act', xb[:, 0:n], ps_x[:, 0:n], [psk], [xb.k])
            pr, prk = self.psum('mB', [2, 3])
            self.mmg(pr[:, 0:n], [(r128[:, :], xb[:, 0:n])], [r128.k, xb.k], prk)
            self.tt(t1[:, 0:n], ps_x[:, 0:n], cs[:, 0, 0:n], ALU.mult, [psk, cs.k], [t1.k])
            self.tt(t2[:, 0:n], pr[:, 0:n], cs[:, 1, 0:n], ALU.mult, [prk, cs.k], [t2.k])
            self.tt(dst, t1[:, 0:n], t2[:, 0:n], ALU.add, [t1.k, t2.k], [wkey])

        for b, (t0, n) in enumerate(BLKS):
            ak = ('aT', b)
            for hf in range(2):
                P.dma('sp', cs[hf * 64:(hf + 1) * 64, 0, 0:n], cosd[:, t0:t0 + n], writes=[cs.k])
                P.dma('sp', cs[hf * 64:(hf + 1) * 64, 1, 0:n], sind[:, t0:t0 + n], writes=[cs.k])
            pk_, pkk = self.psum('mA', [0, 1])
            self.proj(pk_[:, 0:n], ws, (256, 384), lambda kc: aT[:, kc, t0:t0 + n], [ws.k, ak], pkk)
            rope(kx[:, t0:t0 + n], pk_, pkk, n, kx.k)
            for tt_ in range(n // 128):
                tile = t0 // 128 + tt_
                pv, pvk = self.psum('mA', [0, 1])
                self.mmg(pv[:, 0:128], [(aT[:, kc, tile * 128:(tile + 1) * 128], ws[:, kc, 384:512]) for kc in range(8)],
                         [ws.k, ak], pvk)
                self.cp('act', vn[:, tile, :], pv[:, 0:128], [pvk], [vn.k])
                self.cp('dve', vs[:, tile, 0:64], pv[:, 64:128], [pvk], [vs.k])
                self.cp('dve', vs[:, tile, 64:128], pv[:, 0:64], [pvk], [vs.k])
        P.dma('sp', kxs[0:64, :], kx[64:128, :], reads=[kx.k], writes=[kxs.k])
        P.dma('sp', kxs[64:128, :], kx[0:64, :], reads=[kx.k], writes=[kxs.k])
        scale = 64.0 ** -0.5
        for b, (t0, n) in enumerate(BLKS):
            ak = ('aT', b)
            for hf in range(2):
                P.dma('sp', cs[hf * 64:(hf + 1) * 64, 0, 0:n], cosd[:, t0:t0 + n], writes=[cs.k])
                P.dma('sp', cs[hf * 64:(hf + 1) * 64, 1, 0:n], sind[:, t0:t0 + n], writes=[cs.k])
            q_ = qx[b % 2]
            for p_ in range(2):
                pq, pqk = self.psum('mA', [0, 1])
                self.proj(pq[:, 0:n], ws, (p_ * 128, (p_ + 1) * 128), lambda kc: aT[:, kc, t0:t0 + n], [ws.k, ak], pqk)
                rope(q_[:, p_, 0:n], pq, pqk, n, q_.k)
            ob = oblk[b % 2]
            if b == 0:
                tiles = [(0, None), (1, None)]
            else:
                g0 = t0 // 128
                tiles = [(0, None), (1, None)]
                for r in range(-1, 5):
                    j = g0 + r
                    if 2 <= j <= 17:
                        tiles.append((j, msk[:, r + 1, :]))
            for h in range(4):
                g, hh, p_ = h // 2, h % 2, h // 2
                ksrc = kx if g == hh else kxs
                vsrc = vn if g == hh else vs
                self.attn(lambda t, ksrc=ksrc, hh=hh, q_=q_, p_=p_, n=n: [(ksrc[hh * 64:hh * 64 + 64, t * 128:(t + 1) * 128],
                                                                               q_[hh * 64:hh * 64 + 64, p_, 0:n])],
                          lambda t, vsrc=vsrc: vsrc[:, t, :], tiles, scale, n, hh,
                          ob[hh * 64:hh * 64 + 64, p_, 0:n], (ob.k, h), [kx.k, kxs.k, vn.k, vs.k, q_.k],
                          sink_col=esink[:, h:h + 1])
            self.outproj(l, wo, ob, [(ob.k, h) for h in range(4)], b, t0, n)

    def headnorm_gate(self, l, o32, okey, n, t0, b, w, gcols, center, gcol, bcol, oblk, ak):
        aT = self.aT
        ob16 = self.hn_b16
        c32 = self.hn_c32
        blk64 = self.blk64
        for p in range(2):
            src = o32[:, p, 0:n]
            if center:
                self.cp('act', ob16[:, 0:n], src, [okey], [ob16.k])
                pm, pmk = self.psum('mB', [2, 3])
                self.mmg(pm[:, 0:n], [(blk64[:, :], ob16[:, 0:n])], [ob16.k, blk64.k], pmk)
                self.stt(c32[:, 0:n], pm[:, 0:n], -1.0, src, ALU.mult, ALU.add, [okey, pmk], [c32.k])
                cs_ = c32[:, 0:n]
                ck = c32.k
            else:
                cs_ = src
                ck = okey
            self.act(ob16[:, 0:n], cs_, AF.Square, [ck], [ob16.k])
            pv_, pvk = self.psum('mB', [2, 3])
            self.mmg(pv_[:, 0:n], [(blk64[:, :], ob16[:, 0:n])], [ob16.k, blk64.k], pvk)
            rs = self.hn_rs
            self.rstd_from(rs[:, 0:n], pv_[:, 0:n], 1.0, [pvk], [rs.k])
            self.tt(c32[:, 0:n], cs_, rs[:, 0:n], ALU.mult, [ck, rs.k], [c32.k])
            if bcol is not None:
                self.ts(c32[:, 0:n], c32[:, 0:n], self.colv[:, l, gcol + p:gcol + p + 1],
                        self.colv[:, l, bcol + p:bcol + p + 1], ALU.mult, ALU.add, [c32.k, 'colv'], [c32.k])
            else:
                self.ts(c32[:, 0:n], c32[:, 0:n], self.colv[:, l, gcol + p:gcol + p + 1], None, ALU.mult, None,
                        [c32.k, 'colv'], [c32.k])
            pg, pgk = self.psum('mA', [0, 1])
            self.proj(pg[:, 0:n], w, (gcols + p * 128, gcols + (p + 1) * 128), lambda kc: aT[:, kc, t0:t0 + n], [w.k, ak], pgk)
            sg = self.hn_sg
            self.act(sg[:, 0:n], pg[:, 0:n], AF.Silu, [pgk], [sg.k])
            self.tt(oblk[:, p, 0:n], c32[:, 0:n], sg[:, 0:n], ALU.mult, [c32.k, sg.k], [(oblk.k, p)])

    def hn_alloc(self):
        self.hn_b16 = self.ar('hn_b16', [128, 512], BF16)
        self.hn_c32 = self.ar('hn_c32', [128, 512], F32)
        self.hn_rs = self.ar('hn_rs', [128, 512], F32)
        self.hn_sg = self.ar('hn_sg', [128, 512], BF16)
        self.blk64 = self.ar('blk64', [128, 128], BF16)
        self.P.dma('pool', self.blk64[:, :], self.din['blk64'], writes=[self.blk64.k])

    def ret(self, l):
        P = self.P
        aT = self.aT
        ident = self.ident
        r64b = self.ar('r64b', [64, 64], BF16)
        P.dma('pool', r64b[:, :], self.din['r64b'], writes=[r64b.k])
        wo = self.load_wo(l, 3)
        qx = self.ar('qx', [64, 2, T], BF16)
        kx = self.ar('kx', [64, 2, T], BF16)
        kxt = self.ar('kxt', [128, NT, 128], BF16)
        vt = self.ar('vt', [128, NT, 256], BF16)
        Sf = self.ar('Sf', [64, NT, 2, 128], BF16)
        Dsum = self.ar('Dsum', [128, 4, 128], BF16)
        zeta = self.ar('zeta', [128, 2, 4], F32)
        lgcol = self.ar('lgcol', [64, 2, 2], F32)
        xi = self.ar('xi', [64, 2, 2, 128], F32)
        dc = self.ar('dc', [64, 2, 2], F32)
        mark0 = self.ar_off
        w = self.ar('wr_', [128, 8, 512], BF16)
        self.load_w(w[:, :, :], self.w_in[l][:, C_RQ:C_RQ + 512], 8, 512, w.k)
        mark1 = self.ar_off
        lgb = self.ar('lgb', [128, 8], F32)
        rel = self.ar('rel', [128, 4, 128], F32)
        colc = self.ar('colc', [128, 16], F32)
        rowc = self.ar('rowc', [64, 2, 128], F32)
        dtmp = self.ar('dtmp', [128, 2, 128], F32)
        cosd, sind = self.din['cos_ret'], self.din['sin_ret']
        P.dma('sp', lgb[:, :], self.din['ret_decay_logit'][l:l + 1].rearrange("o d h -> o (d h)").broadcast_to([128, 8]),
              writes=[lgb.k])
        P.dma('sp', rel[:, :, :], self.din['ret_rel'].rearrange("r s t -> s r t"), writes=[rel.k])
        P.dma('sp', colc[:, :], self.din['ret_colc'], writes=[colc.k])
        P.dma('sp', rowc[:, :, :], self.din['ret_rowc'].rearrange("p (d t) -> p d t", d=2), writes=[rowc.k])
        self.act(lgb[:, :], lgb[:, :], AF.Exp, [lgb.k], [lgb.k], scale=-1.0)
        self.ts(lgb[:, :], lgb[:, :], 1.0, None, ALU.add, None, [lgb.k], [lgb.k])
        self.act(lgb[:, :], lgb[:, :], AF.Ln, [lgb.k], [lgb.k])
        self.ts(lgb[:, :], lgb[:, :], -1.0, None, ALU.mult, None, [lgb.k], [lgb.k])
        for h in range(4):
            self.act(dtmp[:, 0, :], rel[:, 0, :], AF.Exp, [rel.k, lgb.k], [dtmp.k], scale=lgb[:, h:h + 1])
            self.tt(dtmp[:, 0, :], dtmp[:, 0, :], rel[:, 1, :], ALU.mult, [dtmp.k, rel.k], [dtmp.k])
            self.act(dtmp[:, 1, :], rel[:, 2, :], AF.Exp, [rel.k, lgb.k], [dtmp.k], scale=lgb[:, 4 + h:5 + h])
            self.tt(dtmp[:, 1, :], dtmp[:, 1, :], rel[:, 3, :], ALU.mult, [dtmp.k, rel.k], [dtmp.k])
            self.tt(Dsum[:, h, :], dtmp[:, 0, :], dtmp[:, 1, :], ALU.add, [dtmp.k], [Dsum.k])
        for d in range(2):
            self.act(zeta[:, d, :], lgb[:, d * 4:d * 4 + 4], AF.Exp, [lgb.k, colc.k], [zeta.k], scale=colc[:, d:d + 1])
            for p in range(2):
                self.cp('dve', lgcol[0:32, d, p:p + 1], lgb[0:32, d * 4 + 2 * p:d * 4 + 2 * p + 1], [lgb.k], [lgcol.k])
                self.cp('dve', lgcol[32:64, d, p:p + 1], lgb[32:64, d * 4 + 2 * p + 1:d * 4 + 2 * p + 2], [lgb.k], [lgcol.k])
        for d in range(2):
            for p in range(2):
                self.act(xi[:, d, p, :], rowc[:, d, :], AF.Exp, [rowc.k, lgcol.k], [xi.k], scale=lgcol[:, d, p:p + 1])
        self.act(dc[:, :, :], lgcol[:, :, :], AF.Exp, [lgcol.k], [dc.k], scale=128.0)
        RSTOP = int(os.environ.get('RET_STOP', '9'))
        if RSTOP <= 1:
            return
        self.ar_release(mark1)
        cs = self.ar('cs', [64, 2, 512], F32)
        xb = self.ar('xb', [64, 512], BF16)
        t1 = self.ar('t1', [64, 512], F32)
        t2 = self.ar('t2', [64, 512], F32)

        def rope64(dst, ps_x, psk, n, wkey):
            self.cp('act', xb[:, 0:n], ps_x, [psk], [xb.k])
            pr, prk = self.psum('mB', [2, 3])
            self.mmg(pr[0:64, 0:n], [(r64b[:, :], xb[:, 0:n])], [r64b.k, xb.k], prk)
            self.tt(t1[:, 0:n], ps_x, cs[:, 0, 0:n], ALU.mult, [psk, cs.k], [t1.k])
            self.tt(t2[:, 0:n], pr[0:64, 0:n], cs[:, 1, 0:n], ALU.mult, [prk, cs.k], [t2.k])
            self.tt(t1[:, 0:n], t1[:, 0:n], t2[:, 0:n], ALU.add, [t1.k, t2.k], [t1.k])
            self.cp('act', dst, t1[:, 0:n], [t1.k], [wkey])

        for b, (t0, n) in enumerate(BLKS):
            ak = ('aT', b)
            for hf in range(2):
                P.dma('sp', cs[hf * 32:(hf + 1) * 32, 0, 0:n], cosd[:, t0:t0 + n], writes=[cs.k])
                P.dma('sp', cs[hf * 32:(hf + 1) * 32, 1, 0:n], sind[:, t0:t0 + n], writes=[cs.k])
            for p in range(2):
                pq, pqk = self.psum('mA', [0, 1])
                self.proj(pq[0:64, 0:n], w, (p * 64, (p + 1) * 64), lambda kc: aT[:, kc, t0:t0 + n], [w.k, ak], pqk)
                rope64(qx[:, p, t0:t0 + n], pq[0:64, 0:n], pqk, n, qx.k)
                pk_, pkk = self.psum('mA', [0, 1])
                self.proj(pk_[0:64, 0:n], w, (128 + p * 64, 128 + (p + 1) * 64), lambda kc: aT[:, kc, t0:t0 + n], [w.k, ak], pkk)
                rope64(kx[:, p, t0:t0 + n], pk_[0:64, 0:n], pkk, n, kx.k)
                for tt_ in range(n // 128):
                    tile = t0 // 128 + tt_
                    ptr, ptk = self.psum('mB', [2, 3])
                    self.tr(ptr[:, 0:64], t1[:, tt_ * 128:(tt_ + 1) * 128], ident[0:64, 0:64], [t1.k, 'ident'], ptk)
                    self.cp('act', kxt[:, tile, p * 64:(p + 1) * 64], ptr[:, 0:64], [ptk], [kxt.k])
            for tt_ in range(n // 128):
                tile = t0 // 128 + tt_
                pv, pvk = self.psum('mA', [0, 1])
                self.mmg(pv[:, 0:256], [(aT[:, kc, tile * 128:(tile + 1) * 128], w[:, kc, 256:512]) for kc in range(8)],
                         [w.k, ak], pvk)
                self.cp('act', vt[:, tile, :], pv[:, 0:256], [pvk], [vt.k])

        if RSTOP <= 2:
            return
        self.ar_release(mark0)
        w = self.ar('wg_', [128, 8, 256], BF16)
        self.load_w(w[:, :, :], self.w_in[l][:, C_RG:C_RG + 256], 8, 256, w.k)
        self.hn_alloc()
        S = self.ar('S', [64, 2, 128], F32)
        Sbb = self.ar('Sbb', [64, 2, 128], BF16)
        vz = self.ar('vz', [128, 256], BF16)
        qxm = self.ar('qxm', [64, 2, 2, 128], BF16)
        qxd = self.ar('qxd', [64, 2, 2, 2, 128], BF16)
        rowm = self.ar('rowm', [64, 16], F32)
        P.dma('sp', rowm[:, :], self.din['ret_rowm'], writes=[rowm.k])
        AT = self.ar('AT', [128, 4, 128], BF16)
        o32 = self.ar('o32', [128, 2, 512], F32)
        oblk = self.ar('oblk', [128, 2, 512], BF16)

        def state_step(d, tile):
            self.tt(vz[:, :].rearrange("s (h v) -> s h v", h=4), vt[:, tile, :].rearrange("s (h v) -> s h v", h=4),
                    zeta[:, d, :].unsqueeze(2).broadcast_to([128, 4, 64]), ALU.mult, [vt.k, zeta.k], [vz.k])
            pu, puk = self.psum('mA', [0, 1])
            for p in range(2):
                self.mmg(pu[0:64, p * 128:(p + 1) * 128], [(kxt[:, tile, p * 64:(p + 1) * 64], vz[:, p * 128:(p + 1) * 128])],
                         [kxt.k, vz.k], puk)
            for p in range(2):
                self.stt(S[:, p, :], S[:, p, :], dc[:, d, p:p + 1], pu[0:64, p * 128:(p + 1) * 128], ALU.mult, ALU.add,
                         [S.k, dc.k, puk], [S.k])

        self.memset('dve', S[:, :, :], 0.0, [S.k])
        for tile in range(NT):
            self.cp('act', Sf[:, tile, :, :], S[:, :, :], [S.k], [(Sf.k, tile)])
            if tile < NT - 1:
                state_step(0, tile)
        if RSTOP <= 3:
            return
        self.memset('dve', S[:, :, :], 0.0, [S.k])
        order = [1, 0] + list(range(NT - 1, 1, -1))
        for idx, tile in enumerate(order):
            self.cp('act', Sbb[:, :, :], S[:, :, :], [S.k], [Sbb.k])
            for hh in range(2):
                self.ts(qxm[:, hh, :, :], qx[:, :, tile * 128:(tile + 1) * 128], rowm[:, hh:hh + 1], None, ALU.mult, None,
                        [qx.k, rowm.k], [qxm.k])
            for d in range(2):
                for hh in range(2):
                    self.tt(qxd[:, d, hh, :, :], qxm[:, hh, :, :], xi[:, d, :, :], ALU.mult, [qxm.k, xi.k], [qxd.k])
            pA, pAk = self.psum('rA', [4, 5])
            for h in range(4):
                hh, p = h % 2, h // 2
                self.mmg(pA[:, h * 128:(h + 1) * 128], [(kx[:, p, tile * 128:(tile + 1) * 128], qxm[:, hh, p, :])],
                         [kx.k, qxm.k], pAk)
            self.tt(AT[:, :, :], pA[:, :].rearrange("s (h t) -> s h t", h=4), Dsum[:, :, :], ALU.mult, [pAk, Dsum.k], [AT.k])
            if RSTOP <= 4:
                continue
            pO, pOk = self.psum('rO', [6, 7])
            for h in range(4):
                hh, p = h % 2, h // 2
                self.mmg(pO[:, h * 128:(h + 1) * 128],
                         [(vt[:, tile, p * 128:(p + 1) * 128], AT[:, h, :]),
                          (Sf[:, tile, p, :], qxd[:, 0, hh, p, :]),
                          (Sbb[:, p, :], qxd[:, 1, hh, p, :])],
                         [vt.k, AT.k, (Sf.k, tile), Sbb.k, qxd.k], pOk)
            b = self.blk_of_tile(tile)
            t0, n = BLKS[b]
            off = tile * 128 - t0
            pv4 = pO[:, :].rearrange("q (p j t) -> q p j t", p=2, j=2)
            for hh in range(2):
                self.ts(o32[hh * 64:hh * 64 + 64, :, off:off + 128], pv4[hh * 64:hh * 64 + 64, :, hh, :], 32.0 ** -0.5, None,
                        ALU.mult, None, [pOk], [(o32.k, tile % 4, hh)])
            if RSTOP <= 5:
                continue
            if idx < NT - 1:
                state_step(1, tile)
            if RSTOP <= 6:
                continue
            if tile * 128 == t0:
                ntile = n // 128
                okeys = [(o32.k, (t0 // 128 + j) % 4, hh) for j in range(ntile) for hh in range(2)]
                self.P.op('dve', lambda e: e.tensor_copy(out=o32[:, :, 0:1], in_=o32[:, :, 0:1]), reads=okeys, writes=[o32.k] + okeys)
                self.headnorm_gate(l, o32, o32.k, n, t0, b, w, 0, True, 5, 7, oblk, ('aT', b))
                self.outproj(l, wo, oblk, [(oblk.k, 0), (oblk.k, 1)], b, t0, n)


    def hgrn(self, l):
        P = self.P
        aT = self.aT
        ident = self.ident
        wh = self.ar('wh', [128, 8, 1280], BF16)
        self.load_w(wh[:, :, :], self.w_in[l][:, C_HQ:C_HQ + 1280], 8, 1280, wh.k)
        itm = self.ar('itm', [128, NT, 256], BF16)
        ohg = self.ar('ohg', [128, 2, T], BF16)
        trie = self.ar('trie', [128, 2, 136], F32)
        P.dma('sp', trie[:, :, :], self.din['hg_trie'].rearrange("d s c -> s d c"), writes=[trie.k])
        mt = self.ar('mt', [128, 2, 128], BF16)
        P.dma('pool', mt[:, :, :], self.din['hg_mt'].rearrange("d s c -> s d c"), writes=[mt.k])
        ee = self.ar('ee', [128, 2, 8], BF16)
        P.dma('pool', ee[:, :, :], self.din['hg_e'].rearrange("d s c -> s d c"), writes=[ee.k])
        omlF = self.ar('omlF', [64, 8], F32)
        nomlF = self.ar('nomlF', [64, 8], F32)
        lbB = self.ar('lbB', [128, 512], F32)
        omlB = self.ar('omlB', [128, 512], F32)
        mark = self.ar_off
        st = self.ar('st', [32, 64], F32)
        lbl = self.ar('lbl', [64, 4, 8], F32)
        sF = self.ar('sF', [64, 8], F32)
        cF = self.ar('cF', [64, 8], F32)
        P.dma('sp', st[:, :], self.din['hg_lb_logits'].rearrange("l d (h k) -> (l d h) k", k=64), writes=[st.k])
        pt, pk = self.psum('mA', [0, 1])
        self.tr(pt[0:64, 0:32], st[:, :], ident[0:32, 0:32], [st.k, 'ident'], pk)
        self.act(lbl[:, :, :], pt[0:64, 0:32].rearrange("k (l x) -> k l x", l=4), AF.Exp, [pk], [lbl.k])
        self.tt(sF[:, :], lbl[:, 0, :], lbl[:, 1, :], ALU.add, [lbl.k], [sF.k])
        self.tt(sF[:, :], sF[:, :], lbl[:, 2, :], ALU.add, [lbl.k, sF.k], [sF.k])
        self.tt(sF[:, :], sF[:, :], lbl[:, 3, :], ALU.add, [lbl.k, sF.k], [sF.k])
        self.recip(sF[:, :], sF[:, :], [sF.k], [sF.k])
        self.memset('dve', cF[:, :], 0.0, [cF.k])
        for j in range(1, l + 1):
            self.tt(cF[:, :], cF[:, :], lbl[:, j, :], ALU.add, [lbl.k, cF.k], [cF.k])
        self.tt(cF[:, :], cF[:, :], sF[:, :], ALU.mult, [cF.k, sF.k], [cF.k])
        self.ts(omlF[:, :], cF[:, :], -1.0, 1.0, ALU.mult, ALU.add, [cF.k], [omlF.k])
        self.ts(nomlF[:, :], cF[:, :], -1.0, None, ALU.add, None, [cF.k], [nomlF.k])
        eb_ = self.ar('ebig', [128, 4, 512], F32)
        sB = self.ar('sB', [128, 512], F32)
        P.dma('sp', eb_[:, :, :], self.din['hg_lb_logits'].rearrange("(o l) d c -> o l (d c)", o=1).broadcast_to([128, 4, 512]),
              writes=[eb_.k])
        self.act(eb_[:, :, :], eb_[:, :, :], AF.Exp, [eb_.k], [eb_.k])
        self.tt(sB[:, :], eb_[:, 0, :], eb_[:, 1, :], ALU.add, [eb_.k], [sB.k])
        self.tt(sB[:, :], sB[:, :], eb_[:, 2, :], ALU.add, [eb_.k, sB.k], [sB.k])
        self.tt(sB[:, :], sB[:, :], eb_[:, 3, :], ALU.add, [eb_.k, sB.k], [sB.k])
        self.recip(sB[:, :], sB[:, :], [sB.k], [sB.k])
        self.memset('dve', lbB[:, :], 0.0, [lbB.k])
        for j in range(1, l + 1):
            self.tt(lbB[:, :], lbB[:, :], eb_[:, j, :], ALU.add, [eb_.k, lbB.k], [lbB.k])
        self.tt(lbB[:, :], lbB[:, :], sB[:, :], ALU.mult, [lbB.k, sB.k], [lbB.k])
        self.ts(omlB[:, :], lbB[:, :], -1.0, 1.0, ALU.mult, ALU.add, [lbB.k], [omlB.k])
        self.ar_release(mark)
        for tile in range(NT):
            ak = ('aT', self.blk_of_tile(tile))
            pv, pvk = self.psum('mA', [0, 1])
            self.mmg(pv[:, 0:256], [(aT[:, kc, tile * 128:(tile + 1) * 128], wh[:, kc, 768:1024]) for kc in range(8)],
                     [wh.k, ak], pvk)
            self.cp('act', itm[:, tile, :], pv[:, 0:256], [pvk], [itm.k])
        ftm = self.ar('ftm', [128, 256], F32)
        lftm = self.ar('lftm', [128, 256], F32)
        ktm = self.ar('ktm', [128, 256], F32)
        kbt = self.ar('kbt', [128, 256], BF16)
        kTt = self.ar('kTt', [64, 4, 128], F32)
        ebt = self.ar('ebt', [64, 4, 128], F32)
        enb = self.ar('enb', [64, 4, 128], F32)
        dd = self.ar('dd', [64, 4, 8], F32)
        qb = self.ar('qb', [64, 4, 128], BF16)
        kb = self.ar('kb', [64, 4, 128], BF16)
        vexp = [self.ar('vexp%d' % i, [128, 8, 64], BF16) for i in range(2)]
        u = self.ar('u', [64, 8, 4, 64], F32)
        Spp = [self.ar('S%d' % i, [64, 4, 64], F32) for i in range(2)]
        tmpS = self.ar('tmpS', [64, 4, 64], F32)
        Sbf = self.ar('Sbf', [64, 8, 4, 64], BF16)
        si = 0
        AT = self.ar('AT', [128, 4, 128], BF16)
        vi = 0
        for d in range(2):
            order = list(range(NT)) if d == 0 else [1, 0] + list(range(NT - 1, 1, -1))
            self.memset('dve', Spp[si % 2][:, :, :], 0.0, [Spp[si % 2].k])
            zc = 256 + d * 256
            for tile in order:
                ak = ('aT', self.blk_of_tile(tile))
                tcs = slice(tile * 128, (tile + 1) * 128)
                pz, pzk = self.psum('h0', [0, 1])
                self.mmg(pz[:, 0:256], [(aT[:, kc, tcs], wh[:, kc, zc:zc + 256]) for kc in range(8)], [wh.k, ak], pzk)
                self.act(ftm[:, :], pz[:, 0:256], AF.Sigmoid, [pzk], [ftm.k])
                self.tt(ftm[:, :], ftm[:, :], omlB[:, d * 256:(d + 1) * 256], ALU.mult, [ftm.k, omlB.k], [ftm.k])
                self.tt(ftm[:, :], ftm[:, :], lbB[:, d * 256:(d + 1) * 256], ALU.add, [ftm.k, lbB.k], [ftm.k])
                self.act(lftm[:, :], ftm[:, :], AF.Ln, [ftm.k], [lftm.k])
                self.ts(ktm[:, :], ftm[:, :], -1.0, 1.0, ALU.mult, ALU.add, [ftm.k], [ktm.k])
                pb, pbk = self.psum('h0', [0, 1])
                self.mmg(pb[:, 0:256], [(trie[:, d, 0:128], lftm[:, :])], [trie.k, lftm.k], pbk)
                self.act(ftm[:, :], pb[:, 0:256], AF.Exp, [pbk], [ftm.k], scale=-1.0)
                self.tt(kbt[:, :], ktm[:, :], ftm[:, :], ALU.mult, [ktm.k, ftm.k], [kbt.k])
                pzT, pzTk = self.psum('h0', [0, 1])
                for h in range(4):
                    self.mmg(pzT[0:64, h * 128:(h + 1) * 128],
                             [(wh[:, kc, zc + h * 64:zc + (h + 1) * 64], aT[:, kc, tcs]) for kc in range(8)], [wh.k, ak], pzTk)
                self.act(kTt[:, :, :], pzT[0:64, :].rearrange("k (h t) -> k h t", h=4), AF.Sigmoid, [pzTk], [kTt.k])
                self.tt(kTt[:, :, :], kTt[:, :, :], nomlF[:, d * 4:(d + 1) * 4].unsqueeze(2).broadcast_to([64, 4, 128]),
                        ALU.mult, [kTt.k, nomlF.k], [kTt.k])
                self.tt(kTt[:, :, :], kTt[:, :, :], omlF[:, d * 4:(d + 1) * 4].unsqueeze(2).broadcast_to([64, 4, 128]),
                        ALU.add, [kTt.k, omlF.k], [kTt.k])
                pbT, pbTk = self.psum('h1', [2, 3])
                for h in range(4):
                    self.mmg(pbT[0:64, h * 128:(h + 1) * 128], [(lftm[:, h * 64:(h + 1) * 64], trie[:, d, 0:128])],
                             [lftm.k, trie.k], pbTk)
                ptot, ptotk = self.psum('h1', [2, 3])
                for h in range(4):
                    self.mmg(ptot[0:64, h * 8:(h + 1) * 8], [(lftm[:, h * 64:(h + 1) * 64], trie[:, d, 128:136])],
                             [lftm.k, trie.k], ptotk)
                self.act(ebt[:, :, :], pbT[0:64, :].rearrange("k (h t) -> k h t", h=4), AF.Exp, [pbTk], [ebt.k])
                self.act(enb[:, :, :], pbT[0:64, :].rearrange("k (h t) -> k h t", h=4), AF.Exp, [pbTk], [enb.k], scale=-1.0)
                self.act(dd[:, :, :], ptot[0:64, 0:32].rearrange("k (h j) -> k h j", h=4), AF.Exp, [ptotk], [dd.k])
                pq, pqk = self.psum('h0', [0, 1])
                for h in range(4):
                    self.mmg(pq[0:64, h * 128:(h + 1) * 128],
                             [(wh[:, kc, h * 64:(h + 1) * 64], aT[:, kc, tcs]) for kc in range(8)], [wh.k, ak], pqk)
                self.stt(qb[:, :, :], pq[0:64, :].rearrange("k (h t) -> k h t", h=4), 0.125, ebt[:, :, :], ALU.mult, ALU.mult,
                         [pqk, ebt.k], [qb.k])
                self.tt(kb[:, :, :], kTt[:, :, :], enb[:, :, :], ALU.mult, [kTt.k, enb.k], [kb.k])
                for h in range(4):
                    ve = vexp[vi % 2]
                    vi += 1
                    self.tt(ve[:, :, :], itm[:, tile, h * 64:(h + 1) * 64].unsqueeze(1).broadcast_to([128, 8, 64]),
                            ee[:, d, :].unsqueeze(2).broadcast_to([128, 8, 64]), ALU.mult, [itm.k, ee.k], [ve.k])
                    pu, puk = self.psum('h2', [4, 5])
                    self.mmg(pu[0:64, :], [(kbt[:, h * 64:(h + 1) * 64], ve[:, :, :].rearrange("s j v -> s (j v)"))],
                             [kbt.k, ve.k], puk)
                    self.tt(u[:, :, h, :], pu[0:64, :].rearrange("k (j v) -> k j v", j=8),
                            dd[:, h, :].unsqueeze(2).broadcast_to([64, 8, 64]), ALU.mult, [puk, dd.k], [u.k])
                for j in range(8):
                    S, S2 = Spp[si % 2], Spp[(si + 1) % 2]
                    si += 1
                    self.cp('act', Sbf[:, j, :, :], S[:, :, :], [S.k], [(Sbf.k, j)])
                    self.tt(tmpS[:, :, :], S[:, :, :], dd[:, :, j:j + 1].broadcast_to([64, 4, 64]), ALU.mult,
                            [S.k, dd.k], [tmpS.k])
                    self.tt(S2[:, :, :], tmpS[:, :, :], u[:, j, :, :], ALU.add, [tmpS.k, u.k], [S2.k])
                pA, pAk = self.psum('h3', [6])
                for h in range(4):
                    self.mmg(pA[:, h * 128:(h + 1) * 128], [(kb[:, h, :], qb[:, h, :])], [kb.k, qb.k], pAk)
                self.tt(AT[:, :, :], pA[:, :].rearrange("s (h t) -> s h t", h=4),
                        mt[:, d, :].unsqueeze(1).broadcast_to([128, 4, 128]), ALU.mult, [pAk, mt.k], [AT.k])
                pO, pOk = self.psum('h4', [7])
                rk = [itm.k, AT.k, qb.k] + [(Sbf.k, j) for j in range(8)]
                for h in range(4):
                    p = h // 2
                    P.op('pe', lambda e, h=h, p=p, tile=tile: e.matmul(pO[:, h * 128:(h + 1) * 128], lhsT=itm[:, tile, p * 128:(p + 1) * 128],
                                                           rhs=AT[:, h, :], start=True, stop=False),
                         reads=rk, writes=[pOk], inc=False)
                    for j in range(8):
                        cj = j if d == 0 else 7 - j
                        P.op('pe', lambda e, h=h, p=p, j=j, cj=cj: e.matmul(
                            pO[:, h * 128 + cj * 16:h * 128 + (cj + 1) * 16],
                            lhsT=Sbf[:, j, 2 * p:2 * p + 2, :].rearrange("k h v -> k (h v)"),
                            rhs=qb[:, h, cj * 16:(cj + 1) * 16], start=False, stop=(j == 7)),
                             reads=rk, writes=[pOk], inc=(h == 3 and j == 7))
                pv4 = pO[:, :].rearrange("q (p j t) -> q p j t", p=2, j=2)
                for hh in range(2):
                    if d == 0:
                        self.cp('act' if hh else 'dve', ohg[hh * 64:hh * 64 + 64, :, tcs], pv4[hh * 64:hh * 64 + 64, :, hh, :],
                                [pOk], [(ohg.k, tile, hh)])
                    else:
                        self.tt(ohg[hh * 64:hh * 64 + 64, :, tcs], pv4[hh * 64:hh * 64 + 64, :, hh, :],
                                ohg[hh * 64:hh * 64 + 64, :, tcs], ALU.add, [pOk, (ohg.k, tile, hh)], [(ohg.k, tile, hh)])
        if os.environ.get('HG_DBG'):
            dbg = self.nc.dram_tensor('dbg', [128, 2 * T], F32, kind="ExternalOutput").ap()
            P.dma('pool', dbg.rearrange("q (p t) -> q p t", p=2), ohg[:, :, :],
                  reads=[(ohg.k, t_, hh) for t_ in range(NT) for hh in range(2)], is_output=True)
        self.ar_release(mark)
        wo = self.load_wo(l, 1)
        self.hn_alloc()
        oblk = self.ar('oblk', [128, 2, 512], BF16)
        o32 = self.ar('o32h', [128, 2, 512], F32)
        for b, (t0, n) in enumerate(BLKS):
            okeys = [(ohg.k, t0 // 128 + j, hh) for j in range(n // 128) for hh in range(2)]
            self.cp('dve', o32[:, :, 0:n], ohg[:, :, t0:t0 + n], okeys, [o32.k])
            self.headnorm_gate(l, o32, o32.k, n, t0, b, wh, 1024, False, 3, None, oblk, ('aT', b))
            self.outproj(l, wo, oblk, [(oblk.k, 0), (oblk.k, 1)], b, t0, n)


    def final_out(self, out, gfin):
        P = self.P
        hT, rstd, ident = self.hT, self.rstd, self.ident
        nsq = self.ar('nsq', [128, 8, 512], BF16)
        ntmp = self.ar('ntmp', [128, 8, 512], F32)
        nsq.k, ntmp.k = 'nsq', 'ntmp'
        ost = [self.ar('ost%d' % i, [128, 1024], F32) for i in range(2)]
        oi = 0
        for b, (t0, n) in enumerate(BLKS):
            if b == 0:
                continue
            hk = [('hT', c, b) for c in range(8)]
            if self.final:
                self.act(nsq[:, :, 0:n], hT[:, :, t0:t0 + n], AF.Square, hk, ['nsq'])
                pt, pk = self.psum('nrm', [2, 3])
                self.mmg(pt[:, 0:n], [(self.ones_bf[:, :], nsq[:, c, 0:n]) for c in range(8)], ['nsq', 'ones_bf'], pk)
                self.act(rstd[:, 0:n], pt[:, 0:n], AF.Sqrt, [pk], ['rstd'], bias=self.eps_col[:, 0:1], scale=1.0 / D)
                self.recip(rstd[:, 0:n], rstd[:, 0:n], ['rstd'], ['rstd'])
                self.tt(ntmp[:, :, 0:n], hT[:, :, t0:t0 + n], rstd[:, 0:n].unsqueeze(1).broadcast_to([128, 8, n]),
                        ALU.mult, hk + ['rstd'], ['ntmp'])
                self.tt(ntmp[:, :, 0:n], ntmp[:, :, 0:n], gfin[:, :].unsqueeze(2).broadcast_to([128, 8, n]),
                        ALU.mult, ['ntmp', 'gfin'], ['ntmp'])
            else:
                self.cp('dve', ntmp[:, :, 0:n], hT[:, :, t0:t0 + n], hk, ['ntmp'])
            for tt_ in range(n // 128):
                os_ = ost[oi % 2]
                oi += 1
                for half in range(2):
                    pt, pk = self.psum('tr', [0, 1])
                    for j in range(4):
                        c = half * 4 + j
                        self.tr(pt[:, j * 128:(j + 1) * 128], ntmp[:, c, tt_ * 128:(tt_ + 1) * 128], ident[:, :],
                                ['ntmp', 'ident'], pk)
                    self.cp('act' if half else 'dve', os_[:, half * 512:(half + 1) * 512], pt[:, :], [pk], [(os_.k, half)])
                row0 = t0 - LCTX + tt_ * 128
                P.dma('sp', out[row0:row0 + 128, :], os_[:, :], reads=[(os_.k, 0), (os_.k, 1)], is_output=True)


def _rot_T(half):
    n = 2 * half
    R = np.zeros((n, n), np.float32)
    for m in range(half):
        R[m, m + half] = -1.0
        R[m + half, m] = 1.0
    return R.T.copy()


def _consts():
    sel = np.zeros((NEXP, NEXP * 128), np.float32)
    for e in range(NEXP):
        sel[e, e * 128:(e + 1) * 128] = 1.0
    c = {'ident': np.eye(128, dtype=np.float32), 'sel': sel}
    theta = 10000.0
    tok = np.arange(NLAT)
    row, col = (tok // 64).astype(np.float64), (tok % 64).astype(np.float64)

    def axial(rot_dim):
        nf = rot_dim // 4
        inv = theta ** (-np.arange(nf, dtype=np.float64) / nf)
        inv = inv.astype(np.float32).astype(np.float64)
        ang = np.concatenate([row[:, None] * inv, col[:, None] * inv], axis=1)
        ang = ang.astype(np.float32).astype(np.float64)
        full = np.zeros((T, rot_dim // 2))
        full[LCTX:] = ang
        a2 = np.concatenate([full, full], axis=1)
        return np.cos(a2).T.astype(np.float32).copy(), np.sin(a2).T.astype(np.float32).copy()

    c['cos_mla'], c['sin_mla'] = axial(32)
    c['cos_swa'], c['sin_swa'] = axial(64)
    inv = (theta ** (-np.arange(16, dtype=np.float64) / 16)).astype(np.float32).astype(np.float64)
    ang = (np.arange(T, dtype=np.float64)[:, None] * inv).astype(np.float32).astype(np.float64)
    a2 = np.concatenate([ang, ang], axis=1)
    c['cos_ret'], c['sin_ret'] = np.cos(a2).T.astype(np.float32).copy(), np.sin(a2).T.astype(np.float32).copy()
    rb = np.zeros((96, 96), np.float32)
    rb[64:96, 64:96] = _rot_T(16)
    c['rbig96'] = rb
    r128 = np.zeros((128, 128), np.float32)
    r128[0:64, 0:64] = _rot_T(32)
    r128[64:128, 64:128] = _rot_T(32)
    c['r128'] = r128
    r64b = np.zeros((64, 64), np.float32)
    r64b[0:32, 0:32] = _rot_T(16)
    r64b[32:64, 32:64] = _rot_T(16)
    c['r64b'] = r64b
    m = np.zeros((6, 128, 512), np.float32)
    s_ = np.arange(128)[:, None]
    t_ = np.arange(512)[None, :]
    for r in range(-1, 5):
        m[r + 1] = (np.abs(t_ - s_ - 128 * r) <= 128).astype(np.float32)
    c['swa_mask'] = m
    b64 = np.zeros((128, 128), np.float32)
    b64[0:64, 0:64] = 1.0 / 64
    b64[64:128, 64:128] = 1.0 / 64
    c['blk64'] = b64
    s_ = np.arange(128, dtype=np.float32)[:, None]
    t_ = np.arange(128, dtype=np.float32)[None, :]
    c['ret_rel'] = np.stack([np.maximum(t_ - s_, 0), (t_ >= s_).astype(np.float32),
                             np.maximum(s_ - t_, 0), (s_ >= t_).astype(np.float32)]).astype(np.float32)
    cc = np.zeros((128, 16), np.float32)
    cc[:, 0] = 127.0 - np.arange(128)
    cc[:, 1] = np.arange(128)
    c['ret_colc'] = cc
    rc = np.concatenate([np.arange(128) + 1.0, 128.0 - np.arange(128)])[None, :].repeat(64, axis=0)
    c['ret_rowc'] = rc.astype(np.float32)
    rm = np.zeros((64, 16), np.float32)
    rm[0:32, 0] = 1.0
    rm[32:64, 1] = 1.0
    c['ret_rowm'] = rm
    si = np.arange(128)[:, None]
    ti = np.arange(128)[None, :]
    same = (si // 16) == (ti // 16)
    trif = (same & (si <= ti)).astype(np.float32)
    trib = (same & (si >= ti)).astype(np.float32)
    ef = ((si // 16) == np.arange(8)[None, :]).astype(np.float32)
    eb = ((si // 16) == (7 - np.arange(8))[None, :]).astype(np.float32)
    c['hg_trie'] = np.stack([np.concatenate([trif, ef], axis=1), np.concatenate([trib, eb], axis=1)]).astype(np.float32)
    c['hg_mt'] = np.stack([trif, trib]).astype(np.float32)
    c['hg_e'] = np.stack([ef, eb]).astype(np.float32)
    return c


_CACHE = {}


def _get_prog(key, **kw):
    if key not in _CACHE:
        b = Builder(**kw)
        nc = b.build()
        _CACHE[key] = (nc, b)
    return _CACHE[key]


def run_partial(inputs, layers=(0, 1, 2, 3), mixers=(0, 1, 2, 3), ffn=True, final=True, cores=8):
    nc, b = _get_prog(('p', tuple(layers), tuple(mixers), ffn, final), layers=layers, mixers=mixers, ffn=ffn, final=final)
    consts = _consts()
    f = lambda a: np.ascontiguousarray(np.asarray(a, dtype=np.float32))
    shared = {k: f(inputs[k]) for k in b.din if k in inputs and k not in ('x', 'ctx')}
    for k in consts:
        if k in b.din:
            shared[k] = consts[k]
    in_maps = []
    for i in range(cores):
        m = dict(shared)
        m['x'] = f(inputs['x'][i])
        m['ctx'] = f(inputs['ctx'][i])
        m['c2'] = np.ascontiguousarray(np.stack([np.asarray(inputs['c'][i], np.float32),
                                                 np.asarray(inputs['c_ctx'], np.float32)]))
        in_maps.append(m)
    res = run_bass_kernel_spmd(nc, in_maps, core_ids=list(range(cores)))
    return np.stack([r['out'] for r in res.results], axis=0)


def kernel(**inputs):
    return run_partial(inputs)
```

```python
import os
import numpy as np
import concourse.bass as bass
import concourse.mybir as mybir
from concourse.bass_utils import run_bass_kernel_spmd

AF = mybir.ActivationFunctionType
ALU = mybir.AluOpType
AX = mybir.AxisListType
F32 = mybir.dt.float32
BF16 = mybir.dt.bfloat16

ENGS = ['pe', 'act', 'dve', 'pool', 'sp']
NDMA = 56

D = 1024
T = 2304
NT = 18
LCTX = 256
NLAT = 2048
DEPTH = 4
D_FF = 2816
D_FFE = 3584
NEXP = 8
D_IN = 2912
EPS = 1e-6
BLKS = [(0, 256), (256, 512), (768, 512), (1280, 512), (1792, 512)]
C_MQ, C_MKV, C_MPE = 0, 192, 320
C_HQ, C_HZF, C_HZB, C_HI, C_HG = 352, 608, 864, 1120, 1376
C_SQ, C_SK, C_SV = 1632, 1888, 2016
C_RQ, C_RK, C_RV, C_RG = 2144, 2272, 2400, 2656


class Prog:
    def __init__(self, nc):
        self.nc = nc
        self.q = {e: [] for e in ENGS}
        self.sem = {e: nc.alloc_semaphore('cs_' + e) for e in ENGS}
        self.cnt = {e: 0 for e in ENGS}
        self.waited = {}
        self.lastw = {}
        self.readers = {}
        self.dma_sems = [nc.alloc_semaphore('ds%d' % i) for i in range(NDMA)]
        self.dma_cnt = [0] * NDMA
        self.dma_rr = {'sp': 0, 'pool': 0}
        self.out_tokens = []
        self.ninst = 0

    def _need(self, eng, tok):
        sem, val, prod = tok
        if eng == 'pe' and prod == 'pe':
            return
        k = (eng, sem.num)
        if self.waited.get(k, 0) >= val:
            return
        self.waited[k] = val
        self.q[eng].append(lambda e, sem=sem, val=val: e.wait_ge(sem, val))

    def _deps(self, eng, reads, writes):
        for r in reads:
            t = self.lastw.get(r)
            if t is not None:
                self._need(eng, t)
            if isinstance(r, tuple) and r[0] == 'ps':
                for t in self.readers.get(r, {}).values():
                    if t[2] != eng:
                        self._need(eng, t)
        for w in writes:
            t = self.lastw.get(w)
            if t is not None:
                self._need(eng, t)
            for t in self.readers.get(w, {}).values():
                self._need(eng, t)

    def _record(self, tok, reads, writes):
        for r in reads:
            d = self.readers.setdefault(r, {})
            k = tok[0].num
            if k not in d or d[k][1] < tok[1]:
                d[k] = tok
        for w in writes:
            self.lastw[w] = tok
            self.readers[w] = {}

    def op(self, eng, fn, reads=(), writes=(), inc=True):
        self._deps(eng, reads, writes)
        sem = self.sem[eng]
        self.ninst += 1
        if inc:
            self.cnt[eng] += 1
            tok = (sem, self.cnt[eng], eng)
            self.q[eng].append(lambda e, fn=fn, sem=sem: fn(e).then_inc(sem, 1))
        else:
            tok = (sem, self.cnt[eng] + 1, eng)
            self.q[eng].append(lambda e, fn=fn: fn(e))
        self._record(tok, reads, writes)
        return tok

    def dma(self, eng, out, in_, reads=(), writes=(), is_output=False):
        h = NDMA // 2
        j = self.dma_rr[eng]
        self.dma_rr[eng] = (j + 1) % h
        i = j if eng == 'sp' else h + j
        sem = self.dma_sems[i]
        if self.dma_cnt[i] > 0:
            self._need(eng, (sem, self.dma_cnt[i], 'dma'))
        self._deps(eng, reads, writes)
        self.dma_cnt[i] += 16
        tok = (sem, self.dma_cnt[i], 'dma')
        self.ninst += 1
        self.q[eng].append(
            lambda e, out=out, in_=in_, sem=sem: e.dma_start(out=out, in_=in_).then_inc(sem, 16))
        self._record(tok, reads, writes)
        if is_output:
            self.out_tokens.append(tok)
        return tok

    def barrier(self):
        toks = [(self.sem[e], self.cnt[e], e) for e in ENGS if self.cnt[e] > 0]
        toks += [(self.dma_sems[i], self.dma_cnt[i], 'dma') for i in range(NDMA) if self.dma_cnt[i] > 0]
        for f in ENGS:
            for t in toks:
                self._need(f, t)

    def emit(self):
        nc = self.nc
        for t in self.out_tokens:
            self._need('sp', t)
        for e in ['pe', 'act', 'dve', 'pool']:
            if self.cnt[e] > 0:
                self._need('sp', (self.sem[e], self.cnt[e], e))
        q = self.q
        with nc.Block() as block:
            @block.tensor
            def _(e):
                for f in q['pe']:
                    f(e)

            @block.scalar
            def _(e):
                for f in q['act']:
                    f(e)

            @block.vector
            def _(e):
                for f in q['dve']:
                    f(e)

            @block.gpsimd
            def _(e):
                for f in q['pool']:
                    f(e)

            @block.sync
            def _(e):
                for f in q['sp']:
                    f(e)


class SB:
    def __init__(self, nc, name, shape, dtype):
        self.t = nc.alloc_sbuf_tensor('sb_' + name, list(shape), dtype)
        self.k = name

    def __getitem__(self, idx):
        return self.t[idx]


class View:
    def __init__(self, ap, key):
        self.t = ap
        self.k = key

    def __getitem__(self, idx):
        return self.t[idx]


AR_WORDS = 19456


class Builder:
    def __init__(self, layers, mixers=(0, 1, 2, 3), ffn=True, final=True, h_in=False, h_out=False):
        self.layers = list(layers)
        self.mixers = tuple(mixers)
        self.ffn = ffn
        self.final = final
        self.h_in = h_in
        self.h_out = h_out
        self.nc = nc = bass.Bass("TRN2", target_bir_lowering=False)
        self.P = Prog(nc)
        self.din = {}
        self.ps = [nc.alloc_psum_tensor("psb%d" % i, [128, 512], F32) for i in range(8)]
        self.ps_rr = {}
        self.uid = 0
        self.arena = nc.alloc_sbuf_tensor('arena', [128, AR_WORDS], F32)
        self.ar_off = 0
        self.ar_phase = 0

    def inp(self, name, shape, dtype=F32):
        ap = self.nc.dram_tensor(name, list(shape), dtype, kind="ExternalInput").ap()
        self.din[name] = ap
        return ap

    def sb(self, name, shape, dtype=F32):
        return SB(self.nc, name, shape, dtype)

    def ar(self, name, shape, dtype=F32):
        nel = int(np.prod(shape[1:]))
        nb = nel * (2 if dtype == BF16 else 4)
        nw = ((nb + 31) // 32) * 8
        assert self.ar_off + nw <= AR_WORDS, (name, self.ar_off, nw)
        ap = self.arena[0:shape[0], self.ar_off:self.ar_off + nw]
        self.ar_off += nw
        if dtype == BF16:
            ap = ap.bitcast(BF16)
        ap = ap[:, 0:nel]
        if len(shape) == 3:
            ap = ap.rearrange("p (a b) -> p a b", a=shape[1])
        elif len(shape) == 4:
            ap = ap.rearrange("p (a b c) -> p a b c", a=shape[1], b=shape[2])
        elif len(shape) == 5:
            ap = ap.rearrange("p (a b c d) -> p a b c d", a=shape[1], b=shape[2], c=shape[3])
        self.ar_gen = getattr(self, 'ar_gen', 0)
        return View(ap, ('ar', self.ar_phase, name))

    def ar_release(self, mark):
        self.P.barrier()
        self.ar_off = mark
        self.ar_phase += 1

    def phase(self):
        self.P.barrier()
        self.ar_off = 0
        self.ar_phase += 1

    def psum(self, pool, banks):
        i = self.ps_rr.get(pool, 0)
        self.ps_rr[pool] = i + 1
        b = banks[i % len(banks)]
        return self.ps[b], ('ps', b)

    def mmg(self, out, pairs, reads, wkey):
        n = len(pairs)
        for i, (l, r) in enumerate(pairs):
            self.P.op('pe', lambda e, l=l, r=r, s=(i == 0), t=(i == n - 1): e.matmul(out, lhsT=l, rhs=r, start=s, stop=t),
                      reads=reads, writes=[wkey], inc=(i == n - 1))

    def tr(self, out, in_, ident, reads, wkey):
        self.P.op('pe', lambda e: e.transpose(out, in_, ident), reads=reads, writes=[wkey])

    def act(self, out, in_, func, reads, writes, bias=None, scale=None):
        kw = {}
        if bias is not None:
            kw['bias'] = bias
        if scale is not None:
            kw['scale'] = scale
        self.P.op('act', lambda e: e.activation(out=out, in_=in_, func=func, **kw), reads=reads, writes=writes)

    def tt(self, out, in0, in1, op, reads, writes, eng='dve'):
        self.P.op(eng, lambda e: e.tensor_tensor(out=out, in0=in0, in1=in1, op=op), reads=reads, writes=writes)

    def ts(self, out, in0, s1, s2, op0, op1, reads, writes, eng='dve'):
        if s2 is None:
            self.P.op(eng, lambda e: e.tensor_scalar(out=out, in0=in0, scalar1=s1, scalar2=None, op0=op0),
                      reads=reads, writes=writes)
        else:
            self.P.op(eng, lambda e: e.tensor_scalar(out=out, in0=in0, scalar1=s1, scalar2=s2, op0=op0, op1=op1),
                      reads=reads, writes=writes)

    def stt(self, out, in0, scalar, in1, op0, op1, reads, writes):
        self.P.op('dve', lambda e: e.scalar_tensor_tensor(out=out, in0=in0, scalar=scalar, in1=in1, op0=op0, op1=op1),
                  reads=reads, writes=writes)

    def cp(self, eng, out, in_, reads, writes):
        if eng == 'act':
            self.P.op('act', lambda e: e.copy(out=out, in_=in_), reads=reads, writes=writes)
        else:
            self.P.op(eng, lambda e: e.tensor_copy(out=out, in_=in_), reads=reads, writes=writes)

    def recip(self, out, in_, reads, writes):
        self.P.op('dve', lambda e: e.reciprocal(out=out, in_=in_), reads=reads, writes=writes)

    def memset(self, eng, ap, val, writes):
        self.P.op(eng, lambda e: e.memset(ap, val), writes=writes)

    def rows_to_cols(self, dram_rows, nrows, ncols, dst_ap, dst_key):
        self.uid += 1
        st = self.stage
        k = ('stage', self.uid % 2)
        sl = st[self.uid % 2]
        self.P.dma('sp', sl[0:nrows, 0:ncols], dram_rows, writes=[k])
        pt, pk = self.psum('tr', [0, 1])
        self.tr(pt[0:ncols, 0:nrows], sl[0:nrows, 0:ncols], self.ident[0:nrows, 0:nrows], [k, 'ident'], pk)
        self.cp('dve', dst_ap, pt[0:ncols, 0:nrows], [pk], [dst_key])

    def build(self):
        nc, P = self.nc, self.P
        L = DEPTH
        x = self.inp('x', [NLAT, D])
        ctx = self.inp('ctx', [LCTX, D])
        c2 = self.inp('c2', [2, D])
        identd = self.inp('ident', [128, 128])
        w_ada = self.inp('w_ada', [L, D, 6 * D])
        b_ada = self.inp('b_ada', [L, 6 * D])
        nmg = self.inp('norm_mix_g', [L, D])
        nfg = self.inp('norm_ffn_g', [L, D])
        fng = self.inp('final_norm_g', [D])
        self.w_in = self.inp('w_in', [L, D, D_IN])
        self.w_out = self.inp('w_out', [L, D, D])
        self.fwg = self.inp('ffn_w_gate', [2, D, D_FF])
        self.fwu = self.inp('ffn_w_up', [2, D, D_FF])
        self.fwd = self.inp('ffn_w_down', [2, D_FF, D])
        self.mwr = self.inp('moe_w_router', [2, D, NEXP])
        self.mwg = self.inp('moe_w_gate', [2, NEXP, D, D_FFE])
        self.mwu = self.inp('moe_w_up', [2, NEXP, D, D_FFE])
        self.mwd = self.inp('moe_w_down', [2, NEXP, D_FFE, D])
        seld = self.inp('sel', [NEXP, NEXP * 128])
        for nm, shp in [('mla_q_norm_g', [L, 192]), ('mla_w_uq', [L, 192, 384]), ('mla_kv_norm_g', [L, 128]),
                        ('mla_w_ukv', [L, 128, 512]), ('hg_lb_logits', [L, 2, 256]), ('hg_norm_g', [L, 256]),
                        ('swa_sink', [L, 4]), ('ret_decay_logit', [L, 2, 4]), ('ret_gn_g', [L, 256]),
                        ('ret_gn_b', [L, 256]), ('cos_mla', [32, T]), ('sin_mla', [32, T]), ('cos_swa', [64, T]),
                        ('sin_swa', [64, T]), ('cos_ret', [32, T]), ('sin_ret', [32, T]), ('rbig96', [96, 96]),
                        ('r128', [128, 128]), ('r64b', [64, 64]), ('swa_mask', [6, 128, 512]),
                        ('blk64', [128, 128]), ('ret_rel', [4, 128, 128]), ('ret_colc', [128, 16]),
                        ('ret_rowc', [64, 256]), ('ret_rowm', [64, 16]), ('hg_trie', [2, 128, 136]), ('hg_mt', [2, 128, 128]),
                        ('hg_e', [2, 128, 8])]:
            self.inp(nm, shp)
        if self.h_in:
            hin = self.inp('h_in', [128, 8 * T])
        out = nc.dram_tensor('out', [NLAT, D], F32, kind="ExternalOutput").ap()
        if self.h_out:
            hout = nc.dram_tensor('h_out', [128, 8 * T], F32, kind="ExternalOutput").ap()

        self.hT = hT = self.sb('hT', [128, 8, T], F32)
        self.aT = aT = self.sb('aT', [128, 8, T], BF16)
        self.ident = ident = self.sb('ident', [128, 128], F32)
        self.identb = identb = self.sb('identb', [128, 128], BF16)
        self.ones_bf = ones_bf = self.sb('ones_bf', [128, 128], BF16)
        self.stage = [self.ar('stage%d' % i, [128, 1024], F32).t for i in range(2)]
        self.modT = modT = self.sb('modT', [128, L, 48, 2], F32)
        self.gmix = gmix = self.sb('gmix', [128, L, 8], F32)
        self.gffn = gffn = self.sb('gffn', [128, L, 8], F32)
        self.gfin = gfin = self.sb('gfin', [128, 8], F32)
        self.A1 = A1 = self.sb('A1', [128, 8, 2], F32)
        self.rstd = rstd = self.sb('rstd', [128, 512], F32)
        self.sel = sel = self.sb('sel', [NEXP, NEXP * 128], F32)
        self.eps_col = self.sb('eps_col', [128, 1], F32)
        self.memset('dve', self.eps_col[:, :], EPS, ['eps_col'])
        self.gT = self.sb('gT', [NEXP, T], F32)

        P.dma('sp', ident[:, :], identd, writes=['ident'])
        P.dma('sp', sel[:, :], seld, writes=['sel'])
        self.cp('dve', identb[:, :], ident[:, :], ['ident'], ['identb'])
        self.memset('dve', ones_bf[:, :], 1.0, ['ones_bf'])

        if self.h_in:
            for c in range(8):
                P.dma('sp', hT[:, c, :], hin[:, c * T:(c + 1) * T], writes=[('hT', c, b) for b in range(5)])
        else:
            for t in range(NT):
                src = ctx[t * 128:(t + 1) * 128, :] if t < 2 else x[(t - 2) * 128:(t - 1) * 128, :]
                st = self.stage[t % 2]
                k = ('stage', t % 2)
                P.dma('sp', st[:, :], src, writes=[k])
                b = self.blk_of_tile(t)
                for half in range(2):
                    pt, pk = self.psum('tr', [0, 1])
                    for j in range(4):
                        c = half * 4 + j
                        self.tr(pt[:, j * 128:(j + 1) * 128], st[:, c * 128:(c + 1) * 128], ident[:, :], [k, 'ident'], pk)
                    self.cp('act' if half else 'dve', hT[:, half * 4:half * 4 + 4, t * 128:(t + 1) * 128],
                            pt[:, :].rearrange("p (j n) -> p j n", j=4), [pk],
                            [('hT', half * 4 + j, b) for j in range(4)])

        for l in range(L):
            self.rows_to_cols(nmg[l].rearrange("(c p) -> c p", p=128), 8, 128, gmix[:, l, :], 'gmix')
            self.rows_to_cols(nfg[l].rearrange("(c p) -> c p", p=128), 8, 128, gffn[:, l, :], 'gffn')
        self.rows_to_cols(fng.rearrange("(c p) -> c p", p=128), 8, 128, gfin[:, :], 'gfin')
        bada = self.sb('bada', [128, L, 48], F32)
        for l in range(L):
            self.rows_to_cols(b_ada[l].rearrange("(m p) -> m p", p=128), 48, 128, bada[:, l, :], 'bada')

        self.colv = colv = self.sb('colv', [128, L, 12], F32)
        for l in range(L):
            qg = self.din['mla_q_norm_g'][l]
            self.rows_to_cols(qg[0:128].rearrange("(o p) -> o p", o=1), 1, 128, colv[:, l, 0:1], 'colv')
            self.rows_to_cols(qg[128:192].rearrange("(o p) -> o p", o=1), 1, 64, colv[0:64, l, 1:2], 'colv')
            self.rows_to_cols(self.din['mla_kv_norm_g'][l].rearrange("(o p) -> o p", o=1), 1, 128, colv[:, l, 2:3], 'colv')
            self.rows_to_cols(self.din['hg_norm_g'][l].rearrange("(c p) -> c p", p=128), 2, 128, colv[:, l, 3:5], 'colv')
            self.rows_to_cols(self.din['ret_gn_g'][l].rearrange("(c p) -> c p", p=128), 2, 128, colv[:, l, 5:7], 'colv')
            self.rows_to_cols(self.din['ret_gn_b'][l].rearrange("(c p) -> c p", p=128), 2, 128, colv[:, l, 7:9], 'colv')

        c2s = self.ar('c2s', [2, D], F32)
        scT = self.sb('scT', [128, 8, 2], F32)
        P.dma('sp', c2s[:, :], c2, writes=['c2s'])
        self.act(c2s[:, :], c2s[:, :], AF.Silu, ['c2s'], ['c2s'])
        for c in range(8):
            pt, pk = self.psum('tr', [0, 1])
            self.tr(pt[:, 0:2], c2s[0:2, c * 128:(c + 1) * 128], ident[0:2, 0:2], ['c2s', 'ident'], pk)
            self.cp('dve', scT[:, c, :], pt[:, 0:2], [pk], ['scT'])
        NCB = 768
        wab = [self.ar('wab%d' % i, [128, 8, NCB], F32) for i in range(2)]
        it = 0
        for l in self.layers:
            for cb in range(6 * D // NCB):
                wt = wab[it % 2]
                it += 1
                P.dma('sp', wt[:, :, :], w_ada[l][:, cb * NCB:(cb + 1) * NCB].rearrange("(c p) n -> p c n", p=128),
                      writes=[wt.k])
                pt, pk = self.psum('tr', [0, 1])
                for m in range(NCB // 128):
                    self.mmg(pt[:, m * 2:(m + 1) * 2],
                             [(wt[:, kc, m * 128:(m + 1) * 128], scT[:, kc, :]) for kc in range(8)],
                             [wt.k, 'scT'], pk)
                nm = NCB // 128
                self.tt(modT[:, l, cb * nm:(cb + 1) * nm, :], pt[:, 0:2 * nm].rearrange("p (m r) -> p m r", r=2),
                        bada[:, l, cb * nm:(cb + 1) * nm].unsqueeze(2).broadcast_to([128, nm, 2]), ALU.add,
                        [pk, 'bada'], [('modT', l)])

        for l in self.layers:
            if len(self.mixers) > 0:
                self.phase()
                self.norm_mod(l, gmix, 0, 1, False)
                self.mixer_phase(l)
            if self.ffn:
                moe = (l % 2 == 1)
                self.phase()
                self.norm_mod(l, gffn, 3, 4, moe)
                if moe:
                    self.moe_route(l)
                self.phase()
                if moe:
                    self.moe_ffn(l)
                else:
                    self.dense_ffn(l)
        self.phase()

        if self.h_out:
            for c in range(8):
                P.dma('sp', hout[:, c * T:(c + 1) * T], hT[:, c, :], reads=[('hT', c, b) for b in range(5)],
                      is_output=True)
        self.final_out(out, gfin)
        P.emit()
        return nc

    def blk_of_tile(self, t):
        return 0 if t < 2 else 1 + (t - 2) // 4

    def norm_mod(self, l, gvec, i_shift, i_scale, want_f32):
        P = self.P
        hT, aT, modT, A1, rstd = self.hT, self.aT, self.modT, self.A1, self.rstd
        self.stt(A1[:, :, :], modT[:, l, i_scale * 8:(i_scale + 1) * 8, :], 1.0,
                 gvec[:, l, :].unsqueeze(2).broadcast_to([128, 8, 2]), ALU.add, ALU.mult,
                 [('modT', l), gvec.k], ['A1'])
        nsq = self.ar('nsq', [128, 8, 512], BF16)
        ntmp = self.ar('ntmp', [128, 8, 512], F32)
        if want_f32:
            self.a32 = self.ar('a32', [128, 8, 512], F32)
            self.wr = self.ar('wr', [128, 8, NEXP], F32)
            self.lgT = self.ar('lgT', [NEXP, T], F32)
            self.P.dma('sp', self.wr[:, :, :], self.mwr[l // 2].rearrange("(c p) e -> p c e", p=128), writes=[self.wr.k])
        for b, (t0, n) in enumerate(BLKS):
            r = 1 if b == 0 else 0
            hk = [('hT', c, b) for c in range(8)]
            self.act(nsq[:, :, 0:n], hT[:, :, t0:t0 + n], AF.Square, hk, [nsq.k])
            pt, pk = self.psum('nrm', [2, 3])
            self.mmg(pt[:, 0:n], [(self.ones_bf[:, :], nsq[:, c, 0:n]) for c in range(8)], [nsq.k, 'ones_bf'], pk)
            self.act(rstd[:, 0:n], pt[:, 0:n], AF.Sqrt, [pk], ['rstd'], bias=self.eps_col[:, 0:1], scale=1.0 / D)
            self.recip(rstd[:, 0:n], rstd[:, 0:n], ['rstd'], ['rstd'])
            self.tt(ntmp[:, :, 0:n], hT[:, :, t0:t0 + n], rstd[:, 0:n].unsqueeze(1).broadcast_to([128, 8, n]),
                    ALU.mult, hk + ['rstd'], [ntmp.k])
            for c in range(8):
                if want_f32:
                    self.ts(self.a32[:, c, 0:n], ntmp[:, c, 0:n], A1[:, c, r:r + 1],
                            modT[:, l, i_shift * 8 + c, r:r + 1], ALU.mult, ALU.add,
                            [ntmp.k, 'A1', ('modT', l)], [('a32', c)])
                    self.cp('act', aT[:, c, t0:t0 + n], self.a32[:, c, 0:n], [('a32', c)], [('aT', b)])
                else:
                    self.ts(aT[:, c, t0:t0 + n], ntmp[:, c, 0:n], A1[:, c, r:r + 1],
                            modT[:, l, i_shift * 8 + c, r:r + 1], ALU.mult, ALU.add,
                            [ntmp.k, 'A1', ('modT', l)], [('aT', b)])
            if want_f32:
                self.route_block(l, b, t0, n)

    def load_w(self, dst, dram, kchunks, ncols, key):
        step = max(1, 4096 // ncols)
        for k0 in range(0, kchunks, step):
            k1 = min(kchunks, k0 + step)
            self.P.dma('pool', dst[:, k0:k1, :], dram[k0 * 128:k1 * 128, :].rearrange("(c p) n -> p c n", p=128),
                       writes=[key])

    def ffn_slices(self, dff):
        s = []
        f = 0
        while f < dff:
            n = min(512, dff - f)
            s.append((f, n))
            f += n
        return s

    def ffn_core(self, l, wg_d, wu_d, wd_d, dff, gate_tile=None, tagbase=''):
        P = self.P
        hT, aT, modT = self.hT, self.aT, self.modT
        for (f0, fn) in self.ffn_slices(dff):
            wg, wu, wd = self.fw[self.fit % 2]
            self.fit += 1
            nj = fn // 128
            self.load_w(wg[:, :, 0:fn], wg_d[:, f0:f0 + fn], 8, fn, wg.k)
            self.load_w(wu[:, :, 0:fn], wu_d[:, f0:f0 + fn], 8, fn, wu.k)
            self.load_w(wd[:, 0:nj, :], wd_d[f0:f0 + fn, :], nj, 1024, wd.k)
            def gu(b, t0, n):
                hid = self.fhid[self.fh % 2]
                self.fh += 1
                for j in range(nj):
                    pg, pgk = self.psum('ffg', [0, 1])
                    pu, puk = self.psum('ffu', [2, 3])
                    self.mmg(pg[:, 0:n], [(wg[:, kc, j * 128:(j + 1) * 128], aT[:, kc, t0:t0 + n]) for kc in range(8)],
                             [wg.k, ('aT', b)], pgk)
                    self.mmg(pu[:, 0:n], [(wu[:, kc, j * 128:(j + 1) * 128], aT[:, kc, t0:t0 + n]) for kc in range(8)],
                             [wu.k, ('aT', b)], puk)
                    sg = self.fsg[self.fj % 2]
                    self.fj += 1
                    self.act(sg[:, 0:n], pg[:, 0:n], AF.Silu, [pgk], [sg.k])
                    if gate_tile is not None:
                        self.tt(sg[:, 0:n], sg[:, 0:n], gate_tile[:, t0:t0 + n], ALU.mult, [sg.k, gate_tile.k], [sg.k])
                    self.tt(hid[:, j, 0:n], pu[:, 0:n], sg[:, 0:n], ALU.mult, [puk, sg.k], [(hid.k, j)])
                return hid

            def down(b, t0, n, hid):
                r = 1 if b == 0 else 0
                for oc in range(8):
                    py, pyk = self.psum('ffy', [4, 5, 6, 7])
                    self.mmg(py[:, 0:n], [(wd[:, j, oc * 128:(oc + 1) * 128], hid[:, j, 0:n]) for j in range(nj)],
                             [wd.k] + [(hid.k, j) for j in range(nj)], pyk)
                    self.stt(hT[:, oc, t0:t0 + n], py[:, 0:n], modT[:, l, 5 * 8 + oc, r:r + 1], hT[:, oc, t0:t0 + n],
                             ALU.mult, ALU.add, [pyk, ('modT', l), ('hT', oc, b)], [('hT', oc, b)])

            prev = None
            for b, (t0, n) in enumerate(BLKS):
                hid = gu(b, t0, n)
                if prev is not None:
                    down(*prev)
                prev = (b, t0, n, hid)
            down(*prev)

    def ffn_alloc(self):
        self.fw = [(self.ar('fwg%d' % i, [128, 8, 512], BF16), self.ar('fwu%d' % i, [128, 8, 512], BF16),
                    self.ar('fwd%d' % i, [128, 4, 1024], BF16)) for i in range(2)]
        self.fsg = [self.ar('fsg%d' % i, [128, 512], BF16) for i in range(2)]
        self.fhid = [self.ar('fhid%d' % i, [128, 4, 512], BF16) for i in range(2)]
        self.fit = 0
        self.fj = 0
        self.fh = 0

    def dense_ffn(self, l):
        i = l // 2
        self.ffn_alloc()
        self.ffn_core(l, self.fwg[i], self.fwu[i], self.fwd[i], D_FF)

    def route_block(self, l, b, t0, n):
        pt, pk = self.psum('nrm', [2, 3])
        self.mmg(pt[0:NEXP, 0:n], [(self.wr[:, c, :], self.a32[:, c, 0:n]) for c in range(8)],
                 [self.wr.k] + [('a32', c) for c in range(8)], pk)
        self.cp('dve', self.lgT[:, t0:t0 + n], pt[0:NEXP, 0:n], [pk], ['lgT'])

    def moe_route(self, l):
        ident = self.ident
        lg = self.ar('lg', [128, NT, NEXP], F32)
        mk1 = self.ar('mk1', [128, NT, NEXP], F32)
        mk2 = self.ar('mk2', [128, NT, NEXP], F32)
        m1 = self.ar('m1', [128, NT], F32)
        m2 = self.ar('m2', [128, NT], F32)
        w1 = self.ar('w1', [128, NT], F32)
        gT = self.gT
        lg.k, mk1.k, mk2.k, m1.k, m2.k, w1.k = 'lg', 'mk1', 'mk2', 'm1', 'm2', 'w1'
        for t in range(NT):
            pt, pk = self.psum('tr', [0, 1])
            self.tr(pt[:, 0:NEXP], self.lgT[0:NEXP, t * 128:(t + 1) * 128], ident[0:NEXP, 0:NEXP], ['lgT', 'ident'], pk)
            self.cp('dve', lg[:, t, :], pt[:, 0:NEXP], [pk], ['lg'])
        P = self.P
        P.op('dve', lambda e: e.tensor_reduce(out=m1[:, :], in_=lg[:, :, :], axis=AX.X, op=ALU.max), reads=['lg'], writes=['m1'])
        self.tt(mk1[:, :, :], lg[:, :, :], m1[:, :].unsqueeze(2).broadcast_to([128, NT, NEXP]), ALU.is_equal,
                ['lg', 'm1'], ['mk1'])
        self.stt(mk2[:, :, :], mk1[:, :, :], -1e30, lg[:, :, :], ALU.mult, ALU.add, ['mk1', 'lg'], ['mk2'])
        P.op('dve', lambda e: e.tensor_reduce(out=m2[:, :], in_=mk2[:, :, :], axis=AX.X, op=ALU.max), reads=['mk2'], writes=['m2'])
        self.tt(mk2[:, :, :], mk2[:, :, :], m2[:, :].unsqueeze(2).broadcast_to([128, NT, NEXP]), ALU.is_equal,
                ['mk2', 'm2'], ['mk2'])
        self.tt(w1[:, :], m1[:, :], m2[:, :], ALU.subtract, ['m1', 'm2'], ['w1'])
        self.act(w1[:, :], w1[:, :], AF.Sigmoid, ['w1'], ['w1'])
        self.tt(mk1[:, :, :], mk1[:, :, :], w1[:, :].unsqueeze(2).broadcast_to([128, NT, NEXP]), ALU.mult,
                ['mk1', 'w1'], ['mk1'])
        self.ts(w1[:, :], w1[:, :], -1.0, 1.0, ALU.mult, ALU.add, ['w1'], ['w1'])
        self.tt(mk2[:, :, :], mk2[:, :, :], w1[:, :].unsqueeze(2).broadcast_to([128, NT, NEXP]), ALU.mult,
                ['mk2', 'w1'], ['mk2'])
        self.tt(mk1[:, :, :], mk1[:, :, :], mk2[:, :, :], ALU.add, ['mk1', 'mk2'], ['mk1'])
        for t in range(NT):
            pt, pk = self.psum('tr', [0, 1])
            self.tr(pt[0:NEXP, 0:128], mk1[:, t, :], ident[:, :], ['mk1', 'ident'], pk)
            self.cp('dve', gT[:, t * 128:(t + 1) * 128], pt[0:NEXP, 0:128], [pk], ['gT'])

    def moe_ffn(self, l):
        i = l // 2
        gT = self.gT
        gbc = [self.ar('gbc%d' % j, [128, T], BF16) for j in range(2)]
        self.ffn_alloc()
        for e_ in range(NEXP):
            gb = gbc[e_ % 2]
            for b, (t0, n) in enumerate(BLKS):
                pt, pk = self.psum('nrm', [2, 3])
                self.mmg(pt[:, 0:n], [(self.sel[:, e_ * 128:(e_ + 1) * 128], gT[:, t0:t0 + n])], ['sel', 'gT'], pk)
                self.cp('act', gb[:, t0:t0 + n], pt[:, 0:n], [pk], [gb.k])
            self.ffn_core(l, self.mwg[i][e_], self.mwu[i][e_], self.mwd[i][e_], D_FFE, gate_tile=gb)


    def mixer_phase(self, l):
        if 0 in self.mixers:
            self.phase()
            self.mla(l)
        if 2 in self.mixers:
            self.phase()
            self.swa(l)
        if 3 in self.mixers:
            self.phase()
            self.ret(l)
        if 1 in self.mixers:
            self.phase()
            self.hgrn(l)

    def colvec(self, dst_ap, dram_vec, n, key):
        self.P.dma('sp', dst_ap, dram_vec.rearrange("(p o) -> p o", o=1), writes=[key])

    def proj(self, out, w, wcols, rhs_fn, reads, pk, kchunks=8):
        c0, c1 = wcols
        self.mmg(out, [(w[:, kc, c0:c1], rhs_fn(kc)) for kc in range(kchunks)], reads, pk)

    def rstd_from(self, dst, src_ps, n_feat, reads, writes):
        p = dst.shape[0] if hasattr(dst, 'shape') else 128
        self.act(dst, src_ps, AF.Sqrt, reads, writes, bias=self.eps_col[0:p, 0:1], scale=1.0 / n_feat)
        self.recip(dst, dst, writes, writes)

    def outproj(self, l, wo, o_blk, okeys, b, t0, n):
        r = 1 if b == 0 else 0
        hT, modT = self.hT, self.modT
        for oc in range(8):
            py, pyk = self.psum('ffy', [4, 5, 6, 7])
            self.mmg(py[:, 0:n], [(wo[:, p, oc * 128:(oc + 1) * 128], o_blk[:, p, 0:n]) for p in range(2)],
                     [wo.k] + okeys, pyk)
            self.stt(hT[:, oc, t0:t0 + n], py[:, 0:n], modT[:, l, 2 * 8 + oc, r:r + 1], hT[:, oc, t0:t0 + n],
                     ALU.mult, ALU.add, [pyk, ('modT', l), ('hT', oc, b)], [('hT', oc, b)])

    def load_wo(self, l, grp):
        wo = self.ar('wo', [128, 2, 1024], BF16)
        self.load_w(wo[:, :, :], self.w_out[l][grp * 256:(grp + 1) * 256, :], 2, 1024, wo.k)
        return wo

    def attn(self, spairs, vfn, tiles, scale, n, half, out_ap, out_key, reads, sink_col=None):
        P = self.P
        pO, pOk = self.psum('aO', [4, 6])
        pD, pDk = self.psum('aD', [5, 7])
        nt = len(tiles)

        def smm(i):
            pS, pSk = self.psum('aS', [0, 1])
            self.mmg(pS[:, 0:n], spairs(tiles[i][0]), reads, pSk)
            return pS, pSk

        nxt = smm(0)
        for i, (tile, mask) in enumerate(tiles):
            pS, pSk = nxt
            if i + 1 < nt:
                nxt = smm(i + 1)
            pt = self.PT[self.pti % 3]
            self.pti += 1
            self.act(pt[:, 0:n], pS[:, 0:n], AF.Exp, [pSk], [pt.k], scale=scale)
            if mask is not None:
                self.tt(pt[:, 0:n], pt[:, 0:n], mask[:, 0:n], ALU.mult, [pt.k, 'swamask'], [pt.k])
            v = vfn(tile)
            P.op('pe', lambda e, v=v, pt=pt, s=(i == 0), t=(i == nt - 1): e.matmul(pO[:, 0:n], lhsT=v, rhs=pt[:, 0:n], start=s, stop=t),
                 reads=reads + [pt.k], writes=[pOk], inc=False)
            P.op('pe', lambda e, pt=pt, s=(i == 0), t=(i == nt - 1): e.matmul(pD[:, 0:n], lhsT=self.ones_bf[:, :], rhs=pt[:, 0:n], start=s, stop=t),
                 reads=[pt.k, 'ones_bf'], writes=[pOk, pDk], inc=True)
        r0 = half * 64
        rd = self.rd
        if sink_col is not None:
            self.ts(rd[r0:r0 + 64, 0:n], pD[r0:r0 + 64, 0:n], sink_col[r0:r0 + 64, :], None, ALU.add, None, [pDk, 'esink'], ['rd'])
            self.recip(rd[r0:r0 + 64, 0:n], rd[r0:r0 + 64, 0:n], ['rd'], ['rd'])
        else:
            self.recip(rd[r0:r0 + 64, 0:n], pD[r0:r0 + 64, 0:n], [pDk], ['rd'])
        self.tt(out_ap, pO[r0:r0 + 64, 0:n], rd[r0:r0 + 64, 0:n], ALU.mult, [pOk, 'rd'], [out_key])

    def mla(self, l):
        P = self.P
        aT = self.aT
        wm = self.ar('wm', [128, 8, 352], BF16)
        self.load_w(wm[:, :, :], self.w_in[l][:, 0:352], 8, 352, wm.k)
        wuq = self.ar('wuq', [128, 2, 384], BF16)
        P.dma('pool', wuq[:, 0, :], self.din['mla_w_uq'][l][0:128, :], writes=[wuq.k])
        P.dma('pool', wuq[0:64, 1, :], self.din['mla_w_uq'][l][128:192, :], writes=[wuq.k])
        wukv = self.ar('wukv', [128, 512], BF16)
        P.dma('pool', wukv[:, :], self.din['mla_w_ukv'][l], writes=[wukv.k])
        cv = self.colv
        self.ts(wuq[:, 0, :], wuq[:, 0, :], cv[:, l, 0:1], None, ALU.mult, None, [wuq.k, 'colv'], [wuq.k])
        self.ts(wuq[0:64, 1, :], wuq[0:64, 1, :], cv[0:64, l, 1:2], None, ALU.mult, None, [wuq.k, 'colv'], [wuq.k])
        self.ts(wukv[:, :], wukv[:, :], cv[:, l, 2:3], None, ALU.mult, None, [wukv.k, 'colv'], [wukv.k])
        r32 = self.ar('r32', [32, 32], BF16)
        P.dma('pool', r32[:, :], self.din['r64b'][0:32, 0:32], writes=[r32.k])
        wo = self.load_wo(l, 0)
        kN = self.ar('kN', [64, 4, T], BF16)
        kP = self.ar('kP', [32, T], BF16)
        vt = self.ar('vt', [128, NT, 256], BF16)
        cs = self.ar('cs', [32, 2, 512], F32)
        kvl = self.ar('kvl', [128, 512], BF16)
        sq = self.ar('sq', [128, 2, 512], BF16)
        rs = self.ar('rs', [128, 512], F32)
        rsc = self.ar('rsc', [128, NT], F32)
        xb = self.ar('xb', [32, 512], BF16)
        t1 = self.ar('t1', [32, 512], F32)
        t2 = self.ar('t2', [32, 512], F32)
        qlb = self.ar('qlb', [128, 2, 512], BF16)
        qp32 = self.ar('qp32', [32, 512], F32)
        qn = [self.ar('qn%d' % i, [64, 512], BF16) for i in range(2)]
        qp = [self.ar('qp%d' % i, [32, 512], BF16) for i in range(2)]
        oblk = [self.ar('oblk%d' % i, [128, 2, 512], BF16) for i in range(2)]
        self.PT = [self.ar('PT%d' % i, [128, 512], BF16) for i in range(3)]
        self.pti = 0
        self.rd = self.ar('rd', [128, 512], F32)
        self.rd.k = 'rd'
        cosd, sind = self.din['cos_mla'], self.din['sin_mla']

        def rope32(dst, src, skey, n, wkey):
            self.cp('act', xb[:, 0:n], src, [skey], [xb.k])
            pr, prk = self.psum('mB', [2, 3])
            self.mmg(pr[0:32, 0:n], [(r32[:, :], xb[:, 0:n])], [r32.k, xb.k], prk)
            RV = int(os.environ.get('ROPEV', '9'))
            if RV == 1:
                self.tt(t1[:, 0:n], src, src, ALU.mult, [skey], [t1.k])
                return
            if RV == 5:
                return
            if RV == 3:
                self.tt(t1[:, 0:n], rs[0:32, 0:n], rs[0:32, 0:n], ALU.mult, [rs.k], [t1.k])
                return
            if RV == 4:
                self.tt(rs[0:32, 0:n], cs[:, 0, 0:n], cs[:, 0, 0:n], ALU.mult, [cs.k, rs.k], [rs.k])
                return
            if RV == 2:
                self.tt(t1[:, 0:n], cs[:, 0, 0:n], cs[:, 0, 0:n], ALU.mult, [cs.k], [t1.k])
                return
            self.tt(t1[:, 0:n], src, cs[:, 0, 0:n], ALU.mult, [skey, cs.k, xb.k], [t1.k])
            self.tt(t2[:, 0:n], pr[0:32, 0:n], cs[:, 1, 0:n], ALU.mult, [prk, cs.k], [t2.k])
            self.tt(dst, t1[:, 0:n], t2[:, 0:n], ALU.add, [t1.k, t2.k], [wkey])

        for b, (t0, n) in enumerate(BLKS):
            ak = ('aT', b)
            P.dma('sp', cs[:, 0, 0:n], cosd[:, t0:t0 + n], writes=[cs.k])
            P.dma('sp', cs[:, 1, 0:n], sind[:, t0:t0 + n], writes=[cs.k])
            pk_, pkk = self.psum('mA', [0, 1])
            self.proj(pk_[:, 0:n], wm, (192, 320), lambda kc: aT[:, kc, t0:t0 + n], [wm.k, ak], pkk)
            self.cp('act', kvl[:, 0:n], pk_[:, 0:n], [pkk], [kvl.k])
            self.act(sq[:, 0, 0:n], pk_[:, 0:n], AF.Square, [pkk], [sq.k])
            ps_, psk = self.psum('mB', [2, 3])
            self.mmg(ps_[:, 0:n], [(self.ones_bf[:, :], sq[:, 0, 0:n])], [sq.k, 'ones_bf'], psk)
            self.rstd_from(rs[:, 0:n], ps_[:, 0:n], 128.0, [psk], [rs.k])
            for tt_ in range(n // 128):
                tile = t0 // 128 + tt_
                pc, pck = self.psum('mB', [2, 3])
                self.mmg(pc[:, 0:1], [(sq[:, 0, tt_ * 128:(tt_ + 1) * 128], self.ones_bf[:, 0:1])], [sq.k, 'ones_bf'], pck)
                self.rstd_from(rsc[:, tile:tile + 1], pc[:, 0:1], 128.0, [pck], [rsc.k])
                pv, pvk = self.psum('mA', [0, 1])
                for h in range(4):
                    self.mmg(pv[:, h * 64:(h + 1) * 64], [(kvl[:, tt_ * 128:(tt_ + 1) * 128], wukv[:, h * 128 + 64:h * 128 + 128])],
                             [kvl.k, wukv.k], pvk)
                self.ts(vt[:, tile, :], pv[:, 0:256], rsc[:, tile:tile + 1], None, ALU.mult, None, [pvk, rsc.k], [vt.k])
            for h in range(4):
                pk2, pk2k = self.psum('mA', [0, 1])
                self.mmg(pk2[0:64, 0:n], [(wukv[:, h * 128:h * 128 + 64], kvl[:, 0:n])], [kvl.k, wukv.k], pk2k)
                self.tt(kN[:, h, t0:t0 + n], pk2[0:64, 0:n], rs[0:64, 0:n], ALU.mult, [pk2k, rs.k], [kN.k])
            pp, ppk = self.psum('mA', [0, 1])
            self.proj(pp[0:32, 0:n], wm, (320, 352), lambda kc: aT[:, kc, t0:t0 + n], [wm.k, ak], ppk)
            rope32(kP[:, t0:t0 + n], pp[0:32, 0:n], ppk, n, kP.k)
        STOP = int(os.environ.get('MLA_STOP', '9'))
        if STOP <= 2:
            return
        scale = 96.0 ** -0.5
        qi = 0
        for b, (t0, n) in enumerate(BLKS):
            ak = ('aT', b)
            P.dma('sp', cs[:, 0, 0:n], cosd[:, t0:t0 + n], writes=[cs.k])
            P.dma('sp', cs[:, 1, 0:n], sind[:, t0:t0 + n], writes=[cs.k])
            p0, p0k = self.psum('mA', [0, 1])
            self.proj(p0[:, 0:n], wm, (0, 128), lambda kc: aT[:, kc, t0:t0 + n], [wm.k, ak], p0k)
            p1, p1k = self.psum('mA', [0, 1])
            self.proj(p1[0:64, 0:n], wm, (128, 192), lambda kc: aT[:, kc, t0:t0 + n], [wm.k, ak], p1k)
            self.cp('act', qlb[:, 0, 0:n], p0[:, 0:n], [p0k], [qlb.k])
            self.cp('act', qlb[0:64, 1, 0:n], p1[0:64, 0:n], [p1k], [qlb.k])
            self.act(sq[:, 0, 0:n], p0[:, 0:n], AF.Square, [p0k], [sq.k])
            self.act(sq[0:64, 1, 0:n], p1[0:64, 0:n], AF.Square, [p1k], [sq.k])
            ps_, psk = self.psum('mB', [2, 3])
            self.mmg(ps_[:, 0:n], [(self.ones_bf[:, :], sq[:, 0, 0:n]), (self.ones_bf[0:64, :], sq[0:64, 1, 0:n])],
                     [sq.k, 'ones_bf'], psk)
            self.rstd_from(rs[:, 0:n], ps_[:, 0:n], 192.0, [psk], [rs.k])
            ob = oblk[b % 2]
            tiles = [(0, None), (1, None)] if b == 0 else [(t, None) for t in range(NT)]
            def qprep(h, n=n):
                nonlocal qi
                qn_, qp_ = qn[qi % 2], qp[qi % 2]
                qi += 1
                pq, pqk = self.psum('mB', [2, 3])
                self.mmg(pq[0:64, 0:n], [(wuq[:, 0, h * 96:h * 96 + 64], qlb[:, 0, 0:n]),
                                         (wuq[0:64, 1, h * 96:h * 96 + 64], qlb[0:64, 1, 0:n])], [wuq.k, qlb.k], pqk)
                self.tt(qn_[:, 0:n], pq[0:64, 0:n], rs[0:64, 0:n], ALU.mult, [pqk, rs.k], [qn_.k])
                pq2, pq2k = self.psum('mB', [2, 3])
                self.mmg(pq2[0:32, 0:n], [(wuq[:, 0, h * 96 + 64:h * 96 + 96], qlb[:, 0, 0:n]),
                                          (wuq[0:64, 1, h * 96 + 64:h * 96 + 96], qlb[0:64, 1, 0:n])], [wuq.k, qlb.k], pq2k)
                self.tt(qp32[:, 0:n], pq2[0:32, 0:n], rs[0:32, 0:n], ALU.mult, [pq2k, rs.k], [qp32.k])
                rope32(qp_[:, 0:n], qp32[:, 0:n], qp32.k, n, qp_.k)
                return qn_, qp_

            nxtq = qprep(0)
            for h in range(4):
                qn_, qp_ = nxtq
                if h + 1 < 4:
                    nxtq = qprep(h + 1)
                p_, hh = h // 2, h % 2
                self.attn(lambda t, h=h, qn_=qn_, qp_=qp_, n=n: [(kN[:, h, t * 128:(t + 1) * 128], qn_[:, 0:n]),
                                                                 (kP[:, t * 128:(t + 1) * 128], qp_[:, 0:n])],
                          lambda t, p_=p_: vt[:, t, p_ * 128:(p_ + 1) * 128], tiles, scale, n, hh,
                          ob[hh * 64:hh * 64 + 64, p_, 0:n], (ob.k, h), [kN.k, kP.k, vt.k, qn_.k, qp_.k])
            if STOP <= 5:
                continue
            self.outproj(l, wo, ob, [(ob.k, h) for h in range(4)], b, t0, n)

    def swa(self, l):
        P = self.P
        aT = self.aT
        ws = self.ar('ws', [128, 8, 512], BF16)
        self.load_w(ws[:, :, :], self.w_in[l][:, C_SQ:C_SQ + 512], 8, 512, ws.k)
        r128 = self.ar('r128', [128, 128], BF16)
        P.dma('pool', r128[:, :], self.din['r128'], writes=[r128.k])
        wo = self.load_wo(l, 2)
        msk = self.ar('swamask', [128, 6, 512], BF16)
        msk.k = 'swamask'
        for r in range(6):
            P.dma('pool', msk[:, r, :], self.din['swa_mask'][r], writes=['swamask'])
        esink = self.ar('esink', [128, 4], F32)
        esink.k = 'esink'
        P.dma('sp', esink[:, :], self.din['swa_sink'][l:l + 1, :].broadcast_to([128, 4]), writes=['esink'])
        self.act(esink[:, :], esink[:, :], AF.Exp, ['esink'], ['esink'])
        kx = self.ar('kx', [128, T], BF16)
        kxs = self.ar('kxs', [128, T], BF16)
        vn = self.ar('vn', [128, NT, 128], BF16)
        vs = self.ar('vs', [128, NT, 128], BF16)
        cs = self.ar('cs', [128, 2, 512], F32)
        xb = self.ar('xb', [128, 512], BF16)
        t1 = self.ar('t1', [128, 512], F32)
        t2 = self.ar('t2', [128, 512], F32)
        qx = [self.ar('qx%d' % i, [128, 2, 512], BF16) for i in range(2)]
        oblk = [self.ar('oblk%d' % i, [128, 2, 512], BF16) for i in range(2)]
        self.PT = [self.ar('PT%d' % i, [128, 512], BF16) for i in range(3)]
        self.pti = 0
        self.rd = self.ar('rd', [128, 512], F32)
        self.rd.k = 'rd'
        cosd, sind = self.din['cos_swa'], self.din['sin_swa']

        def rope(dst, ps_x, psk, n, wkey):
            self.cp('act', xb[:, 0:n], ps_x[:, 0:n], [psk], [xb.k])
            pr, prk = self.psum('mB', [2, 3])
            self.mmg(pr[:, 0:n], [(r128[:, :], xb[:, 0:n])], [r128.k, xb.k], prk)
            self.tt(t1[:, 0:n], ps_x[:, 0:n], cs[:, 0, 0:n], ALU.mult, [psk, cs.k], [t1.k])
            self.tt(t2[:, 0:n], pr[:, 0:n], cs[:, 1, 0:n], ALU.mult, [prk, cs.k], [t2.k])
            self.tt(dst, t1[:, 0:n], t2[:, 0:n], ALU.add, [t1.k, t2.k], [wkey])

        for b, (t0, n) in enumerate(BLKS):
            ak = ('aT', b)
            for hf in range(2):
                P.dma('sp', cs[hf * 64:(hf + 1) * 64, 0, 0:n], cosd[:, t0:t0 + n], writes=[cs.k])
                P.dma('sp', cs[hf * 64:(hf + 1) * 64, 1, 0:n], sind[:, t0:t0 + n], writes=[cs.k])
            pk_, pkk = self.psum('mA', [0, 1])
            self.proj(pk_[:, 0:n], ws, (256, 384), lambda kc: aT[:, kc, t0:t0 + n], [ws.k, ak], pkk)
            rope(kx[:, t0:t0 + n], pk_, pkk, n, kx.k)
            for tt_ in range(n // 128):
                tile = t0 // 128 + tt_
                pv, pvk = self.psum('mA', [0, 1])
                self.mmg(pv[:, 0:128], [(aT[:, kc, tile * 128:(tile + 1) * 128], ws[:, kc, 384:512]) for kc in range(8)],
                         [ws.k, ak], pvk)
                self.cp('act', vn[:, tile, :], pv[:, 0:128], [pvk], [vn.k])
                self.cp('dve', vs[:, tile, 0:64], pv[:, 64:128], [pvk], [vs.k])
                self.cp('dve', vs[:, tile, 64:128], pv[:, 0:64], [pvk], [vs.k])
        P.dma('sp', kxs[0:64, :], kx[64:128, :], reads=[kx.k], writes=[kxs.k])
        P.dma('sp', kxs[64:128, :], kx[0:64, :], reads=[kx.k], writes=[kxs.k])
        scale = 64.0 ** -0.5
        for b, (t0, n) in enumerate(BLKS):
            ak = ('aT', b)
            for hf in range(2):
                P.dma('sp', cs[hf * 64:(hf + 1) * 64, 0, 0:n], cosd[:, t0:t0 + n], writes=[cs.k])
                P.dma('sp', cs[hf * 64:(hf + 1) * 64, 1, 0:n], sind[:, t0:t0 + n], writes=[cs.k])
            q_ = qx[b % 2]
            for p_ in range(2):
                pq, pqk = self.psum('mA', [0, 1])
                self.proj(pq[:, 0:n], ws, (p_ * 128, (p_ + 1) * 128), lambda kc: aT[:, kc, t0:t0 + n], [ws.k, ak], pqk)
                rope(q_[:, p_, 0:n], pq, pqk, n, q_.k)
            ob = oblk[b % 2]
            if b == 0:
                tiles = [(0, None), (1, None)]
            else:
                g0 = t0 // 128
                tiles = [(0, None), (1, None)]
                for r in range(-1, 5):
                    j = g0 + r
                    if 2 <= j <= 17:
                        tiles.append((j, msk[:, r + 1, :]))
            for h in range(4):
                g, hh, p_ = h // 2, h % 2, h // 2
                ksrc = kx if g == hh else kxs
                vsrc = vn if g == hh else vs
                self.attn(lambda t, ksrc=ksrc, hh=hh, q_=q_, p_=p_, n=n: [(ksrc[hh * 64:hh * 64 + 64, t * 128:(t + 1) * 128],
                                                                               q_[hh * 64:hh * 64 + 64, p_, 0:n])],
                          lambda t, vsrc=vsrc: vsrc[:, t, :], tiles, scale, n, hh,
                          ob[hh * 64:hh * 64 + 64, p_, 0:n], (ob.k, h), [kx.k, kxs.k, vn.k, vs.k, q_.k],
                          sink_col=esink[:, h:h + 1])
            self.outproj(l, wo, ob, [(ob.k, h) for h in range(4)], b, t0, n)

    def headnorm_gate(self, l, o32, okey, n, t0, b, w, gcols, center, gcol, bcol, oblk, ak):
        aT = self.aT
        ob16 = self.hn_b16
        c32 = self.hn_c32
        blk64 = self.blk64
        for p in range(2):
            src = o32[:, p, 0:n]
            if center:
                self.cp('act', ob16[:, 0:n], src, [okey], [ob16.k])
                pm, pmk = self.psum('mB', [2, 3])
                self.mmg(pm[:, 0:n], [(blk64[:, :], ob16[:, 0:n])], [ob16.k, blk64.k], pmk)
                self.stt(c32[:, 0:n], pm[:, 0:n], -1.0, src, ALU.mult, ALU.add, [okey, pmk], [c32.k])
                cs_ = c32[:, 0:n]
                ck = c32.k
            else:
                cs_ = src
                ck = okey
            self.act(ob16[:, 0:n], cs_, AF.Square, [ck], [ob16.k])
            pv_, pvk = self.psum('mB', [2, 3])
            self.mmg(pv_[:, 0:n], [(blk64[:, :], ob16[:, 0:n])], [ob16.k, blk64.k], pvk)
            rs = self.hn_rs
            self.rstd_from(rs[:, 0:n], pv_[:, 0:n], 1.0, [pvk], [rs.k])
            self.tt(c32[:, 0:n], cs_, rs[:, 0:n], ALU.mult, [ck, rs.k], [c32.k])
            if bcol is not None:
                self.ts(c32[:, 0:n], c32[:, 0:n], self.colv[:, l, gcol + p:gcol + p + 1],
                        self.colv[:, l, bcol + p:bcol + p + 1], ALU.mult, ALU.add, [c32.k, 'colv'], [c32.k])
            else:
                self.ts(c32[:, 0:n], c32[:, 0:n], self.colv[:, l, gcol + p:gcol + p + 1], None, ALU.mult, None,
                        [c32.k, 'colv'], [c32.k])
            pg, pgk = self.psum('mA', [0, 1])
            self.proj(pg[:, 0:n], w, (gcols + p * 128, gcols + (p + 1) * 128), lambda kc: aT[:, kc, t0:t0 + n], [w.k, ak], pgk)
            sg = self.hn_sg
            self.act(sg[:, 0:n], pg[:, 0:n], AF.Silu, [pgk], [sg.k])
            self.tt(oblk[:, p, 0:n], c32[:, 0:n], sg[:, 0:n], ALU.mult, [c32.k, sg.k], [(oblk.k, p)])

    def hn_alloc(self):
        self.hn_b16 = self.ar('hn_b16', [128, 512], BF16)
        self.hn_c32 = self.ar('hn_c32', [128, 512], F32)
        self.hn_rs = self.ar('hn_rs', [128, 512], F32)
        self.hn_sg = self.ar('hn_sg', [128, 512], BF16)
        self.blk64 = self.ar('blk64', [128, 128], BF16)
        self.P.dma('pool', self.blk64[:, :], self.din['blk64'], writes=[self.blk64.k])

    def ret(self, l):
        P = self.P
        aT = self.aT
        ident = self.ident
        r64b = self.ar('r64b', [64, 64], BF16)
        P.dma('pool', r64b[:, :], self.din['r64b'], writes=[r64b.k])
        wo = self.load_wo(l, 3)
        qx = self.ar('qx', [64, 2, T], BF16)
        kx = self.ar('kx', [64, 2, T], BF16)
        kxt = self.ar('kxt', [128, NT, 128], BF16)
        vt = self.ar('vt', [128, NT, 256], BF16)
        Sf = self.ar('Sf', [64, NT, 2, 128], BF16)
        Dsum = self.ar('Dsum', [128, 4, 128], BF16)
        zeta = self.ar('zeta', [128, 2, 4], F32)
        lgcol = self.ar('lgcol', [64, 2, 2], F32)
        xi = self.ar('xi', [64, 2, 2, 128], F32)
        dc = self.ar('dc', [64, 2, 2], F32)
        mark0 = self.ar_off
        w = self.ar('wr_', [128, 8, 512], BF16)
        self.load_w(w[:, :, :], self.w_in[l][:, C_RQ:C_RQ + 512], 8, 512, w.k)
        mark1 = self.ar_off
        lgb = self.ar('lgb', [128, 8], F32)
        rel = self.ar('rel', [128, 4, 128], F32)
        colc = self.ar('colc', [128, 16], F32)
        rowc = self.ar('rowc', [64, 2, 128], F32)
        dtmp = self.ar('dtmp', [128, 2, 128], F32)
        cosd, sind = self.din['cos_ret'], self.din['sin_ret']
        P.dma('sp', lgb[:, :], self.din['ret_decay_logit'][l:l + 1].rearrange("o d h -> o (d h)").broadcast_to([128, 8]),
              writes=[lgb.k])
        P.dma('sp', rel[:, :, :], self.din['ret_rel'].rearrange("r s t -> s r t"), writes=[rel.k])
        P.dma('sp', colc[:, :], self.din['ret_colc'], writes=[colc.k])
        P.dma('sp', rowc[:, :, :], self.din['ret_rowc'].rearrange("p (d t) -> p d t", d=2), writes=[rowc.k])
        self.act(lgb[:, :], lgb[:, :], AF.Exp, [lgb.k], [lgb.k], scale=-1.0)
        self.ts(lgb[:, :], lgb[:, :], 1.0, None, ALU.add, None, [lgb.k], [lgb.k])
        self.act(lgb[:, :], lgb[:, :], AF.Ln, [lgb.k], [lgb.k])
        self.ts(lgb[:, :], lgb[:, :], -1.0, None, ALU.mult, None, [lgb.k], [lgb.k])
        for h in range(4):
            self.act(dtmp[:, 0, :], rel[:, 0, :], AF.Exp, [rel.k, lgb.k], [dtmp.k], scale=lgb[:, h:h + 1])
            self.tt(dtmp[:, 0, :], dtmp[:, 0, :], rel[:, 1, :], ALU.mult, [dtmp.k, rel.k], [dtmp.k])
            self.act(dtmp[:, 1, :], rel[:, 2, :], AF.Exp, [rel.k, lgb.k], [dtmp.k], scale=lgb[:, 4 + h:5 + h])
            self.tt(dtmp[:, 1, :], dtmp[:, 1, :], rel[:, 3, :], ALU.mult, [dtmp.k, rel.k], [dtmp.k])
            self.tt(Dsum[:, h, :], dtmp[:, 0, :], dtmp[:, 1, :], ALU.add, [dtmp.k], [Dsum.k])
        for d in range(2):
            self.act(zeta[:, d, :], lgb[:, d * 4:d * 4 + 4], AF.Exp, [lgb.k, colc.k], [zeta.k], scale=colc[:, d:d + 1])
            for p in range(2):
                self.cp('dve', lgcol[0:32, d, p:p + 1], lgb[0:32, d * 4 + 2 * p:d * 4 + 2 * p + 1], [lgb.k], [lgcol.k])
                self.cp('dve', lgcol[32:64, d, p:p + 1], lgb[32:64, d * 4 + 2 * p + 1:d * 4 + 2 * p + 2], [lgb.k], [lgcol.k])
        for d in range(2):
            for p in range(2):
                self.act(xi[:, d, p, :], rowc[:, d, :], AF.Exp, [rowc.k, lgcol.k], [xi.k], scale=lgcol[:, d, p:p + 1])
        self.act(dc[:, :, :], lgcol[:, :, :], AF.Exp, [lgcol.k], [dc.k], scale=128.0)
        RSTOP = int(os.environ.get('RET_STOP', '9'))
        if RSTOP <= 1:
            return
        self.ar_release(mark1)
        cs = self.ar('cs', [64, 2, 512], F32)
        xb = self.ar('xb', [64, 512], BF16)
        t1 = self.ar('t1', [64, 512], F32)
        t2 = self.ar('t2', [64, 512], F32)

        def rope64(dst, ps_x, psk, n, wkey):
            self.cp('act', xb[:, 0:n], ps_x, [psk], [xb.k])
            pr, prk = self.psum('mB', [2, 3])
            self.mmg(pr[0:64, 0:n], [(r64b[:, :], xb[:, 0:n])], [r64b.k, xb.k], prk)
            self.tt(t1[:, 0:n], ps_x, cs[:, 0, 0:n], ALU.mult, [psk, cs.k], [t1.k])
            self.tt(t2[:, 0:n], pr[0:64, 0:n], cs[:, 1, 0:n], ALU.mult, [prk, cs.k], [t2.k])
            self.tt(t1[:, 0:n], t1[:, 0:n], t2[:, 0:n], ALU.add, [t1.k, t2.k], [t1.k])
            self.cp('act', dst, t1[:, 0:n], [t1.k], [wkey])

        for b, (t0, n) in enumerate(BLKS):
            ak = ('aT', b)
            for hf in range(2):
                P.dma('sp', cs[hf * 32:(hf + 1) * 32, 0, 0:n], cosd[:, t0:t0 + n], writes=[cs.k])
                P.dma('sp', cs[hf * 32:(hf + 1) * 32, 1, 0:n], sind[:, t0:t0 + n], writes=[cs.k])
            for p in range(2):
                pq, pqk = self.psum('mA', [0, 1])
                self.proj(pq[0:64, 0:n], w, (p * 64, (p + 1) * 64), lambda kc: aT[:, kc, t0:t0 + n], [w.k, ak], pqk)
                rope64(qx[:, p, t0:t0 + n], pq[0:64, 0:n], pqk, n, qx.k)
                pk_, pkk = self.psum('mA', [0, 1])
                self.proj(pk_[0:64, 0:n], w, (128 + p * 64, 128 + (p + 1) * 64), lambda kc: aT[:, kc, t0:t0 + n], [w.k, ak], pkk)
                rope64(kx[:, p, t0:t0 + n], pk_[0:64, 0:n], pkk, n, kx.k)
                for tt_ in range(n // 128):
                    tile = t0 // 128 + tt_
                    ptr, ptk = self.psum('mB', [2, 3])
                    self.tr(ptr[:, 0:64], t1[:, tt_ * 128:(tt_ + 1) * 128], ident[0:64, 0:64], [t1.k, 'ident'], ptk)
                    self.cp('act', kxt[:, tile, p * 64:(p + 1) * 64], ptr[:, 0:64], [ptk], [kxt.k])
            for tt_ in range(n // 128):
                tile = t0 // 128 + tt_
                pv, pvk = self.psum('mA', [0, 1])
                self.mmg(pv[:, 0:256], [(aT[:, kc, tile * 128:(tile + 1) * 128], w[:, kc, 256:512]) for kc in range(8)],
                         [w.k, ak], pvk)
                self.cp('act', vt[:, tile, :], pv[:, 0:256], [pvk], [vt.k])

        if RSTOP <= 2:
            return
        self.ar_release(mark0)
        w = self.ar('wg_', [128, 8, 256], BF16)
        self.load_w(w[:, :, :], self.w_in[l][:, C_RG:C_RG + 256], 8, 256, w.k)
        self.hn_alloc()
        S = self.ar('S', [64, 2, 128], F32)
        Sbb = self.ar('Sbb', [64, 2, 128], BF16)
        vz = self.ar('vz', [128, 256], BF16)
        qxm = self.ar('qxm', [64, 2, 2, 128], BF16)
        qxd = self.ar('qxd', [64, 2, 2, 2, 128], BF16)
        rowm = self.ar('rowm', [64, 16], F32)
        P.dma('sp', rowm[:, :], self.din['ret_rowm'], writes=[rowm.k])
        AT = self.ar('AT', [128, 4, 128], BF16)
        o32 = self.ar('o32', [128, 2, 512], F32)
        oblk = self.ar('oblk', [128, 2, 512], BF16)

        def state_step(d, tile):
            self.tt(vz[:, :].rearrange("s (h v) -> s h v", h=4), vt[:, tile, :].rearrange("s (h v) -> s h v", h=4),
                    zeta[:, d, :].unsqueeze(2).broadcast_to([128, 4, 64]), ALU.mult, [vt.k, zeta.k], [vz.k])
            pu, puk = self.psum('mA', [0, 1])
            for p in range(2):
                self.mmg(pu[0:64, p * 128:(p + 1) * 128], [(kxt[:, tile, p * 64:(p + 1) * 64], vz[:, p * 128:(p + 1) * 128])],
                         [kxt.k, vz.k], puk)
            for p in range(2):
                self.stt(S[:, p, :], S[:, p, :], dc[:, d, p:p + 1], pu[0:64, p * 128:(p + 1) * 128], ALU.mult, ALU.add,
                         [S.k, dc.k, puk], [S.k])

        self.memset('dve', S[:, :, :], 0.0, [S.k])
        for tile in range(NT):
            self.cp('act', Sf[:, tile, :, :], S[:, :, :], [S.k], [(Sf.k, tile)])
            if tile < NT - 1:
                state_step(0, tile)
        if RSTOP <= 3:
            return
        self.memset('dve', S[:, :, :], 0.0, [S.k])
        order = [1, 0] + list(range(NT - 1, 1, -1))
        for idx, tile in enumerate(order):
            self.cp('act', Sbb[:, :, :], S[:, :, :], [S.k], [Sbb.k])
            for hh in range(2):
                self.ts(qxm[:, hh, :, :], qx[:, :, tile * 128:(tile + 1) * 128], rowm[:, hh:hh + 1], None, ALU.mult, None,
                        [qx.k, rowm.k], [qxm.k])
            for d in range(2):
                for hh in range(2):
                    self.tt(qxd[:, d, hh, :, :], qxm[:, hh, :, :], xi[:, d, :, :], ALU.mult, [qxm.k, xi.k], [qxd.k])
            pA, pAk = self.psum('rA', [4, 5])
            for h in range(4):
                hh, p = h % 2, h // 2
                self.mmg(pA[:, h * 128:(h + 1) * 128], [(kx[:, p, tile * 128:(tile + 1) * 128], qxm[:, hh, p, :])],
                         [kx.k, qxm.k], pAk)
            self.tt(AT[:, :, :], pA[:, :].rearrange("s (h t) -> s h t", h=4), Dsum[:, :, :], ALU.mult, [pAk, Dsum.k], [AT.k])
            if RSTOP <= 4:
                continue
            pO, pOk = self.psum('rO', [6, 7])
            for h in range(4):
                hh, p = h % 2, h // 2
                self.mmg(pO[:, h * 128:(h + 1) * 128],
                         [(vt[:, tile, p * 128:(p + 1) * 128], AT[:, h, :]),
                          (Sf[:, tile, p, :], qxd[:, 0, hh, p, :]),
                          (Sbb[:, p, :], qxd[:, 1, hh, p, :])],
                         [vt.k, AT.k, (Sf.k, tile), Sbb.k, qxd.k], pOk)
            b = self.blk_of_tile(tile)
            t0, n = BLKS[b]
            off = tile * 128 - t0
            pv4 = pO[:, :].rearrange("q (p j t) -> q p j t", p=2, j=2)
            for hh in range(2):
                self.ts(o32[hh * 64:hh * 64 + 64, :, off:off + 128], pv4[hh * 64:hh * 64 + 64, :, hh, :], 32.0 ** -0.5, None,
                        ALU.mult, None, [pOk], [(o32.k, tile % 4, hh)])
            if RSTOP <= 5:
                continue
            if idx < NT - 1:
                state_step(1, tile)
            if RSTOP <= 6:
                continue
            if tile * 128 == t0:
                ntile = n // 128
                okeys = [(o32.k, (t0 // 128 + j) % 4, hh) for j in range(ntile) for hh in range(2)]
                self.P.op('dve', lambda e: e.tensor_copy(out=o32[:, :, 0:1], in_=o32[:, :, 0:1]), reads=okeys, writes=[o32.k] + okeys)
                self.headnorm_gate(l, o32, o32.k, n, t0, b, w, 0, True, 5, 7, oblk, ('aT', b))
                self.outproj(l, wo, oblk, [(oblk.k, 0), (oblk.k, 1)], b, t0, n)


    def hgrn(self, l):
        P = self.P
        aT = self.aT
        ident = self.ident
        wh = self.ar('wh', [128, 8, 1280], BF16)
        self.load_w(wh[:, :, :], self.w_in[l][:, C_HQ:C_HQ + 1280], 8, 1280, wh.k)
        itm = self.ar('itm', [128, NT, 256], BF16)
        ohg = self.ar('ohg', [128, 2, T], BF16)
        trie = self.ar('trie', [128, 2, 136], F32)
        P.dma('sp', trie[:, :, :], self.din['hg_trie'].rearrange("d s c -> s d c"), writes=[trie.k])
        mt = self.ar('mt', [128, 2, 128], BF16)
        P.dma('pool', mt[:, :, :], self.din['hg_mt'].rearrange("d s c -> s d c"), writes=[mt.k])
        ee = self.ar('ee', [128, 2, 8], BF16)
        P.dma('pool', ee[:, :, :], self.din['hg_e'].rearrange("d s c -> s d c"), writes=[ee.k])
        omlF = self.ar('omlF', [64, 8], F32)
        nomlF = self.ar('nomlF', [64, 8], F32)
        lbB = self.ar('lbB', [128, 512], F32)
        omlB = self.ar('omlB', [128, 512], F32)
        mark = self.ar_off
        st = self.ar('st', [32, 64], F32)
        lbl = self.ar('lbl', [64, 4, 8], F32)
        sF = self.ar('sF', [64, 8], F32)
        cF = self.ar('cF', [64, 8], F32)
        P.dma('sp', st[:, :], self.din['hg_lb_logits'].rearrange("l d (h k) -> (l d h) k", k=64), writes=[st.k])
        pt, pk = self.psum('mA', [0, 1])
        self.tr(pt[0:64, 0:32], st[:, :], ident[0:32, 0:32], [st.k, 'ident'], pk)
        self.act(lbl[:, :, :], pt[0:64, 0:32].rearrange("k (l x) -> k l x", l=4), AF.Exp, [pk], [lbl.k])
        self.tt(sF[:, :], lbl[:, 0, :], lbl[:, 1, :], ALU.add, [lbl.k], [sF.k])
        self.tt(sF[:, :], sF[:, :], lbl[:, 2, :], ALU.add, [lbl.k, sF.k], [sF.k])
        self.tt(sF[:, :], sF[:, :], lbl[:, 3, :], ALU.add, [lbl.k, sF.k], [sF.k])
        self.recip(sF[:, :], sF[:, :], [sF.k], [sF.k])
        self.memset('dve', cF[:, :], 0.0, [cF.k])
        for j in range(1, l + 1):
            self.tt(cF[:, :], cF[:, :], lbl[:, j, :], ALU.add, [lbl.k, cF.k], [cF.k])
        self.tt(cF[:, :], cF[:, :], sF[:, :], ALU.mult, [cF.k, sF.k], [cF.k])
        self.ts(omlF[:, :], cF[:, :], -1.0, 1.0, ALU.mult, ALU.add, [cF.k], [omlF.k])
        self.ts(nomlF[:, :], cF[:, :], -1.0, None, ALU.add, None, [cF.k], [nomlF.k])
        eb_ = self.ar('ebig', [128, 4, 512], F32)
        sB = self.ar('sB', [128, 512], F32)
        P.dma('sp', eb_[:, :, :], self.din['hg_lb_logits'].rearrange("(o l) d c -> o l (d c)", o=1).broadcast_to([128, 4, 512]),
              writes=[eb_.k])
        self.act(eb_[:, :, :], eb_[:, :, :], AF.Exp, [eb_.k], [eb_.k])
        self.tt(sB[:, :], eb_[:, 0, :], eb_[:, 1, :], ALU.add, [eb_.k], [sB.k])
        self.tt(sB[:, :], sB[:, :], eb_[:, 2, :], ALU.add, [eb_.k, sB.k], [sB.k])
        self.tt(sB[:, :], sB[:, :], eb_[:, 3, :], ALU.add, [eb_.k, sB.k], [sB.k])
        self.recip(sB[:, :], sB[:, :], [sB.k], [sB.k])
        self.memset('dve', lbB[:, :], 0.0, [lbB.k])
        for j in range(1, l + 1):
            self.tt(lbB[:, :], lbB[:, :], eb_[:, j, :], ALU.add, [eb_.k, lbB.k], [lbB.k])
        self.tt(lbB[:, :], lbB[:, :], sB[:, :], ALU.mult, [lbB.k, sB.k], [lbB.k])
        self.ts(omlB[:, :], lbB[:, :], -1.0, 1.0, ALU.mult, ALU.add, [lbB.k], [omlB.k])
        self.ar_release(mark)
        for tile in range(NT):
            ak = ('aT', self.blk_of_tile(tile))
            pv, pvk = self.psum('mA', [0, 1])
            self.mmg(pv[:, 0:256], [(aT[:, kc, tile * 128:(tile + 1) * 128], wh[:, kc, 768:1024]) for kc in range(8)],
                     [wh.k, ak], pvk)
            self.cp('act', itm[:, tile, :], pv[:, 0:256], [pvk], [itm.k])
        ftm = self.ar('ftm', [128, 256], F32)
        lftm = self.ar('lftm', [128, 256], F32)
        ktm = self.ar('ktm', [128, 256], F32)
        kbt = self.ar('kbt', [128, 256], BF16)
        kTt = self.ar('kTt', [64, 4, 128], F32)
        ebt = self.ar('ebt', [64, 4, 128], F32)
        enb = self.ar('enb', [64, 4, 128], F32)
        dd = self.ar('dd', [64, 4, 8], F32)
        qb = self.ar('qb', [64, 4, 128], BF16)
        kb = self.ar('kb', [64, 4, 128], BF16)
        vexp = [self.ar('vexp%d' % i, [128, 8, 64], BF16) for i in range(2)]
        u = self.ar('u', [64, 8, 4, 64], F32)
        Spp = [self.ar('S%d' % i, [64, 4, 64], F32) for i in range(2)]
        tmpS = self.ar('tmpS', [64, 4, 64], F32)
        Sbf = self.ar('Sbf', [64, 8, 4, 64], BF16)
        si = 0
        AT = self.ar('AT', [128, 4, 128], BF16)
        vi = 0
        for d in range(2):
            order = list(range(NT)) if d == 0 else [1, 0] + list(range(NT - 1, 1, -1))
            self.memset('dve', Spp[si % 2][:, :, :], 0.0, [Spp[si % 2].k])
            zc = 256 + d * 256
            for tile in order:
                ak = ('aT', self.blk_of_tile(tile))
                tcs = slice(tile * 128, (tile + 1) * 128)
                pz, pzk = self.psum('hz', [0])
                self.mmg(pz[:, 0:256], [(aT[:, kc, tcs], wh[:, kc, zc:zc + 256]) for kc in range(8)], [wh.k, ak], pzk)
                pzT, pzTk = self.psum('hzT', [1])
                for h in range(4):
                    self.mmg(pzT[0:64, h * 128:(h + 1) * 128],
                             [(wh[:, kc, zc + h * 64:zc + (h + 1) * 64], aT[:, kc, tcs]) for kc in range(8)], [wh.k, ak], pzTk)
                pq, pqk = self.psum('hq', [2])
                for h in range(4):
                    self.mmg(pq[0:64, h * 128:(h + 1) * 128],
                             [(wh[:, kc, h * 64:(h + 1) * 64], aT[:, kc, tcs]) for kc in range(8)], [wh.k, ak], pqk)
                self.act(ftm[:, :], pz[:, 0:256], AF.Sigmoid, [pzk], [ftm.k])
                self.act(kTt[:, :, :], pzT[0:64, :].rearrange("k (h t) -> k h t", h=4), AF.Sigmoid, [pzTk], [kTt.k])
                self.tt(ftm[:, :], ftm[:, :], omlB[:, d * 256:(d + 1) * 256], ALU.mult, [ftm.k, omlB.k], [ftm.k])
                self.tt(ftm[:, :], ftm[:, :], lbB[:, d * 256:(d + 1) * 256], ALU.add, [ftm.k, lbB.k], [ftm.k])
                self.act(lftm[:, :], ftm[:, :], AF.Ln, [ftm.k], [lftm.k])
                self.tt(kTt[:, :, :], kTt[:, :, :], nomlF[:, d * 4:(d + 1) * 4].unsqueeze(2).broadcast_to([64, 4, 128]),
                        ALU.mult, [kTt.k, nomlF.k], [kTt.k])
                self.tt(kTt[:, :, :], kTt[:, :, :], omlF[:, d * 4:(d + 1) * 4].unsqueeze(2).broadcast_to([64, 4, 128]),
                        ALU.add, [kTt.k, omlF.k], [kTt.k])
                self.ts(ktm[:, :], ftm[:, :], -1.0, 1.0, ALU.mult, ALU.add, [ftm.k], [ktm.k])
                pb, pbk = self.psum('hb', [3])
                self.mmg(pb[:, 0:256], [(trie[:, d, 0:128], lftm[:, :])], [trie.k, lftm.k], pbk)
                pbT, pbTk = self.psum('hbT', [4])
                for h in range(4):
                    self.mmg(pbT[0:64, h * 128:(h + 1) * 128], [(lftm[:, h * 64:(h + 1) * 64], trie[:, d, 0:128])],
                             [lftm.k, trie.k], pbTk)
                ptot, ptotk = self.psum('htot', [5])
                for h in range(4):
                    self.mmg(ptot[0:64, h * 8:(h + 1) * 8], [(lftm[:, h * 64:(h + 1) * 64], trie[:, d, 128:136])],
                             [lftm.k, trie.k], ptotk)
                self.act(ftm[:, :], pb[:, 0:256], AF.Exp, [pbk], [ftm.k], scale=-1.0)
                self.act(ebt[:, :, :], pbT[0:64, :].rearrange("k (h t) -> k h t", h=4), AF.Exp, [pbTk], [ebt.k])
                self.act(enb[:, :, :], pbT[0:64, :].rearrange("k (h t) -> k h t", h=4), AF.Exp, [pbTk], [enb.k], scale=-1.0)
                self.act(dd[:, :, :], ptot[0:64, 0:32].rearrange("k (h j) -> k h j", h=4), AF.Exp, [ptotk], [dd.k])
                self.tt(kbt[:, :], ktm[:, :], ftm[:, :], ALU.mult, [ktm.k, ftm.k], [kbt.k])
                self.stt(qb[:, :, :], pq[0:64, :].rearrange("k (h t) -> k h t", h=4), 0.125, ebt[:, :, :], ALU.mult, ALU.mult,
                         [pqk, ebt.k], [qb.k])
                self.tt(kb[:, :, :], kTt[:, :, :], enb[:, :, :], ALU.mult, [kTt.k, enb.k], [kb.k])
                pA, pAk = self.psum('h3', [6])
                for h in range(4):
                    self.mmg(pA[:, h * 128:(h + 1) * 128], [(kb[:, h, :], qb[:, h, :])], [kb.k, qb.k], pAk)
                self.tt(AT[:, :, :], pA[:, :].rearrange("s (h t) -> s h t", h=4),
                        mt[:, d, :].unsqueeze(1).broadcast_to([128, 4, 128]), ALU.mult, [pAk, mt.k], [AT.k])
                for hp in range(2):
                    ves = []
                    for h in (2 * hp, 2 * hp + 1):
                        ve = vexp[h % 2]
                        self.tt(ve[:, :, :], itm[:, tile, h * 64:(h + 1) * 64].unsqueeze(1).broadcast_to([128, 8, 64]),
                                ee[:, d, :].unsqueeze(2).broadcast_to([128, 8, 64]), ALU.mult, [itm.k, ee.k], [ve.k])
                        ves.append(ve)
                    pus = []
                    for h, ve in zip((2 * hp, 2 * hp + 1), ves):
                        pu, puk = self.psum('h2', [0, 1])
                        self.mmg(pu[0:64, :], [(kbt[:, h * 64:(h + 1) * 64], ve[:, :, :].rearrange("s j v -> s (j v)"))],
                                 [kbt.k, ve.k], puk)
                        pus.append((pu, puk))
                    for h, (pu, puk) in zip((2 * hp, 2 * hp + 1), pus):
                        self.tt(u[:, :, h, :], pu[0:64, :].rearrange("k (j v) -> k j v", j=8),
                                dd[:, h, :].unsqueeze(2).broadcast_to([64, 8, 64]), ALU.mult, [puk, dd.k], [u.k])
                for j in range(8):
                    S, S2 = Spp[si % 2], Spp[(si + 1) % 2]
                    si += 1
                    self.cp('act', Sbf[:, j, :, :], S[:, :, :], [S.k], [(Sbf.k, j)])
                    self.tt(tmpS[:, :, :], S[:, :, :], dd[:, :, j:j + 1].broadcast_to([64, 4, 64]), ALU.mult,
                            [S.k, dd.k], [tmpS.k])
                    self.tt(S2[:, :, :], tmpS[:, :, :], u[:, j, :, :], ALU.add, [tmpS.k, u.k], [S2.k])
                pO, pOk = self.psum('h4', [7])
                rk = [itm.k, AT.k, qb.k] + [(Sbf.k, j) for j in range(8)]
                for h in range(4):
                    p = h // 2
                    P.op('pe', lambda e, h=h, p=p, tile=tile: e.matmul(pO[:, h * 128:(h + 1) * 128], lhsT=itm[:, tile, p * 128:(p + 1) * 128],
                                                           rhs=AT[:, h, :], start=True, stop=False),
                         reads=rk, writes=[pOk], inc=False)
                    for j in range(8):
                        cj = j if d == 0 else 7 - j
                        P.op('pe', lambda e, h=h, p=p, j=j, cj=cj: e.matmul(
                            pO[:, h * 128 + cj * 16:h * 128 + (cj + 1) * 16],
                            lhsT=Sbf[:, j, 2 * p:2 * p + 2, :].rearrange("k h v -> k (h v)"),
                            rhs=qb[:, h, cj * 16:(cj + 1) * 16], start=False, stop=(j == 7)),
                             reads=rk, writes=[pOk], inc=(h == 3 and j == 7))
                pv4 = pO[:, :].rearrange("q (p j t) -> q p j t", p=2, j=2)
                for hh in range(2):
                    if d == 0:
                        self.cp('act' if hh else 'dve', ohg[hh * 64:hh * 64 + 64, :, tcs], pv4[hh * 64:hh * 64 + 64, :, hh, :],
                                [pOk], [(ohg.k, tile, hh)])
                    else:
                        self.tt(ohg[hh * 64:hh * 64 + 64, :, tcs], pv4[hh * 64:hh * 64 + 64, :, hh, :],
                                ohg[hh * 64:hh * 64 + 64, :, tcs], ALU.add, [pOk, (ohg.k, tile, hh)], [(ohg.k, tile, hh)])
        if os.environ.get('HG_DBG'):
            dbg = self.nc.dram_tensor('dbg', [128, 2 * T], F32, kind="ExternalOutput").ap()
            P.dma('pool', dbg.rearrange("q (p t) -> q p t", p=2), ohg[:, :, :],
                  reads=[(ohg.k, t_, hh) for t_ in range(NT) for hh in range(2)], is_output=True)
        self.ar_release(mark)
        wo = self.load_wo(l, 1)
        self.hn_alloc()
        oblk = self.ar('oblk', [128, 2, 512], BF16)
        o32 = self.ar('o32h', [128, 2, 512], F32)
        for b, (t0, n) in enumerate(BLKS):
            okeys = [(ohg.k, t0 // 128 + j, hh) for j in range(n // 128) for hh in range(2)]
            self.cp('dve', o32[:, :, 0:n], ohg[:, :, t0:t0 + n], okeys, [o32.k])
            self.headnorm_gate(l, o32, o32.k, n, t0, b, wh, 1024, False, 3, None, oblk, ('aT', b))
            self.outproj(l, wo, oblk, [(oblk.k, 0), (oblk.k, 1)], b, t0, n)


    def final_out(self, out, gfin):
        P = self.P
        hT, rstd, ident = self.hT, self.rstd, self.ident
        nsq = self.ar('nsq', [128, 8, 512], BF16)
        ntmp = self.ar('ntmp', [128, 8, 512], F32)
        nsq.k, ntmp.k = 'nsq', 'ntmp'
        ost = [self.ar('ost%d' % i, [128, 1024], F32) for i in range(2)]
        oi = 0
        for b, (t0, n) in enumerate(BLKS):
            if b == 0:
                continue
            hk = [('hT', c, b) for c in range(8)]
            if self.final:
                self.act(nsq[:, :, 0:n], hT[:, :, t0:t0 + n], AF.Square, hk, ['nsq'])
                pt, pk = self.psum('nrm', [2, 3])
                self.mmg(pt[:, 0:n], [(self.ones_bf[:, :], nsq[:, c, 0:n]) for c in range(8)], ['nsq', 'ones_bf'], pk)
                self.act(rstd[:, 0:n], pt[:, 0:n], AF.Sqrt, [pk], ['rstd'], bias=self.eps_col[:, 0:1], scale=1.0 / D)
                self.recip(rstd[:, 0:n], rstd[:, 0:n], ['rstd'], ['rstd'])
                self.tt(ntmp[:, :, 0:n], hT[:, :, t0:t0 + n], rstd[:, 0:n].unsqueeze(1).broadcast_to([128, 8, n]),
                        ALU.mult, hk + ['rstd'], ['ntmp'])
                self.tt(ntmp[:, :, 0:n], ntmp[:, :, 0:n], gfin[:, :].unsqueeze(2).broadcast_to([128, 8, n]),
                        ALU.mult, ['ntmp', 'gfin'], ['ntmp'])
            else:
                self.cp('dve', ntmp[:, :, 0:n], hT[:, :, t0:t0 + n], hk, ['ntmp'])
            for tt_ in range(n // 128):
                os_ = ost[oi % 2]
                oi += 1
                for half in range(2):
                    pt, pk = self.psum('tr', [0, 1])
                    for j in range(4):
                        c = half * 4 + j
                        self.tr(pt[:, j * 128:(j + 1) * 128], ntmp[:, c, tt_ * 128:(tt_ + 1) * 128], ident[:, :],
                                ['ntmp', 'ident'], pk)
                    self.cp('act' if half else 'dve', os_[:, half * 512:(half + 1) * 512], pt[:, :], [pk], [(os_.k, half)])
                row0 = t0 - LCTX + tt_ * 128
                P.dma('sp', out[row0:row0 + 128, :], os_[:, :], reads=[(os_.k, 0), (os_.k, 1)], is_output=True)


def _rot_T(half):
    n = 2 * half
    R = np.zeros((n, n), np.float32)
    for m in range(half):
        R[m, m + half] = -1.0
        R[m + half, m] = 1.0
    return R.T.copy()


def _consts():
    sel = np.zeros((NEXP, NEXP * 128), np.float32)
    for e in range(NEXP):
        sel[e, e * 128:(e + 1) * 128] = 1.0
    c = {'ident': np.eye(128, dtype=np.float32), 'sel': sel}
    theta = 10000.0
    tok = np.arange(NLAT)
    row, col = (tok // 64).astype(np.float64), (tok % 64).astype(np.float64)

    def axial(rot_dim):
        nf = rot_dim // 4
        inv = theta ** (-np.arange(nf, dtype=np.float64) / nf)
        inv = inv.astype(np.float32).astype(np.float64)
        ang = np.concatenate([row[:, None] * inv, col[:, None] * inv], axis=1)
        ang = ang.astype(np.float32).astype(np.float64)
        full = np.zeros((T, rot_dim // 2))
        full[LCTX:] = ang
        a2 = np.concatenate([full, full], axis=1)
        return np.cos(a2).T.astype(np.float32).copy(), np.sin(a2).T.astype(np.float32).copy()

    c['cos_mla'], c['sin_mla'] = axial(32)
    c['cos_swa'], c['sin_swa'] = axial(64)
    inv = (theta ** (-np.arange(16, dtype=np.float64) / 16)).astype(np.float32).astype(np.float64)
    ang = (np.arange(T, dtype=np.float64)[:, None] * inv).astype(np.float32).astype(np.float64)
    a2 = np.concatenate([ang, ang], axis=1)
    c['cos_ret'], c['sin_ret'] = np.cos(a2).T.astype(np.float32).copy(), np.sin(a2).T.astype(np.float32).copy()
    rb = np.zeros((96, 96), np.float32)
    rb[64:96, 64:96] = _rot_T(16)
    c['rbig96'] = rb
    r128 = np.zeros((128, 128), np.float32)
    r128[0:64, 0:64] = _rot_T(32)
    r128[64:128, 64:128] = _rot_T(32)
    c['r128'] = r128
    r64b = np.zeros((64, 64), np.float32)
    r64b[0:32, 0:32] = _rot_T(16)
    r64b[32:64, 32:64] = _rot_T(16)
    c['r64b'] = r64b
    m = np.zeros((6, 128, 512), np.float32)
    s_ = np.arange(128)[:, None]
    t_ = np.arange(512)[None, :]
    for r in range(-1, 5):
        m[r + 1] = (np.abs(t_ - s_ - 128 * r) <= 128).astype(np.float32)
    c['swa_mask'] = m
    b64 = np.zeros((128, 128), np.float32)
    b64[0:64, 0:64] = 1.0 / 64
    b64[64:128, 64:128] = 1.0 / 64
    c['blk64'] = b64
    s_ = np.arange(128, dtype=np.float32)[:, None]
    t_ = np.arange(128, dtype=np.float32)[None, :]
    c['ret_rel'] = np.stack([np.maximum(t_ - s_, 0), (t_ >= s_).astype(np.float32),
                             np.maximum(s_ - t_, 0), (s_ >= t_).astype(np.float32)]).astype(np.float32)
    cc = np.zeros((128, 16), np.float32)
    cc[:, 0] = 127.0 - np.arange(128)
    cc[:, 1] = np.arange(128)
    c['ret_colc'] = cc
    rc = np.concatenate([np.arange(128) + 1.0, 128.0 - np.arange(128)])[None, :].repeat(64, axis=0)
    c['ret_rowc'] = rc.astype(np.float32)
    rm = np.zeros((64, 16), np.float32)
    rm[0:32, 0] = 1.0
    rm[32:64, 1] = 1.0
    c['ret_rowm'] = rm
    si = np.arange(128)[:, None]
    ti = np.arange(128)[None, :]
    same = (si // 16) == (ti // 16)
    trif = (same & (si <= ti)).astype(np.float32)
    trib = (same & (si >= ti)).astype(np.float32)
    ef = ((si // 16) == np.arange(8)[None, :]).astype(np.float32)
    eb = ((si // 16) == (7 - np.arange(8))[None, :]).astype(np.float32)
    c['hg_trie'] = np.stack([np.concatenate([trif, ef], axis=1), np.concatenate([trib, eb], axis=1)]).astype(np.float32)
    c['hg_mt'] = np.stack([trif, trib]).astype(np.float32)
    c['hg_e'] = np.stack([ef, eb]).astype(np.float32)
    return c


_CACHE = {}


def _get_prog(key, **kw):
    if key not in _CACHE:
        b = Builder(**kw)
        nc = b.build()
        _CACHE[key] = (nc, b)
    return _CACHE[key]


def run_partial(inputs, layers=(0, 1, 2, 3), mixers=(0, 1, 2, 3), ffn=True, final=True, cores=8):
    nc, b = _get_prog(('p', tuple(layers), tuple(mixers), ffn, final), layers=layers, mixers=mixers, ffn=ffn, final=final)
    consts = _consts()
    f = lambda a: np.ascontiguousarray(np.asarray(a, dtype=np.float32))
    shared = {k: f(inputs[k]) for k in b.din if k in inputs and k not in ('x', 'ctx')}
    for k in consts:
        if k in b.din:
            shared[k] = consts[k]
    in_maps = []
    for i in range(cores):
        m = dict(shared)
        m['x'] = f(inputs['x'][i])
        m['ctx'] = f(inputs['ctx'][i])
        m['c2'] = np.ascontiguousarray(np.stack([np.asarray(inputs['c'][i], np.float32),
                                                 np.asarray(inputs['c_ctx'], np.float32)]))
        in_maps.append(m)
    res = run_bass_kernel_spmd(nc, in_maps, core_ids=list(range(cores)))
    return np.stack([r['out'] for r in res.results], axis=0)


def kernel(**inputs):
    return run_partial(inputs)
```

```python
import os
import numpy as np
import concourse.bass as bass
import concourse.mybir as mybir
from concourse.bass_utils import run_bass_kernel_spmd

AF = mybir.ActivationFunctionType
ALU = mybir.AluOpType
AX = mybir.AxisListType
F32 = mybir.dt.float32
BF16 = mybir.dt.bfloat16

ENGS = ['pe', 'act', 'dve', 'pool', 'sp']
NDMA = 56

D = 1024
T = 2304
NT = 18
LCTX = 256
NLAT = 2048
DEPTH = 4
D_FF = 2816
D_FFE = 3584
NEXP = 8
D_IN = 2912
EPS = 1e-6
BLKS = [(0, 256), (256, 512), (768, 512), (1280, 512), (1792, 512)]
C_MQ, C_MKV, C_MPE = 0, 192, 320
C_HQ, C_HZF, C_HZB, C_HI, C_HG = 352, 608, 864, 1120, 1376
C_SQ, C_SK, C_SV = 1632, 1888, 2016
C_RQ, C_RK, C_RV, C_RG = 2144, 2272, 2400, 2656


class Prog:
    def __init__(self, nc):
        self.nc = nc
        self.q = {e: [] for e in ENGS}
        self.sem = {e: nc.alloc_semaphore('cs_' + e) for e in ENGS}
        self.cnt = {e: 0 for e in ENGS}
        self.waited = {}
        self.lastw = {}
        self.readers = {}
        self.dma_sems = [nc.alloc_semaphore('ds%d' % i) for i in range(NDMA)]
        self.dma_cnt = [0] * NDMA
        self.dma_rr = {'sp': 0, 'pool': 0}
        self.out_tokens = []
        self.ninst = 0

    def _need(self, eng, tok):
        sem, val, prod = tok
        if eng == 'pe' and prod == 'pe':
            return
        k = (eng, sem.num)
        if self.waited.get(k, 0) >= val:
            return
        self.waited[k] = val
        self.q[eng].append(lambda e, sem=sem, val=val: e.wait_ge(sem, val))

    def _deps(self, eng, reads, writes):
        for r in reads:
            t = self.lastw.get(r)
            if t is not None:
                self._need(eng, t)
            if isinstance(r, tuple) and r[0] == 'ps':
                for t in self.readers.get(r, {}).values():
                    if t[2] != eng:
                        self._need(eng, t)
        for w in writes:
            t = self.lastw.get(w)
            if t is not None:
                self._need(eng, t)
            for t in self.readers.get(w, {}).values():
                self._need(eng, t)

    def _record(self, tok, reads, writes):
        for r in reads:
            d = self.readers.setdefault(r, {})
            k = tok[0].num
            if k not in d or d[k][1] < tok[1]:
                d[k] = tok
        for w in writes:
            self.lastw[w] = tok
            self.readers[w] = {}

    def op(self, eng, fn, reads=(), writes=(), inc=True):
        self._deps(eng, reads, writes)
        sem = self.sem[eng]
        self.ninst += 1
        if inc:
            self.cnt[eng] += 1
            tok = (sem, self.cnt[eng], eng)
            self.q[eng].append(lambda e, fn=fn, sem=sem: fn(e).then_inc(sem, 1))
        else:
            tok = (sem, self.cnt[eng] + 1, eng)
            self.q[eng].append(lambda e, fn=fn: fn(e))
        self._record(tok, reads, writes)
        return tok

    def dma(self, eng, out, in_, reads=(), writes=(), is_output=False):
        h = NDMA // 2
        j = self.dma_rr[eng]
        self.dma_rr[eng] = (j + 1) % h
        i = j if eng == 'sp' else h + j
        sem = self.dma_sems[i]
        if self.dma_cnt[i] > 0:
            self._need(eng, (sem, self.dma_cnt[i], 'dma'))
        self._deps(eng, reads, writes)
        self.dma_cnt[i] += 16
        tok = (sem, self.dma_cnt[i], 'dma')
        self.ninst += 1
        self.q[eng].append(
            lambda e, out=out, in_=in_, sem=sem: e.dma_start(out=out, in_=in_).then_inc(sem, 16))
        self._record(tok, reads, writes)
        if is_output:
            self.out_tokens.append(tok)
        return tok

    def barrier(self):
        toks = [(self.sem[e], self.cnt[e], e) for e in ENGS if self.cnt[e] > 0]
        toks += [(self.dma_sems[i], self.dma_cnt[i], 'dma') for i in range(NDMA) if self.dma_cnt[i] > 0]
        for f in ENGS:
            for t in toks:
                self._need(f, t)

    def emit(self):
        nc = self.nc
        for t in self.out_tokens:
            self._need('sp', t)
        for e in ['pe', 'act', 'dve', 'pool']:
            if self.cnt[e] > 0:
                self._need('sp', (self.sem[e], self.cnt[e], e))
        q = self.q
        with nc.Block() as block:
            @block.tensor
            def _(e):
                for f in q['pe']:
                    f(e)

            @block.scalar
            def _(e):
                for f in q['act']:
                    f(e)

            @block.vector
            def _(e):
                for f in q['dve']:
                    f(e)

            @block.gpsimd
            def _(e):
                for f in q['pool']:
                    f(e)

            @block.sync
            def _(e):
                for f in q['sp']:
                    f(e)


class SB:
    def __init__(self, nc, name, shape, dtype):
        self.t = nc.alloc_sbuf_tensor('sb_' + name, list(shape), dtype)
        self.k = name

    def __getitem__(self, idx):
        return self.t[idx]


class View:
    def __init__(self, ap, key):
        self.t = ap
        self.k = key

    def __getitem__(self, idx):
        return self.t[idx]


AR_WORDS = 19456


class Builder:
    def __init__(self, layers, mixers=(0, 1, 2, 3), ffn=True, final=True, h_in=False, h_out=False):
        self.layers = list(layers)
        self.mixers = tuple(mixers)
        self.ffn = ffn
        self.final = final
        self.h_in = h_in
        self.h_out = h_out
        self.nc = nc = bass.Bass("TRN2", target_bir_lowering=False)
        self.P = Prog(nc)
        self.din = {}
        self.ps = [nc.alloc_psum_tensor("psb%d" % i, [128, 512], F32) for i in range(8)]
        self.ps_rr = {}
        self.uid = 0
        self.arena = nc.alloc_sbuf_tensor('arena', [128, AR_WORDS], F32)
        self.ar_off = 0
        self.ar_phase = 0

    def inp(self, name, shape, dtype=F32):
        ap = self.nc.dram_tensor(name, list(shape), dtype, kind="ExternalInput").ap()
        self.din[name] = ap
        return ap

    def sb(self, name, shape, dtype=F32):
        return SB(self.nc, name, shape, dtype)

    def ar(self, name, shape, dtype=F32):
        nel = int(np.prod(shape[1:]))
        nb = nel * (2 if dtype == BF16 else 4)
        nw = ((nb + 31) // 32) * 8
        assert self.ar_off + nw <= AR_WORDS, (name, self.ar_off, nw)
        ap = self.arena[0:shape[0], self.ar_off:self.ar_off + nw]
        self.ar_off += nw
        if dtype == BF16:
            ap = ap.bitcast(BF16)
        ap = ap[:, 0:nel]
        if len(shape) == 3:
            ap = ap.rearrange("p (a b) -> p a b", a=shape[1])
        elif len(shape) == 4:
            ap = ap.rearrange("p (a b c) -> p a b c", a=shape[1], b=shape[2])
        elif len(shape) == 5:
            ap = ap.rearrange("p (a b c d) -> p a b c d", a=shape[1], b=shape[2], c=shape[3])
        self.ar_gen = getattr(self, 'ar_gen', 0)
        return View(ap, ('ar', self.ar_phase, name))

    def ar_release(self, mark):
        self.P.barrier()
        self.ar_off = mark
        self.ar_phase += 1

    def phase(self):
        self.P.barrier()
        self.ar_off = 0
        self.ar_phase += 1

    def psum(self, pool, banks):
        i = self.ps_rr.get(pool, 0)
        self.ps_rr[pool] = i + 1
        b = banks[i % len(banks)]
        return self.ps[b], ('ps', b)

    def mmg(self, out, pairs, reads, wkey):
        n = len(pairs)
        for i, (l, r) in enumerate(pairs):
            self.P.op('pe', lambda e, l=l, r=r, s=(i == 0), t=(i == n - 1): e.matmul(out, lhsT=l, rhs=r, start=s, stop=t),
                      reads=reads, writes=[wkey], inc=(i == n - 1))

    def tr(self, out, in_, ident, reads, wkey):
        self.P.op('pe', lambda e: e.transpose(out, in_, ident), reads=reads, writes=[wkey])

    def act(self, out, in_, func, reads, writes, bias=None, scale=None):
        kw = {}
        if bias is not None:
            kw['bias'] = bias
        if scale is not None:
            kw['scale'] = scale
        self.P.op('act', lambda e: e.activation(out=out, in_=in_, func=func, **kw), reads=reads, writes=writes)

    def tt(self, out, in0, in1, op, reads, writes, eng='dve'):
        self.P.op(eng, lambda e: e.tensor_tensor(out=out, in0=in0, in1=in1, op=op), reads=reads, writes=writes)

    def ts(self, out, in0, s1, s2, op0, op1, reads, writes, eng='dve'):
        if s2 is None:
            self.P.op(eng, lambda e: e.tensor_scalar(out=out, in0=in0, scalar1=s1, scalar2=None, op0=op0),
                      reads=reads, writes=writes)
        else:
            self.P.op(eng, lambda e: e.tensor_scalar(out=out, in0=in0, scalar1=s1, scalar2=s2, op0=op0, op1=op1),
                      reads=reads, writes=writes)

    def stt(self, out, in0, scalar, in1, op0, op1, reads, writes):
        self.P.op('dve', lambda e: e.scalar_tensor_tensor(out=out, in0=in0, scalar=scalar, in1=in1, op0=op0, op1=op1),
                  reads=reads, writes=writes)

    def cp(self, eng, out, in_, reads, writes):
        if eng == 'act':
            self.P.op('act', lambda e: e.copy(out=out, in_=in_), reads=reads, writes=writes)
        else:
            self.P.op(eng, lambda e: e.tensor_copy(out=out, in_=in_), reads=reads, writes=writes)

    def recip(self, out, in_, reads, writes):
        self.P.op('dve', lambda e: e.reciprocal(out=out, in_=in_), reads=reads, writes=writes)

    def memset(self, eng, ap, val, writes):
        self.P.op(eng, lambda e: e.memset(ap, val), writes=writes)

    def rows_to_cols(self, dram_rows, nrows, ncols, dst_ap, dst_key):
        self.uid += 1
        st = self.stage
        k = ('stage', self.uid % 2)
        sl = st[self.uid % 2]
        self.P.dma('sp', sl[0:nrows, 0:ncols], dram_rows, writes=[k])
        pt, pk = self.psum('tr', [0, 1])
        self.tr(pt[0:ncols, 0:nrows], sl[0:nrows, 0:ncols], self.ident[0:nrows, 0:nrows], [k, 'ident'], pk)
        self.cp('dve', dst_ap, pt[0:ncols, 0:nrows], [pk], [dst_key])

    def build(self):
        nc, P = self.nc, self.P
        L = DEPTH
        x = self.inp('x', [NLAT, D])
        ctx = self.inp('ctx', [LCTX, D])
        c2 = self.inp('c2', [2, D])
        identd = self.inp('ident', [128, 128])
        w_ada = self.inp('w_ada', [L, D, 6 * D])
        b_ada = self.inp('b_ada', [L, 6 * D])
        nmg = self.inp('norm_mix_g', [L, D])
        nfg = self.inp('norm_ffn_g', [L, D])
        fng = self.inp('final_norm_g', [D])
        self.w_in = self.inp('w_in', [L, D, D_IN])
        self.w_out = self.inp('w_out', [L, D, D])
        self.fwg = self.inp('ffn_w_gate', [2, D, D_FF])
        self.fwu = self.inp('ffn_w_up', [2, D, D_FF])
        self.fwd = self.inp('ffn_w_down', [2, D_FF, D])
        self.mwr = self.inp('moe_w_router', [2, D, NEXP])
        self.mwg = self.inp('moe_w_gate', [2, NEXP, D, D_FFE])
        self.mwu = self.inp('moe_w_up', [2, NEXP, D, D_FFE])
        self.mwd = self.inp('moe_w_down', [2, NEXP, D_FFE, D])
        seld = self.inp('sel', [NEXP, NEXP * 128])
        for nm, shp in [('mla_q_norm_g', [L, 192]), ('mla_w_uq', [L, 192, 384]), ('mla_kv_norm_g', [L, 128]),
                        ('mla_w_ukv', [L, 128, 512]), ('hg_lb_logits', [L, 2, 256]), ('hg_norm_g', [L, 256]),
                        ('swa_sink', [L, 4]), ('ret_decay_logit', [L, 2, 4]), ('ret_gn_g', [L, 256]),
                        ('ret_gn_b', [L, 256]), ('cos_mla', [32, T]), ('sin_mla', [32, T]), ('cos_swa', [64, T]),
                        ('sin_swa', [64, T]), ('cos_ret', [32, T]), ('sin_ret', [32, T]), ('rbig96', [96, 96]),
                        ('r128', [128, 128]), ('r64b', [64, 64]), ('swa_mask', [6, 128, 512]),
                        ('blk64', [128, 128]), ('ret_rel', [4, 128, 128]), ('ret_colc', [128, 16]),
                        ('ret_rowc', [64, 256]), ('ret_rowm', [64, 16]), ('hg_trie', [2, 128, 136]), ('hg_mt', [2, 128, 128]),
                        ('hg_e', [2, 128, 8])]:
            self.inp(nm, shp)
        if self.h_in:
            hin = self.inp('h_in', [128, 8 * T])
        out = nc.dram_tensor('out', [NLAT, D], F32, kind="ExternalOutput").ap()
        if self.h_out:
            hout = nc.dram_tensor('h_out', [128, 8 * T], F32, kind="ExternalOutput").ap()

        self.hT = hT = self.sb('hT', [128, 8, T], F32)
        self.aT = aT = self.sb('aT', [128, 8, T], BF16)
        self.ident = ident = self.sb('ident', [128, 128], F32)
        self.identb = identb = self.sb('identb', [128, 128], BF16)
        self.ones_bf = ones_bf = self.sb('ones_bf', [128, 128], BF16)
        self.stage = [self.ar('stage%d' % i, [128, 1024], F32).t for i in range(2)]
        self.modT = modT = self.sb('modT', [128, L, 48, 2], F32)
        self.gmix = gmix = self.sb('gmix', [128, L, 8], F32)
        self.gffn = gffn = self.sb('gffn', [128, L, 8], F32)
        self.gfin = gfin = self.sb('gfin', [128, 8], F32)
        self.A1 = A1 = self.sb('A1', [128, 8, 2], F32)
        self.rstd = rstd = self.sb('rstd', [128, 512], F32)
        self.sel = sel = self.sb('sel', [NEXP, NEXP * 128], F32)
        self.eps_col = self.sb('eps_col', [128, 1], F32)
        self.memset('dve', self.eps_col[:, :], EPS, ['eps_col'])
        self.gT = self.sb('gT', [NEXP, T], F32)

        P.dma('sp', ident[:, :], identd, writes=['ident'])
        P.dma('sp', sel[:, :], seld, writes=['sel'])
        self.cp('dve', identb[:, :], ident[:, :], ['ident'], ['identb'])
        self.memset('dve', ones_bf[:, :], 1.0, ['ones_bf'])

        if self.h_in:
            for c in range(8):
                P.dma('sp', hT[:, c, :], hin[:, c * T:(c + 1) * T], writes=[('hT', c, b) for b in range(5)])
        else:
            for t in range(NT):
                src = ctx[t * 128:(t + 1) * 128, :] if t < 2 else x[(t - 2) * 128:(t - 1) * 128, :]
                st = self.stage[t % 2]
                k = ('stage', t % 2)
                P.dma('sp', st[:, :], src, writes=[k])
                b = self.blk_of_tile(t)
                for half in range(2):
                    pt, pk = self.psum('tr', [0, 1])
                    for j in range(4):
                        c = half * 4 + j
                        self.tr(pt[:, j * 128:(j + 1) * 128], st[:, c * 128:(c + 1) * 128], ident[:, :], [k, 'ident'], pk)
                    self.cp('act' if half else 'dve', hT[:, half * 4:half * 4 + 4, t * 128:(t + 1) * 128],
                            pt[:, :].rearrange("p (j n) -> p j n", j=4), [pk],
                            [('hT', half * 4 + j, b) for j in range(4)])

        for l in range(L):
            self.rows_to_cols(nmg[l].rearrange("(c p) -> c p", p=128), 8, 128, gmix[:, l, :], 'gmix')
            self.rows_to_cols(nfg[l].rearrange("(c p) -> c p", p=128), 8, 128, gffn[:, l, :], 'gffn')
        self.rows_to_cols(fng.rearrange("(c p) -> c p", p=128), 8, 128, gfin[:, :], 'gfin')
        bada = self.sb('bada', [128, L, 48], F32)
        for l in range(L):
            self.rows_to_cols(b_ada[l].rearrange("(m p) -> m p", p=128), 48, 128, bada[:, l, :], 'bada')

        self.colv = colv = self.sb('colv', [128, L, 12], F32)
        for l in range(L):
            qg = self.din['mla_q_norm_g'][l]
            self.rows_to_cols(qg[0:128].rearrange("(o p) -> o p", o=1), 1, 128, colv[:, l, 0:1], 'colv')
            self.rows_to_cols(qg[128:192].rearrange("(o p) -> o p", o=1), 1, 64, colv[0:64, l, 1:2], 'colv')
            self.rows_to_cols(self.din['mla_kv_norm_g'][l].rearrange("(o p) -> o p", o=1), 1, 128, colv[:, l, 2:3], 'colv')
            self.rows_to_cols(self.din['hg_norm_g'][l].rearrange("(c p) -> c p", p=128), 2, 128, colv[:, l, 3:5], 'colv')
            self.rows_to_cols(self.din['ret_gn_g'][l].rearrange("(c p) -> c p", p=128), 2, 128, colv[:, l, 5:7], 'colv')
            self.rows_to_cols(self.din['ret_gn_b'][l].rearrange("(c p) -> c p", p=128), 2, 128, colv[:, l, 7:9], 'colv')

        c2s = self.ar('c2s', [2, D], F32)
        scT = self.sb('scT', [128, 8, 2], F32)
        P.dma('sp', c2s[:, :], c2, writes=['c2s'])
        self.act(c2s[:, :], c2s[:, :], AF.Silu, ['c2s'], ['c2s'])
        for c in range(8):
            pt, pk = self.psum('tr', [0, 1])
            self.tr(pt[:, 0:2], c2s[0:2, c * 128:(c + 1) * 128], ident[0:2, 0:2], ['c2s', 'ident'], pk)
            self.cp('dve', scT[:, c, :], pt[:, 0:2], [pk], ['scT'])
        NCB = 768
        wab = [self.ar('wab%d' % i, [128, 8, NCB], F32) for i in range(2)]
        it = 0
        for l in self.layers:
            for cb in range(6 * D // NCB):
                wt = wab[it % 2]
                it += 1
                P.dma('sp', wt[:, :, :], w_ada[l][:, cb * NCB:(cb + 1) * NCB].rearrange("(c p) n -> p c n", p=128),
                      writes=[wt.k])
                pt, pk = self.psum('tr', [0, 1])
                for m in range(NCB // 128):
                    self.mmg(pt[:, m * 2:(m + 1) * 2],
                             [(wt[:, kc, m * 128:(m + 1) * 128], scT[:, kc, :]) for kc in range(8)],
                             [wt.k, 'scT'], pk)
                nm = NCB // 128
                self.tt(modT[:, l, cb * nm:(cb + 1) * nm, :], pt[:, 0:2 * nm].rearrange("p (m r) -> p m r", r=2),
                        bada[:, l, cb * nm:(cb + 1) * nm].unsqueeze(2).broadcast_to([128, nm, 2]), ALU.add,
                        [pk, 'bada'], [('modT', l)])

        for l in self.layers:
            if len(self.mixers) > 0:
                self.phase()
                self.norm_mod(l, gmix, 0, 1, False)
                self.mixer_phase(l)
            if self.ffn:
                moe = (l % 2 == 1)
                self.phase()
                self.norm_mod(l, gffn, 3, 4, moe)
                if moe:
                    self.moe_route(l)
                self.phase()
                if moe:
                    self.moe_ffn(l)
                else:
                    self.dense_ffn(l)
        self.phase()

        if self.h_out:
            for c in range(8):
                P.dma('sp', hout[:, c * T:(c + 1) * T], hT[:, c, :], reads=[('hT', c, b) for b in range(5)],
                      is_output=True)
        self.final_out(out, gfin)
        P.emit()
        return nc

    def blk_of_tile(self, t):
        return 0 if t < 2 else 1 + (t - 2) // 4

    def norm_mod(self, l, gvec, i_shift, i_scale, want_f32):
        P = self.P
        hT, aT, modT, A1, rstd = self.hT, self.aT, self.modT, self.A1, self.rstd
        self.stt(A1[:, :, :], modT[:, l, i_scale * 8:(i_scale + 1) * 8, :], 1.0,
                 gvec[:, l, :].unsqueeze(2).broadcast_to([128, 8, 2]), ALU.add, ALU.mult,
                 [('modT', l), gvec.k], ['A1'])
        nsq = self.ar('nsq', [128, 8, 512], BF16)
        ntmp = self.ar('ntmp', [128, 8, 512], F32)
        if want_f32:
            self.a32 = self.ar('a32', [128, 8, 512], F32)
            self.wr = self.ar('wr', [128, 8, NEXP], F32)
            self.lgT = self.ar('lgT', [NEXP, T], F32)
            self.P.dma('sp', self.wr[:, :, :], self.mwr[l // 2].rearrange("(c p) e -> p c e", p=128), writes=[self.wr.k])
        for b, (t0, n) in enumerate(BLKS):
            r = 1 if b == 0 else 0
            hk = [('hT', c, b) for c in range(8)]
            self.act(nsq[:, :, 0:n], hT[:, :, t0:t0 + n], AF.Square, hk, [nsq.k])
            pt, pk = self.psum('nrm', [2, 3])
            self.mmg(pt[:, 0:n], [(self.ones_bf[:, :], nsq[:, c, 0:n]) for c in range(8)], [nsq.k, 'ones_bf'], pk)
            self.act(rstd[:, 0:n], pt[:, 0:n], AF.Sqrt, [pk], ['rstd'], bias=self.eps_col[:, 0:1], scale=1.0 / D)
            self.recip(rstd[:, 0:n], rstd[:, 0:n], ['rstd'], ['rstd'])
            self.tt(ntmp[:, :, 0:n], hT[:, :, t0:t0 + n], rstd[:, 0:n].unsqueeze(1).broadcast_to([128, 8, n]),
                    ALU.mult, hk + ['rstd'], [ntmp.k])
            for c in range(8):
                if want_f32:
                    self.ts(self.a32[:, c, 0:n], ntmp[:, c, 0:n], A1[:, c, r:r + 1],
                            modT[:, l, i_shift * 8 + c, r:r + 1], ALU.mult, ALU.add,
                            [ntmp.k, 'A1', ('modT', l)], [('a32', c)])
                    self.cp('act', aT[:, c, t0:t0 + n], self.a32[:, c, 0:n], [('a32', c)], [('aT', b)])
                else:
                    self.ts(aT[:, c, t0:t0 + n], ntmp[:, c, 0:n], A1[:, c, r:r + 1],
                            modT[:, l, i_shift * 8 + c, r:r + 1], ALU.mult, ALU.add,
                            [ntmp.k, 'A1', ('modT', l)], [('aT', b)])
            if want_f32:
                self.route_block(l, b, t0, n)

    def load_w(self, dst, dram, kchunks, ncols, key):
        step = max(1, 4096 // ncols)
        for k0 in range(0, kchunks, step):
            k1 = min(kchunks, k0 + step)
            self.P.dma('pool', dst[:, k0:k1, :], dram[k0 * 128:k1 * 128, :].rearrange("(c p) n -> p c n", p=128),
                       writes=[key])

    def ffn_slices(self, dff):
        s = []
        f = 0
        while f < dff:
            n = min(512, dff - f)
            s.append((f, n))
            f += n
        return s

    def ffn_core(self, l, wg_d, wu_d, wd_d, dff, gate_tile=None, tagbase=''):
        P = self.P
        hT, aT, modT = self.hT, self.aT, self.modT
        for (f0, fn) in self.ffn_slices(dff):
            wg, wu, wd = self.fw[self.fit % 2]
            self.fit += 1
            nj = fn // 128
            self.load_w(wg[:, :, 0:fn], wg_d[:, f0:f0 + fn], 8, fn, wg.k)
            self.load_w(wu[:, :, 0:fn], wu_d[:, f0:f0 + fn], 8, fn, wu.k)
            self.load_w(wd[:, 0:nj, :], wd_d[f0:f0 + fn, :], nj, 1024, wd.k)
            def gu(b, t0, n):
                hid = self.fhid[self.fh % 2]
                self.fh += 1
                for j in range(nj):
                    pg, pgk = self.psum('ffg', [0, 1])
                    pu, puk = self.psum('ffu', [2, 3])
                    self.mmg(pg[:, 0:n], [(wg[:, kc, j * 128:(j + 1) * 128], aT[:, kc, t0:t0 + n]) for kc in range(8)],
                             [wg.k, ('aT', b)], pgk)
                    self.mmg(pu[:, 0:n], [(wu[:, kc, j * 128:(j + 1) * 128], aT[:, kc, t0:t0 + n]) for kc in range(8)],
                             [wu.k, ('aT', b)], puk)
                    sg = self.fsg[self.fj % 2]
                    self.fj += 1
                    self.act(sg[:, 0:n], pg[:, 0:n], AF.Silu, [pgk], [sg.k])
                    if gate_tile is not None:
                        self.tt(sg[:, 0:n], sg[:, 0:n], gate_tile[:, t0:t0 + n], ALU.mult, [sg.k, gate_tile.k], [sg.k])
                    self.tt(hid[:, j, 0:n], pu[:, 0:n], sg[:, 0:n], ALU.mult, [puk, sg.k], [(hid.k, j)])
                return hid

            def down(b, t0, n, hid):
                r = 1 if b == 0 else 0
                for oc in range(8):
                    py, pyk = self.psum('ffy', [4, 5, 6, 7])
                    self.mmg(py[:, 0:n], [(wd[:, j, oc * 128:(oc + 1) * 128], hid[:, j, 0:n]) for j in range(nj)],
                             [wd.k] + [(hid.k, j) for j in range(nj)], pyk)
                    self.stt(hT[:, oc, t0:t0 + n], py[:, 0:n], modT[:, l, 5 * 8 + oc, r:r + 1], hT[:, oc, t0:t0 + n],
                             ALU.mult, ALU.add, [pyk, ('modT', l), ('hT', oc, b)], [('hT', oc, b)])

            prev = None
            for b, (t0, n) in enumerate(BLKS):
                hid = gu(b, t0, n)
                if prev is not None:
                    down(*prev)
                prev = (b, t0, n, hid)
            down(*prev)

    def ffn_alloc(self):
        self.fw = [(self.ar('fwg%d' % i, [128, 8, 512], BF16), self.ar('fwu%d' % i, [128, 8, 512], BF16),
                    self.ar('fwd%d' % i, [128, 4, 1024], BF16)) for i in range(2)]
        self.fsg = [self.ar('fsg%d' % i, [128, 512], BF16) for i in range(2)]
        self.fhid = [self.ar('fhid%d' % i, [128, 4, 512], BF16) for i in range(2)]
        self.fit = 0
        self.fj = 0
        self.fh = 0

    def dense_ffn(self, l):
        i = l // 2
        self.ffn_alloc()
        self.ffn_core(l, self.fwg[i], self.fwu[i], self.fwd[i], D_FF)

    def route_block(self, l, b, t0, n):
        pt, pk = self.psum('nrm', [2, 3])
        self.mmg(pt[0:NEXP, 0:n], [(self.wr[:, c, :], self.a32[:, c, 0:n]) for c in range(8)],
                 [self.wr.k] + [('a32', c) for c in range(8)], pk)
        self.cp('dve', self.lgT[:, t0:t0 + n], pt[0:NEXP, 0:n], [pk], ['lgT'])

    def moe_route(self, l):
        ident = self.ident
        lg = self.ar('lg', [128, NT, NEXP], F32)
        mk1 = self.ar('mk1', [128, NT, NEXP], F32)
        mk2 = self.ar('mk2', [128, NT, NEXP], F32)
        m1 = self.ar('m1', [128, NT], F32)
        m2 = self.ar('m2', [128, NT], F32)
        w1 = self.ar('w1', [128, NT], F32)
        gT = self.gT
        lg.k, mk1.k, mk2.k, m1.k, m2.k, w1.k = 'lg', 'mk1', 'mk2', 'm1', 'm2', 'w1'
        for t in range(NT):
            pt, pk = self.psum('tr', [0, 1])
            self.tr(pt[:, 0:NEXP], self.lgT[0:NEXP, t * 128:(t + 1) * 128], ident[0:NEXP, 0:NEXP], ['lgT', 'ident'], pk)
            self.cp('dve', lg[:, t, :], pt[:, 0:NEXP], [pk], ['lg'])
        P = self.P
        P.op('dve', lambda e: e.tensor_reduce(out=m1[:, :], in_=lg[:, :, :], axis=AX.X, op=ALU.max), reads=['lg'], writes=['m1'])
        self.tt(mk1[:, :, :], lg[:, :, :], m1[:, :].unsqueeze(2).broadcast_to([128, NT, NEXP]), ALU.is_equal,
                ['lg', 'm1'], ['mk1'])
        self.stt(mk2[:, :, :], mk1[:, :, :], -1e30, lg[:, :, :], ALU.mult, ALU.add, ['mk1', 'lg'], ['mk2'])
        P.op('dve', lambda e: e.tensor_reduce(out=m2[:, :], in_=mk2[:, :, :], axis=AX.X, op=ALU.max), reads=['mk2'], writes=['m2'])
        self.tt(mk2[:, :, :], mk2[:, :, :], m2[:, :].unsqueeze(2).broadcast_to([128, NT, NEXP]), ALU.is_equal,
                ['mk2', 'm2'], ['mk2'])
        self.tt(w1[:, :], m1[:, :], m2[:, :], ALU.subtract, ['m1', 'm2'], ['w1'])
        self.act(w1[:, :], w1[:, :], AF.Sigmoid, ['w1'], ['w1'])
        self.tt(mk1[:, :, :], mk1[:, :, :], w1[:, :].unsqueeze(2).broadcast_to([128, NT, NEXP]), ALU.mult,
                ['mk1', 'w1'], ['mk1'])
        self.ts(w1[:, :], w1[:, :], -1.0, 1.0, ALU.mult, ALU.add, ['w1'], ['w1'])
        self.tt(mk2[:, :, :], mk2[:, :, :], w1[:, :].unsqueeze(2).broadcast_to([128, NT, NEXP]), ALU.mult,
                ['mk2', 'w1'], ['mk2'])
        self.tt(mk1[:, :, :], mk1[:, :, :], mk2[:, :, :], ALU.add, ['mk1', 'mk2'], ['mk1'])
        for t in range(NT):
            pt, pk = self.psum('tr', [0, 1])
            self.tr(pt[0:NEXP, 0:128], mk1[:, t, :], ident[:, :], ['mk1', 'ident'], pk)
            self.cp('dve', gT[:, t * 128:(t + 1) * 128], pt[0:NEXP, 0:128], [pk], ['gT'])

    def moe_ffn(self, l):
        i = l // 2
        gT = self.gT
        gbc = [self.ar('gbc%d' % j, [128, T], BF16) for j in range(2)]
        self.ffn_alloc()
        for e_ in range(NEXP):
            gb = gbc[e_ % 2]
            for b, (t0, n) in enumerate(BLKS):
                pt, pk = self.psum('nrm', [2, 3])
                self.mmg(pt[:, 0:n], [(self.sel[:, e_ * 128:(e_ + 1) * 128], gT[:, t0:t0 + n])], ['sel', 'gT'], pk)
                self.cp('act', gb[:, t0:t0 + n], pt[:, 0:n], [pk], [gb.k])
            self.ffn_core(l, self.mwg[i][e_], self.mwu[i][e_], self.mwd[i][e_], D_FFE, gate_tile=gb)


    def mixer_phase(self, l):
        if 0 in self.mixers:
            self.phase()
            self.mla(l)
        if 2 in self.mixers:
            self.phase()
            self.swa(l)
        if 3 in self.mixers:
            self.phase()
            self.ret(l)
        if 1 in self.mixers:
            self.phase()
            self.hgrn(l)

    def colvec(self, dst_ap, dram_vec, n, key):
        self.P.dma('sp', dst_ap, dram_vec.rearrange("(p o) -> p o", o=1), writes=[key])

    def proj(self, out, w, wcols, rhs_fn, reads, pk, kchunks=8):
        c0, c1 = wcols
        self.mmg(out, [(w[:, kc, c0:c1], rhs_fn(kc)) for kc in range(kchunks)], reads, pk)

    def rstd_from(self, dst, src_ps, n_feat, reads, writes):
        p = dst.shape[0] if hasattr(dst, 'shape') else 128
        self.act(dst, src_ps, AF.Sqrt, reads, writes, bias=self.eps_col[0:p, 0:1], scale=1.0 / n_feat)
        self.recip(dst, dst, writes, writes)

    def outproj(self, l, wo, o_blk, okeys, b, t0, n):
        r = 1 if b == 0 else 0
        hT, modT = self.hT, self.modT
        for oc in range(8):
            py, pyk = self.psum('ffy', [4, 5, 6, 7])
            self.mmg(py[:, 0:n], [(wo[:, p, oc * 128:(oc + 1) * 128], o_blk[:, p, 0:n]) for p in range(2)],
                     [wo.k] + okeys, pyk)
            self.stt(hT[:, oc, t0:t0 + n], py[:, 0:n], modT[:, l, 2 * 8 + oc, r:r + 1], hT[:, oc, t0:t0 + n],
                     ALU.mult, ALU.add, [pyk, ('modT', l), ('hT', oc, b)], [('hT', oc, b)])

    def load_wo(self, l, grp):
        wo = self.ar('wo', [128, 2, 1024], BF16)
        self.load_w(wo[:, :, :], self.w_out[l][grp * 256:(grp + 1) * 256, :], 2, 1024, wo.k)
        return wo

    def attn(self, spairs, vfn, tiles, scale, n, half, out_ap, out_key, reads, sink_col=None):
        P = self.P
        pO, pOk = self.psum('aO', [4, 6])
        pD, pDk = self.psum('aD', [5, 7])
        nt = len(tiles)

        def smm(i):
            pS, pSk = self.psum('aS', [0, 1])
            self.mmg(pS[:, 0:n], spairs(tiles[i][0]), reads, pSk)
            return pS, pSk

        nxt = smm(0)
        for i, (tile, mask) in enumerate(tiles):
            pS, pSk = nxt
            if i + 1 < nt:
                nxt = smm(i + 1)
            pt = self.PT[self.pti % 3]
            self.pti += 1
            self.act(pt[:, 0:n], pS[:, 0:n], AF.Exp, [pSk], [pt.k], scale=scale)
            if mask is not None:
                self.tt(pt[:, 0:n], pt[:, 0:n], mask[:, 0:n], ALU.mult, [pt.k, 'swamask'], [pt.k])
            v = vfn(tile)
            P.op('pe', lambda e, v=v, pt=pt, s=(i == 0), t=(i == nt - 1): e.matmul(pO[:, 0:n], lhsT=v, rhs=pt[:, 0:n], start=s, stop=t),
                 reads=reads + [pt.k], writes=[pOk], inc=False)
            P.op('pe', lambda e, pt=pt, s=(i == 0), t=(i == nt - 1): e.matmul(pD[:, 0:n], lhsT=self.ones_bf[:, :], rhs=pt[:, 0:n], start=s, stop=t),
                 reads=[pt.k, 'ones_bf'], writes=[pOk, pDk], inc=True)
        r0 = half * 64
        rd = self.rd
        if sink_col is not None:
            self.ts(rd[r0:r0 + 64, 0:n], pD[r0:r0 + 64, 0:n], sink_col[r0:r0 + 64, :], None, ALU.add, None, [pDk, 'esink'], ['rd'])
            self.recip(rd[r0:r0 + 64, 0:n], rd[r0:r0 + 64, 0:n], ['rd'], ['rd'])
        else:
            self.recip(rd[r0:r0 + 64, 0:n], pD[r0:r0 + 64, 0:n], [pDk], ['rd'])
        self.tt(out_ap, pO[r0:r0 + 64, 0:n], rd[r0:r0 + 64, 0:n], ALU.mult, [pOk, 'rd'], [out_key])

    def mla(self, l):
        P = self.P
        aT = self.aT
        wm = self.ar('wm', [128, 8, 352], BF16)
        self.load_w(wm[:, :, :], self.w_in[l][:, 0:352], 8, 352, wm.k)
        wuq = self.ar('wuq', [128, 2, 384], BF16)
        P.dma('pool', wuq[:, 0, :], self.din['mla_w_uq'][l][0:128, :], writes=[wuq.k])
        P.dma('pool', wuq[0:64, 1, :], self.din['mla_w_uq'][l][128:192, :], writes=[wuq.k])
        wukv = self.ar('wukv', [128, 512], BF16)
        P.dma('pool', wukv[:, :], self.din['mla_w_ukv'][l], writes=[wukv.k])
        cv = self.colv
        self.ts(wuq[:, 0, :], wuq[:, 0, :], cv[:, l, 0:1], None, ALU.mult, None, [wuq.k, 'colv'], [wuq.k])
        self.ts(wuq[0:64, 1, :], wuq[0:64, 1, :], cv[0:64, l, 1:2], None, ALU.mult, None, [wuq.k, 'colv'], [wuq.k])
        self.ts(wukv[:, :], wukv[:, :], cv[:, l, 2:3], None, ALU.mult, None, [wukv.k, 'colv'], [wukv.k])
        r32 = self.ar('r32', [32, 32], BF16)
        P.dma('pool', r32[:, :], self.din['r64b'][0:32, 0:32], writes=[r32.k])
        wo = self.load_wo(l, 0)
        kN = self.ar('kN', [64, 4, T], BF16)
        kP = self.ar('kP', [32, T], BF16)
        vt = self.ar('vt', [128, NT, 256], BF16)
        cs = self.ar('cs', [32, 2, 512], F32)
        kvl = self.ar('kvl', [128, 512], BF16)
        sq = self.ar('sq', [128, 2, 512], BF16)
        rs = self.ar('rs', [128, 512], F32)
        rsc = self.ar('rsc', [128, NT], F32)
        xb = self.ar('xb', [32, 512], BF16)
        t1 = self.ar('t1', [32, 512], F32)
        t2 = self.ar('t2', [32, 512], F32)
        qlb = self.ar('qlb', [128, 2, 512], BF16)
        qp32 = self.ar('qp32', [32, 512], F32)
        qn = [self.ar('qn%d' % i, [64, 512], BF16) for i in range(2)]
        qp = [self.ar('qp%d' % i, [32, 512], BF16) for i in range(2)]
        oblk = [self.ar('oblk%d' % i, [128, 2, 512], BF16) for i in range(2)]
        self.PT = [self.ar('PT%d' % i, [128, 512], BF16) for i in range(3)]
        self.pti = 0
        self.rd = self.ar('rd', [128, 512], F32)
        self.rd.k = 'rd'
        cosd, sind = self.din['cos_mla'], self.din['sin_mla']

        def rope32(dst, src, skey, n, wkey):
            self.cp('act', xb[:, 0:n], src, [skey], [xb.k])
            pr, prk = self.psum('mB', [2, 3])
            self.mmg(pr[0:32, 0:n], [(r32[:, :], xb[:, 0:n])], [r32.k, xb.k], prk)
            RV = int(os.environ.get('ROPEV', '9'))
            if RV == 1:
                self.tt(t1[:, 0:n], src, src, ALU.mult, [skey], [t1.k])
                return
            if RV == 5:
                return
            if RV == 3:
                self.tt(t1[:, 0:n], rs[0:32, 0:n], rs[0:32, 0:n], ALU.mult, [rs.k], [t1.k])
                return
            if RV == 4:
                self.tt(rs[0:32, 0:n], cs[:, 0, 0:n], cs[:, 0, 0:n], ALU.mult, [cs.k, rs.k], [rs.k])
                return
            if RV == 2:
                self.tt(t1[:, 0:n], cs[:, 0, 0:n], cs[:, 0, 0:n], ALU.mult, [cs.k], [t1.k])
                return
            self.tt(t1[:, 0:n], src, cs[:, 0, 0:n], ALU.mult, [skey, cs.k, xb.k], [t1.k])
            self.tt(t2[:, 0:n], pr[0:32, 0:n], cs[:, 1, 0:n], ALU.mult, [prk, cs.k], [t2.k])
            self.tt(dst, t1[:, 0:n], t2[:, 0:n], ALU.add, [t1.k, t2.k], [wkey])

        for b, (t0, n) in enumerate(BLKS):
            ak = ('aT', b)
            P.dma('sp', cs[:, 0, 0:n], cosd[:, t0:t0 + n], writes=[cs.k])
            P.dma('sp', cs[:, 1, 0:n], sind[:, t0:t0 + n], writes=[cs.k])
            pk_, pkk = self.psum('mA', [0, 1])
            self.proj(pk_[:, 0:n], wm, (192, 320), lambda kc: aT[:, kc, t0:t0 + n], [wm.k, ak], pkk)
            self.cp('act', kvl[:, 0:n], pk_[:, 0:n], [pkk], [kvl.k])
            self.act(sq[:, 0, 0:n], pk_[:, 0:n], AF.Square, [pkk], [sq.k])
            ps_, psk = self.psum('mB', [2, 3])
            self.mmg(ps_[:, 0:n], [(self.ones_bf[:, :], sq[:, 0, 0:n])], [sq.k, 'ones_bf'], psk)
            self.rstd_from(rs[:, 0:n], ps_[:, 0:n], 128.0, [psk], [rs.k])
            pc, pck = self.psum('mB', [2, 3])
            for tt_ in range(n // 128):
                self.mmg(pc[:, tt_:tt_ + 1], [(sq[:, 0, tt_ * 128:(tt_ + 1) * 128], self.ones_bf[:, 0:1])], [sq.k, 'ones_bf'], pck)
            tl0 = t0 // 128
            self.rstd_from(rsc[:, tl0:tl0 + n // 128], pc[:, 0:n // 128], 128.0, [pck], [rsc.k])
            for tt_ in range(n // 128):
                tile = t0 // 128 + tt_
                pv, pvk = self.psum('mA', [0, 1])
                for h in range(4):
                    self.mmg(pv[:, h * 64:(h + 1) * 64], [(kvl[:, tt_ * 128:(tt_ + 1) * 128], wukv[:, h * 128 + 64:h * 128 + 128])],
                             [kvl.k, wukv.k], pvk)
                self.ts(vt[:, tile, :], pv[:, 0:256], rsc[:, tile:tile + 1], None, ALU.mult, None, [pvk, rsc.k], [vt.k])
            for h in range(4):
                pk2, pk2k = self.psum('mA', [0, 1])
                self.mmg(pk2[0:64, 0:n], [(wukv[:, h * 128:h * 128 + 64], kvl[:, 0:n])], [kvl.k, wukv.k], pk2k)
                self.tt(kN[:, h, t0:t0 + n], pk2[0:64, 0:n], rs[0:64, 0:n], ALU.mult, [pk2k, rs.k], [kN.k])
            pp, ppk = self.psum('mA', [0, 1])
            self.proj(pp[0:32, 0:n], wm, (320, 352), lambda kc: aT[:, kc, t0:t0 + n], [wm.k, ak], ppk)
            rope32(kP[:, t0:t0 + n], pp[0:32, 0:n], ppk, n, kP.k)
        STOP = int(os.environ.get('MLA_STOP', '9'))
        if STOP <= 2:
            return
        scale = 96.0 ** -0.5
        qi = 0
        for b, (t0, n) in enumerate(BLKS):
            ak = ('aT', b)
            P.dma('sp', cs[:, 0, 0:n], cosd[:, t0:t0 + n], writes=[cs.k])
            P.dma('sp', cs[:, 1, 0:n], sind[:, t0:t0 + n], writes=[cs.k])
            p0, p0k = self.psum('mA', [0, 1])
            self.proj(p0[:, 0:n], wm, (0, 128), lambda kc: aT[:, kc, t0:t0 + n], [wm.k, ak], p0k)
            p1, p1k = self.psum('mA', [0, 1])
            self.proj(p1[0:64, 0:n], wm, (128, 192), lambda kc: aT[:, kc, t0:t0 + n], [wm.k, ak], p1k)
            self.cp('act', qlb[:, 0, 0:n], p0[:, 0:n], [p0k], [qlb.k])
            self.cp('act', qlb[0:64, 1, 0:n], p1[0:64, 0:n], [p1k], [qlb.k])
            self.act(sq[:, 0, 0:n], p0[:, 0:n], AF.Square, [p0k], [sq.k])
            self.act(sq[0:64, 1, 0:n], p1[0:64, 0:n], AF.Square, [p1k], [sq.k])
            ps_, psk = self.psum('mB', [2, 3])
            self.mmg(ps_[:, 0:n], [(self.ones_bf[:, :], sq[:, 0, 0:n]), (self.ones_bf[0:64, :], sq[0:64, 1, 0:n])],
                     [sq.k, 'ones_bf'], psk)
            self.rstd_from(rs[:, 0:n], ps_[:, 0:n], 192.0, [psk], [rs.k])
            ob = oblk[b % 2]
            tiles = [(0, None), (1, None)] if b == 0 else [(t, None) for t in range(NT)]
            def qprep(h, n=n):
                nonlocal qi
                qn_, qp_ = qn[qi % 2], qp[qi % 2]
                qi += 1
                pq, pqk = self.psum('mB', [2, 3])
                self.mmg(pq[0:64, 0:n], [(wuq[:, 0, h * 96:h * 96 + 64], qlb[:, 0, 0:n]),
                                         (wuq[0:64, 1, h * 96:h * 96 + 64], qlb[0:64, 1, 0:n])], [wuq.k, qlb.k], pqk)
                self.tt(qn_[:, 0:n], pq[0:64, 0:n], rs[0:64, 0:n], ALU.mult, [pqk, rs.k], [qn_.k])
                pq2, pq2k = self.psum('mB', [2, 3])
                self.mmg(pq2[0:32, 0:n], [(wuq[:, 0, h * 96 + 64:h * 96 + 96], qlb[:, 0, 0:n]),
                                          (wuq[0:64, 1, h * 96 + 64:h * 96 + 96], qlb[0:64, 1, 0:n])], [wuq.k, qlb.k], pq2k)
                self.tt(qp32[:, 0:n], pq2[0:32, 0:n], rs[0:32, 0:n], ALU.mult, [pq2k, rs.k], [qp32.k])
                rope32(qp_[:, 0:n], qp32[:, 0:n], qp32.k, n, qp_.k)
                return qn_, qp_

            nxtq = qprep(0)
            for h in range(4):
                qn_, qp_ = nxtq
                if h + 1 < 4:
                    nxtq = qprep(h + 1)
                p_, hh = h // 2, h % 2
                self.attn(lambda t, h=h, qn_=qn_, qp_=qp_, n=n: [(kN[:, h, t * 128:(t + 1) * 128], qn_[:, 0:n]),
                                                                 (kP[:, t * 128:(t + 1) * 128], qp_[:, 0:n])],
                          lambda t, p_=p_: vt[:, t, p_ * 128:(p_ + 1) * 128], tiles, scale, n, hh,
                          ob[hh * 64:hh * 64 + 64, p_, 0:n], (ob.k, h), [kN.k, kP.k, vt.k, qn_.k, qp_.k])
            if STOP <= 5:
                continue
            self.outproj(l, wo, ob, [(ob.k, h) for h in range(4)], b, t0, n)

    def swa(self, l):
        P = self.P
        aT = self.aT
        ws = self.ar('ws', [128, 8, 512], BF16)
        self.load_w(ws[:, :, :], self.w_in[l][:, C_SQ:C_SQ + 512], 8, 512, ws.k)
        r128 = self.ar('r128', [128, 128], BF16)
        P.dma('pool', r128[:, :], self.din['r128'], writes=[r128.k])
        wo = self.load_wo(l, 2)
        msk = self.ar('swamask', [128, 6, 512], BF16)
        msk.k = 'swamask'
        for r in range(6):
            P.dma('pool', msk[:, r, :], self.din['swa_mask'][r], writes=['swamask'])
        esink = self.ar('esink', [128, 4], F32)
        esink.k = 'esink'
        P.dma('sp', esink[:, :], self.din['swa_sink'][l:l + 1, :].broadcast_to([128, 4]), writes=['esink'])
        self.act(esink[:, :], esink[:, :], AF.Exp, ['esink'], ['esink'])
        kx = self.ar('kx', [128, T], BF16)
        kxs = self.ar('kxs', [128, T], BF16)
        vn = self.ar('vn', [128, NT, 128], BF16)
        vs = self.ar('vs', [128, NT, 128], BF16)
        cs = self.ar('cs', [128, 2, 512], F32)
        xb = self.ar('xb', [128, 512], BF16)
        t1 = self.ar('t1', [128, 512], F32)
        t2 = self.ar('t2', [128, 512], F32)
        qx = [self.ar('qx%d' % i, [128, 2, 512], BF16) for i in range(2)]
        oblk = [self.ar('oblk%d' % i, [128, 2, 512], BF16) for i in range(2)]
        self.PT = [self.ar('PT%d' % i, [128, 512], BF16) for i in range(3)]
        self.pti = 0
        self.rd = self.ar('rd', [128, 512], F32)
        self.rd.k = 'rd'
        cosd, sind = self.din['cos_swa'], self.din['sin_swa']

        def rope(dst, ps_x, psk, n, wkey):
            self.cp('act', xb[:, 0:n], ps_x[:, 0:n], [psk], [xb.k])
            pr, prk = self.psum('mB', [2, 3])
            self.mmg(pr[:, 0:n], [(r128[:, :], xb[:, 0:n])], [r128.k, xb.k], prk)
            self.tt(t1[:, 0:n], ps_x[:, 0:n], cs[:, 0, 0:n], ALU.mult, [psk, cs.k], [t1.k])
            self.tt(t2[:, 0:n], pr[:, 0:n], cs[:, 1, 0:n], ALU.mult, [prk, cs.k], [t2.k])
            self.tt(dst, t1[:, 0:n], t2[:, 0:n], ALU.add, [t1.k, t2.k], [wkey])

        for b, (t0, n) in enumerate(BLKS):
            ak = ('aT', b)
            for hf in range(2):
                P.dma('sp', cs[hf * 64:(hf + 1) * 64, 0, 0:n], cosd[:, t0:t0 + n], writes=[cs.k])
                P.dma('sp', cs[hf * 64:(hf + 1) * 64, 1, 0:n], sind[:, t0:t0 + n], writes=[cs.k])
            pk_, pkk = self.psum('mA', [0, 1])
            self.proj(pk_[:, 0:n], ws, (256, 384), lambda kc: aT[:, kc, t0:t0 + n], [ws.k, ak], pkk)
            rope(kx[:, t0:t0 + n], pk_, pkk, n, kx.k)
            for tt_ in range(n // 128):
                tile = t0 // 128 + tt_
                pv, pvk = self.psum('mA', [0, 1])
                self.mmg(pv[:, 0:128], [(aT[:, kc, tile * 128:(tile + 1) * 128], ws[:, kc, 384:512]) for kc in range(8)],
                         [ws.k, ak], pvk)
                self.cp('act', vn[:, tile, :], pv[:, 0:128], [pvk], [vn.k])
                self.cp('dve', vs[:, tile, 0:64], pv[:, 64:128], [pvk], [vs.k])
                self.cp('dve', vs[:, tile, 64:128], pv[:, 0:64], [pvk], [vs.k])
        P.dma('sp', kxs[0:64, :], kx[64:128, :], reads=[kx.k], writes=[kxs.k])
        P.dma('sp', kxs[64:128, :], kx[0:64, :], reads=[kx.k], writes=[kxs.k])
        scale = 64.0 ** -0.5
        for b, (t0, n) in enumerate(BLKS):
            ak = ('aT', b)
            for hf in range(2):
                P.dma('sp', cs[hf * 64:(hf + 1) * 64, 0, 0:n], cosd[:, t0:t0 + n], writes=[cs.k])
                P.dma('sp', cs[hf * 64:(hf + 1) * 64, 1, 0:n], sind[:, t0:t0 + n], writes=[cs.k])
            q_ = qx[b % 2]
            for p_ in range(2):
                pq, pqk = self.psum('mA', [0, 1])
                self.proj(pq[:, 0:n], ws, (p_ * 128, (p_ + 1) * 128), lambda kc: aT[:, kc, t0:t0 + n], [ws.k, ak], pqk)
                rope(q_[:, p_, 0:n], pq, pqk, n, q_.k)
            ob = oblk[b % 2]
            if b == 0:
                tiles = [(0, None), (1, None)]
            else:
                g0 = t0 // 128
                tiles = [(0, None), (1, None)]
                for r in range(-1, 5):
                    j = g0 + r
                    if 2 <= j <= 17:
                        tiles.append((j, msk[:, r + 1, :]))
            for h in range(4):
                g, hh, p_ = h // 2, h % 2, h // 2
                ksrc = kx if g == hh else kxs
                vsrc = vn if g == hh else vs
                self.attn(lambda t, ksrc=ksrc, hh=hh, q_=q_, p_=p_, n=n: [(ksrc[hh * 64:hh * 64 + 64, t * 128:(t + 1) * 128],
                                                                               q_[hh * 64:hh * 64 + 64, p_, 0:n])],
                          lambda t, vsrc=vsrc: vsrc[:, t, :], tiles, scale, n, hh,
                          ob[hh * 64:hh * 64 + 64, p_, 0:n], (ob.k, h), [kx.k, kxs.k, vn.k, vs.k, q_.k],
                          sink_col=esink[:, h:h + 1])
            self.outproj(l, wo, ob, [(ob.k, h) for h in range(4)], b, t0, n)

    def headnorm_gate(self, l, o32, okey, n, t0, b, w, gcols, center, gcol, bcol, oblk, ak):
        aT = self.aT
        ob16 = self.hn_b16
        c32 = self.hn_c32
        blk64 = self.blk64
        for p in range(2):
            src = o32[:, p, 0:n]
            if center:
                self.cp('act', ob16[:, 0:n], src, [okey], [ob16.k])
                pm, pmk = self.psum('mB', [2, 3])
                self.mmg(pm[:, 0:n], [(blk64[:, :], ob16[:, 0:n])], [ob16.k, blk64.k], pmk)
                self.stt(c32[:, 0:n], pm[:, 0:n], -1.0, src, ALU.mult, ALU.add, [okey, pmk], [c32.k])
                cs_ = c32[:, 0:n]
                ck = c32.k
            else:
                cs_ = src
                ck = okey
            self.act(ob16[:, 0:n], cs_, AF.Square, [ck], [ob16.k])
            pv_, pvk = self.psum('mB', [2, 3])
            self.mmg(pv_[:, 0:n], [(blk64[:, :], ob16[:, 0:n])], [ob16.k, blk64.k], pvk)
            rs = self.hn_rs
            self.rstd_from(rs[:, 0:n], pv_[:, 0:n], 1.0, [pvk], [rs.k])
            self.tt(c32[:, 0:n], cs_, rs[:, 0:n], ALU.mult, [ck, rs.k], [c32.k])
            if bcol is not None:
                self.ts(c32[:, 0:n], c32[:, 0:n], self.colv[:, l, gcol + p:gcol + p + 1],
                        self.colv[:, l, bcol + p:bcol + p + 1], ALU.mult, ALU.add, [c32.k, 'colv'], [c32.k])
            else:
                self.ts(c32[:, 0:n], c32[:, 0:n], self.colv[:, l, gcol + p:gcol + p + 1], None, ALU.mult, None,
                        [c32.k, 'colv'], [c32.k])
            pg, pgk = self.psum('mA', [0, 1])
            self.proj(pg[:, 0:n], w, (gcols + p * 128, gcols + (p + 1) * 128), lambda kc: aT[:, kc, t0:t0 + n], [w.k, ak], pgk)
            sg = self.hn_sg
            self.act(sg[:, 0:n], pg[:, 0:n], AF.Silu, [pgk], [sg.k])
            self.tt(oblk[:, p, 0:n], c32[:, 0:n], sg[:, 0:n], ALU.mult, [c32.k, sg.k], [(oblk.k, p)])

    def hn_alloc(self):
        self.hn_b16 = self.ar('hn_b16', [128, 512], BF16)
        self.hn_c32 = self.ar('hn_c32', [128, 512], F32)
        self.hn_rs = self.ar('hn_rs', [128, 512], F32)
        self.hn_sg = self.ar('hn_sg', [128, 512], BF16)
        self.blk64 = self.ar('blk64', [128, 128], BF16)
        self.P.dma('pool', self.blk64[:, :], self.din['blk64'], writes=[self.blk64.k])

    def ret(self, l):
        P = self.P
        aT = self.aT
        ident = self.ident
        r64b = self.ar('r64b', [64, 64], BF16)
        P.dma('pool', r64b[:, :], self.din['r64b'], writes=[r64b.k])
        wo = self.load_wo(l, 3)
        qx = self.ar('qx', [64, 2, T], BF16)
        kx = self.ar('kx', [64, 2, T], BF16)
        kxt = self.ar('kxt', [128, NT, 128], BF16)
        vt = self.ar('vt', [128, NT, 256], BF16)
        Sf = self.ar('Sf', [64, NT, 2, 128], BF16)
        Dsum = self.ar('Dsum', [128, 4, 128], BF16)
        zeta = self.ar('zeta', [128, 2, 4], F32)
        lgcol = self.ar('lgcol', [64, 2, 2], F32)
        xi = self.ar('xi', [64, 2, 2, 128], F32)
        dc = self.ar('dc', [64, 2, 2], F32)
        mark0 = self.ar_off
        w = self.ar('wr_', [128, 8, 512], BF16)
        self.load_w(w[:, :, :], self.w_in[l][:, C_RQ:C_RQ + 512], 8, 512, w.k)
        mark1 = self.ar_off
        lgb = self.ar('lgb', [128, 8], F32)
        rel = self.ar('rel', [128, 4, 128], F32)
        colc = self.ar('colc', [128, 16], F32)
        rowc = self.ar('rowc', [64, 2, 128], F32)
        dtmp = self.ar('dtmp', [128, 2, 128], F32)
        cosd, sind = self.din['cos_ret'], self.din['sin_ret']
        P.dma('sp', lgb[:, :], self.din['ret_decay_logit'][l:l + 1].rearrange("o d h -> o (d h)").broadcast_to([128, 8]),
              writes=[lgb.k])
        P.dma('sp', rel[:, :, :], self.din['ret_rel'].rearrange("r s t -> s r t"), writes=[rel.k])
        P.dma('sp', colc[:, :], self.din['ret_colc'], writes=[colc.k])
        P.dma('sp', rowc[:, :, :], self.din['ret_rowc'].rearrange("p (d t) -> p d t", d=2), writes=[rowc.k])
        self.act(lgb[:, :], lgb[:, :], AF.Exp, [lgb.k], [lgb.k], scale=-1.0)
        self.ts(lgb[:, :], lgb[:, :], 1.0, None, ALU.add, None, [lgb.k], [lgb.k])
        self.act(lgb[:, :], lgb[:, :], AF.Ln, [lgb.k], [lgb.k])
        self.ts(lgb[:, :], lgb[:, :], -1.0, None, ALU.mult, None, [lgb.k], [lgb.k])
        for h in range(4):
            self.act(dtmp[:, 0, :], rel[:, 0, :], AF.Exp, [rel.k, lgb.k], [dtmp.k], scale=lgb[:, h:h + 1])
            self.tt(dtmp[:, 0, :], dtmp[:, 0, :], rel[:, 1, :], ALU.mult, [dtmp.k, rel.k], [dtmp.k])
            self.act(dtmp[:, 1, :], rel[:, 2, :], AF.Exp, [rel.k, lgb.k], [dtmp.k], scale=lgb[:, 4 + h:5 + h])
            self.tt(dtmp[:, 1, :], dtmp[:, 1, :], rel[:, 3, :], ALU.mult, [dtmp.k, rel.k], [dtmp.k])
            self.tt(Dsum[:, h, :], dtmp[:, 0, :], dtmp[:, 1, :], ALU.add, [dtmp.k], [Dsum.k])
        for d in range(2):
            self.act(zeta[:, d, :], lgb[:, d * 4:d * 4 + 4], AF.Exp, [lgb.k, colc.k], [zeta.k], scale=colc[:, d:d + 1])
            for p in range(2):
                self.cp('dve', lgcol[0:32, d, p:p + 1], lgb[0:32, d * 4 + 2 * p:d * 4 + 2 * p + 1], [lgb.k], [lgcol.k])
                self.cp('dve', lgcol[32:64, d, p:p + 1], lgb[32:64, d * 4 + 2 * p + 1:d * 4 + 2 * p + 2], [lgb.k], [lgcol.k])
        for d in range(2):
            for p in range(2):
                self.act(xi[:, d, p, :], rowc[:, d, :], AF.Exp, [rowc.k, lgcol.k], [xi.k], scale=lgcol[:, d, p:p + 1])
        self.act(dc[:, :, :], lgcol[:, :, :], AF.Exp, [lgcol.k], [dc.k], scale=128.0)
        RSTOP = int(os.environ.get('RET_STOP', '9'))
        if RSTOP <= 1:
            return
        self.ar_release(mark1)
        cs = self.ar('cs', [64, 2, 512], F32)
        xb = self.ar('xb', [64, 512], BF16)
        t1 = self.ar('t1', [64, 512], F32)
        t2 = self.ar('t2', [64, 512], F32)

        def rope64(dst, ps_x, psk, n, wkey):
            self.cp('act', xb[:, 0:n], ps_x, [psk], [xb.k])
            pr, prk = self.psum('mB', [2, 3])
            self.mmg(pr[0:64, 0:n], [(r64b[:, :], xb[:, 0:n])], [r64b.k, xb.k], prk)
            self.tt(t1[:, 0:n], ps_x, cs[:, 0, 0:n], ALU.mult, [psk, cs.k], [t1.k])
            self.tt(t2[:, 0:n], pr[0:64, 0:n], cs[:, 1, 0:n], ALU.mult, [prk, cs.k], [t2.k])
            self.tt(t1[:, 0:n], t1[:, 0:n], t2[:, 0:n], ALU.add, [t1.k, t2.k], [t1.k])
            self.cp('act', dst, t1[:, 0:n], [t1.k], [wkey])

        for b, (t0, n) in enumerate(BLKS):
            ak = ('aT', b)
            for hf in range(2):
                P.dma('sp', cs[hf * 32:(hf + 1) * 32, 0, 0:n], cosd[:, t0:t0 + n], writes=[cs.k])
                P.dma('sp', cs[hf * 32:(hf + 1) * 32, 1, 0:n], sind[:, t0:t0 + n], writes=[cs.k])
            for p in range(2):
                pq, pqk = self.psum('mA', [0, 1])
                self.proj(pq[0:64, 0:n], w, (p * 64, (p + 1) * 64), lambda kc: aT[:, kc, t0:t0 + n], [w.k, ak], pqk)
                rope64(qx[:, p, t0:t0 + n], pq[0:64, 0:n], pqk, n, qx.k)
                pk_, pkk = self.psum('mA', [0, 1])
                self.proj(pk_[0:64, 0:n], w, (128 + p * 64, 128 + (p + 1) * 64), lambda kc: aT[:, kc, t0:t0 + n], [w.k, ak], pkk)
                rope64(kx[:, p, t0:t0 + n], pk_[0:64, 0:n], pkk, n, kx.k)
                for tt_ in range(n // 128):
                    tile = t0 // 128 + tt_
                    ptr, ptk = self.psum('mB', [2, 3])
                    self.tr(ptr[:, 0:64], t1[:, tt_ * 128:(tt_ + 1) * 128], ident[0:64, 0:64], [t1.k, 'ident'], ptk)
                    self.cp('act', kxt[:, tile, p * 64:(p + 1) * 64], ptr[:, 0:64], [ptk], [kxt.k])
            for tt_ in range(n // 128):
                tile = t0 // 128 + tt_
                pv, pvk = self.psum('mA', [0, 1])
                self.mmg(pv[:, 0:256], [(aT[:, kc, tile * 128:(tile + 1) * 128], w[:, kc, 256:512]) for kc in range(8)],
                         [w.k, ak], pvk)
                self.cp('act', vt[:, tile, :], pv[:, 0:256], [pvk], [vt.k])

        if RSTOP <= 2:
            return
        self.ar_release(mark0)
        w = self.ar('wg_', [128, 8, 256], BF16)
        self.load_w(w[:, :, :], self.w_in[l][:, C_RG:C_RG + 256], 8, 256, w.k)
        self.hn_alloc()
        S = self.ar('S', [64, 2, 128], F32)
        Sbb = self.ar('Sbb', [64, 2, 128], BF16)
        vz = self.ar('vz', [128, 256], BF16)
        qxm = self.ar('qxm', [64, 2, 2, 128], BF16)
        qxd = self.ar('qxd', [64, 2, 2, 2, 128], BF16)
        rowm = self.ar('rowm', [64, 16], F32)
        P.dma('sp', rowm[:, :], self.din['ret_rowm'], writes=[rowm.k])
        AT = self.ar('AT', [128, 4, 128], BF16)
        o32 = self.ar('o32', [128, 2, 512], F32)
        oblk = self.ar('oblk', [128, 2, 512], BF16)

        def state_step(d, tile):
            self.tt(vz[:, :].rearrange("s (h v) -> s h v", h=4), vt[:, tile, :].rearrange("s (h v) -> s h v", h=4),
                    zeta[:, d, :].unsqueeze(2).broadcast_to([128, 4, 64]), ALU.mult, [vt.k, zeta.k], [vz.k])
            pu, puk = self.psum('mA', [0, 1])
            for p in range(2):
                self.mmg(pu[0:64, p * 128:(p + 1) * 128], [(kxt[:, tile, p * 64:(p + 1) * 64], vz[:, p * 128:(p + 1) * 128])],
                         [kxt.k, vz.k], puk)
            for p in range(2):
                self.stt(S[:, p, :], S[:, p, :], dc[:, d, p:p + 1], pu[0:64, p * 128:(p + 1) * 128], ALU.mult, ALU.add,
                         [S.k, dc.k, puk], [S.k])

        self.memset('dve', S[:, :, :], 0.0, [S.k])
        for tile in range(NT):
            self.cp('act', Sf[:, tile, :, :], S[:, :, :], [S.k], [(Sf.k, tile)])
            if tile < NT - 1:
                state_step(0, tile)
        if RSTOP <= 3:
            return
        self.memset('dve', S[:, :, :], 0.0, [S.k])
        order = [1, 0] + list(range(NT - 1, 1, -1))
        for idx, tile in enumerate(order):
            self.cp('act', Sbb[:, :, :], S[:, :, :], [S.k], [Sbb.k])
            for hh in range(2):
                self.ts(qxm[:, hh, :, :], qx[:, :, tile * 128:(tile + 1) * 128], rowm[:, hh:hh + 1], None, ALU.mult, None,
                        [qx.k, rowm.k], [qxm.k])
            for d in range(2):
                for hh in range(2):
                    self.tt(qxd[:, d, hh, :, :], qxm[:, hh, :, :], xi[:, d, :, :], ALU.mult, [qxm.k, xi.k], [qxd.k])
            pA, pAk = self.psum('rA', [4, 5])
            for h in range(4):
                hh, p = h % 2, h // 2
                self.mmg(pA[:, h * 128:(h + 1) * 128], [(kx[:, p, tile * 128:(tile + 1) * 128], qxm[:, hh, p, :])],
                         [kx.k, qxm.k], pAk)
            self.tt(AT[:, :, :], pA[:, :].rearrange("s (h t) -> s h t", h=4), Dsum[:, :, :], ALU.mult, [pAk, Dsum.k], [AT.k])
            if RSTOP <= 4:
                continue
            pO, pOk = self.psum('rO', [6, 7])
            for h in range(4):
                hh, p = h % 2, h // 2
                self.mmg(pO[:, h * 128:(h + 1) * 128],
                         [(vt[:, tile, p * 128:(p + 1) * 128], AT[:, h, :]),
                          (Sf[:, tile, p, :], qxd[:, 0, hh, p, :]),
                          (Sbb[:, p, :], qxd[:, 1, hh, p, :])],
                         [vt.k, AT.k, (Sf.k, tile), Sbb.k, qxd.k], pOk)
            b = self.blk_of_tile(tile)
            t0, n = BLKS[b]
            off = tile * 128 - t0
            pv4 = pO[:, :].rearrange("q (p j t) -> q p j t", p=2, j=2)
            for hh in range(2):
                self.ts(o32[hh * 64:hh * 64 + 64, :, off:off + 128], pv4[hh * 64:hh * 64 + 64, :, hh, :], 32.0 ** -0.5, None,
                        ALU.mult, None, [pOk], [(o32.k, tile % 4, hh)])
            if RSTOP <= 5:
                continue
            if idx < NT - 1:
                state_step(1, tile)
            if RSTOP <= 6:
                continue
            if tile * 128 == t0:
                ntile = n // 128
                okeys = [(o32.k, (t0 // 128 + j) % 4, hh) for j in range(ntile) for hh in range(2)]
                self.P.op('dve', lambda e: e.tensor_copy(out=o32[:, :, 0:1], in_=o32[:, :, 0:1]), reads=okeys, writes=[o32.k] + okeys)
                self.headnorm_gate(l, o32, o32.k, n, t0, b, w, 0, True, 5, 7, oblk, ('aT', b))
                self.outproj(l, wo, oblk, [(oblk.k, 0), (oblk.k, 1)], b, t0, n)


    def hgrn(self, l):
        P = self.P
        aT = self.aT
        ident = self.ident
        wh = self.ar('wh', [128, 8, 1280], BF16)
        self.load_w(wh[:, :, :], self.w_in[l][:, C_HQ:C_HQ + 1280], 8, 1280, wh.k)
        itm = self.ar('itm', [128, NT, 256], BF16)
        ohg = self.ar('ohg', [128, 2, T], BF16)
        trie = self.ar('trie', [128, 2, 136], F32)
        P.dma('sp', trie[:, :, :], self.din['hg_trie'].rearrange("d s c -> s d c"), writes=[trie.k])
        mt = self.ar('mt', [128, 2, 128], BF16)
        P.dma('pool', mt[:, :, :], self.din['hg_mt'].rearrange("d s c -> s d c"), writes=[mt.k])
        ee = self.ar('ee', [128, 2, 8], BF16)
        P.dma('pool', ee[:, :, :], self.din['hg_e'].rearrange("d s c -> s d c"), writes=[ee.k])
        omlF = self.ar('omlF', [64, 8], F32)
        nomlF = self.ar('nomlF', [64, 8], F32)
        lbB = self.ar('lbB', [128, 512], F32)
        omlB = self.ar('omlB', [128, 512], F32)
        mark = self.ar_off
        st = self.ar('st', [32, 64], F32)
        lbl = self.ar('lbl', [64, 4, 8], F32)
        sF = self.ar('sF', [64, 8], F32)
        cF = self.ar('cF', [64, 8], F32)
        P.dma('sp', st[:, :], self.din['hg_lb_logits'].rearrange("l d (h k) -> (l d h) k", k=64), writes=[st.k])
        pt, pk = self.psum('mA', [0, 1])
        self.tr(pt[0:64, 0:32], st[:, :], ident[0:32, 0:32], [st.k, 'ident'], pk)
        self.act(lbl[:, :, :], pt[0:64, 0:32].rearrange("k (l x) -> k l x", l=4), AF.Exp, [pk], [lbl.k])
        self.tt(sF[:, :], lbl[:, 0, :], lbl[:, 1, :], ALU.add, [lbl.k], [sF.k])
        self.tt(sF[:, :], sF[:, :], lbl[:, 2, :], ALU.add, [lbl.k, sF.k], [sF.k])
        self.tt(sF[:, :], sF[:, :], lbl[:, 3, :], ALU.add, [lbl.k, sF.k], [sF.k])
        self.recip(sF[:, :], sF[:, :], [sF.k], [sF.k])
        self.memset('dve', cF[:, :], 0.0, [cF.k])
        for j in range(1, l + 1):
            self.tt(cF[:, :], cF[:, :], lbl[:, j, :], ALU.add, [lbl.k, cF.k], [cF.k])
        self.tt(cF[:, :], cF[:, :], sF[:, :], ALU.mult, [cF.k, sF.k], [cF.k])
        self.ts(omlF[:, :], cF[:, :], -1.0, 1.0, ALU.mult, ALU.add, [cF.k], [omlF.k])
        self.ts(nomlF[:, :], cF[:, :], -1.0, None, ALU.add, None, [cF.k], [nomlF.k])
        eb_ = self.ar('ebig', [128, 4, 512], F32)
        sB = self.ar('sB', [128, 512], F32)
        P.dma('sp', eb_[:, :, :], self.din['hg_lb_logits'].rearrange("(o l) d c -> o l (d c)", o=1).broadcast_to([128, 4, 512]),
              writes=[eb_.k])
        self.act(eb_[:, :, :], eb_[:, :, :], AF.Exp, [eb_.k], [eb_.k])
        self.tt(sB[:, :], eb_[:, 0, :], eb_[:, 1, :], ALU.add, [eb_.k], [sB.k])
        self.tt(sB[:, :], sB[:, :], eb_[:, 2, :], ALU.add, [eb_.k, sB.k], [sB.k])
        self.tt(sB[:, :], sB[:, :], eb_[:, 3, :], ALU.add, [eb_.k, sB.k], [sB.k])
        self.recip(sB[:, :], sB[:, :], [sB.k], [sB.k])
        self.memset('dve', lbB[:, :], 0.0, [lbB.k])
        for j in range(1, l + 1):
            self.tt(lbB[:, :], lbB[:, :], eb_[:, j, :], ALU.add, [eb_.k, lbB.k], [lbB.k])
        self.tt(lbB[:, :], lbB[:, :], sB[:, :], ALU.mult, [lbB.k, sB.k], [lbB.k])
        self.ts(omlB[:, :], lbB[:, :], -1.0, 1.0, ALU.mult, ALU.add, [lbB.k], [omlB.k])
        self.ar_release(mark)
        for tile in range(NT):
            ak = ('aT', self.blk_of_tile(tile))
            pv, pvk = self.psum('mA', [0, 1])
            self.mmg(pv[:, 0:256], [(aT[:, kc, tile * 128:(tile + 1) * 128], wh[:, kc, 768:1024]) for kc in range(8)],
                     [wh.k, ak], pvk)
            self.cp('act', itm[:, tile, :], pv[:, 0:256], [pvk], [itm.k])
        ftm = self.ar('ftm', [128, 256], F32)
        lftm = self.ar('lftm', [128, 256], F32)
        ktm = self.ar('ktm', [128, 256], F32)
        kbt = self.ar('kbt', [128, 256], BF16)
        kTt = self.ar('kTt', [64, 4, 128], F32)
        ebt = self.ar('ebt', [64, 4, 128], F32)
        enb = self.ar('enb', [64, 4, 128], F32)
        dd = self.ar('dd', [64, 4, 8], F32)
        qb = self.ar('qb', [64, 4, 128], BF16)
        kb = self.ar('kb', [64, 4, 128], BF16)
        vexp = [self.ar('vexp%d' % i, [128, 8, 64], BF16) for i in range(2)]
        u = self.ar('u', [64, 8, 4, 64], F32)
        Spp = [self.ar('S%d' % i, [64, 4, 64], F32) for i in range(2)]
        tmpS = self.ar('tmpS', [64, 4, 64], F32)
        Sbf = self.ar('Sbf', [64, 8, 4, 64], BF16)
        si = 0
        AT = self.ar('AT', [128, 4, 128], BF16)
        vi = 0
        for d in range(2):
            order = list(range(NT)) if d == 0 else [1, 0] + list(range(NT - 1, 1, -1))
            self.memset('dve', Spp[si % 2][:, :, :], 0.0, [(Spp[si % 2].k, h) for h in range(4)])
            zc = 256 + d * 256
            for tile in order:
                ak = ('aT', self.blk_of_tile(tile))
                tcs = slice(tile * 128, (tile + 1) * 128)
                pz, pzk = self.psum('hz', [0])
                self.mmg(pz[:, 0:256], [(aT[:, kc, tcs], wh[:, kc, zc:zc + 256]) for kc in range(8)], [wh.k, ak], pzk)
                pzT, pzTk = self.psum('hzT', [1])
                for h in range(4):
                    self.mmg(pzT[0:64, h * 128:(h + 1) * 128],
                             [(wh[:, kc, zc + h * 64:zc + (h + 1) * 64], aT[:, kc, tcs]) for kc in range(8)], [wh.k, ak], pzTk)
                pq, pqk = self.psum('hq', [2])
                for h in range(4):
                    self.mmg(pq[0:64, h * 128:(h + 1) * 128],
                             [(wh[:, kc, h * 64:(h + 1) * 64], aT[:, kc, tcs]) for kc in range(8)], [wh.k, ak], pqk)
                self.act(ftm[:, :], pz[:, 0:256], AF.Sigmoid, [pzk], [ftm.k])
                self.act(kTt[:, :, :], pzT[0:64, :].rearrange("k (h t) -> k h t", h=4), AF.Sigmoid, [pzTk], [kTt.k])
                self.tt(ftm[:, :], ftm[:, :], omlB[:, d * 256:(d + 1) * 256], ALU.mult, [ftm.k, omlB.k], [ftm.k])
                self.tt(ftm[:, :], ftm[:, :], lbB[:, d * 256:(d + 1) * 256], ALU.add, [ftm.k, lbB.k], [ftm.k])
                self.act(lftm[:, :], ftm[:, :], AF.Ln, [ftm.k], [lftm.k])
                self.tt(kTt[:, :, :], kTt[:, :, :], nomlF[:, d * 4:(d + 1) * 4].unsqueeze(2).broadcast_to([64, 4, 128]),
                        ALU.mult, [kTt.k, nomlF.k], [kTt.k])
                self.tt(kTt[:, :, :], kTt[:, :, :], omlF[:, d * 4:(d + 1) * 4].unsqueeze(2).broadcast_to([64, 4, 128]),
                        ALU.add, [kTt.k, omlF.k], [kTt.k])
                self.ts(ktm[:, :], ftm[:, :], -1.0, 1.0, ALU.mult, ALU.add, [ftm.k], [ktm.k])
                pb, pbk = self.psum('hb', [3])
                self.mmg(pb[:, 0:256], [(trie[:, d, 0:128], lftm[:, :])], [trie.k, lftm.k], pbk)
                pbT, pbTk = self.psum('hbT', [4])
                for h in range(4):
                    self.mmg(pbT[0:64, h * 128:(h + 1) * 128], [(lftm[:, h * 64:(h + 1) * 64], trie[:, d, 0:128])],
                             [lftm.k, trie.k], pbTk)
                ptot, ptotk = self.psum('htot', [5])
                for h in range(4):
                    self.mmg(ptot[0:64, h * 8:(h + 1) * 8], [(lftm[:, h * 64:(h + 1) * 64], trie[:, d, 128:136])],
                             [lftm.k, trie.k], ptotk)
                self.act(ftm[:, :], pb[:, 0:256], AF.Exp, [pbk], [ftm.k], scale=-1.0)
                self.act(ebt[:, :, :], pbT[0:64, :].rearrange("k (h t) -> k h t", h=4), AF.Exp, [pbTk], [ebt.k])
                self.act(enb[:, :, :], pbT[0:64, :].rearrange("k (h t) -> k h t", h=4), AF.Exp, [pbTk], [enb.k], scale=-1.0)
                self.act(dd[:, :, :], ptot[0:64, 0:32].rearrange("k (h j) -> k h j", h=4), AF.Exp, [ptotk], [dd.k])
                self.tt(kbt[:, :], ktm[:, :], ftm[:, :], ALU.mult, [ktm.k, ftm.k], [kbt.k])
                self.stt(qb[:, :, :], pq[0:64, :].rearrange("k (h t) -> k h t", h=4), 0.125, ebt[:, :, :], ALU.mult, ALU.mult,
                         [pqk, ebt.k], [qb.k])
                self.tt(kb[:, :, :], kTt[:, :, :], enb[:, :, :], ALU.mult, [kTt.k, enb.k], [kb.k])
                pA, pAk = self.psum('h3', [6])
                for h in range(4):
                    self.mmg(pA[:, h * 128:(h + 1) * 128], [(kb[:, h, :], qb[:, h, :])], [kb.k, qb.k], pAk)
                self.tt(AT[:, :, :], pA[:, :].rearrange("s (h t) -> s h t", h=4),
                        mt[:, d, :].unsqueeze(1).broadcast_to([128, 4, 128]), ALU.mult, [pAk, mt.k], [AT.k])
                for hp in range(2):
                    ves = []
                    for h in (2 * hp, 2 * hp + 1):
                        ve = vexp[h % 2]
                        self.tt(ve[:, :, :], itm[:, tile, h * 64:(h + 1) * 64].unsqueeze(1).broadcast_to([128, 8, 64]),
                                ee[:, d, :].unsqueeze(2).broadcast_to([128, 8, 64]), ALU.mult, [itm.k, ee.k], [ve.k])
                        ves.append(ve)
                    pus = []
                    for h, ve in zip((2 * hp, 2 * hp + 1), ves):
                        pu, puk = self.psum('h2', [0, 1])
                        self.mmg(pu[0:64, :], [(kbt[:, h * 64:(h + 1) * 64], ve[:, :, :].rearrange("s j v -> s (j v)"))],
                                 [kbt.k, ve.k], puk)
                        pus.append((pu, puk))
                    for h, (pu, puk) in zip((2 * hp, 2 * hp + 1), pus):
                        self.tt(u[:, :, h, :], pu[0:64, :].rearrange("k (j v) -> k j v", j=8),
                                dd[:, h, :].unsqueeze(2).broadcast_to([64, 8, 64]), ALU.mult, [puk, dd.k], [u.k])
                for j in range(8):
                    S, S2 = Spp[si % 2], Spp[(si + 1) % 2]
                    si += 1
                    self.cp('act', Sbf[:, j, :, :], S[:, :, :], [(S.k, h) for h in range(4)], [(Sbf.k, j)])
                    for h in range(4):
                        self.stt(S2[:, h, :], S[:, h, :], dd[:, h, j:j + 1], u[:, j, h, :], ALU.mult, ALU.add,
                                 [(S.k, h), dd.k, u.k], [(S2.k, h)])
                pO, pOk = self.psum('h4', [7])
                rk = [itm.k, AT.k, qb.k] + [(Sbf.k, j) for j in range(8)]
                for h in range(4):
                    p = h // 2
                    P.op('pe', lambda e, h=h, p=p, tile=tile: e.matmul(pO[:, h * 128:(h + 1) * 128], lhsT=itm[:, tile, p * 128:(p + 1) * 128],
                                                           rhs=AT[:, h, :], start=True, stop=False),
                         reads=rk, writes=[pOk], inc=False)
                    for j in range(8):
                        cj = j if d == 0 else 7 - j
                        P.op('pe', lambda e, h=h, p=p, j=j, cj=cj: e.matmul(
                            pO[:, h * 128 + cj * 16:h * 128 + (cj + 1) * 16],
                            lhsT=Sbf[:, j, 2 * p:2 * p + 2, :].rearrange("k h v -> k (h v)"),
                            rhs=qb[:, h, cj * 16:(cj + 1) * 16], start=False, stop=(j == 7)),
                             reads=rk, writes=[pOk], inc=(h == 3 and j == 7))
                pv4 = pO[:, :].rearrange("q (p j t) -> q p j t", p=2, j=2)
                for hh in range(2):
                    if d == 0:
                        self.cp('act' if hh else 'dve', ohg[hh * 64:hh * 64 + 64, :, tcs], pv4[hh * 64:hh * 64 + 64, :, hh, :],
                                [pOk], [(ohg.k, tile, hh)])
                    else:
                        self.tt(ohg[hh * 64:hh * 64 + 64, :, tcs], pv4[hh * 64:hh * 64 + 64, :, hh, :],
                                ohg[hh * 64:hh * 64 + 64, :, tcs], ALU.add, [pOk, (ohg.k, tile, hh)], [(ohg.k, tile, hh)])
        if os.environ.get('HG_DBG'):
            dbg = self.nc.dram_tensor('dbg', [128, 2 * T], F32, kind="ExternalOutput").ap()
            P.dma('pool', dbg.rearrange("q (p t) -> q p t", p=2), ohg[:, :, :],
                  reads=[(ohg.k, t_, hh) for t_ in range(NT) for hh in range(2)], is_output=True)
        self.ar_release(mark)
        wo = self.load_wo(l, 1)
        self.hn_alloc()
        oblk = self.ar('oblk', [128, 2, 512], BF16)
        o32 = self.ar('o32h', [128, 2, 512], F32)
        for b, (t0, n) in enumerate(BLKS):
            okeys = [(ohg.k, t0 // 128 + j, hh) for j in range(n // 128) for hh in range(2)]
            self.cp('dve', o32[:, :, 0:n], ohg[:, :, t0:t0 + n], okeys, [o32.k])
            self.headnorm_gate(l, o32, o32.k, n, t0, b, wh, 1024, False, 3, None, oblk, ('aT', b))
            self.outproj(l, wo, oblk, [(oblk.k, 0), (oblk.k, 1)], b, t0, n)


    def final_out(self, out, gfin):
        P = self.P
        hT, rstd, ident = self.hT, self.rstd, self.ident
        nsq = self.ar('nsq', [128, 8, 512], BF16)
        ntmp = self.ar('ntmp', [128, 8, 512], F32)
        nsq.k, ntmp.k = 'nsq', 'ntmp'
        ost = [self.ar('ost%d' % i, [128, 1024], F32) for i in range(2)]
        oi = 0
        for b, (t0, n) in enumerate(BLKS):
            if b == 0:
                continue
            hk = [('hT', c, b) for c in range(8)]
            if self.final:
                self.act(nsq[:, :, 0:n], hT[:, :, t0:t0 + n], AF.Square, hk, ['nsq'])
                pt, pk = self.psum('nrm', [2, 3])
                self.mmg(pt[:, 0:n], [(self.ones_bf[:, :], nsq[:, c, 0:n]) for c in range(8)], ['nsq', 'ones_bf'], pk)
                self.act(rstd[:, 0:n], pt[:, 0:n], AF.Sqrt, [pk], ['rstd'], bias=self.eps_col[:, 0:1], scale=1.0 / D)
                self.recip(rstd[:, 0:n], rstd[:, 0:n], ['rstd'], ['rstd'])
                self.tt(ntmp[:, :, 0:n], hT[:, :, t0:t0 + n], rstd[:, 0:n].unsqueeze(1).broadcast_to([128, 8, n]),
                        ALU.mult, hk + ['rstd'], ['ntmp'])
                self.tt(ntmp[:, :, 0:n], ntmp[:, :, 0:n], gfin[:, :].unsqueeze(2).broadcast_to([128, 8, n]),
                        ALU.mult, ['ntmp', 'gfin'], ['ntmp'])
            else:
                self.cp('dve', ntmp[:, :, 0:n], hT[:, :, t0:t0 + n], hk, ['ntmp'])
            for tt_ in range(n // 128):
                os_ = ost[oi % 2]
                oi += 1
                for half in range(2):
                    pt, pk = self.psum('tr', [0, 1])
                    for j in range(4):
                        c = half * 4 + j
                        self.tr(pt[:, j * 128:(j + 1) * 128], ntmp[:, c, tt_ * 128:(tt_ + 1) * 128], ident[:, :],
                                ['ntmp', 'ident'], pk)
                    self.cp('act' if half else 'dve', os_[:, half * 512:(half + 1) * 512], pt[:, :], [pk], [(os_.k, half)])
                row0 = t0 - LCTX + tt_ * 128
                P.dma('sp', out[row0:row0 + 128, :], os_[:, :], reads=[(os_.k, 0), (os_.k, 1)], is_output=True)


def _rot_T(half):
    n = 2 * half
    R = np.zeros((n, n), np.float32)
    for m in range(half):
        R[m, m + half] = -1.0
        R[m + half, m] = 1.0
    return R.T.copy()


def _consts():
    sel = np.zeros((NEXP, NEXP * 128), np.float32)
    for e in range(NEXP):
        sel[e, e * 128:(e + 1) * 128] = 1.0
    c = {'ident': np.eye(128, dtype=np.float32), 'sel': sel}
    theta = 10000.0
    tok = np.arange(NLAT)
    row, col = (tok // 64).astype(np.float64), (tok % 64).astype(np.float64)

    def axial(rot_dim):
        nf = rot_dim // 4
        inv = theta ** (-np.arange(nf, dtype=np.float64) / nf)
        inv = inv.astype(np.float32).astype(np.float64)
        ang = np.concatenate([row[:, None] * inv, col[:, None] * inv], axis=1)
        ang = ang.astype(np.float32).astype(np.float64)
        full = np.zeros((T, rot_dim // 2))
        full[LCTX:] = ang
        a2 = np.concatenate([full, full], axis=1)
        return np.cos(a2).T.astype(np.float32).copy(), np.sin(a2).T.astype(np.float32).copy()

    c['cos_mla'], c['sin_mla'] = axial(32)
    c['cos_swa'], c['sin_swa'] = axial(64)
    inv = (theta ** (-np.arange(16, dtype=np.float64) / 16)).astype(np.float32).astype(np.float64)
    ang = (np.arange(T, dtype=np.float64)[:, None] * inv).astype(np.float32).astype(np.float64)
    a2 = np.concatenate([ang, ang], axis=1)
    c['cos_ret'], c['sin_ret'] = np.cos(a2).T.astype(np.float32).copy(), np.sin(a2).T.astype(np.float32).copy()
    rb = np.zeros((96, 96), np.float32)
    rb[64:96, 64:96] = _rot_T(16)
    c['rbig96'] = rb
    r128 = np.zeros((128, 128), np.float32)
    r128[0:64, 0:64] = _rot_T(32)
    r128[64:128, 64:128] = _rot_T(32)
    c['r128'] = r128
    r64b = np.zeros((64, 64), np.float32)
    r64b[0:32, 0:32] = _rot_T(16)
    r64b[32:64, 32:64] = _rot_T(16)
    c['r64b'] = r64b
    m = np.zeros((6, 128, 512), np.float32)
    s_ = np.arange(128)[:, None]
    t_ = np.arange(512)[None, :]
    for r in range(-1, 5):
        m[r + 1] = (np.abs(t_ - s_ - 128 * r) <= 128).astype(np.float32)
    c['swa_mask'] = m
    b64 = np.zeros((128, 128), np.float32)
    b64[0:64, 0:64] = 1.0 / 64
    b64[64:128, 64:128] = 1.0 / 64
    c['blk64'] = b64
    s_ = np.arange(128, dtype=np.float32)[:, None]
    t_ = np.arange(128, dtype=np.float32)[None, :]
    c['ret_rel'] = np.stack([np.maximum(t_ - s_, 0), (t_ >= s_).astype(np.float32),
                             np.maximum(s_ - t_, 0), (s_ >= t_).astype(np.float32)]).astype(np.float32)
    cc = np.zeros((128, 16), np.float32)
    cc[:, 0] = 127.0 - np.arange(128)
    cc[:, 1] = np.arange(128)
    c['ret_colc'] = cc
    rc = np.concatenate([np.arange(128) + 1.0, 128.0 - np.arange(128)])[None, :].repeat(64, axis=0)
    c['ret_rowc'] = rc.astype(np.float32)
    rm = np.zeros((64, 16), np.float32)
    rm[0:32, 0] = 1.0
    rm[32:64, 1] = 1.0
    c['ret_rowm'] = rm
    si = np.arange(128)[:, None]
    ti = np.arange(128)[None, :]
    same = (si // 16) == (ti // 16)
    trif = (same & (si <= ti)).astype(np.float32)
    trib = (same & (si >= ti)).astype(np.float32)
    ef = ((si // 16) == np.arange(8)[None, :]).astype(np.float32)
    eb = ((si // 16) == (7 - np.arange(8))[None, :]).astype(np.float32)
    c['hg_trie'] = np.stack([np.concatenate([trif, ef], axis=1), np.concatenate([trib, eb], axis=1)]).astype(np.float32)
    c['hg_mt'] = np.stack([trif, trib]).astype(np.float32)
    c['hg_e'] = np.stack([ef, eb]).astype(np.float32)
    return c


_CACHE = {}


def _get_prog(key, **kw):
    if key not in _CACHE:
        b = Builder(**kw)
        nc = b.build()
        _CACHE[key] = (nc, b)
    return _CACHE[key]


def run_partial(inputs, layers=(0, 1, 2, 3), mixers=(0, 1, 2, 3), ffn=True, final=True, cores=8):
    nc, b = _get_prog(('p', tuple(layers), tuple(mixers), ffn, final), layers=layers, mixers=mixers, ffn=ffn, final=final)
    consts = _consts()
    f = lambda a: np.ascontiguousarray(np.asarray(a, dtype=np.float32))
    shared = {k: f(inputs[k]) for k in b.din if k in inputs and k not in ('x', 'ctx')}
    for k in consts:
        if k in b.din:
            shared[k] = consts[k]
    in_maps = []
    for i in range(cores):
        m = dict(shared)
        m['x'] = f(inputs['x'][i])
        m['ctx'] = f(inputs['ctx'][i])
        m['c2'] = np.ascontiguousarray(np.stack([np.asarray(inputs['c'][i], np.float32),
                                                 np.asarray(inputs['c_ctx'], np.float32)]))
        in_maps.append(m)
    res = run_bass_kernel_spmd(nc, in_maps, core_ids=list(range(cores)))
    return np.stack([r['out'] for r in res.results], axis=0)


def kernel(**inputs):
    return run_partial(inputs)
```

```python
import os
import numpy as np
import concourse.bass as bass
import concourse.mybir as mybir
from concourse.bass_utils import run_bass_kernel_spmd

AF = mybir.ActivationFunctionType
ALU = mybir.AluOpType
AX = mybir.AxisListType
F32 = mybir.dt.float32
BF16 = mybir.dt.bfloat16

ENGS = ['pe', 'act', 'dve', 'pool', 'sp']
NDMA = 56

D = 1024
T = 2304
NT = 18
LCTX = 256
NLAT = 2048
DEPTH = 4
D_FF = 2816
D_FFE = 3584
NEXP = 8
D_IN = 2912
EPS = 1e-6
BLKS = [(0, 256), (256, 512), (768, 512), (1280, 512), (1792, 512)]
C_MQ, C_MKV, C_MPE = 0, 192, 320
C_HQ, C_HZF, C_HZB, C_HI, C_HG = 352, 608, 864, 1120, 1376
C_SQ, C_SK, C_SV = 1632, 1888, 2016
C_RQ, C_RK, C_RV, C_RG = 2144, 2272, 2400, 2656


class Prog:
    def __init__(self, nc):
        self.nc = nc
        self.q = {e: [] for e in ENGS}
        self.sem = {e: nc.alloc_semaphore('cs_' + e) for e in ENGS}
        self.cnt = {e: 0 for e in ENGS}
        self.waited = {}
        self.lastw = {}
        self.readers = {}
        self.dma_sems = [nc.alloc_semaphore('ds%d' % i) for i in range(NDMA)]
        self.dma_cnt = [0] * NDMA
        self.dma_rr = {'sp': 0, 'pool': 0}
        self.out_tokens = []
        self.ninst = 0

    def _need(self, eng, tok):
        sem, val, prod = tok
        if eng == 'pe' and prod == 'pe':
            return
        k = (eng, sem.num)
        if self.waited.get(k, 0) >= val:
            return
        self.waited[k] = val
        self.q[eng].append(lambda e, sem=sem, val=val: e.wait_ge(sem, val))

    def _deps(self, eng, reads, writes):
        for r in reads:
            t = self.lastw.get(r)
            if t is not None:
                self._need(eng, t)
            if isinstance(r, tuple) and r[0] == 'ps':
                for t in self.readers.get(r, {}).values():
                    if t[2] != eng:
                        self._need(eng, t)
        for w in writes:
            t = self.lastw.get(w)
            if t is not None:
                self._need(eng, t)
            for t in self.readers.get(w, {}).values():
                self._need(eng, t)

    def _record(self, tok, reads, writes):
        for r in reads:
            d = self.readers.setdefault(r, {})
            k = tok[0].num
            if k not in d or d[k][1] < tok[1]:
                d[k] = tok
        for w in writes:
            self.lastw[w] = tok
            self.readers[w] = {}

    def op(self, eng, fn, reads=(), writes=(), inc=True):
        self._deps(eng, reads, writes)
        sem = self.sem[eng]
        self.ninst += 1
        if inc:
            self.cnt[eng] += 1
            tok = (sem, self.cnt[eng], eng)
            self.q[eng].append(lambda e, fn=fn, sem=sem: fn(e).then_inc(sem, 1))
        else:
            tok = (sem, self.cnt[eng] + 1, eng)
            self.q[eng].append(lambda e, fn=fn: fn(e))
        self._record(tok, reads, writes)
        return tok

    def dma(self, eng, out, in_, reads=(), writes=(), is_output=False):
        h = NDMA // 2
        j = self.dma_rr[eng]
        self.dma_rr[eng] = (j + 1) % h
        i = j if eng == 'sp' else h + j
        sem = self.dma_sems[i]
        if self.dma_cnt[i] > 0:
            self._need(eng, (sem, self.dma_cnt[i], 'dma'))
        self._deps(eng, reads, writes)
        self.dma_cnt[i] += 16
        tok = (sem, self.dma_cnt[i], 'dma')
        self.ninst += 1
        self.q[eng].append(
            lambda e, out=out, in_=in_, sem=sem: e.dma_start(out=out, in_=in_).then_inc(sem, 16))
        self._record(tok, reads, writes)
        if is_output:
            self.out_tokens.append(tok)
        return tok

    def barrier(self):
        toks = [(self.sem[e], self.cnt[e], e) for e in ENGS if self.cnt[e] > 0]
        toks += [(self.dma_sems[i], self.dma_cnt[i], 'dma') for i in range(NDMA) if self.dma_cnt[i] > 0]
        for f in ENGS:
            for t in toks:
                self._need(f, t)

    def emit(self):
        nc = self.nc
        for t in self.out_tokens:
            self._need('sp', t)
        for e in ['pe', 'act', 'dve', 'pool']:
            if self.cnt[e] > 0:
                self._need('sp', (self.sem[e], self.cnt[e], e))
        q = self.q
        with nc.Block() as block:
            @block.tensor
            def _(e):
                for f in q['pe']:
                    f(e)

            @block.scalar
            def _(e):
                for f in q['act']:
                    f(e)

            @block.vector
            def _(e):
                for f in q['dve']:
                    f(e)

            @block.gpsimd
            def _(e):
                for f in q['pool']:
                    f(e)

            @block.sync
            def _(e):
                for f in q['sp']:
                    f(e)


class SB:
    def __init__(self, nc, name, shape, dtype):
        self.t = nc.alloc_sbuf_tensor('sb_' + name, list(shape), dtype)
        self.k = name

    def __getitem__(self, idx):
        return self.t[idx]


class View:
    def __init__(self, ap, key):
        self.t = ap
        self.k = key

    def __getitem__(self, idx):
        return self.t[idx]


AR_WORDS = 19456


class Builder:
    def __init__(self, layers, mixers=(0, 1, 2, 3), ffn=True, final=True, h_in=False, h_out=False):
        self.layers = list(layers)
        self.mixers = tuple(mixers)
        self.ffn = ffn
        self.final = final
        self.h_in = h_in
        self.h_out = h_out
        self.nc = nc = bass.Bass("TRN2", target_bir_lowering=False)
        self.P = Prog(nc)
        self.din = {}
        self.ps = [nc.alloc_psum_tensor("psb%d" % i, [128, 512], F32) for i in range(8)]
        self.ps_rr = {}
        self.uid = 0
        self.arena = nc.alloc_sbuf_tensor('arena', [128, AR_WORDS], F32)
        self.ar_off = 0
        self.ar_phase = 0

    def inp(self, name, shape, dtype=F32):
        ap = self.nc.dram_tensor(name, list(shape), dtype, kind="ExternalInput").ap()
        self.din[name] = ap
        return ap

    def sb(self, name, shape, dtype=F32):
        return SB(self.nc, name, shape, dtype)

    def ar(self, name, shape, dtype=F32):
        nel = int(np.prod(shape[1:]))
        nb = nel * (2 if dtype == BF16 else 4)
        nw = ((nb + 31) // 32) * 8
        assert self.ar_off + nw <= AR_WORDS, (name, self.ar_off, nw)
        ap = self.arena[0:shape[0], self.ar_off:self.ar_off + nw]
        self.ar_off += nw
        if dtype == BF16:
            ap = ap.bitcast(BF16)
        ap = ap[:, 0:nel]
        if len(shape) == 3:
            ap = ap.rearrange("p (a b) -> p a b", a=shape[1])
        elif len(shape) == 4:
            ap = ap.rearrange("p (a b c) -> p a b c", a=shape[1], b=shape[2])
        elif len(shape) == 5:
            ap = ap.rearrange("p (a b c d) -> p a b c d", a=shape[1], b=shape[2], c=shape[3])
        self.ar_gen = getattr(self, 'ar_gen', 0)
        return View(ap, ('ar', self.ar_phase, name))

    def ar_release(self, mark):
        self.P.barrier()
        self.ar_off = mark
        self.ar_phase += 1

    def phase(self):
        self.P.barrier()
        self.ar_off = 0
        self.ar_phase += 1

    def psum(self, pool, banks):
        i = self.ps_rr.get(pool, 0)
        self.ps_rr[pool] = i + 1
        b = banks[i % len(banks)]
        return self.ps[b], ('ps', b)

    def mmg(self, out, pairs, reads, wkey):
        n = len(pairs)
        for i, (l, r) in enumerate(pairs):
            self.P.op('pe', lambda e, l=l, r=r, s=(i == 0), t=(i == n - 1): e.matmul(out, lhsT=l, rhs=r, start=s, stop=t),
                      reads=reads, writes=[wkey], inc=(i == n - 1))

    def tr(self, out, in_, ident, reads, wkey):
        self.P.op('pe', lambda e: e.transpose(out, in_, ident), reads=reads, writes=[wkey])

    def act(self, out, in_, func, reads, writes, bias=None, scale=None):
        kw = {}
        if bias is not None:
            kw['bias'] = bias
        if scale is not None:
            kw['scale'] = scale
        self.P.op('act', lambda e: e.activation(out=out, in_=in_, func=func, **kw), reads=reads, writes=writes)

    def tt(self, out, in0, in1, op, reads, writes, eng='dve'):
        self.P.op(eng, lambda e: e.tensor_tensor(out=out, in0=in0, in1=in1, op=op), reads=reads, writes=writes)

    def ts(self, out, in0, s1, s2, op0, op1, reads, writes, eng='dve'):
        if s2 is None:
            self.P.op(eng, lambda e: e.tensor_scalar(out=out, in0=in0, scalar1=s1, scalar2=None, op0=op0),
                      reads=reads, writes=writes)
        else:
            self.P.op(eng, lambda e: e.tensor_scalar(out=out, in0=in0, scalar1=s1, scalar2=s2, op0=op0, op1=op1),
                      reads=reads, writes=writes)

    def stt(self, out, in0, scalar, in1, op0, op1, reads, writes):
        self.P.op('dve', lambda e: e.scalar_tensor_tensor(out=out, in0=in0, scalar=scalar, in1=in1, op0=op0, op1=op1),
                  reads=reads, writes=writes)

    def cp(self, eng, out, in_, reads, writes):
        if eng == 'act':
            self.P.op('act', lambda e: e.copy(out=out, in_=in_), reads=reads, writes=writes)
        else:
            self.P.op(eng, lambda e: e.tensor_copy(out=out, in_=in_), reads=reads, writes=writes)

    def recip(self, out, in_, reads, writes):
        self.P.op('dve', lambda e: e.reciprocal(out=out, in_=in_), reads=reads, writes=writes)

    def memset(self, eng, ap, val, writes):
        self.P.op(eng, lambda e: e.memset(ap, val), writes=writes)

    def rows_to_cols(self, dram_rows, nrows, ncols, dst_ap, dst_key):
        self.uid += 1
        st = self.stage
        k = ('stage', self.uid % 2)
        sl = st[self.uid % 2]
        self.P.dma('sp', sl[0:nrows, 0:ncols], dram_rows, writes=[k])
        pt, pk = self.psum('tr', [0, 1])
        self.tr(pt[0:ncols, 0:nrows], sl[0:nrows, 0:ncols], self.ident[0:nrows, 0:nrows], [k, 'ident'], pk)
        self.cp('dve', dst_ap, pt[0:ncols, 0:nrows], [pk], [dst_key])

    def build(self):
        nc, P = self.nc, self.P
        L = DEPTH
        x = self.inp('x', [NLAT, D])
        ctx = self.inp('ctx', [LCTX, D])
        c2 = self.inp('c2', [2, D])
        identd = self.inp('ident', [128, 128])
        w_ada = self.inp('w_ada', [L, D, 6 * D])
        b_ada = self.inp('b_ada', [L, 6 * D])
        nmg = self.inp('norm_mix_g', [L, D])
        nfg = self.inp('norm_ffn_g', [L, D])
        fng = self.inp('final_norm_g', [D])
        self.w_in = self.inp('w_in', [L, D, D_IN])
        self.w_out = self.inp('w_out', [L, D, D])
        self.fwg = self.inp('ffn_w_gate', [2, D, D_FF])
        self.fwu = self.inp('ffn_w_up', [2, D, D_FF])
        self.fwd = self.inp('ffn_w_down', [2, D_FF, D])
        self.mwr = self.inp('moe_w_router', [2, D, NEXP])
        self.mwg = self.inp('moe_w_gate', [2, NEXP, D, D_FFE])
        self.mwu = self.inp('moe_w_up', [2, NEXP, D, D_FFE])
        self.mwd = self.inp('moe_w_down', [2, NEXP, D_FFE, D])
        seld = self.inp('sel', [NEXP, NEXP * 128])
        for nm, shp in [('mla_q_norm_g', [L, 192]), ('mla_w_uq', [L, 192, 384]), ('mla_kv_norm_g', [L, 128]),
                        ('mla_w_ukv', [L, 128, 512]), ('hg_lb_logits', [L, 2, 256]), ('hg_norm_g', [L, 256]),
                        ('swa_sink', [L, 4]), ('ret_decay_logit', [L, 2, 4]), ('ret_gn_g', [L, 256]),
                        ('ret_gn_b', [L, 256]), ('cos_mla', [32, T]), ('sin_mla', [32, T]), ('cos_swa', [64, T]),
                        ('sin_swa', [64, T]), ('cos_ret', [32, T]), ('sin_ret', [32, T]), ('rbig96', [96, 96]),
                        ('r128', [128, 128]), ('r64b', [64, 64]), ('swa_mask', [6, 128, 512]),
                        ('blk64', [128, 128]), ('ret_rel', [4, 128, 128]), ('ret_colc', [128, 16]),
                        ('ret_rowc', [64, 256]), ('ret_rowm', [64, 16]), ('hg_trie', [2, 128, 136]), ('hg_mt', [2, 128, 128]),
                        ('hg_e', [2, 128, 8])]:
            self.inp(nm, shp)
        if self.h_in:
            hin = self.inp('h_in', [128, 8 * T])
        out = nc.dram_tensor('out', [NLAT, D], F32, kind="ExternalOutput").ap()
        if self.h_out:
            hout = nc.dram_tensor('h_out', [128, 8 * T], F32, kind="ExternalOutput").ap()

        self.hT = hT = self.sb('hT', [128, 8, T], F32)
        self.aT = aT = self.sb('aT', [128, 8, T], BF16)
        self.ident = ident = self.sb('ident', [128, 128], F32)
        self.identb = identb = self.sb('identb', [128, 128], BF16)
        self.ones_bf = ones_bf = self.sb('ones_bf', [128, 128], BF16)
        self.stage = [self.ar('stage%d' % i, [128, 1024], F32).t for i in range(2)]
        self.modT = modT = self.sb('modT', [128, L, 48, 2], F32)
        self.gmix = gmix = self.sb('gmix', [128, L, 8], F32)
        self.gffn = gffn = self.sb('gffn', [128, L, 8], F32)
        self.gfin = gfin = self.sb('gfin', [128, 8], F32)
        self.A1 = A1 = self.sb('A1', [128, 8, 2], F32)
        self.rstd = rstd = self.sb('rstd', [128, 512], F32)
        self.sel = sel = self.sb('sel', [NEXP, NEXP * 128], F32)
        self.eps_col = self.sb('eps_col', [128, 1], F32)
        self.memset('dve', self.eps_col[:, :], EPS, ['eps_col'])
        self.gT = self.sb('gT', [NEXP, T], F32)

        P.dma('sp', ident[:, :], identd, writes=['ident'])
        P.dma('sp', sel[:, :], seld, writes=['sel'])
        self.cp('dve', identb[:, :], ident[:, :], ['ident'], ['identb'])
        self.memset('dve', ones_bf[:, :], 1.0, ['ones_bf'])

        if self.h_in:
            for c in range(8):
                P.dma('sp', hT[:, c, :], hin[:, c * T:(c + 1) * T], writes=[('hT', c, b) for b in range(5)])
        else:
            for t in range(NT):
                src = ctx[t * 128:(t + 1) * 128, :] if t < 2 else x[(t - 2) * 128:(t - 1) * 128, :]
                st = self.stage[t % 2]
                k = ('stage', t % 2)
                P.dma('sp', st[:, :], src, writes=[k])
                b = self.blk_of_tile(t)
                for half in range(2):
                    pt, pk = self.psum('tr', [0, 1])
                    for j in range(4):
                        c = half * 4 + j
                        self.tr(pt[:, j * 128:(j + 1) * 128], st[:, c * 128:(c + 1) * 128], ident[:, :], [k, 'ident'], pk)
                    self.cp('act' if half else 'dve', hT[:, half * 4:half * 4 + 4, t * 128:(t + 1) * 128],
                            pt[:, :].rearrange("p (j n) -> p j n", j=4), [pk],
                            [('hT', half * 4 + j, b) for j in range(4)])

        for l in range(L):
            self.rows_to_cols(nmg[l].rearrange("(c p) -> c p", p=128), 8, 128, gmix[:, l, :], 'gmix')
            self.rows_to_cols(nfg[l].rearrange("(c p) -> c p", p=128), 8, 128, gffn[:, l, :], 'gffn')
        self.rows_to_cols(fng.rearrange("(c p) -> c p", p=128), 8, 128, gfin[:, :], 'gfin')
        bada = self.sb('bada', [128, L, 48], F32)
        for l in range(L):
            self.rows_to_cols(b_ada[l].rearrange("(m p) -> m p", p=128), 48, 128, bada[:, l, :], 'bada')

        self.colv = colv = self.sb('colv', [128, L, 12], F32)
        for l in range(L):
            qg = self.din['mla_q_norm_g'][l]
            self.rows_to_cols(qg[0:128].rearrange("(o p) -> o p", o=1), 1, 128, colv[:, l, 0:1], 'colv')
            self.rows_to_cols(qg[128:192].rearrange("(o p) -> o p", o=1), 1, 64, colv[0:64, l, 1:2], 'colv')
            self.rows_to_cols(self.din['mla_kv_norm_g'][l].rearrange("(o p) -> o p", o=1), 1, 128, colv[:, l, 2:3], 'colv')
            self.rows_to_cols(self.din['hg_norm_g'][l].rearrange("(c p) -> c p", p=128), 2, 128, colv[:, l, 3:5], 'colv')
            self.rows_to_cols(self.din['ret_gn_g'][l].rearrange("(c p) -> c p", p=128), 2, 128, colv[:, l, 5:7], 'colv')
            self.rows_to_cols(self.din['ret_gn_b'][l].rearrange("(c p) -> c p", p=128), 2, 128, colv[:, l, 7:9], 'colv')

        c2s = self.ar('c2s', [2, D], F32)
        scT = self.sb('scT', [128, 8, 2], F32)
        P.dma('sp', c2s[:, :], c2, writes=['c2s'])
        self.act(c2s[:, :], c2s[:, :], AF.Silu, ['c2s'], ['c2s'])
        for c in range(8):
            pt, pk = self.psum('tr', [0, 1])
            self.tr(pt[:, 0:2], c2s[0:2, c * 128:(c + 1) * 128], ident[0:2, 0:2], ['c2s', 'ident'], pk)
            self.cp('dve', scT[:, c, :], pt[:, 0:2], [pk], ['scT'])
        NCB = 768
        wab = [self.ar('wab%d' % i, [128, 8, NCB], F32) for i in range(2)]
        it = 0
        for l in self.layers:
            for cb in range(6 * D // NCB):
                wt = wab[it % 2]
                it += 1
                P.dma('sp', wt[:, :, :], w_ada[l][:, cb * NCB:(cb + 1) * NCB].rearrange("(c p) n -> p c n", p=128),
                      writes=[wt.k])
                pt, pk = self.psum('tr', [0, 1])
                for m in range(NCB // 128):
                    self.mmg(pt[:, m * 2:(m + 1) * 2],
                             [(wt[:, kc, m * 128:(m + 1) * 128], scT[:, kc, :]) for kc in range(8)],
                             [wt.k, 'scT'], pk)
                nm = NCB // 128
                self.tt(modT[:, l, cb * nm:(cb + 1) * nm, :], pt[:, 0:2 * nm].rearrange("p (m r) -> p m r", r=2),
                        bada[:, l, cb * nm:(cb + 1) * nm].unsqueeze(2).broadcast_to([128, nm, 2]), ALU.add,
                        [pk, 'bada'], [('modT', l)])

        for l in self.layers:
            if len(self.mixers) > 0:
                self.phase()
                self.norm_mod(l, gmix, 0, 1, False)
                self.mixer_phase(l)
            if self.ffn:
                moe = (l % 2 == 1)
                self.phase()
                self.norm_mod(l, gffn, 3, 4, moe)
                if moe:
                    self.moe_route(l)
                self.phase()
                if moe:
                    self.moe_ffn(l)
                else:
                    self.dense_ffn(l)
        self.phase()

        if self.h_out:
            for c in range(8):
                P.dma('sp', hout[:, c * T:(c + 1) * T], hT[:, c, :], reads=[('hT', c, b) for b in range(5)],
                      is_output=True)
        self.final_out(out, gfin)
        P.emit()
        return nc

    def blk_of_tile(self, t):
        return 0 if t < 2 else 1 + (t - 2) // 4

    def norm_mod(self, l, gvec, i_shift, i_scale, want_f32):
        P = self.P
        hT, aT, modT, A1, rstd = self.hT, self.aT, self.modT, self.A1, self.rstd
        self.stt(A1[:, :, :], modT[:, l, i_scale * 8:(i_scale + 1) * 8, :], 1.0,
                 gvec[:, l, :].unsqueeze(2).broadcast_to([128, 8, 2]), ALU.add, ALU.mult,
                 [('modT', l), gvec.k], ['A1'])
        nsq = self.ar('nsq', [128, 8, 512], BF16)
        ntmp = self.ar('ntmp', [128, 8, 512], F32)
        if want_f32:
            self.a32 = self.ar('a32', [128, 8, 512], F32)
            self.wr = self.ar('wr', [128, 8, NEXP], F32)
            self.lgT = self.ar('lgT', [NEXP, T], F32)
            self.P.dma('sp', self.wr[:, :, :], self.mwr[l // 2].rearrange("(c p) e -> p c e", p=128), writes=[self.wr.k])
        for b, (t0, n) in enumerate(BLKS):
            r = 1 if b == 0 else 0
            hk = [('hT', c, b) for c in range(8)]
            self.act(nsq[:, :, 0:n], hT[:, :, t0:t0 + n], AF.Square, hk, [nsq.k])
            pt, pk = self.psum('nrm', [2, 3])
            self.mmg(pt[:, 0:n], [(self.ones_bf[:, :], nsq[:, c, 0:n]) for c in range(8)], [nsq.k, 'ones_bf'], pk)
            self.act(rstd[:, 0:n], pt[:, 0:n], AF.Sqrt, [pk], ['rstd'], bias=self.eps_col[:, 0:1], scale=1.0 / D)
            self.recip(rstd[:, 0:n], rstd[:, 0:n], ['rstd'], ['rstd'])
            self.tt(ntmp[:, :, 0:n], hT[:, :, t0:t0 + n], rstd[:, 0:n].unsqueeze(1).broadcast_to([128, 8, n]),
                    ALU.mult, hk + ['rstd'], [ntmp.k])
            for c in range(8):
                if want_f32:
                    self.ts(self.a32[:, c, 0:n], ntmp[:, c, 0:n], A1[:, c, r:r + 1],
                            modT[:, l, i_shift * 8 + c, r:r + 1], ALU.mult, ALU.add,
                            [ntmp.k, 'A1', ('modT', l)], [('a32', c)])
                    self.cp('act', aT[:, c, t0:t0 + n], self.a32[:, c, 0:n], [('a32', c)], [('aT', b)])
                else:
                    self.ts(aT[:, c, t0:t0 + n], ntmp[:, c, 0:n], A1[:, c, r:r + 1],
                            modT[:, l, i_shift * 8 + c, r:r + 1], ALU.mult, ALU.add,
                            [ntmp.k, 'A1', ('modT', l)], [('aT', b)])
            if want_f32:
                self.route_block(l, b, t0, n)

    def load_w(self, dst, dram, kchunks, ncols, key):
        step = max(1, 4096 // ncols)
        for k0 in range(0, kchunks, step):
            k1 = min(kchunks, k0 + step)
            self.P.dma('pool', dst[:, k0:k1, :], dram[k0 * 128:k1 * 128, :].rearrange("(c p) n -> p c n", p=128),
                       writes=[key])

    def ffn_slices(self, dff):
        s = []
        f = 0
        while f < dff:
            n = min(512, dff - f)
            s.append((f, n))
            f += n
        return s

    def ffn_core(self, l, wg_d, wu_d, wd_d, dff, gate_tile=None, tagbase=''):
        P = self.P
        hT, aT, modT = self.hT, self.aT, self.modT
        for (f0, fn) in self.ffn_slices(dff):
            wg, wu, wd = self.fw[self.fit % 2]
            self.fit += 1
            nj = fn // 128
            self.load_w(wg[:, :, 0:fn], wg_d[:, f0:f0 + fn], 8, fn, wg.k)
            self.load_w(wu[:, :, 0:fn], wu_d[:, f0:f0 + fn], 8, fn, wu.k)
            self.load_w(wd[:, 0:nj, :], wd_d[f0:f0 + fn, :], nj, 1024, wd.k)
            def gu(b, t0, n):
                hid = self.fhid[self.fh % 2]
                self.fh += 1
                for j in range(nj):
                    pg, pgk = self.psum('ffg', [0, 1])
                    pu, puk = self.psum('ffu', [2, 3])
                    self.mmg(pg[:, 0:n], [(wg[:, kc, j * 128:(j + 1) * 128], aT[:, kc, t0:t0 + n]) for kc in range(8)],
                             [wg.k, ('aT', b)], pgk)
                    self.mmg(pu[:, 0:n], [(wu[:, kc, j * 128:(j + 1) * 128], aT[:, kc, t0:t0 + n]) for kc in range(8)],
                             [wu.k, ('aT', b)], puk)
                    sg = self.fsg[self.fj % 2]
                    self.fj += 1
                    self.act(sg[:, 0:n], pg[:, 0:n], AF.Silu, [pgk], [sg.k])
                    if gate_tile is not None:
                        self.tt(sg[:, 0:n], sg[:, 0:n], gate_tile[:, t0:t0 + n], ALU.mult, [sg.k, gate_tile.k], [sg.k])
                    self.tt(hid[:, j, 0:n], pu[:, 0:n], sg[:, 0:n], ALU.mult, [puk, sg.k], [(hid.k, j)])
                return hid

            def down(b, t0, n, hid):
                r = 1 if b == 0 else 0
                for oc in range(8):
                    py, pyk = self.psum('ffy', [4, 5, 6, 7])
                    self.mmg(py[:, 0:n], [(wd[:, j, oc * 128:(oc + 1) * 128], hid[:, j, 0:n]) for j in range(nj)],
                             [wd.k] + [(hid.k, j) for j in range(nj)], pyk)
                    self.stt(hT[:, oc, t0:t0 + n], py[:, 0:n], modT[:, l, 5 * 8 + oc, r:r + 1], hT[:, oc, t0:t0 + n],
                             ALU.mult, ALU.add, [pyk, ('modT', l), ('hT', oc, b)], [('hT', oc, b)])

            prev = None
            for b, (t0, n) in enumerate(BLKS):
                hid = gu(b, t0, n)
                if prev is not None:
                    down(*prev)
                prev = (b, t0, n, hid)
            down(*prev)

    def ffn_alloc(self):
        self.fw = [(self.ar('fwg%d' % i, [128, 8, 512], BF16), self.ar('fwu%d' % i, [128, 8, 512], BF16),
                    self.ar('fwd%d' % i, [128, 4, 1024], BF16)) for i in range(2)]
        self.fsg = [self.ar('fsg%d' % i, [128, 512], BF16) for i in range(2)]
        self.fhid = [self.ar('fhid%d' % i, [128, 4, 512], BF16) for i in range(2)]
        self.fit = 0
        self.fj = 0
        self.fh = 0

    def dense_ffn(self, l):
        i = l // 2
        self.ffn_alloc()
        self.ffn_core(l, self.fwg[i], self.fwu[i], self.fwd[i], D_FF)

    def route_block(self, l, b, t0, n):
        pt, pk = self.psum('nrm', [2, 3])
        self.mmg(pt[0:NEXP, 0:n], [(self.wr[:, c, :], self.a32[:, c, 0:n]) for c in range(8)],
                 [self.wr.k] + [('a32', c) for c in range(8)], pk)
        self.cp('dve', self.lgT[:, t0:t0 + n], pt[0:NEXP, 0:n], [pk], ['lgT'])

    def moe_route(self, l):
        ident = self.ident
        lg = self.ar('lg', [128, NT, NEXP], F32)
        mk1 = self.ar('mk1', [128, NT, NEXP], F32)
        mk2 = self.ar('mk2', [128, NT, NEXP], F32)
        m1 = self.ar('m1', [128, NT], F32)
        m2 = self.ar('m2', [128, NT], F32)
        w1 = self.ar('w1', [128, NT], F32)
        gT = self.gT
        lg.k, mk1.k, mk2.k, m1.k, m2.k, w1.k = 'lg', 'mk1', 'mk2', 'm1', 'm2', 'w1'
        for t in range(NT):
            pt, pk = self.psum('tr', [0, 1])
            self.tr(pt[:, 0:NEXP], self.lgT[0:NEXP, t * 128:(t + 1) * 128], ident[0:NEXP, 0:NEXP], ['lgT', 'ident'], pk)
            self.cp('dve', lg[:, t, :], pt[:, 0:NEXP], [pk], ['lg'])
        P = self.P
        P.op('dve', lambda e: e.tensor_reduce(out=m1[:, :], in_=lg[:, :, :], axis=AX.X, op=ALU.max), reads=['lg'], writes=['m1'])
        self.tt(mk1[:, :, :], lg[:, :, :], m1[:, :].unsqueeze(2).broadcast_to([128, NT, NEXP]), ALU.is_equal,
                ['lg', 'm1'], ['mk1'])
        self.stt(mk2[:, :, :], mk1[:, :, :], -1e30, lg[:, :, :], ALU.mult, ALU.add, ['mk1', 'lg'], ['mk2'])
        P.op('dve', lambda e: e.tensor_reduce(out=m2[:, :], in_=mk2[:, :, :], axis=AX.X, op=ALU.max), reads=['mk2'], writes=['m2'])
        self.tt(mk2[:, :, :], mk2[:, :, :], m2[:, :].unsqueeze(2).broadcast_to([128, NT, NEXP]), ALU.is_equal,
                ['mk2', 'm2'], ['mk2'])
        self.tt(w1[:, :], m1[:, :], m2[:, :], ALU.subtract, ['m1', 'm2'], ['w1'])
        self.act(w1[:, :], w1[:, :], AF.Sigmoid, ['w1'], ['w1'])
        self.tt(mk1[:, :, :], mk1[:, :, :], w1[:, :].unsqueeze(2).broadcast_to([128, NT, NEXP]), ALU.mult,
                ['mk1', 'w1'], ['mk1'])
        self.ts(w1[:, :], w1[:, :], -1.0, 1.0, ALU.mult, ALU.add, ['w1'], ['w1'])
        self.tt(mk2[:, :, :], mk2[:, :, :], w1[:, :].unsqueeze(2).broadcast_to([128, NT, NEXP]), ALU.mult,
                ['mk2', 'w1'], ['mk2'])
        self.tt(mk1[:, :, :], mk1[:, :, :], mk2[:, :, :], ALU.add, ['mk1', 'mk2'], ['mk1'])
        for t in range(NT):
            pt, pk = self.psum('tr', [0, 1])
            self.tr(pt[0:NEXP, 0:128], mk1[:, t, :], ident[:, :], ['mk1', 'ident'], pk)
            self.cp('dve', gT[:, t * 128:(t + 1) * 128], pt[0:NEXP, 0:128], [pk], ['gT'])

    def moe_ffn(self, l):
        i = l // 2
        gT = self.gT
        gbc = [self.ar('gbc%d' % j, [128, T], BF16) for j in range(2)]
        self.ffn_alloc()
        for e_ in range(NEXP):
            gb = gbc[e_ % 2]
            for b, (t0, n) in enumerate(BLKS):
                pt, pk = self.psum('nrm', [2, 3])
                self.mmg(pt[:, 0:n], [(self.sel[:, e_ * 128:(e_ + 1) * 128], gT[:, t0:t0 + n])], ['sel', 'gT'], pk)
                self.cp('act', gb[:, t0:t0 + n], pt[:, 0:n], [pk], [gb.k])
            self.ffn_core(l, self.mwg[i][e_], self.mwu[i][e_], self.mwd[i][e_], D_FFE, gate_tile=gb)


    def mixer_phase(self, l):
        if 0 in self.mixers:
            self.phase()
            self.mla(l)
        if 2 in self.mixers:
            self.phase()
            self.swa(l)
        if 3 in self.mixers:
            self.phase()
            self.ret(l)
        if 1 in self.mixers:
            self.phase()
            self.hgrn(l)

    def colvec(self, dst_ap, dram_vec, n, key):
        self.P.dma('sp', dst_ap, dram_vec.rearrange("(p o) -> p o", o=1), writes=[key])

    def proj(self, out, w, wcols, rhs_fn, reads, pk, kchunks=8):
        c0, c1 = wcols
        self.mmg(out, [(w[:, kc, c0:c1], rhs_fn(kc)) for kc in range(kchunks)], reads, pk)

    def rstd_from(self, dst, src_ps, n_feat, reads, writes):
        p = dst.shape[0] if hasattr(dst, 'shape') else 128
        self.act(dst, src_ps, AF.Sqrt, reads, writes, bias=self.eps_col[0:p, 0:1], scale=1.0 / n_feat)
        self.recip(dst, dst, writes, writes)

    def outproj(self, l, wo, o_blk, okeys, b, t0, n):
        r = 1 if b == 0 else 0
        hT, modT = self.hT, self.modT
        for oc in range(8):
            py, pyk = self.psum('ffy', [4, 5, 6, 7])
            self.mmg(py[:, 0:n], [(wo[:, p, oc * 128:(oc + 1) * 128], o_blk[:, p, 0:n]) for p in range(2)],
                     [wo.k] + okeys, pyk)
            self.stt(hT[:, oc, t0:t0 + n], py[:, 0:n], modT[:, l, 2 * 8 + oc, r:r + 1], hT[:, oc, t0:t0 + n],
                     ALU.mult, ALU.add, [pyk, ('modT', l), ('hT', oc, b)], [('hT', oc, b)])

    def load_wo(self, l, grp):
        wo = self.ar('wo', [128, 2, 1024], BF16)
        self.load_w(wo[:, :, :], self.w_out[l][grp * 256:(grp + 1) * 256, :], 2, 1024, wo.k)
        return wo

    def attn(self, spairs, vfn, tiles, scale, n, half, out_ap, out_key, reads, sink_col=None):
        P = self.P
        pO, pOk = self.psum('aO', [4, 6])
        pD, pDk = self.psum('aD', [5, 7])
        nt = len(tiles)

        def smm(i):
            pS, pSk = self.psum('aS', [0, 1])
            self.mmg(pS[:, 0:n], spairs(tiles[i][0]), reads, pSk)
            return pS, pSk

        nxt = smm(0)
        for i, (tile, mask) in enumerate(tiles):
            pS, pSk = nxt
            if i + 1 < nt:
                nxt = smm(i + 1)
            pt = self.PT[self.pti % 3]
            self.pti += 1
            self.act(pt[:, 0:n], pS[:, 0:n], AF.Exp, [pSk], [pt.k], scale=scale)
            if mask is not None:
                self.tt(pt[:, 0:n], pt[:, 0:n], mask[:, 0:n], ALU.mult, [pt.k, 'swamask'], [pt.k])
            v = vfn(tile)
            P.op('pe', lambda e, v=v, pt=pt, s=(i == 0), t=(i == nt - 1): e.matmul(pO[:, 0:n], lhsT=v, rhs=pt[:, 0:n], start=s, stop=t),
                 reads=reads + [pt.k], writes=[pOk], inc=False)
            P.op('pe', lambda e, pt=pt, s=(i == 0), t=(i == nt - 1): e.matmul(pD[:, 0:n], lhsT=self.ones_bf[:, :], rhs=pt[:, 0:n], start=s, stop=t),
                 reads=[pt.k, 'ones_bf'], writes=[pOk, pDk], inc=True)
        r0 = half * 64
        rd = self.rd
        if sink_col is not None:
            self.ts(rd[r0:r0 + 64, 0:n], pD[r0:r0 + 64, 0:n], sink_col[r0:r0 + 64, :], None, ALU.add, None, [pDk, 'esink'], ['rd'])
            self.recip(rd[r0:r0 + 64, 0:n], rd[r0:r0 + 64, 0:n], ['rd'], ['rd'])
        else:
            self.recip(rd[r0:r0 + 64, 0:n], pD[r0:r0 + 64, 0:n], [pDk], ['rd'])
        self.tt(out_ap, pO[r0:r0 + 64, 0:n], rd[r0:r0 + 64, 0:n], ALU.mult, [pOk, 'rd'], [out_key])

    def mla(self, l):
        P = self.P
        aT = self.aT
        wm = self.ar('wm', [128, 8, 352], BF16)
        self.load_w(wm[:, :, :], self.w_in[l][:, 0:352], 8, 352, wm.k)
        wuq = self.ar('wuq', [128, 2, 384], BF16)
        P.dma('pool', wuq[:, 0, :], self.din['mla_w_uq'][l][0:128, :], writes=[wuq.k])
        P.dma('pool', wuq[0:64, 1, :], self.din['mla_w_uq'][l][128:192, :], writes=[wuq.k])
        wukv = self.ar('wukv', [128, 512], BF16)
        P.dma('pool', wukv[:, :], self.din['mla_w_ukv'][l], writes=[wukv.k])
        cv = self.colv
        self.ts(wuq[:, 0, :], wuq[:, 0, :], cv[:, l, 0:1], None, ALU.mult, None, [wuq.k, 'colv'], [wuq.k])
        self.ts(wuq[0:64, 1, :], wuq[0:64, 1, :], cv[0:64, l, 1:2], None, ALU.mult, None, [wuq.k, 'colv'], [wuq.k])
        self.ts(wukv[:, :], wukv[:, :], cv[:, l, 2:3], None, ALU.mult, None, [wukv.k, 'colv'], [wukv.k])
        r32 = self.ar('r32', [32, 32], BF16)
        P.dma('pool', r32[:, :], self.din['r64b'][0:32, 0:32], writes=[r32.k])
        wo = self.load_wo(l, 0)
        kN = self.ar('kN', [64, 4, T], BF16)
        kP = self.ar('kP', [32, T], BF16)
        vt = self.ar('vt', [128, NT, 256], BF16)
        cs = self.ar('cs', [32, 2, 512], F32)
        kvl = self.ar('kvl', [128, 512], BF16)
        sq = self.ar('sq', [128, 2, 512], BF16)
        rs = self.ar('rs', [128, 512], F32)
        rsc = self.ar('rsc', [128, NT], F32)
        xb = self.ar('xb', [32, 512], BF16)
        t1 = self.ar('t1', [32, 512], F32)
        t2 = self.ar('t2', [32, 512], F32)
        qlb = self.ar('qlb', [128, 2, 512], BF16)
        qp32 = self.ar('qp32', [32, 512], F32)
        qn = [self.ar('qn%d' % i, [64, 512], BF16) for i in range(2)]
        qp = [self.ar('qp%d' % i, [32, 512], BF16) for i in range(2)]
        oblk = [self.ar('oblk%d' % i, [128, 2, 512], BF16) for i in range(2)]
        self.PT = [self.ar('PT%d' % i, [128, 512], BF16) for i in range(3)]
        self.pti = 0
        self.rd = self.ar('rd', [128, 512], F32)
        self.rd.k = 'rd'
        cosd, sind = self.din['cos_mla'], self.din['sin_mla']

        def rope32(dst, src, skey, n, wkey):
            self.cp('act', xb[:, 0:n], src, [skey], [xb.k])
            pr, prk = self.psum('mB', [2, 3])
            self.mmg(pr[0:32, 0:n], [(r32[:, :], xb[:, 0:n])], [r32.k, xb.k], prk)
            RV = int(os.environ.get('ROPEV', '9'))
            if RV == 1:
                self.tt(t1[:, 0:n], src, src, ALU.mult, [skey], [t1.k])
                return
            if RV == 5:
                return
            if RV == 3:
                self.tt(t1[:, 0:n], rs[0:32, 0:n], rs[0:32, 0:n], ALU.mult, [rs.k], [t1.k])
                return
            if RV == 4:
                self.tt(rs[0:32, 0:n], cs[:, 0, 0:n], cs[:, 0, 0:n], ALU.mult, [cs.k, rs.k], [rs.k])
                return
            if RV == 2:
                self.tt(t1[:, 0:n], cs[:, 0, 0:n], cs[:, 0, 0:n], ALU.mult, [cs.k], [t1.k])
                return
            self.tt(t1[:, 0:n], src, cs[:, 0, 0:n], ALU.mult, [skey, cs.k, xb.k], [t1.k])
            self.tt(t2[:, 0:n], pr[0:32, 0:n], cs[:, 1, 0:n], ALU.mult, [prk, cs.k], [t2.k])
            self.tt(dst, t1[:, 0:n], t2[:, 0:n], ALU.add, [t1.k, t2.k], [wkey])

        for b, (t0, n) in enumerate(BLKS):
            ak = ('aT', b)
            P.dma('sp', cs[:, 0, 0:n], cosd[:, t0:t0 + n], writes=[cs.k])
            P.dma('sp', cs[:, 1, 0:n], sind[:, t0:t0 + n], writes=[cs.k])
            pk_, pkk = self.psum('mA', [0, 1])
            self.proj(pk_[:, 0:n], wm, (192, 320), lambda kc: aT[:, kc, t0:t0 + n], [wm.k, ak], pkk)
            self.cp('act', kvl[:, 0:n], pk_[:, 0:n], [pkk], [kvl.k])
            self.act(sq[:, 0, 0:n], pk_[:, 0:n], AF.Square, [pkk], [sq.k])
            ps_, psk = self.psum('mB', [2, 3])
            self.mmg(ps_[:, 0:n], [(self.ones_bf[:, :], sq[:, 0, 0:n])], [sq.k, 'ones_bf'], psk)
            self.rstd_from(rs[:, 0:n], ps_[:, 0:n], 128.0, [psk], [rs.k])
            pc, pck = self.psum('mB', [2, 3])
            for tt_ in range(n // 128):
                self.mmg(pc[:, tt_:tt_ + 1], [(sq[:, 0, tt_ * 128:(tt_ + 1) * 128], self.ones_bf[:, 0:1])], [sq.k, 'ones_bf'], pck)
            tl0 = t0 // 128
            self.rstd_from(rsc[:, tl0:tl0 + n // 128], pc[:, 0:n // 128], 128.0, [pck], [rsc.k])
            for tt_ in range(n // 128):
                tile = t0 // 128 + tt_
                pv, pvk = self.psum('mA', [0, 1])
                for h in range(4):
                    self.mmg(pv[:, h * 64:(h + 1) * 64], [(kvl[:, tt_ * 128:(tt_ + 1) * 128], wukv[:, h * 128 + 64:h * 128 + 128])],
                             [kvl.k, wukv.k], pvk)
                self.ts(vt[:, tile, :], pv[:, 0:256], rsc[:, tile:tile + 1], None, ALU.mult, None, [pvk, rsc.k], [vt.k])
            for h in range(4):
                pk2, pk2k = self.psum('mA', [0, 1])
                self.mmg(pk2[0:64, 0:n], [(wukv[:, h * 128:h * 128 + 64], kvl[:, 0:n])], [kvl.k, wukv.k], pk2k)
                self.tt(kN[:, h, t0:t0 + n], pk2[0:64, 0:n], rs[0:64, 0:n], ALU.mult, [pk2k, rs.k], [kN.k])
            pp, ppk = self.psum('mA', [0, 1])
            self.proj(pp[0:32, 0:n], wm, (320, 352), lambda kc: aT[:, kc, t0:t0 + n], [wm.k, ak], ppk)
            rope32(kP[:, t0:t0 + n], pp[0:32, 0:n], ppk, n, kP.k)
        STOP = int(os.environ.get('MLA_STOP', '9'))
        if STOP <= 2:
            return
        scale = 96.0 ** -0.5
        qi = 0
        for b, (t0, n) in enumerate(BLKS):
            ak = ('aT', b)
            P.dma('sp', cs[:, 0, 0:n], cosd[:, t0:t0 + n], writes=[cs.k])
            P.dma('sp', cs[:, 1, 0:n], sind[:, t0:t0 + n], writes=[cs.k])
            p0, p0k = self.psum('mA', [0, 1])
            self.proj(p0[:, 0:n], wm, (0, 128), lambda kc: aT[:, kc, t0:t0 + n], [wm.k, ak], p0k)
            p1, p1k = self.psum('mA', [0, 1])
            self.proj(p1[0:64, 0:n], wm, (128, 192), lambda kc: aT[:, kc, t0:t0 + n], [wm.k, ak], p1k)
            self.cp('act', qlb[:, 0, 0:n], p0[:, 0:n], [p0k], [qlb.k])
            self.cp('act', qlb[0:64, 1, 0:n], p1[0:64, 0:n], [p1k], [qlb.k])
            self.act(sq[:, 0, 0:n], p0[:, 0:n], AF.Square, [p0k], [sq.k])
            self.act(sq[0:64, 1, 0:n], p1[0:64, 0:n], AF.Square, [p1k], [sq.k])
            ps_, psk = self.psum('mB', [2, 3])
            self.mmg(ps_[:, 0:n], [(self.ones_bf[:, :], sq[:, 0, 0:n]), (self.ones_bf[0:64, :], sq[0:64, 1, 0:n])],
                     [sq.k, 'ones_bf'], psk)
            self.rstd_from(rs[:, 0:n], ps_[:, 0:n], 192.0, [psk], [rs.k])
            ob = oblk[b % 2]
            tiles = [(0, None), (1, None)] if b == 0 else [(t, None) for t in range(NT)]
            def qprep(h, n=n):
                nonlocal qi
                qn_, qp_ = qn[qi % 2], qp[qi % 2]
                qi += 1
                pq, pqk = self.psum('mB', [2, 3])
                self.mmg(pq[0:64, 0:n], [(wuq[:, 0, h * 96:h * 96 + 64], qlb[:, 0, 0:n]),
                                         (wuq[0:64, 1, h * 96:h * 96 + 64], qlb[0:64, 1, 0:n])], [wuq.k, qlb.k], pqk)
                self.tt(qn_[:, 0:n], pq[0:64, 0:n], rs[0:64, 0:n], ALU.mult, [pqk, rs.k], [qn_.k])
                pq2, pq2k = self.psum('mB', [2, 3])
                self.mmg(pq2[0:32, 0:n], [(wuq[:, 0, h * 96 + 64:h * 96 + 96], qlb[:, 0, 0:n]),
                                          (wuq[0:64, 1, h * 96 + 64:h * 96 + 96], qlb[0:64, 1, 0:n])], [wuq.k, qlb.k], pq2k)
                self.tt(qp32[:, 0:n], pq2[0:32, 0:n], rs[0:32, 0:n], ALU.mult, [pq2k, rs.k], [qp32.k])
                rope32(qp_[:, 0:n], qp32[:, 0:n], qp32.k, n, qp_.k)
                return qn_, qp_

            nxtq = qprep(0)
            for h in range(4):
                qn_, qp_ = nxtq
                if h + 1 < 4:
                    nxtq = qprep(h + 1)
                p_, hh = h // 2, h % 2
                self.attn(lambda t, h=h, qn_=qn_, qp_=qp_, n=n: [(kN[:, h, t * 128:(t + 1) * 128], qn_[:, 0:n]),
                                                                 (kP[:, t * 128:(t + 1) * 128], qp_[:, 0:n])],
                          lambda t, p_=p_: vt[:, t, p_ * 128:(p_ + 1) * 128], tiles, scale, n, hh,
                          ob[hh * 64:hh * 64 + 64, p_, 0:n], (ob.k, h), [kN.k, kP.k, vt.k, qn_.k, qp_.k])
            if STOP <= 5:
                continue
            self.outproj(l, wo, ob, [(ob.k, h) for h in range(4)], b, t0, n)

    def swa(self, l):
        P = self.P
        aT = self.aT
        ws = self.ar('ws', [128, 8, 512], BF16)
        self.load_w(ws[:, :, :], self.w_in[l][:, C_SQ:C_SQ + 512], 8, 512, ws.k)
        r128 = self.ar('r128', [128, 128], BF16)
        P.dma('pool', r128[:, :], self.din['r128'], writes=[r128.k])
        wo = self.load_wo(l, 2)
        msk = self.ar('swamask', [128, 6, 512], BF16)
        msk.k = 'swamask'
        for r in range(6):
            P.dma('pool', msk[:, r, :], self.din['swa_mask'][r], writes=['swamask'])
        esink = self.ar('esink', [128, 4], F32)
        esink.k = 'esink'
        P.dma('sp', esink[:, :], self.din['swa_sink'][l:l + 1, :].broadcast_to([128, 4]), writes=['esink'])
        self.act(esink[:, :], esink[:, :], AF.Exp, ['esink'], ['esink'])
        kx = self.ar('kx', [128, T], BF16)
        kxs = self.ar('kxs', [128, T], BF16)
        vn = self.ar('vn', [128, NT, 128], BF16)
        vs = self.ar('vs', [128, NT, 128], BF16)
        cs = self.ar('cs', [128, 2, 512], F32)
        xb = self.ar('xb', [128, 512], BF16)
        t1 = self.ar('t1', [128, 512], F32)
        t2 = self.ar('t2', [128, 512], F32)
        qx = [self.ar('qx%d' % i, [128, 2, 512], BF16) for i in range(2)]
        oblk = [self.ar('oblk%d' % i, [128, 2, 512], BF16) for i in range(2)]
        self.PT = [self.ar('PT%d' % i, [128, 512], BF16) for i in range(3)]
        self.pti = 0
        self.rd = self.ar('rd', [128, 512], F32)
        self.rd.k = 'rd'
        cosd, sind = self.din['cos_swa'], self.din['sin_swa']

        def rope(dst, ps_x, psk, n, wkey):
            self.cp('act', xb[:, 0:n], ps_x[:, 0:n], [psk], [xb.k])
            pr, prk = self.psum('mB', [2, 3])
            self.mmg(pr[:, 0:n], [(r128[:, :], xb[:, 0:n])], [r128.k, xb.k], prk)
            self.tt(t1[:, 0:n], ps_x[:, 0:n], cs[:, 0, 0:n], ALU.mult, [psk, cs.k], [t1.k])
            self.tt(t2[:, 0:n], pr[:, 0:n], cs[:, 1, 0:n], ALU.mult, [prk, cs.k], [t2.k])
            self.tt(dst, t1[:, 0:n], t2[:, 0:n], ALU.add, [t1.k, t2.k], [wkey])

        for b, (t0, n) in enumerate(BLKS):
            ak = ('aT', b)
            for hf in range(2):
                P.dma('sp', cs[hf * 64:(hf + 1) * 64, 0, 0:n], cosd[:, t0:t0 + n], writes=[cs.k])
                P.dma('sp', cs[hf * 64:(hf + 1) * 64, 1, 0:n], sind[:, t0:t0 + n], writes=[cs.k])
            pk_, pkk = self.psum('mA', [0, 1])
            self.proj(pk_[:, 0:n], ws, (256, 384), lambda kc: aT[:, kc, t0:t0 + n], [ws.k, ak], pkk)
            rope(kx[:, t0:t0 + n], pk_, pkk, n, kx.k)
            for tt_ in range(n // 128):
                tile = t0 // 128 + tt_
                pv, pvk = self.psum('mA', [0, 1])
                self.mmg(pv[:, 0:128], [(aT[:, kc, tile * 128:(tile + 1) * 128], ws[:, kc, 384:512]) for kc in range(8)],
                         [ws.k, ak], pvk)
                self.cp('act', vn[:, tile, :], pv[:, 0:128], [pvk], [vn.k])
                self.cp('dve', vs[:, tile, 0:64], pv[:, 64:128], [pvk], [vs.k])
                self.cp('dve', vs[:, tile, 64:128], pv[:, 0:64], [pvk], [vs.k])
        P.dma('sp', kxs[0:64, :], kx[64:128, :], reads=[kx.k], writes=[kxs.k])
        P.dma('sp', kxs[64:128, :], kx[0:64, :], reads=[kx.k], writes=[kxs.k])
        scale = 64.0 ** -0.5
        for b, (t0, n) in enumerate(BLKS):
            ak = ('aT', b)
            for hf in range(2):
                P.dma('sp', cs[hf * 64:(hf + 1) * 64, 0, 0:n], cosd[:, t0:t0 + n], writes=[cs.k])
                P.dma('sp', cs[hf * 64:(hf + 1) * 64, 1, 0:n], sind[:, t0:t0 + n], writes=[cs.k])
            q_ = qx[b % 2]
            for p_ in range(2):
                pq, pqk = self.psum('mA', [0, 1])
                self.proj(pq[:, 0:n], ws, (p_ * 128, (p_ + 1) * 128), lambda kc: aT[:, kc, t0:t0 + n], [ws.k, ak], pqk)
                rope(q_[:, p_, 0:n], pq, pqk, n, q_.k)
            ob = oblk[b % 2]
            if b == 0:
                tiles = [(0, None), (1, None)]
            else:
                g0 = t0 // 128
                tiles = [(0, None), (1, None)]
                for r in range(-1, 5):
                    j = g0 + r
                    if 2 <= j <= 17:
                        tiles.append((j, msk[:, r + 1, :]))
            for h in range(4):
                g, hh, p_ = h // 2, h % 2, h // 2
                ksrc = kx if g == hh else kxs
                vsrc = vn if g == hh else vs
                self.attn(lambda t, ksrc=ksrc, hh=hh, q_=q_, p_=p_, n=n: [(ksrc[hh * 64:hh * 64 + 64, t * 128:(t + 1) * 128],
                                                                               q_[hh * 64:hh * 64 + 64, p_, 0:n])],
                          lambda t, vsrc=vsrc: vsrc[:, t, :], tiles, scale, n, hh,
                          ob[hh * 64:hh * 64 + 64, p_, 0:n], (ob.k, h), [kx.k, kxs.k, vn.k, vs.k, q_.k],
                          sink_col=esink[:, h:h + 1])
            self.outproj(l, wo, ob, [(ob.k, h) for h in range(4)], b, t0, n)

    def headnorm_gate(self, l, o32, okey, n, t0, b, w, gcols, center, gcol, bcol, oblk, ak):
        aT = self.aT
        ob16 = self.hn_b16
        c32 = self.hn_c32
        blk64 = self.blk64
        for p in range(2):
            src = o32[:, p, 0:n]
            if center:
                self.cp('act', ob16[:, 0:n], src, [okey], [ob16.k])
                pm, pmk = self.psum('mB', [2, 3])
                self.mmg(pm[:, 0:n], [(blk64[:, :], ob16[:, 0:n])], [ob16.k, blk64.k], pmk)
                self.stt(c32[:, 0:n], pm[:, 0:n], -1.0, src, ALU.mult, ALU.add, [okey, pmk], [c32.k])
                cs_ = c32[:, 0:n]
                ck = c32.k
            else:
                cs_ = src
                ck = okey
            self.act(ob16[:, 0:n], cs_, AF.Square, [ck], [ob16.k])
            pv_, pvk = self.psum('mB', [2, 3])
            self.mmg(pv_[:, 0:n], [(blk64[:, :], ob16[:, 0:n])], [ob16.k, blk64.k], pvk)
            rs = self.hn_rs
            self.rstd_from(rs[:, 0:n], pv_[:, 0:n], 1.0, [pvk], [rs.k])
            self.tt(c32[:, 0:n], cs_, rs[:, 0:n], ALU.mult, [ck, rs.k], [c32.k])
            if bcol is not None:
                self.ts(c32[:, 0:n], c32[:, 0:n], self.colv[:, l, gcol + p:gcol + p + 1],
                        self.colv[:, l, bcol + p:bcol + p + 1], ALU.mult, ALU.add, [c32.k, 'colv'], [c32.k])
            else:
                self.ts(c32[:, 0:n], c32[:, 0:n], self.colv[:, l, gcol + p:gcol + p + 1], None, ALU.mult, None,
                        [c32.k, 'colv'], [c32.k])
            pg, pgk = self.psum('mA', [0, 1])
            self.proj(pg[:, 0:n], w, (gcols + p * 128, gcols + (p + 1) * 128), lambda kc: aT[:, kc, t0:t0 + n], [w.k, ak], pgk)
            sg = self.hn_sg
            self.act(sg[:, 0:n], pg[:, 0:n], AF.Silu, [pgk], [sg.k])
            self.tt(oblk[:, p, 0:n], c32[:, 0:n], sg[:, 0:n], ALU.mult, [c32.k, sg.k], [(oblk.k, p)])

    def hn_alloc(self):
        self.hn_b16 = self.ar('hn_b16', [128, 512], BF16)
        self.hn_c32 = self.ar('hn_c32', [128, 512], F32)
        self.hn_rs = self.ar('hn_rs', [128, 512], F32)
        self.hn_sg = self.ar('hn_sg', [128, 512], BF16)
        self.blk64 = self.ar('blk64', [128, 128], BF16)
        self.P.dma('pool', self.blk64[:, :], self.din['blk64'], writes=[self.blk64.k])

    def ret(self, l):
        P = self.P
        aT = self.aT
        ident = self.ident
        r64b = self.ar('r64b', [64, 64], BF16)
        P.dma('pool', r64b[:, :], self.din['r64b'], writes=[r64b.k])
        wo = self.load_wo(l, 3)
        qx = self.ar('qx', [64, 2, T], BF16)
        kx = self.ar('kx', [64, 2, T], BF16)
        kxt = self.ar('kxt', [128, NT, 128], BF16)
        vt = self.ar('vt', [128, NT, 256], BF16)
        Sf = self.ar('Sf', [64, NT, 2, 128], BF16)
        Dsum = self.ar('Dsum', [128, 4, 128], BF16)
        zeta = self.ar('zeta', [128, 2, 4], F32)
        lgcol = self.ar('lgcol', [64, 2, 2], F32)
        xi = self.ar('xi', [64, 2, 2, 128], F32)
        dc = self.ar('dc', [64, 2, 2], F32)
        mark0 = self.ar_off
        w = self.ar('wr_', [128, 8, 512], BF16)
        self.load_w(w[:, :, :], self.w_in[l][:, C_RQ:C_RQ + 512], 8, 512, w.k)
        mark1 = self.ar_off
        lgb = self.ar('lgb', [128, 8], F32)
        rel = self.ar('rel', [128, 4, 128], F32)
        colc = self.ar('colc', [128, 16], F32)
        rowc = self.ar('rowc', [64, 2, 128], F32)
        dtmp = self.ar('dtmp', [128, 2, 128], F32)
        cosd, sind = self.din['cos_ret'], self.din['sin_ret']
        P.dma('sp', lgb[:, :], self.din['ret_decay_logit'][l:l + 1].rearrange("o d h -> o (d h)").broadcast_to([128, 8]),
              writes=[lgb.k])
        P.dma('sp', rel[:, :, :], self.din['ret_rel'].rearrange("r s t -> s r t"), writes=[rel.k])
        P.dma('sp', colc[:, :], self.din['ret_colc'], writes=[colc.k])
        P.dma('sp', rowc[:, :, :], self.din['ret_rowc'].rearrange("p (d t) -> p d t", d=2), writes=[rowc.k])
        self.act(lgb[:, :], lgb[:, :], AF.Exp, [lgb.k], [lgb.k], scale=-1.0)
        self.ts(lgb[:, :], lgb[:, :], 1.0, None, ALU.add, None, [lgb.k], [lgb.k])
        self.act(lgb[:, :], lgb[:, :], AF.Ln, [lgb.k], [lgb.k])
        self.ts(lgb[:, :], lgb[:, :], -1.0, None, ALU.mult, None, [lgb.k], [lgb.k])
        for h in range(4):
            self.act(dtmp[:, 0, :], rel[:, 0, :], AF.Exp, [rel.k, lgb.k], [dtmp.k], scale=lgb[:, h:h + 1])
            self.tt(dtmp[:, 0, :], dtmp[:, 0, :], rel[:, 1, :], ALU.mult, [dtmp.k, rel.k], [dtmp.k])
            self.act(dtmp[:, 1, :], rel[:, 2, :], AF.Exp, [rel.k, lgb.k], [dtmp.k], scale=lgb[:, 4 + h:5 + h])
            self.tt(dtmp[:, 1, :], dtmp[:, 1, :], rel[:, 3, :], ALU.mult, [dtmp.k, rel.k], [dtmp.k])
            self.tt(Dsum[:, h, :], dtmp[:, 0, :], dtmp[:, 1, :], ALU.add, [dtmp.k], [Dsum.k])
        for d in range(2):
            self.act(zeta[:, d, :], lgb[:, d * 4:d * 4 + 4], AF.Exp, [lgb.k, colc.k], [zeta.k], scale=colc[:, d:d + 1])
            for p in range(2):
                self.cp('dve', lgcol[0:32, d, p:p + 1], lgb[0:32, d * 4 + 2 * p:d * 4 + 2 * p + 1], [lgb.k], [lgcol.k])
                self.cp('dve', lgcol[32:64, d, p:p + 1], lgb[32:64, d * 4 + 2 * p + 1:d * 4 + 2 * p + 2], [lgb.k], [lgcol.k])
        for d in range(2):
            for p in range(2):
                self.act(xi[:, d, p, :], rowc[:, d, :], AF.Exp, [rowc.k, lgcol.k], [xi.k], scale=lgcol[:, d, p:p + 1])
        self.act(dc[:, :, :], lgcol[:, :, :], AF.Exp, [lgcol.k], [dc.k], scale=128.0)
        RSTOP = int(os.environ.get('RET_STOP', '9'))
        if RSTOP <= 1:
            return
        self.ar_release(mark1)
        cs = self.ar('cs', [64, 2, 512], F32)
        xb = self.ar('xb', [64, 512], BF16)
        t1 = self.ar('t1', [64, 512], F32)
        t2 = self.ar('t2', [64, 512], F32)

        def rope64(dst, ps_x, psk, n, wkey):
            self.cp('act', xb[:, 0:n], ps_x, [psk], [xb.k])
            pr, prk = self.psum('mB', [2, 3])
            self.mmg(pr[0:64, 0:n], [(r64b[:, :], xb[:, 0:n])], [r64b.k, xb.k], prk)
            self.tt(t1[:, 0:n], ps_x, cs[:, 0, 0:n], ALU.mult, [psk, cs.k], [t1.k])
            self.tt(t2[:, 0:n], pr[0:64, 0:n], cs[:, 1, 0:n], ALU.mult, [prk, cs.k], [t2.k])
            self.tt(t1[:, 0:n], t1[:, 0:n], t2[:, 0:n], ALU.add, [t1.k, t2.k], [t1.k])
            self.cp('act', dst, t1[:, 0:n], [t1.k], [wkey])

        for b, (t0, n) in enumerate(BLKS):
            ak = ('aT', b)
            for hf in range(2):
                P.dma('sp', cs[hf * 32:(hf + 1) * 32, 0, 0:n], cosd[:, t0:t0 + n], writes=[cs.k])
                P.dma('sp', cs[hf * 32:(hf + 1) * 32, 1, 0:n], sind[:, t0:t0 + n], writes=[cs.k])
            for p in range(2):
                pq, pqk = self.psum('mA', [0, 1])
                self.proj(pq[0:64, 0:n], w, (p * 64, (p + 1) * 64), lambda kc: aT[:, kc, t0:t0 + n], [w.k, ak], pqk)
                rope64(qx[:, p, t0:t0 + n], pq[0:64, 0:n], pqk, n, qx.k)
                pk_, pkk = self.psum('mA', [0, 1])
                self.proj(pk_[0:64, 0:n], w, (128 + p * 64, 128 + (p + 1) * 64), lambda kc: aT[:, kc, t0:t0 + n], [w.k, ak], pkk)
                rope64(kx[:, p, t0:t0 + n], pk_[0:64, 0:n], pkk, n, kx.k)
                for tt_ in range(n // 128):
                    tile = t0 // 128 + tt_
                    ptr, ptk = self.psum('mB', [2, 3])
                    self.tr(ptr[:, 0:64], t1[:, tt_ * 128:(tt_ + 1) * 128], ident[0:64, 0:64], [t1.k, 'ident'], ptk)
                    self.cp('act', kxt[:, tile, p * 64:(p + 1) * 64], ptr[:, 0:64], [ptk], [kxt.k])
            for tt_ in range(n // 128):
                tile = t0 // 128 + tt_
                pv, pvk = self.psum('mA', [0, 1])
                self.mmg(pv[:, 0:256], [(aT[:, kc, tile * 128:(tile + 1) * 128], w[:, kc, 256:512]) for kc in range(8)],
                         [w.k, ak], pvk)
                self.cp('act', vt[:, tile, :], pv[:, 0:256], [pvk], [vt.k])

        if RSTOP <= 2:
            return
        self.ar_release(mark0)
        w = self.ar('wg_', [128, 8, 256], BF16)
        self.load_w(w[:, :, :], self.w_in[l][:, C_RG:C_RG + 256], 8, 256, w.k)
        self.hn_alloc()
        S = self.ar('S', [64, 2, 128], F32)
        Sbb = self.ar('Sbb', [64, 2, 128], BF16)
        vz = self.ar('vz', [128, 256], BF16)
        qxm = self.ar('qxm', [64, 2, 2, 128], BF16)
        qxd = self.ar('qxd', [64, 2, 2, 2, 128], BF16)
        rowm = self.ar('rowm', [64, 16], F32)
        P.dma('sp', rowm[:, :], self.din['ret_rowm'], writes=[rowm.k])
        AT = self.ar('AT', [128, 4, 128], BF16)
        o32 = self.ar('o32', [128, 2, 512], F32)
        oblk = self.ar('oblk', [128, 2, 512], BF16)

        def state_step(d, tile):
            self.tt(vz[:, :].rearrange("s (h v) -> s h v", h=4), vt[:, tile, :].rearrange("s (h v) -> s h v", h=4),
                    zeta[:, d, :].unsqueeze(2).broadcast_to([128, 4, 64]), ALU.mult, [vt.k, zeta.k], [vz.k])
            pu, puk = self.psum('mA', [0, 1])
            for p in range(2):
                self.mmg(pu[0:64, p * 128:(p + 1) * 128], [(kxt[:, tile, p * 64:(p + 1) * 64], vz[:, p * 128:(p + 1) * 128])],
                         [kxt.k, vz.k], puk)
            for p in range(2):
                self.stt(S[:, p, :], S[:, p, :], dc[:, d, p:p + 1], pu[0:64, p * 128:(p + 1) * 128], ALU.mult, ALU.add,
                         [S.k, dc.k, puk], [S.k])

        self.memset('dve', S[:, :, :], 0.0, [S.k])
        for tile in range(NT):
            self.cp('act', Sf[:, tile, :, :], S[:, :, :], [S.k], [(Sf.k, tile)])
            if tile < NT - 1:
                state_step(0, tile)
        if RSTOP <= 3:
            return
        self.memset('dve', S[:, :, :], 0.0, [S.k])
        order = [1, 0] + list(range(NT - 1, 1, -1))
        for idx, tile in enumerate(order):
            self.cp('act', Sbb[:, :, :], S[:, :, :], [S.k], [Sbb.k])
            for hh in range(2):
                self.ts(qxm[:, hh, :, :], qx[:, :, tile * 128:(tile + 1) * 128], rowm[:, hh:hh + 1], None, ALU.mult, None,
                        [qx.k, rowm.k], [qxm.k])
            for d in range(2):
                for hh in range(2):
                    self.tt(qxd[:, d, hh, :, :], qxm[:, hh, :, :], xi[:, d, :, :], ALU.mult, [qxm.k, xi.k], [qxd.k])
            pA, pAk = self.psum('rA', [4, 5])
            for h in range(4):
                hh, p = h % 2, h // 2
                self.mmg(pA[:, h * 128:(h + 1) * 128], [(kx[:, p, tile * 128:(tile + 1) * 128], qxm[:, hh, p, :])],
                         [kx.k, qxm.k], pAk)
            self.tt(AT[:, :, :], pA[:, :].rearrange("s (h t) -> s h t", h=4), Dsum[:, :, :], ALU.mult, [pAk, Dsum.k], [AT.k])
            if RSTOP <= 4:
                continue
            pO, pOk = self.psum('rO', [6, 7])
            for h in range(4):
                hh, p = h % 2, h // 2
                self.mmg(pO[:, h * 128:(h + 1) * 128],
                         [(vt[:, tile, p * 128:(p + 1) * 128], AT[:, h, :]),
                          (Sf[:, tile, p, :], qxd[:, 0, hh, p, :]),
                          (Sbb[:, p, :], qxd[:, 1, hh, p, :])],
                         [vt.k, AT.k, (Sf.k, tile), Sbb.k, qxd.k], pOk)
            b = self.blk_of_tile(tile)
            t0, n = BLKS[b]
            off = tile * 128 - t0
            pv4 = pO[:, :].rearrange("q (p j t) -> q p j t", p=2, j=2)
            for hh in range(2):
                self.ts(o32[hh * 64:hh * 64 + 64, :, off:off + 128], pv4[hh * 64:hh * 64 + 64, :, hh, :], 32.0 ** -0.5, None,
                        ALU.mult, None, [pOk], [(o32.k, tile % 4, hh)])
            if RSTOP <= 5:
                continue
            if idx < NT - 1:
                state_step(1, tile)
            if RSTOP <= 6:
                continue
            if tile * 128 == t0:
                ntile = n // 128
                okeys = [(o32.k, (t0 // 128 + j) % 4, hh) for j in range(ntile) for hh in range(2)]
                self.P.op('dve', lambda e: e.tensor_copy(out=o32[:, :, 0:1], in_=o32[:, :, 0:1]), reads=okeys, writes=[o32.k] + okeys)
                self.headnorm_gate(l, o32, o32.k, n, t0, b, w, 0, True, 5, 7, oblk, ('aT', b))
                self.outproj(l, wo, oblk, [(oblk.k, 0), (oblk.k, 1)], b, t0, n)


    def hgrn(self, l):
        P = self.P
        aT = self.aT
        ident = self.ident
        wh = self.ar('wh', [128, 8, 1280], BF16)
        self.load_w(wh[:, :, :], self.w_in[l][:, C_HQ:C_HQ + 1280], 8, 1280, wh.k)
        itm = self.ar('itm', [128, NT, 256], BF16)
        ohg = self.ar('ohg', [128, 2, T], BF16)
        trie = self.ar('trie', [128, 2, 136], F32)
        P.dma('sp', trie[:, :, :], self.din['hg_trie'].rearrange("d s c -> s d c"), writes=[trie.k])
        mt = self.ar('mt', [128, 2, 128], BF16)
        P.dma('pool', mt[:, :, :], self.din['hg_mt'].rearrange("d s c -> s d c"), writes=[mt.k])
        ee = self.ar('ee', [128, 2, 8], BF16)
        P.dma('pool', ee[:, :, :], self.din['hg_e'].rearrange("d s c -> s d c"), writes=[ee.k])
        omlF = self.ar('omlF', [64, 8], F32)
        nomlF = self.ar('nomlF', [64, 8], F32)
        lbB = self.ar('lbB', [128, 512], F32)
        omlB = self.ar('omlB', [128, 512], F32)
        mark = self.ar_off
        st = self.ar('st', [32, 64], F32)
        lbl = self.ar('lbl', [64, 4, 8], F32)
        sF = self.ar('sF', [64, 8], F32)
        cF = self.ar('cF', [64, 8], F32)
        P.dma('sp', st[:, :], self.din['hg_lb_logits'].rearrange("l d (h k) -> (l d h) k", k=64), writes=[st.k])
        pt, pk = self.psum('mA', [0, 1])
        self.tr(pt[0:64, 0:32], st[:, :], ident[0:32, 0:32], [st.k, 'ident'], pk)
        self.act(lbl[:, :, :], pt[0:64, 0:32].rearrange("k (l x) -> k l x", l=4), AF.Exp, [pk], [lbl.k])
        self.tt(sF[:, :], lbl[:, 0, :], lbl[:, 1, :], ALU.add, [lbl.k], [sF.k])
        self.tt(sF[:, :], sF[:, :], lbl[:, 2, :], ALU.add, [lbl.k, sF.k], [sF.k])
        self.tt(sF[:, :], sF[:, :], lbl[:, 3, :], ALU.add, [lbl.k, sF.k], [sF.k])
        self.recip(sF[:, :], sF[:, :], [sF.k], [sF.k])
        self.memset('dve', cF[:, :], 0.0, [cF.k])
        for j in range(1, l + 1):
            self.tt(cF[:, :], cF[:, :], lbl[:, j, :], ALU.add, [lbl.k, cF.k], [cF.k])
        self.tt(cF[:, :], cF[:, :], sF[:, :], ALU.mult, [cF.k, sF.k], [cF.k])
        self.ts(omlF[:, :], cF[:, :], -1.0, 1.0, ALU.mult, ALU.add, [cF.k], [omlF.k])
        self.ts(nomlF[:, :], cF[:, :], -1.0, None, ALU.add, None, [cF.k], [nomlF.k])
        eb_ = self.ar('ebig', [128, 4, 512], F32)
        sB = self.ar('sB', [128, 512], F32)
        P.dma('sp', eb_[:, :, :], self.din['hg_lb_logits'].rearrange("(o l) d c -> o l (d c)", o=1).broadcast_to([128, 4, 512]),
              writes=[eb_.k])
        self.act(eb_[:, :, :], eb_[:, :, :], AF.Exp, [eb_.k], [eb_.k])
        self.tt(sB[:, :], eb_[:, 0, :], eb_[:, 1, :], ALU.add, [eb_.k], [sB.k])
        self.tt(sB[:, :], sB[:, :], eb_[:, 2, :], ALU.add, [eb_.k, sB.k], [sB.k])
        self.tt(sB[:, :], sB[:, :], eb_[:, 3, :], ALU.add, [eb_.k, sB.k], [sB.k])
        self.recip(sB[:, :], sB[:, :], [sB.k], [sB.k])
        self.memset('dve', lbB[:, :], 0.0, [lbB.k])
        for j in range(1, l + 1):
            self.tt(lbB[:, :], lbB[:, :], eb_[:, j, :], ALU.add, [eb_.k, lbB.k], [lbB.k])
        self.tt(lbB[:, :], lbB[:, :], sB[:, :], ALU.mult, [lbB.k, sB.k], [lbB.k])
        self.ts(omlB[:, :], lbB[:, :], -1.0, 1.0, ALU.mult, ALU.add, [lbB.k], [omlB.k])
        self.ar_release(mark)
        for tile in range(NT):
            ak = ('aT', self.blk_of_tile(tile))
            pv, pvk = self.psum('mA', [0, 1])
            self.mmg(pv[:, 0:256], [(aT[:, kc, tile * 128:(tile + 1) * 128], wh[:, kc, 768:1024]) for kc in range(8)],
                     [wh.k, ak], pvk)
            self.cp('act', itm[:, tile, :], pv[:, 0:256], [pvk], [itm.k])
        ftm = self.ar('ftm', [128, 256], F32)
        lftm = self.ar('lftm', [128, 256], F32)
        ktm = self.ar('ktm', [128, 256], F32)
        kbt = self.ar('kbt', [128, 256], BF16)
        kTt = self.ar('kTt', [64, 4, 128], F32)
        ebt = self.ar('ebt', [64, 4, 128], F32)
        enb = self.ar('enb', [64, 4, 128], F32)
        dd = self.ar('dd', [64, 4, 8], F32)
        qb = self.ar('qb', [64, 4, 128], BF16)
        kb = self.ar('kb', [64, 4, 128], BF16)
        vexp = [self.ar('vexp%d' % i, [128, 8, 64], BF16) for i in range(4)]
        u = self.ar('u', [64, 8, 4, 64], F32)
        Spp = [self.ar('S%d' % i, [64, 4, 64], F32) for i in range(2)]
        tmpS = self.ar('tmpS', [64, 4, 64], F32)
        Sbf = self.ar('Sbf', [64, 8, 4, 64], BF16)
        si = 0
        AT = self.ar('AT', [128, 4, 128], BF16)
        vi = 0
        for d in range(2):
            order = list(range(NT)) if d == 0 else [1, 0] + list(range(NT - 1, 1, -1))
            self.memset('dve', Spp[si % 2][:, :, :], 0.0, [(Spp[si % 2].k, h) for h in range(4)])
            zc = 256 + d * 256
            for tile in order:
                ak = ('aT', self.blk_of_tile(tile))
                tcs = slice(tile * 128, (tile + 1) * 128)
                pz, pzk = self.psum('hz', [0])
                self.mmg(pz[:, 0:256], [(aT[:, kc, tcs], wh[:, kc, zc:zc + 256]) for kc in range(8)], [wh.k, ak], pzk)
                pzT, pzTk = self.psum('hzT', [1])
                for h in range(4):
                    self.mmg(pzT[0:64, h * 128:(h + 1) * 128],
                             [(wh[:, kc, zc + h * 64:zc + (h + 1) * 64], aT[:, kc, tcs]) for kc in range(8)], [wh.k, ak], pzTk)
                pq, pqk = self.psum('hq', [2])
                for h in range(4):
                    self.mmg(pq[0:64, h * 128:(h + 1) * 128],
                             [(wh[:, kc, h * 64:(h + 1) * 64], aT[:, kc, tcs]) for kc in range(8)], [wh.k, ak], pqk)
                self.act(ftm[:, :], pz[:, 0:256], AF.Sigmoid, [pzk], [ftm.k])
                self.act(kTt[:, :, :], pzT[0:64, :].rearrange("k (h t) -> k h t", h=4), AF.Sigmoid, [pzTk], [kTt.k])
                self.tt(ftm[:, :], ftm[:, :], omlB[:, d * 256:(d + 1) * 256], ALU.mult, [ftm.k, omlB.k], [ftm.k])
                self.tt(ftm[:, :], ftm[:, :], lbB[:, d * 256:(d + 1) * 256], ALU.add, [ftm.k, lbB.k], [ftm.k])
                self.act(lftm[:, :], ftm[:, :], AF.Ln, [ftm.k], [lftm.k])
                self.tt(kTt[:, :, :], kTt[:, :, :], nomlF[:, d * 4:(d + 1) * 4].unsqueeze(2).broadcast_to([64, 4, 128]),
                        ALU.mult, [kTt.k, nomlF.k], [kTt.k])
                self.tt(kTt[:, :, :], kTt[:, :, :], omlF[:, d * 4:(d + 1) * 4].unsqueeze(2).broadcast_to([64, 4, 128]),
                        ALU.add, [kTt.k, omlF.k], [kTt.k])
                self.ts(ktm[:, :], ftm[:, :], -1.0, 1.0, ALU.mult, ALU.add, [ftm.k], [ktm.k])
                pb, pbk = self.psum('hb', [3])
                self.mmg(pb[:, 0:256], [(trie[:, d, 0:128], lftm[:, :])], [trie.k, lftm.k], pbk)
                pbT, pbTk = self.psum('hbT', [4])
                for h in range(4):
                    self.mmg(pbT[0:64, h * 128:(h + 1) * 128], [(lftm[:, h * 64:(h + 1) * 64], trie[:, d, 0:128])],
                             [lftm.k, trie.k], pbTk)
                ptot, ptotk = self.psum('htot', [5])
                for h in range(4):
                    self.mmg(ptot[0:64, h * 8:(h + 1) * 8], [(lftm[:, h * 64:(h + 1) * 64], trie[:, d, 128:136])],
                             [lftm.k, trie.k], ptotk)
                self.act(ftm[:, :], pb[:, 0:256], AF.Exp, [pbk], [ftm.k], scale=-1.0)
                self.act(ebt[:, :, :], pbT[0:64, :].rearrange("k (h t) -> k h t", h=4), AF.Exp, [pbTk], [ebt.k])
                self.act(enb[:, :, :], pbT[0:64, :].rearrange("k (h t) -> k h t", h=4), AF.Exp, [pbTk], [enb.k], scale=-1.0)
                self.act(dd[:, :, :], ptot[0:64, 0:32].rearrange("k (h j) -> k h j", h=4), AF.Exp, [ptotk], [dd.k])
                self.tt(kbt[:, :], ktm[:, :], ftm[:, :], ALU.mult, [ktm.k, ftm.k], [kbt.k])
                self.stt(qb[:, :, :], pq[0:64, :].rearrange("k (h t) -> k h t", h=4), 0.125, ebt[:, :, :], ALU.mult, ALU.mult,
                         [pqk, ebt.k], [qb.k])
                self.tt(kb[:, :, :], kTt[:, :, :], enb[:, :, :], ALU.mult, [kTt.k, enb.k], [kb.k])
                pA, pAk = self.psum('h3', [6])
                for h in range(4):
                    self.mmg(pA[:, h * 128:(h + 1) * 128], [(kb[:, h, :], qb[:, h, :])], [kb.k, qb.k], pAk)
                self.tt(AT[:, :, :], pA[:, :].rearrange("s (h t) -> s h t", h=4),
                        mt[:, d, :].unsqueeze(1).broadcast_to([128, 4, 128]), ALU.mult, [pAk, mt.k], [AT.k])
                for hp in range(2):
                    ves = []
                    for h in (2 * hp, 2 * hp + 1):
                        ve = vexp[h]
                        self.tt(ve[:, :, :], itm[:, tile, h * 64:(h + 1) * 64].unsqueeze(1).broadcast_to([128, 8, 64]),
                                ee[:, d, :].unsqueeze(2).broadcast_to([128, 8, 64]), ALU.mult, [itm.k, ee.k], [ve.k], eng='pool')
                        ves.append(ve)
                    pus = []
                    for h, ve in zip((2 * hp, 2 * hp + 1), ves):
                        pu, puk = self.psum('h2', [0, 1])
                        self.mmg(pu[0:64, :], [(kbt[:, h * 64:(h + 1) * 64], ve[:, :, :].rearrange("s j v -> s (j v)"))],
                                 [kbt.k, ve.k], puk)
                        pus.append((pu, puk))
                    for h, (pu, puk) in zip((2 * hp, 2 * hp + 1), pus):
                        self.tt(u[:, :, h, :], pu[0:64, :].rearrange("k (j v) -> k j v", j=8),
                                dd[:, h, :].unsqueeze(2).broadcast_to([64, 8, 64]), ALU.mult, [puk, dd.k], [u.k])
                for j in range(8):
                    S, S2 = Spp[si % 2], Spp[(si + 1) % 2]
                    si += 1
                    self.cp('act', Sbf[:, j, :, :], S[:, :, :], [(S.k, h) for h in range(4)], [(Sbf.k, j)])
                    for h in range(4):
                        self.stt(S2[:, h, :], S[:, h, :], dd[:, h, j:j + 1], u[:, j, h, :], ALU.mult, ALU.add,
                                 [(S.k, h), dd.k, u.k], [(S2.k, h)])
                pO, pOk = self.psum('h4', [7])
                rk = [itm.k, AT.k, qb.k] + [(Sbf.k, j) for j in range(8)]
                for h in range(4):
                    p = h // 2
                    P.op('pe', lambda e, h=h, p=p, tile=tile: e.matmul(pO[:, h * 128:(h + 1) * 128], lhsT=itm[:, tile, p * 128:(p + 1) * 128],
                                                           rhs=AT[:, h, :], start=True, stop=False),
                         reads=rk, writes=[pOk], inc=False)
                    for j in range(8):
                        cj = j if d == 0 else 7 - j
                        P.op('pe', lambda e, h=h, p=p, j=j, cj=cj: e.matmul(
                            pO[:, h * 128 + cj * 16:h * 128 + (cj + 1) * 16],
                            lhsT=Sbf[:, j, 2 * p:2 * p + 2, :].rearrange("k h v -> k (h v)"),
                            rhs=qb[:, h, cj * 16:(cj + 1) * 16], start=False, stop=(j == 7)),
                             reads=rk, writes=[pOk], inc=(h == 3 and j == 7))
                pv4 = pO[:, :].rearrange("q (p j t) -> q p j t", p=2, j=2)
                for hh in range(2):
                    if d == 0:
                        self.cp('act' if hh else 'dve', ohg[hh * 64:hh * 64 + 64, :, tcs], pv4[hh * 64:hh * 64 + 64, :, hh, :],
                                [pOk], [(ohg.k, tile, hh)])
                    else:
                        self.tt(ohg[hh * 64:hh * 64 + 64, :, tcs], pv4[hh * 64:hh * 64 + 64, :, hh, :],
                                ohg[hh * 64:hh * 64 + 64, :, tcs], ALU.add, [pOk, (ohg.k, tile, hh)], [(ohg.k, tile, hh)])
        if os.environ.get('HG_DBG'):
            dbg = self.nc.dram_tensor('dbg', [128, 2 * T], F32, kind="ExternalOutput").ap()
            P.dma('pool', dbg.rearrange("q (p t) -> q p t", p=2), ohg[:, :, :],
                  reads=[(ohg.k, t_, hh) for t_ in range(NT) for hh in range(2)], is_output=True)
        self.ar_release(mark)
        wo = self.load_wo(l, 1)
        self.hn_alloc()
        oblk = self.ar('oblk', [128, 2, 512], BF16)
        o32 = self.ar('o32h', [128, 2, 512], F32)
        for b, (t0, n) in enumerate(BLKS):
            okeys = [(ohg.k, t0 // 128 + j, hh) for j in range(n // 128) for hh in range(2)]
            self.cp('dve', o32[:, :, 0:n], ohg[:, :, t0:t0 + n], okeys, [o32.k])
            self.headnorm_gate(l, o32, o32.k, n, t0, b, wh, 1024, False, 3, None, oblk, ('aT', b))
            self.outproj(l, wo, oblk, [(oblk.k, 0), (oblk.k, 1)], b, t0, n)


    def final_out(self, out, gfin):
        P = self.P
        hT, rstd, ident = self.hT, self.rstd, self.ident
        nsq = self.ar('nsq', [128, 8, 512], BF16)
        ntmp = self.ar('ntmp', [128, 8, 512], F32)
        nsq.k, ntmp.k = 'nsq', 'ntmp'
        ost = [self.ar('ost%d' % i, [128, 1024], F32) for i in range(2)]
        oi = 0
        for b, (t0, n) in enumerate(BLKS):
            if b == 0:
                continue
            hk = [('hT', c, b) for c in range(8)]
            if self.final:
                self.act(nsq[:, :, 0:n], hT[:, :, t0:t0 + n], AF.Square, hk, ['nsq'])
                pt, pk = self.psum('nrm', [2, 3])
                self.mmg(pt[:, 0:n], [(self.ones_bf[:, :], nsq[:, c, 0:n]) for c in range(8)], ['nsq', 'ones_bf'], pk)
                self.act(rstd[:, 0:n], pt[:, 0:n], AF.Sqrt, [pk], ['rstd'], bias=self.eps_col[:, 0:1], scale=1.0 / D)
                self.recip(rstd[:, 0:n], rstd[:, 0:n], ['rstd'], ['rstd'])
                self.tt(ntmp[:, :, 0:n], hT[:, :, t0:t0 + n], rstd[:, 0:n].unsqueeze(1).broadcast_to([128, 8, n]),
                        ALU.mult, hk + ['rstd'], ['ntmp'])
                self.tt(ntmp[:, :, 0:n], ntmp[:, :, 0:n], gfin[:, :].unsqueeze(2).broadcast_to([128, 8, n]),
                        ALU.mult, ['ntmp', 'gfin'], ['ntmp'])
            else:
                self.cp('dve', ntmp[:, :, 0:n], hT[:, :, t0:t0 + n], hk, ['ntmp'])
            for tt_ in range(n // 128):
                os_ = ost[oi % 2]
                oi += 1
                for half in range(2):
                    pt, pk = self.psum('tr', [0, 1])
                    for j in range(4):
                        c = half * 4 + j
                        self.tr(pt[:, j * 128:(j + 1) * 128], ntmp[:, c, tt_ * 128:(tt_ + 1) * 128], ident[:, :],
                                ['ntmp', 'ident'], pk)
                    self.cp('act' if half else 'dve', os_[:, half * 512:(half + 1) * 512], pt[:, :], [pk], [(os_.k, half)])
                row0 = t0 - LCTX + tt_ * 128
                P.dma('sp', out[row0:row0 + 128, :], os_[:, :], reads=[(os_.k, 0), (os_.k, 1)], is_output=True)


def _rot_T(half):
    n = 2 * half
    R = np.zeros((n, n), np.float32)
    for m in range(half):
        R[m, m + half] = -1.0
        R[m + half, m] = 1.0
    return R.T.copy()


def _consts():
    sel = np.zeros((NEXP, NEXP * 128), np.float32)
    for e in range(NEXP):
        sel[e, e * 128:(e + 1) * 128] = 1.0
    c = {'ident': np.eye(128, dtype=np.float32), 'sel': sel}
    theta = 10000.0
    tok = np.arange(NLAT)
    row, col = (tok // 64).astype(np.float64), (tok % 64).astype(np.float64)

    def axial(rot_dim):
        nf = rot_dim // 4
        inv = theta ** (-np.arange(nf, dtype=np.float64) / nf)
        inv = inv.astype(np.float32).astype(np.float64)
        ang = np.concatenate([row[:, None] * inv, col[:, None] * inv], axis=1)
        ang = ang.astype(np.float32).astype(np.float64)
        full = np.zeros((T, rot_dim // 2))
        full[LCTX:] = ang
        a2 = np.concatenate([full, full], axis=1)
        return np.cos(a2).T.astype(np.float32).copy(), np.sin(a2).T.astype(np.float32).copy()

    c['cos_mla'], c['sin_mla'] = axial(32)
    c['cos_swa'], c['sin_swa'] = axial(64)
    inv = (theta ** (-np.arange(16, dtype=np.float64) / 16)).astype(np.float32).astype(np.float64)
    ang = (np.arange(T, dtype=np.float64)[:, None] * inv).astype(np.float32).astype(np.float64)
    a2 = np.concatenate([ang, ang], axis=1)
    c['cos_ret'], c['sin_ret'] = np.cos(a2).T.astype(np.float32).copy(), np.sin(a2).T.astype(np.float32).copy()
    rb = np.zeros((96, 96), np.float32)
    rb[64:96, 64:96] = _rot_T(16)
    c['rbig96'] = rb
    r128 = np.zeros((128, 128), np.float32)
    r128[0:64, 0:64] = _rot_T(32)
    r128[64:128, 64:128] = _rot_T(32)
    c['r128'] = r128
    r64b = np.zeros((64, 64), np.float32)
    r64b[0:32, 0:32] = _rot_T(16)
    r64b[32:64, 32:64] = _rot_T(16)
    c['r64b'] = r64b
    m = np.zeros((6, 128, 512), np.float32)
    s_ = np.arange(128)[:, None]
    t_ = np.arange(512)[None, :]
    for r in range(-1, 5):
        m[r + 1] = (np.abs(t_ - s_ - 128 * r) <= 128).astype(np.float32)
    c['swa_mask'] = m
    b64 = np.zeros((128, 128), np.float32)
    b64[0:64, 0:64] = 1.0 / 64
    b64[64:128, 64:128] = 1.0 / 64
    c['blk64'] = b64
    s_ = np.arange(128, dtype=np.float32)[:, None]
    t_ = np.arange(128, dtype=np.float32)[None, :]
    c['ret_rel'] = np.stack([np.maximum(t_ - s_, 0), (t_ >= s_).astype(np.float32),
                             np.maximum(s_ - t_, 0), (s_ >= t_).astype(np.float32)]).astype(np.float32)
    cc = np.zeros((128, 16), np.float32)
    cc[:, 0] = 127.0 - np.arange(128)
    cc[:, 1] = np.arange(128)
    c['ret_colc'] = cc
    rc = np.concatenate([np.arange(128) + 1.0, 128.0 - np.arange(128)])[None, :].repeat(64, axis=0)
    c['ret_rowc'] = rc.astype(np.float32)
    rm = np.zeros((64, 16), np.float32)
    rm[0:32, 0] = 1.0
    rm[32:64, 1] = 1.0
    c['ret_rowm'] = rm
    si = np.arange(128)[:, None]
    ti = np.arange(128)[None, :]
    same = (si // 16) == (ti // 16)
    trif = (same & (si <= ti)).astype(np.float32)
    trib = (same & (si >= ti)).astype(np.float32)
    ef = ((si // 16) == np.arange(8)[None, :]).astype(np.float32)
    eb = ((si // 16) == (7 - np.arange(8))[None, :]).astype(np.float32)
    c['hg_trie'] = np.stack([np.concatenate([trif, ef], axis=1), np.concatenate([trib, eb], axis=1)]).astype(np.float32)
    c['hg_mt'] = np.stack([trif, trib]).astype(np.float32)
    c['hg_e'] = np.stack([ef, eb]).astype(np.float32)
    return c


_CACHE = {}


def _get_prog(key, **kw):
    if key not in _CACHE:
        b = Builder(**kw)
        nc = b.build()
        _CACHE[key] = (nc, b)
    return _CACHE[key]


def run_partial(inputs, layers=(0, 1, 2, 3), mixers=(0, 1, 2, 3), ffn=True, final=True, cores=8):
    nc, b = _get_prog(('p', tuple(layers), tuple(mixers), ffn, final), layers=layers, mixers=mixers, ffn=ffn, final=final)
    consts = _consts()
    f = lambda a: np.ascontiguousarray(np.asarray(a, dtype=np.float32))
    shared = {k: f(inputs[k]) for k in b.din if k in inputs and k not in ('x', 'ctx')}
    for k in consts:
        if k in b.din:
            shared[k] = consts[k]
    in_maps = []
    for i in range(cores):
        m = dict(shared)
        m['x'] = f(inputs['x'][i])
        m['ctx'] = f(inputs['ctx'][i])
        m['c2'] = np.ascontiguousarray(np.stack([np.asarray(inputs['c'][i], np.float32),
                                                 np.asarray(inputs['c_ctx'], np.float32)]))
        in_maps.append(m)
    res = run_bass_kernel_spmd(nc, in_maps, core_ids=list(range(cores)))
    return np.stack([r['out'] for r in res.results], axis=0)


def kernel(**inputs):
    return run_partial(inputs)
```
